# Optimizing a Trainium2 kernel written in Bass

```python
import math
import jax
import jax.numpy as jnp
from jax import lax
import numpy as np

D_MODEL = 1024
BATCH = 2
SEQ = 8192
DEPTH = 2

EPS = 1e-6
ROPE_THETA = 10000.0
Q_BLOCK = 128

A_HEADS = 4
A_DK = 64
A_DV = 2 * A_DK
A_QK_W = A_HEADS * 2 * A_DK
A_V_W = A_HEADS * A_DV

B_PAIRS = ((128, 1), (512, 4), (2048, 16))
B_GROUPS = len(B_PAIRS)
B_HEADS = 4
B_DH = 64
B_W = B_GROUPS * B_HEADS * B_DH
B_OUT_W = B_HEADS * B_DH

IN_SPLITS = [A_QK_W, A_QK_W, A_V_W, B_W, B_W, B_W, D_MODEL, D_MODEL]
IN_W = sum(IN_SPLITS)

N_GROUPS = 4
EXPERTS_PER_GROUP = 8
N_EXPERTS = N_GROUPS * EXPERTS_PER_GROUP
TOP_K = 2
D_EXPERT = 256

kernel_name = 'gated_hybrid_diffattn_dilated_hmoe_adaln'


def rms_norm(x, gain=None):
    xf = x.astype(jnp.float32)
    y = xf * lax.rsqrt(jnp.mean(xf * xf, axis=-1, keepdims=True) + EPS)
    if gain is not None:
        y = y * gain.astype(jnp.float32)
    return y.astype(x.dtype)


def rope_tables(positions, dim):
    inv_freq = ROPE_THETA ** (-jnp.arange(0, dim, 2, dtype=jnp.float32) / dim)
    ang = positions.astype(jnp.float32)[..., None] * inv_freq
    return jnp.cos(ang)[:, :, None, :], jnp.sin(ang)[:, :, None, :]


def apply_rope(x, cos, sin):
    xf = x.astype(jnp.float32)
    x1, x2 = jnp.split(xf, 2, axis=-1)
    out = jnp.concatenate([x1 * cos - x2 * sin, x2 * cos + x1 * sin], axis=-1)
    return out.astype(x.dtype)


def differential_attention(q, k, v, lam, lam_init, subln):
    bsz, seq = q.shape[0], q.shape[1]
    n_blocks = seq // Q_BLOCK
    q_blocks = q.reshape(bsz, n_blocks, Q_BLOCK, A_HEADS, 2, A_DK).transpose(1, 0, 2, 3, 4, 5)
    k = k.reshape(bsz, seq, A_HEADS, 2, A_DK)
    scale = A_DK ** -0.5

    def one_block(qb):
        s = jnp.einsum('bqhmd,bkhmd->bhmqk', qb, k).astype(jnp.float32) * scale
        p = jax.nn.softmax(s, axis=-1)
        a = p[:, :, 0] - lam * p[:, :, 1]
        return jnp.einsum('bhqk,bkhd->bqhd', a.astype(v.dtype), v)

    o = lax.map(one_block, q_blocks)
    o = o.transpose(1, 0, 2, 3, 4).reshape(bsz, seq, A_HEADS, A_DV)
    o = rms_norm(o, subln) * (1.0 - lam_init)
    return o.reshape(bsz, seq, A_V_W)


def dilated_window_attention(q, k, v, window, dilation):
    bsz, seq, heads, dh = q.shape
    radius = window // (2 * dilation)
    length = seq // dilation
    n = bsz * dilation

    def to_strided(t):
        return t.reshape(bsz, length, dilation, heads, dh).transpose(0, 2, 1, 3, 4).reshape(n, length, heads, dh)

    qs, ks, vs = to_strided(q), to_strided(k), to_strided(v)
    nb = -(-length // radius)
    lp = nb * radius
    qs = jnp.pad(qs, ((0, 0), (0, lp - length), (0, 0), (0, 0))).reshape(n, nb, radius, heads, dh)

    def windows(t):
        tp = jnp.pad(t, ((0, 0), (radius, lp - length + radius), (0, 0), (0, 0)))
        tp = tp.reshape(n, nb + 2, radius, heads, dh)
        return jnp.concatenate([tp[:, :-2], tp[:, 1:-1], tp[:, 2:]], axis=2)

    kw, vw = windows(ks), windows(vs)
    blk = jnp.arange(nb)[:, None] * radius
    qpos = blk + jnp.arange(radius)[None, :]
    kpos = blk - radius + jnp.arange(3 * radius)[None, :]
    rel = kpos[:, None, :] - qpos[:, :, None]
    valid = (jnp.abs(rel) <= radius) & (kpos[:, None, :] >= 0) & (kpos[:, None, :] < length)

    s = jnp.einsum('nbqhd,nbkhd->nbhqk', qs, kw).astype(jnp.float32) * (dh ** -0.5)
    s = jnp.where(valid[None, :, None], s, -jnp.inf)
    m = jnp.max(s, axis=-1, keepdims=True)
    e = jnp.exp(s - m)
    l = jnp.sum(e, axis=-1, keepdims=True)
    o = jnp.einsum('nbhqk,nbkhd->nbqhd', (e / l).astype(v.dtype), vw)
    lse = (m + jnp.log(l))[..., 0].transpose(0, 1, 3, 2)

    o = o.reshape(n, lp, heads, dh)[:, :length]
    lse = lse.reshape(n, lp, heads)[:, :length]
    o = o.reshape(bsz, dilation, length, heads, dh).transpose(0, 2, 1, 3, 4).reshape(bsz, seq, heads, dh)
    lse = lse.reshape(bsz, dilation, length, heads).transpose(0, 2, 1, 3).reshape(bsz, seq, heads)
    return o, lse


def token_mixer(h, cos, sin, lam, lam_init, w_in, qn_a, kn_a, subln_a, qn_b, kn_b, w_pa, w_pb, w_o):
    bsz, seq, _ = h.shape
    proj = h @ w_in
    cuts = [int(t) for t in np.cumsum(IN_SPLITS)[:-1]]
    qa, ka, va, qb, kb, vb, ga, gb = jnp.split(proj, cuts, axis=-1)

    qa = apply_rope(rms_norm(qa.reshape(bsz, seq, 2 * A_HEADS, A_DK), qn_a), cos, sin)
    ka = apply_rope(rms_norm(ka.reshape(bsz, seq, 2 * A_HEADS, A_DK), kn_a), cos, sin)
    va = va.reshape(bsz, seq, A_HEADS, A_DV)
    out_a = differential_attention(qa, ka, va, lam, lam_init, subln_a)

    qb = apply_rope(rms_norm(qb.reshape(bsz, seq, B_GROUPS * B_HEADS, B_DH), qn_b), cos, sin)
    kb = apply_rope(rms_norm(kb.reshape(bsz, seq, B_GROUPS * B_HEADS, B_DH), kn_b), cos, sin)
    qb = qb.reshape(bsz, seq, B_GROUPS, B_HEADS, B_DH)
    kb = kb.reshape(bsz, seq, B_GROUPS, B_HEADS, B_DH)
    vb = vb.reshape(bsz, seq, B_GROUPS, B_HEADS, B_DH)
    outs, lses = [], []
    for g, (window, dilation) in enumerate(B_PAIRS):
        o_g, lse_g = dilated_window_attention(qb[:, :, g], kb[:, :, g], vb[:, :, g], window, dilation)
        outs.append(o_g)
        lses.append(lse_g)
    wts = jax.nn.softmax(jnp.stack(lses, axis=-1), axis=-1)
    out_b = jnp.sum(jnp.stack(outs, axis=-2) * wts[..., None].astype(h.dtype), axis=-2)
    out_b = out_b.reshape(bsz, seq, B_OUT_W)

    merged = jax.nn.sigmoid(ga) * (out_a @ w_pa) + jax.nn.sigmoid(gb) * (out_b @ w_pb)
    return merged @ w_o


def hierarchical_moe(h, w_r1, b_r1, w_r2, b_r2, w_e_gate, w_e_up, w_e_down):
    bsz, seq, d = h.shape
    t = h.reshape(bsz * seq, d)
    p_group = jax.nn.softmax((t @ w_r1 + b_r1).astype(jnp.float32), axis=-1)
    g_val, g_idx = lax.top_k(p_group, 1)
    logits = (t @ w_r2 + b_r2).astype(jnp.float32).reshape(-1, N_GROUPS, EXPERTS_PER_GROUP)
    in_group = logits[jnp.arange(logits.shape[0]), g_idx[:, 0]]
    top_l, top_i = lax.top_k(in_group, TOP_K)
    weights = g_val * jax.nn.softmax(top_l, axis=-1)
    expert_id = g_idx * EXPERTS_PER_GROUP + top_i
    combine = jnp.einsum('tk,tke->te', weights,
                         jax.nn.one_hot(expert_id, N_EXPERTS, dtype=jnp.float32)).astype(h.dtype)
    y = jnp.zeros_like(t)
    for e in range(N_EXPERTS):
        hid = jax.nn.silu(t @ w_e_gate[e]) * (t @ w_e_up[e])
        y = y + combine[:, e:e + 1] * (hid @ w_e_down[e])
    return y.reshape(bsz, seq, d)


def setup_inputs(seed: int = 0) -> dict:
    key = jax.random.key(seed)
    ks = jax.random.split(key, 26)

    def dense(k, shape, fan_in, gain=1.0):
        return jax.random.normal(k, shape, jnp.float32) * (gain * fan_in ** -0.5)

    def near_one(k, shape):
        return 1.0 + 0.02 * jax.random.normal(k, shape, jnp.float32)

    def small(k, shape, s):
        return s * jax.random.normal(k, shape, jnp.float32)

    offsets = jax.random.randint(ks[2], (BATCH, 1), 0, 4096, dtype=jnp.int32)
    positions = (offsets + jnp.arange(SEQ, dtype=jnp.int32)[None, :]).astype(jnp.int32)
    return {
        'x': jax.random.normal(ks[0], (BATCH, SEQ, D_MODEL), jnp.float32),
        'c': jax.random.normal(ks[1], (BATCH, D_MODEL), jnp.float32),
        'positions': positions,
        'w_ada': dense(ks[3], (DEPTH, D_MODEL, 6 * D_MODEL), D_MODEL, 0.5),
        'b_ada': small(ks[4], (DEPTH, 6 * D_MODEL), 0.01),
        'w_in': dense(ks[5], (DEPTH, D_MODEL, IN_W), D_MODEL),
        'qn_a': near_one(ks[6], (DEPTH, A_DK)),
        'kn_a': near_one(ks[7], (DEPTH, A_DK)),
        'lam_q1': small(ks[8], (DEPTH, A_DK), 0.1),
        'lam_k1': small(ks[9], (DEPTH, A_DK), 0.1),
        'lam_q2': small(ks[10], (DEPTH, A_DK), 0.1),
        'lam_k2': small(ks[11], (DEPTH, A_DK), 0.1),
        'subln_a': near_one(ks[12], (DEPTH, A_DV)),
        'qn_b': near_one(ks[13], (DEPTH, B_DH)),
        'kn_b': near_one(ks[14], (DEPTH, B_DH)),
        'w_pa': dense(ks[15], (DEPTH, A_V_W, D_MODEL), A_V_W),
        'w_pb': dense(ks[16], (DEPTH, B_OUT_W, D_MODEL), B_OUT_W),
        'w_o': dense(ks[17], (DEPTH, D_MODEL, D_MODEL), D_MODEL),
        'w_r1': dense(ks[18], (DEPTH, D_MODEL, N_GROUPS), D_MODEL),
        'b_r1': small(ks[19], (DEPTH, N_GROUPS), 0.01),
        'w_r2': dense(ks[20], (DEPTH, D_MODEL, N_EXPERTS), D_MODEL),
        'b_r2': small(ks[21], (DEPTH, N_EXPERTS), 0.01),
        'w_e_gate': dense(ks[22], (DEPTH, N_EXPERTS, D_MODEL, D_EXPERT), D_MODEL),
        'w_e_up': dense(ks[23], (DEPTH, N_EXPERTS, D_MODEL, D_EXPERT), D_MODEL),
        'w_e_down': dense(ks[24], (DEPTH, N_EXPERTS, D_EXPERT, D_MODEL), D_EXPERT),
    }


def reference(x, c, positions, w_ada, b_ada, w_in, qn_a, kn_a, lam_q1, lam_k1, lam_q2, lam_k2,
              subln_a, qn_b, kn_b, w_pa, w_pb, w_o, w_r1, b_r1, w_r2, b_r2,
              w_e_gate, w_e_up, w_e_down):
    cos, sin = rope_tables(positions, A_DK)
    c_act = jax.nn.silu(c)
    for layer in range(DEPTH):
        mod = (c_act @ w_ada[layer] + b_ada[layer])[:, None, :]
        shift1, scale1, gate1, shift2, scale2, gate2 = jnp.split(mod, 6, axis=-1)
        lam_init = 0.8 - 0.6 * math.exp(-0.3 * layer)
        lam = (jnp.exp(jnp.sum(lam_q1[layer].astype(jnp.float32) * lam_k1[layer].astype(jnp.float32)))
               - jnp.exp(jnp.sum(lam_q2[layer].astype(jnp.float32) * lam_k2[layer].astype(jnp.float32)))
               + lam_init)

        h = rms_norm(x) * (1 + scale1) + shift1
        x = x + gate1 * token_mixer(h, cos, sin, lam, lam_init, w_in[layer], qn_a[layer], kn_a[layer],
                                    subln_a[layer], qn_b[layer], kn_b[layer],
                                    w_pa[layer], w_pb[layer], w_o[layer])

        h = rms_norm(x) * (1 + scale2) + shift2
        x = x + gate2 * hierarchical_moe(h, w_r1[layer], b_r1[layer], w_r2[layer], b_r2[layer],
                                         w_e_gate[layer], w_e_up[layer], w_e_down[layer])
    return x
```

```python
import math
import numpy as np
from contextlib import ExitStack
import concourse.bass as bass
import concourse.mybir as mybir
from concourse.bass_utils import run_bass_kernel_spmd

F32 = mybir.dt.float32
BF16 = mybir.dt.bfloat16
I32 = mybir.dt.int32
ALU = mybir.AluOpType
AF = mybir.ActivationFunctionType
AX = mybir.AxisListType

ENGS = ['pe', 'act', 'dve', 'pool', 'sp']
CHUNK = 1024

NCORES = 8
T = 2048
SEQ = 8192
D = 1024
EPS = 1e-6
NEG = -30000.0
WIN = 4096
PADW = 1024
NEXP = 32


class Op:
    __slots__ = ('eng', 'fn', 'src', 'pos', 'signal', 'waits', 'know', 'is_dma', 'cnt')


class V:
    __slots__ = ('ap', 'keys')

    def __init__(self, ap, keys):
        self.ap = ap
        self.keys = keys


class Buf:
    def __init__(self, arena_name, handle, esz, off, n):
        self.an = arena_name
        self.t = handle
        self.esz = esz
        self.off = off
        self.n = n

    def keys(self, a, b):
        ch = 2048 if self.an.startswith('ps') else CHUNK
        lo = ((self.off + a) * self.esz) // ch
        hi = ((self.off + b) * self.esz - 1) // ch
        return [(self.an, i) for i in range(lo, hi + 1)]

    def v(self, a=0, b=None, p0=0, p1=128, step=1, pat=None, **kw):
        if b is None:
            b = self.n
        assert 0 <= a < b <= self.n, (a, b, self.n)
        if step == 1:
            ap = self.t[p0:p1, self.off + a:self.off + b]
        else:
            ap = self.t[p0:p1, self.off + a:self.off + b:step]
        if pat is not None:
            ap = ap.rearrange(pat, **kw)
        return V(ap, self.keys(a, b))

    def sub(self, off, n):
        assert off + n <= self.n
        return Buf(self.an, self.t, self.esz, self.off + off, n)


class DBuf:
    def __init__(self, name, ap):
        self.name = name
        self.ap = ap

    def v(self, ap=None, key=None):
        return V(self.ap if ap is None else ap, [(self.name, key)])


class Sched:
    def __init__(self, nc, n_dma_sems=16):
        self.nc = nc
        self.ops = {e: [] for e in ENGS}
        self.ncomp = {e: 0 for e in ENGS}
        self.last_w = {}
        self.readers = {}
        self.known = {e: {} for e in ENGS}
        self.n_dma_sems = n_dma_sems
        self.dma_last = [None] * n_dma_sems
        self.dma_cnt = [0] * n_dma_sems
        self.dma_rr = 0
        self.cc_cnt = 0
        self.cc_last = None

    def _need(self, e, d):
        k = self.known[e]
        if k.get(d.src, -1) >= d.pos:
            return None
        d.signal = True
        for s, p in d.know.items():
            if k.get(s, -1) < p:
                k[s] = p
        return d

    def add(self, eng, fn, reads=(), writes=(), dma=False, cc=False):
        op = Op()
        op.eng = eng
        op.fn = fn
        op.is_dma = dma
        op.signal = dma
        deps = []
        seen = set()
        pr_ = [k for k in reads if isinstance(k[0], str) and k[0].startswith('ps')]
        if pr_:
            writes = list(writes) + pr_
        for k in reads:
            w = self.last_w.get(k)
            if w is not None and id(w) not in seen:
                seen.add(id(w)); deps.append(w)
        for k in writes:
            w = self.last_w.get(k)
            if w is not None and id(w) not in seen:
                seen.add(id(w)); deps.append(w)
            for r in self.readers.get(k, ()):
                if id(r) not in seen:
                    seen.add(id(r)); deps.append(r)
        if cc:
            op.src = ('cc', 0)
            op.pos = self.cc_cnt
            self.cc_cnt += 1
            if self.cc_last is not None and id(self.cc_last) not in seen:
                seen.add(id(self.cc_last)); deps.append(self.cc_last)
            self.cc_last = op
        elif dma:
            slot = self.dma_rr
            self.dma_rr = (self.dma_rr + 1) % self.n_dma_sems
            prev = self.dma_last[slot]
            if prev is not None and id(prev) not in seen:
                seen.add(id(prev)); deps.append(prev)
            op.src = ('dma', slot)
            op.pos = self.dma_cnt[slot]
            self.dma_cnt[slot] += 1
            self.dma_last[slot] = op
        else:
            op.src = eng
            op.pos = self.ncomp[eng]
            self.ncomp[eng] += 1
        waits = []
        deps.sort(key=lambda d: -d.pos)
        for d in deps:
            if (not dma) and (not d.is_dma) and d.src == eng:
                if eng == 'pe':
                    continue
                if op.pos - d.pos > 2:
                    continue
            w = self._need(eng, d)
            if w is not None:
                waits.append(w)
        op.waits = waits
        know = dict(self.known[eng])
        know[op.src] = op.pos
        op.know = know
        for k in reads:
            self.readers.setdefault(k, []).append(op)
        for k in writes:
            self.last_w[k] = op
            self.readers[k] = []
        self.ops[eng].append(op)
        return op

    def finish(self):
        op = Op()
        op.eng = 'sp'; op.fn = None; op.is_dma = False; op.signal = False
        op.src = 'sp'; op.pos = self.ncomp['sp']; self.ncomp['sp'] += 1
        waits = []
        for d in list(self.dma_last) + [self.cc_last]:
            if d is not None:
                w = self._need('sp', d)
                if w is not None:
                    waits.append(w)
        for e in ['pe', 'act', 'dve', 'pool']:
            comp = [o for o in self.ops[e] if not o.is_dma]
            if comp:
                w = self._need('sp', comp[-1])
                if w is not None:
                    waits.append(w)
        op.waits = waits
        op.know = {}
        self.ops['sp'].append(op)

    def emit(self, sems):
        nc = self.nc
        for e in ENGS:
            c = 0
            for o in self.ops[e]:
                if o.is_dma:
                    continue
                if o.signal:
                    c += 1
                o.cnt = c
        engobj = {'pe': nc.tensor, 'act': nc.scalar, 'dve': nc.vector, 'pool': nc.gpsimd, 'sp': nc.sync}

        def run(e):
            eo = engobj[e]
            for o in self.ops[e]:
                for d in o.waits:
                    if d.is_dma and d.src[0] == 'cc':
                        eo.wait_ge(sems[d.src], d.pos + 1)
                    elif d.is_dma:
                        eo.wait_ge(sems[d.src], 16 * (d.pos + 1))
                    else:
                        eo.wait_ge(sems[d.src], d.cnt)
                if o.fn is None:
                    continue
                ins = o.fn(eo)
                if o.is_dma and o.src[0] == 'cc':
                    ins.then_inc(sems[o.src])
                elif o.is_dma:
                    ins.then_inc(sems[o.src], 16)
                elif o.signal:
                    ins.then_inc(sems[e], 1)
        return run


class K:
    def __init__(self, nf32, nbf16):
        self.nc = bass.Bass("TRN2", target_bir_lowering=False)
        self.es = ExitStack()
        nc = self.nc
        self.S = Sched(nc)
        es = self.es
        self.af_t = es.enter_context(nc.sbuf_tensor("arena_f", [128, nf32], F32))
        self.ab_t = es.enter_context(nc.sbuf_tensor("arena_b", [128, nbf16], BF16))
        self.AF_ = Buf("af", self.af_t, 4, 0, nf32)
        self.AB_ = Buf("ab", self.ab_t, 2, 0, nbf16)
        self.ps = []
        for i in range(4):
            t = es.enter_context(nc.psum_tensor("ps%d" % i, [128, 1024], F32))
            self.ps.append(Buf("ps%d" % i, t, 4, 0, 1024))
        self.sems = {}
        for e in ENGS:
            self.sems[e] = es.enter_context(nc.semaphore("s_" + e))
        for i in range(self.S.n_dma_sems):
            self.sems[('dma', i)] = es.enter_context(nc.semaphore("d%d" % i))
        self.sems[('cc', 0)] = es.enter_context(nc.semaphore("ccsem"))
        self.fo = 0
        self.bo = 0
        self.dram = {}

    def din(self, name, shape, dt):
        ap = self.nc.dram_tensor(name, list(shape), dt, kind="ExternalInput").ap()
        d = DBuf(name, ap)
        self.dram[name] = d
        return d

    def dout(self, name, shape, dt):
        ap = self.nc.dram_tensor(name, list(shape), dt, kind="ExternalOutput").ap()
        d = DBuf(name, ap)
        self.dram[name] = d
        return d

    def fa(self, n):
        b = self.AF_.sub(self.fo, n)
        self.fo += n
        return b

    def ba(self, n):
        b = self.AB_.sub(self.bo, n)
        self.bo += n
        return b

    def mm(self, out, lhsT, rhs, start=True, stop=True):
        self.S.add('pe', lambda e: e.matmul(out.ap, lhsT.ap, rhs.ap, start=start, stop=stop),
                   reads=lhsT.keys + rhs.keys, writes=out.keys)

    def tr(self, out, in_, ident):
        self.S.add('pe', lambda e: e.transpose(out.ap, in_.ap, ident.ap),
                   reads=in_.keys + ident.keys, writes=out.keys)

    def act(self, out, in_, func, scale=1.0, bias=0.0, accum=None):
        reads = list(in_.keys)
        kw = {}
        if isinstance(bias, V):
            reads += bias.keys
            kw['bias'] = bias.ap
        else:
            kw['bias'] = float(bias)
        if isinstance(scale, V):
            reads += scale.keys
            kw['scale'] = scale.ap
        else:
            kw['scale'] = float(scale)
        writes = list(out.keys)
        if accum is not None:
            writes += accum.keys
            kw['accum_out'] = accum.ap
        self.S.add('act', lambda e: e.activation(out=out.ap, in_=in_.ap, func=func, **kw),
                   reads=reads, writes=writes)

    def tt(self, eng, out, in0, in1, op):
        self.S.add(eng, lambda e: e.tensor_tensor(out=out.ap, in0=in0.ap, in1=in1.ap, op=op),
                   reads=in0.keys + in1.keys, writes=out.keys)

    def ts(self, eng, out, in0, s1, s2=None, op0=ALU.mult, op1=None):
        reads = list(in0.keys)
        a1 = s1
        a2 = s2
        if isinstance(s1, V):
            reads += s1.keys; a1 = s1.ap
        if isinstance(s2, V):
            reads += s2.keys; a2 = s2.ap
        if op1 is None:
            self.S.add(eng, lambda e: e.tensor_scalar(out=out.ap, in0=in0.ap, scalar1=a1, scalar2=None, op0=op0),
                       reads=reads, writes=out.keys)
        else:
            self.S.add(eng, lambda e: e.tensor_scalar(out=out.ap, in0=in0.ap, scalar1=a1, scalar2=a2, op0=op0, op1=op1),
                       reads=reads, writes=out.keys)

    def stt(self, eng, out, in0, scalar, in1, op0, op1):
        reads = in0.keys + in1.keys
        a = scalar
        if isinstance(scalar, V):
            reads = reads + scalar.keys; a = scalar.ap
        self.S.add(eng, lambda e: e.scalar_tensor_tensor(out=out.ap, in0=in0.ap, scalar=a, in1=in1.ap, op0=op0, op1=op1),
                   reads=reads, writes=out.keys)

    def cp(self, eng, out, in_):
        if eng == 'act':
            self.S.add('act', lambda e: e.copy(out=out.ap, in_=in_.ap), reads=in_.keys, writes=out.keys)
        else:
            self.S.add(eng, lambda e: e.tensor_copy(out=out.ap, in_=in_.ap), reads=in_.keys, writes=out.keys)

    def red(self, eng, out, in_, op, axis=AX.X):
        self.S.add(eng, lambda e: e.tensor_reduce(out=out.ap, in_=in_.ap, axis=axis, op=op),
                   reads=in_.keys, writes=out.keys)

    def recip(self, out, in_):
        self.S.add('dve', lambda e: e.reciprocal(out=out.ap, in_=in_.ap), reads=in_.keys, writes=out.keys)

    def memset(self, eng, out, val):
        self.S.add(eng, lambda e: e.memset(out.ap, val), writes=out.keys)

    def dma(self, eng, out, in_):
        self.S.add(eng, lambda e: e.dma_start(out=out.ap, in_=in_.ap), reads=in_.keys, writes=out.keys, dma=True)

    def finalize(self):
        S = self.S
        S.finish()
        run = S.emit(self.sems)
        with self.nc.Block() as block:
            @block.tensor
            def _(e): run('pe')
            @block.scalar
            def _(e): run('act')
            @block.vector
            def _(e): run('dve')
            @block.gpsimd
            def _(e): run('pool')
            @block.sync
            def _(e): run('sp')
        self.es.close()
        return self.nc


def load_consts(k, need_rope):
    c = {}
    cin = k.din("consts", [128, 5 * 128], F32)
    cf = k.fa(256).sub(0, 128)
    k.dma('sp', cf.v(), V(cin.ap[:, 0:128], cin.v().keys))
    c['ident_f'] = cf
    cb = k.ba(5 * 128)
    k.dma('pool', cb.v(), cin.v())
    c['ident_b'] = cb.sub(0, 128)
    c['onesbd'] = cb.sub(128, 128)
    c['rotT'] = cb.sub(256, 128)
    c['maskA'] = cb.sub(384, 128)
    c['maskB'] = cb.sub(512, 128)
    ones = k.ba(128)
    k.ba(256)
    k.memset('pool', ones.v(), 1.0)
    c['ones_b'] = ones
    return c


def host_consts():
    ident = np.eye(128, dtype=np.float32)
    onesbd = np.zeros((128, 128), np.float32)
    onesbd[:64, :64] = 1.0
    onesbd[64:, 64:] = 1.0
    rotT = np.zeros((128, 128), np.float32)
    for m in range(128):
        j = m % 64
        if j < 32:
            rotT[m + 32, m] = -1.0
        else:
            rotT[m - 32, m] = 1.0
    u = np.arange(128)[:, None]
    a = np.arange(128)[None, :]
    maskA = np.where(u >= a, 0.0, NEG).astype(np.float32)
    maskB = np.where(u <= a, 0.0, NEG).astype(np.float32)
    return np.concatenate([ident, onesbd, rotT, maskA, maskB], axis=1)


TWO_PI = 2.0 * math.pi
C1 = 6.28125
C2 = float(np.float32(TWO_PI - 6.28125))
C3 = float(TWO_PI - 6.28125 - float(np.float32(TWO_PI - 6.28125)))


WIN_GROUPS = [('ka', 512), ('kb', 768), ('va', 512), ('vb', 768), ('qa', 512), ('qb', 768), ('ga', 1024), ('gb', 1024)]


def rms_mod(k, xT, hT, c, scale_col, shift_col, work_f, work_b, h32_hook=None):
    sq = [work_b.sub(i * 512, 512) for i in range(2)]
    sd = work_f.sub(0, 512)
    rs = work_f.sub(512, 512)
    tmp = [work_f.sub(1024 + i * 512, 512) for i in range(2)]
    for tb in range(4):
        pss = k.ps[tb % 2].sub(0, 512)
        for cc in range(8):
            s = sq[cc % 2]
            k.act(s.v(), xT.v(cc * T + tb * 512, cc * T + tb * 512 + 512), AF.Square)
            k.mm(pss.v(), c['ones_b'].v(), s.v(), start=(cc == 0), stop=(cc == 7))
        k.act(sd.v(), pss.v(), AF.Sqrt, scale=1.0 / D, bias=EPS)
        k.recip(rs.v(), sd.v())
        for cc in range(8):
            t = tmp[cc % 2]
            k.tt('pool', t.v(), xT.v(cc * T + tb * 512, cc * T + tb * 512 + 512), rs.v(), ALU.mult)
            if h32_hook is not None:
                h32 = h32_hook(tb, cc)
                k.ts('dve', h32.v(), t.v(), scale_col.v(cc, cc + 1), shift_col.v(cc, cc + 1), ALU.mult, ALU.add)
                k.cp('pool', hT.v(cc * T + tb * 512, cc * T + tb * 512 + 512), h32.v())
            else:
                k.ts('dve', hT.v(cc * T + tb * 512, cc * T + tb * 512 + 512), t.v(),
                     scale_col.v(cc, cc + 1), shift_col.v(cc, cc + 1), ALU.mult, ALU.add)
        if h32_hook is not None:
            h32_hook(tb, None)


def body_A(k, c, xT, D, l):
    stg = 99
    ccol_d = D['c_col']
    pos_d = D['posb']
    invf_d = D['invf']
    wada_d = D['w_ada%d' % l]
    bada_d = D['b_ada%d' % l]
    gains_d = D['gains%d' % l]
    wg_d = {n: D['w_%s%d' % (n, l)] for n, nc_ in WIN_GROUPS}
    kTa_o = D['kTa_loc']
    kTb_o = D['kTb_loc']
    qTa_o = D['qTa_s']
    qTb_o = D['qTb_s']
    va_o = D['va_loc']
    vb_o = D['vb_loc']
    gates_o = D['gates_s']
    mod_o = D['mod_s']

    cosT = k.fa(T)
    sinT = k.fa(T)
    small = k.fa(256)
    work_f = k.fa(4096)
    hT = k.ba(8 * T)
    wring = [k.ba(8192) for _ in range(3)]
    work_b = k.ba(2048)
    stage = [k.ba(2048) for _ in range(2)]
    vst = [k.ba(1024) for _ in range(2)]

    ccol = small.sub(0, 8)
    cact = k.ba(8)
    bada = small.sub(8, 48)
    mod = small.sub(56, 48)
    gains = small.sub(104, 4)
    invf = small.sub(108, 1)

    k.dma('sp', ccol.v(), ccol_d.v())
    k.dma('sp', bada.v(), bada_d.v())
    k.dma('sp', gains.v(), gains_d.v())
    k.dma('sp', invf.v(), invf_d.v())

    ang = work_f.sub(0, T)
    kk = work_f.sub(T, T)
    posi_v = V(kk.v().ap.bitcast(I32), kk.v().keys)
    k.dma('sp', posi_v, pos_d.v())
    k.cp('dve', ang.v(), posi_v)
    k.ts('dve', ang.v(), ang.v(), invf.v(), None, ALU.mult)
    k.ts('dve', kk.v(), ang.v(), 1.0 / TWO_PI, 12582912.0, ALU.mult, ALU.add)
    k.ts('dve', kk.v(), kk.v(), 12582912.0, None, ALU.subtract)
    k.stt('dve', ang.v(), kk.v(), -C1, ang.v(), ALU.mult, ALU.add)
    k.stt('dve', ang.v(), kk.v(), -C2, ang.v(), ALU.mult, ALU.add)
    k.stt('dve', ang.v(), kk.v(), -C3, ang.v(), ALU.mult, ALU.add)
    k.ts('dve', ang.v(), ang.v(), math.pi, -math.pi, ALU.min, ALU.max)
    k.act(sinT.v(), ang.v(), AF.Sin)
    k.act(kk.v(), ang.v(), AF.Sin, scale=0.5)
    k.tt('dve', kk.v(), kk.v(), kk.v(), ALU.mult)
    k.ts('dve', cosT.v(), kk.v(), -2.0, 1.0, ALU.mult, ALU.add)

    k.act(cact.v(), ccol.v(), AF.Silu)
    psm = k.ps[3].sub(0, 48)
    for s in range(6):
        wb = wring[s % 3]
        k.dma('pool', wb.v(), V(wada_d.ap[s], wada_d.v(key=s).keys))
        for cc in range(8):
            for kc in range(8):
                k.mm(psm.v(s * 8 + cc, s * 8 + cc + 1),
                     wb.v(kc * 1024 + cc * 128, kc * 1024 + cc * 128 + 128),
                     cact.v(kc, kc + 1), start=(kc == 0), stop=(kc == 7))
    k.tt('dve', mod.v(), psm.v(), bada.v(), ALU.add)
    k.dma('sp', mod_o.v(), mod.v())
    sc1 = small.sub(152, 8)
    k.ts('dve', sc1.v(), mod.v(8, 16), 1.0, None, ALU.add)

    rms_mod(k, xT, hT, c, sc1, mod.sub(0, 8), work_f, work_b)

    raw = [work_f.sub(i * 512, 512) for i in range(2)]
    rst = [work_f.sub(1024 + i * 512, 512) for i in range(2)]
    t1 = [work_f.sub(2048 + i * 512, 512) for i in range(2)]
    t2 = [work_f.sub(3072 + i * 512, 512) for i in range(2)]
    sqb = [work_b.sub(i * 512, 512) for i in range(2)]
    qnb = [work_b.sub(1024 + i * 512, 512) for i in range(2)]
    cnt = [0]

    def qk_block(psb, gcol, outv, tb):
        i = cnt[0] % 2
        cnt[0] += 1
        k.act(sqb[i].v(), psb.v(), AF.Square)
        k.ts('dve', raw[i].v(), psb.v(), gcol, None, ALU.mult)
        ps2 = k.ps[2].sub(i * 512, 512)
        k.mm(ps2.v(), c['onesbd'].v(), sqb[i].v())
        k.act(rst[i].v(), ps2.v(), AF.Sqrt, scale=1.0 / 64, bias=EPS)
        k.recip(rst[i].v(), rst[i].v())
        k.tt('pool', qnb[i].v(), raw[i].v(), rst[i].v(), ALU.mult)
        ps3 = k.ps[3].sub(i * 512, 512)
        k.mm(ps3.v(), c['rotT'].v(), qnb[i].v())
        k.tt('pool', t1[i].v(), qnb[i].v(), cosT.v(tb * 512, tb * 512 + 512), ALU.mult)
        k.tt('dve', t2[i].v(), ps3.v(), sinT.v(tb * 512, tb * 512 + 512), ALU.mult)
        k.tt('pool', outv, t1[i].v(), t2[i].v(), ALU.add)

    pcnt = [0]
    def wload_g(gj):
        nm_, nc__ = WIN_GROUPS[gj]
        k.dma('pool', wring[gj % 3].v(0, 8 * nc__), wg_d[nm_].v())

    wload_g(0)
    wload_g(1)
    for gi, (name, ncol) in enumerate(WIN_GROUPS):
        wb = wring[gi % 3]
        if name == 'qa':
            k.after_kv()
        if gi + 2 < len(WIN_GROUPS):
            wload_g(gi + 2)
        if name in ('ka', 'kb', 'qa', 'qb'):
            npair = ncol // 128
            gidx = {'qa': 0, 'ka': 1, 'qb': 2, 'kb': 3}[name]
            od = {'ka': kTa_o, 'kb': kTb_o, 'qa': qTa_o, 'qb': qTb_o}[name]
            for pr in range(npair):
                st = stage[pr % 2]
                for tb in range(4):
                    psb = k.ps[pcnt[0] % 2].sub(512 * ((pcnt[0] // 2) % 2), 512)
                    pcnt[0] += 1
                    for kc in range(8):
                        k.mm(psb.v(), wb.v(kc * ncol + pr * 128, kc * ncol + pr * 128 + 128),
                             hT.v(kc * T + tb * 512, kc * T + tb * 512 + 512), start=(kc == 0), stop=(kc == 7))
                    qk_block(psb, gains.v(gidx, gidx + 1), st.v(tb * 512, tb * 512 + 512), tb)
                if name in ('ka', 'kb'):
                    odp = od[pr // 2]
                    k.dma('sp', V(odp.ap[(pr % 2) * 128:(pr % 2 + 1) * 128, :], odp.v().keys), st.v())
                else:
                    k.dma('sp', V(od.ap[:, pr * T:(pr + 1) * T], od.v().keys), st.v())
        elif name in ('va', 'vb'):
            nh_, dh_, d_ = (4, 129, 128) if name == 'va' else (12, 64, 64)
            od = va_o if name == 'va' else vb_o
            w_ = nh_ * dh_
            for i in range(2):
                k.memset('pool', vst[i].v(0, w_), 1.0)
            for tt_ in range(16):
                st = vst[tt_ % 2]
                for n0 in range(0, ncol, 512):
                    nn = min(512, ncol - n0)
                    psb = k.ps[pcnt[0] % 2].sub(512 * ((pcnt[0] // 2) % 2), 512)
                    pcnt[0] += 1
                    for kc in range(8):
                        k.mm(psb.v(0, nn), hT.v(kc * T + tt_ * 128, kc * T + tt_ * 128 + 128),
                             wb.v(kc * ncol + n0, kc * ncol + n0 + nn), start=(kc == 0), stop=(kc == 7))
                    h0 = n0 // d_
                    nhh = nn // d_
                    outv = V(st.v(h0 * dh_, (h0 + nhh) * dh_, pat="p (h c) -> p h c", c=dh_).ap[:, :, 0:d_],
                             st.keys(h0 * dh_, (h0 + nhh) * dh_))
                    inv = V(psb.v(0, nn, pat="p (h c) -> p h c", c=d_).ap, psb.keys(0, nn))
                    k.cp('act', outv, inv)
                if name == 'va':
                    for h4 in range(4):
                        k.dma('sp', V(od[h4].ap[tt_ * 128:(tt_ + 1) * 128, :], od[h4].v().keys), st.v(h4 * 129, h4 * 129 + 129))
                else:
                    for g3 in range(3):
                        k.dma('sp', V(od[g3].ap[tt_ * 128:(tt_ + 1) * 128, :], od[g3].v().keys), st.v(g3 * 256, g3 * 256 + 256))
        else:
            gi_ = 0 if name == 'ga' else 1
            for cc in range(8):
                st = stage[cc % 2]
                for tb in range(4):
                    psb = k.ps[pcnt[0] % 2].sub(512 * ((pcnt[0] // 2) % 2), 512)
                    pcnt[0] += 1
                    for kc in range(8):
                        k.mm(psb.v(), wb.v(kc * ncol + cc * 128, kc * ncol + cc * 128 + 128),
                             hT.v(kc * T + tb * 512, kc * T + tb * 512 + 512), start=(kc == 0), stop=(kc == 7))
                    k.act(st.v(tb * 512, tb * 512 + 512), psb.v(), AF.Sigmoid)
                o0 = (gi_ * 8 + cc) * T
                k.dma('sp', V(gates_o.ap[:, o0:o0 + T], gates_o.v().keys), st.v())
    return


def body_B(k, c, xT, D, l):
    mod_d = D['mod_s']
    qTa_d = D['qTa_s']
    qTb_d = D['qTb_s']
    kTa_d = D['kTa_all']
    va_d = D['va_all']
    kTb_d = D['kTb_all']
    vb_d = D['vb_all']
    gates_d = D['gates_s']
    lam_d = D['lamp%d' % l]
    subln_d = D['subln%d' % l]
    wpa_d = D['w_pa%d' % l]
    wpb_d = D['w_pb%d' % l]
    wo_d = D['w_o%d' % l]
    wr_d = D['w_r%d' % l]
    br_d = D['b_r%d' % l]
    sel_d = D['sel']
    ehd_d = D['ehd']
    we_d = D['w_e%d' % l]

    small = k.fa(1024)
    fwork = k.fa(25600 - k.fo)
    mod = small.sub(0, 48)
    lamp = small.sub(48, 258)
    subln = small.sub(320, 128)
    br = small.sub(448, 36)
    sc2 = small.sub(484, 8)
    lamcol = small.sub(492, 4)
    tmp64 = small.sub(512, 128)
    wr = small.sub(640, 288)

    k.dma('sp', mod.v(), mod_d.v())
    k.dma('sp', lamp.v(), lam_d.v())
    k.dma('sp', subln.v(), subln_d.v())
    k.dma('sp', br.v(), br_d.v())
    k.dma('sp', wr.v(), wr_d.v())

    k.tt('dve', tmp64.v(0, 64), lamp.v(0, 64), lamp.v(64, 128), ALU.mult)
    k.tt('dve', tmp64.v(64, 128), lamp.v(128, 192), lamp.v(192, 256), ALU.mult)
    k.red('dve', lamcol.v(0, 2), tmp64.v(0, 128, pat="p (a w) -> p a w", w=64), ALU.add)
    k.act(lamcol.v(0, 2), lamcol.v(0, 2), AF.Exp)
    k.tt('dve', lamcol.v(3, 4), lamcol.v(0, 1), lamcol.v(1, 2), ALU.subtract)
    k.tt('dve', lamcol.v(0, 1), lamcol.v(3, 4), lamp.v(256, 257), ALU.add)
    k.ts('dve', lamcol.v(1, 2), lamcol.v(0, 1), -1.0, None, ALU.mult)
    k.ts('dve', sc2.v(), mod.v(32, 40), 1.0, None, ALU.add)

    AB = k.AB_
    b0 = k.bo
    qTa = AB.sub(b0 + 0, 8192)
    oaT = AB.sub(b0 + 8192, 8192)
    obT = AB.sub(b0 + 16384, 4096)
    qTb = AB.sub(b0 + 20480, 12288)
    kring = [AB.sub(b0 + 32768 + i * 2048, 2048) for i in range(3)]
    vring = [AB.sub(b0 + 38912 + i * 2048, 2048) for i in range(3)]
    PT = [AB.sub(b0 + 45104 + i * 1024, 1024) for i in range(2)]

    k.dma('sp', qTa.v(), qTa_d.v())
    PT4 = [AB.sub(b0 + 45056 + i * 1024, 1024) for i in range(4)]
    tsum = AB.sub(b0 + 49152, 1024)
    acc = fwork.sub(1024, 1024)
    dsb = fwork.sub(2048, 512)
    rsb = fwork.sub(2560, 512)
    r2 = fwork.sub(3072, 512)
    slcol = fwork.sub(0, 1)
    k.tt('dve', slcol.v(), subln.v(0, 1), lamp.v(257, 258), ALU.mult)
    ld = [0]
    for h in range(4):
        for qb in range(4):
            OT = k.ps[2]

            SB = [0, 1, 3]

            def qk(kt, kbuf):
                pss = k.ps[SB[kt % 3]]
                for m in range(2):
                    k.mm(pss.v(m * 512, m * 512 + 512),
                         kbuf.v((kt % 16) * 128, (kt % 16) * 128 + 128, p0=m * 64, p1=m * 64 + 64),
                         qTa.v(h * T + qb * 512, h * T + qb * 512 + 512, p0=m * 64, p1=m * 64 + 64))

            bufs = {}

            def load(ch):
                i = ld[0] % 3
                ld[0] += 1
                kb_, vb_ = kring[i], vring[i]
                k.dma('sp', kb_.v(), V(kTa_d[h // 2].ap[ch * 256 + (h % 2) * 128:ch * 256 + (h % 2) * 128 + 128, :], kTa_d[h // 2].v().keys))
                src = va_d[h].ap[ch * 2048:(ch + 1) * 2048, 0:128].rearrange("(t p) c -> p t c", p=128)
                k.dma('sp', V(vb_.v(0, 2048, pat="p (t c) -> p t c", c=128).ap, vb_.keys(0, 2048)), V(src, va_d[h].v().keys))
                bufs[ch] = (kb_, vb_)

            load(0)
            load(1)
            qk(0, bufs[0][0])
            qk(1, bufs[0][0])
            for kt in range(64):
                ch = kt // 16
                if kt % 16 == 0 and ch + 2 < 4:
                    load(ch + 2)
                if kt + 2 < 64:
                    qk(kt + 2, bufs[(kt + 2) // 16][0])
                pt = PT4[kt % 4]
                k.act(pt.v(), k.ps[SB[kt % 3]].v(), AF.Exp, scale=0.125)
                vb_ = bufs[ch][1]
                for m in range(2):
                    k.mm(OT.v(m * 512, m * 512 + 512), vb_.v((kt % 16) * 128, (kt % 16) * 128 + 128),
                         pt.v(m * 512, m * 512 + 512), start=(kt == 0), stop=(kt == 63))
                if kt % 4 == 1:
                    k.tt('dve', tsum.v(), PT4[(kt - 1) % 4].v(), pt.v(), ALU.add)
                elif kt % 4 == 3:
                    k.tt('dve', tsum.v(), tsum.v(), PT4[(kt - 1) % 4].v(), ALU.add)
                    k.tt('dve', tsum.v(), tsum.v(), pt.v(), ALU.add)
                    if kt == 3:
                        k.cp('dve', acc.v(), tsum.v())
                    else:
                        k.tt('dve', acc.v(), acc.v(), tsum.v(), ALU.add)
            k.cp('dve', tsum.v(), acc.v())
            for m in range(2):
                psd = k.ps[0].sub(m * 512, 512)
                k.mm(psd.v(), c['ones_b'].v(), tsum.v(m * 512, m * 512 + 512))
            k.recip(rsb.v(), k.ps[0].v(0, 512))
            k.recip(r2.v(), k.ps[0].v(512, 1024))
            k.tt('dve', dsb.v(), OT.v(0, 512), rsb.v(), ALU.mult)
            k.tt('dve', r2.v(), OT.v(512, 1024), r2.v(), ALU.mult)
            k.stt('dve', dsb.v(), r2.v(), lamcol.v(1, 2), dsb.v(), ALU.mult, ALU.add)
            sqA = PT4[0].sub(0, 512)
            k.act(sqA.v(), dsb.v(), AF.Square)
            pss_ = k.ps[1].sub(0, 512)
            k.mm(pss_.v(), c['ones_b'].v(), sqA.v())
            k.act(rsb.v(), pss_.v(), AF.Sqrt, scale=1.0 / 128, bias=EPS)
            k.recip(rsb.v(), rsb.v())
            k.stt('dve', oaT.v(h * T + qb * 512, h * T + qb * 512 + 512), dsb.v(), slcol.v(), rsb.v(), ALU.mult, ALU.mult)

    k.dma('sp', qTb.v(), qTb_d.v())
    kbb = [AB.sub(b0 + i * 4096, 4096) for i in range(2)]
    vtr = [AB.sub(b0 + 32768 + i * 260, 260) for i in range(8)]
    numT = fwork.sub(512, 2 * T)
    denT = fwork.sub(512 + 2 * T, T)
    Osb = fwork.sub(512 + 3 * T, 260)
    vld = [0]
    vtile_no = [0]
    U32 = mybir.dt.uint32
    idx_t = k.idx_t
    kidx = Buf("idx_t", idx_t, 4, 0, 24)
    vidx = Buf("idx_t", idx_t, 4, 24, 69)
    vmask = fwork.sub(7200, 69)
    k.dma('sp', vmask.v(), D['vmask'].v())
    kTb_view = [d_.ap.rearrange("r (h c) -> (r h) c", h=2) for d_ in kTb_d]
    vstg = [AB.sub(b0 + 32768 + 8 * 260 + i * 256, 256) for i in range(4)]

    def igather(outv, src_ap, src_d, idxv):
        k.S.add('pool', lambda e: e.indirect_dma_start(out=outv.ap, out_offset=None, in_=src_ap,
                                                       in_offset=bass.IndirectOffsetOnAxis(ap=idxv.ap.bitcast(U32), axis=0)),
                reads=src_d.v().keys + idxv.keys, writes=outv.keys, dma=True)
    for g, dil in enumerate([1, 4, 16]):
        for j in range(2):
            for seg in range(4):
                col = (g * 2 + j) * 4 + seg
                igather(kbb[j].v(seg * 1024, seg * 1024 + 1024), kTb_view[g], kTb_d[g], kidx.v(col, col + 1))
        ntile = 16 // dil
        for r in range(dil):
            vt = {}

            def vload(n):
                i = vld[0] % 8
                vld[0] += 1
                tno = vtile_no[0]
                vtile_no[0] += 1
                vs_ = vstg[tno % 4]
                igather(vs_.v(), vb_d[g].ap, vb_d[g], vidx.v(tno, tno + 1))
                k.ts('dve', V(vtr[i].v(0, 260, pat="p (h c) -> p h c", c=65).ap[:, :, 0:64], vtr[i].keys(0, 260)),
                     V(vs_.v(0, 256, pat="p (h c) -> p h c", c=64).ap, vs_.keys(0, 256)), vmask.v(tno, tno + 1), None, ALU.mult)
                k.ts('dve', V(vtr[i].v(0, 260, pat="p (h c) -> p h c", c=65).ap[:, :, 64], vtr[i].keys(0, 260)),
                     c['ones_b'].v(0, 4), vmask.v(tno, tno + 1), None, ALU.mult)
                vt[n] = vtr[i]

            vload(0)
            for m in range(ntile):
                vload(m + 1)
                pss = k.ps[m % 2]
                i0 = 128 * m * dil + r
                for hh in range(4):
                    j, half = hh // 2, hh % 2
                    for ab in range(2):
                        n = m + ab
                        w0 = PADW + (128 * n - 64) * dil + r
                        col = (hh * 2 + ab) * 128
                        k.mm(pss.v(col, col + 128), c['ident_b'].v(), c['maskA' if ab == 0 else 'maskB'].v(), start=True, stop=False)
                        k.mm(pss.v(col, col + 128),
                             kbb[j].v(w0, w0 + 128 * dil - (dil - 1), p0=half * 64, p1=half * 64 + 64, step=dil),
                             qTb.v((g * 2 + j) * T + i0, (g * 2 + j) * T + i0 + 128 * dil - (dil - 1), p0=half * 64, p1=half * 64 + 64, step=dil),
                             start=False, stop=True)
                pt = PT[m % 2]
                k.act(pt.v(), pss.v(), AF.Exp, scale=0.125)
                Ops = k.ps[2 + (m % 2)].sub(0, 260)
                for hh in range(4):
                    for ab in range(2):
                        col = (hh * 2 + ab) * 128
                        k.mm(Ops.v(hh * 65, hh * 65 + 65), pt.v(col, col + 128), vt[m + ab].v(hh * 65, hh * 65 + 65),
                             start=(ab == 0), stop=(ab == 1))
                k.cp('dve', V(Osb.v(0, 256, pat="p (h c) -> p h c", c=64).ap, Osb.keys(0, 256)),
                     V(Ops.v(0, 260, pat="p (h c) -> p h c", c=65).ap[:, :, 0:64], Ops.keys(0, 260)))
                k.cp('dve', Osb.v(256, 260), V(Ops.v(0, 260, pat="p (h c) -> p h c", c=65).ap[:, :, 64], Ops.keys(0, 260)))
                pT_ = k.ps[2 + (m % 2)]
                for j in range(2):
                    k.tr(pT_.v(512 + j * 128, 512 + j * 128 + 128), Osb.v(j * 128, j * 128 + 128), c['ident_f'].v())
                k.tr(pT_.v(768, 896, p0=0, p1=4), Osb.v(256, 260), c['ident_f'].v())
                for j in range(2):
                    dst = numT.v(j * T + i0, j * T + i0 + 128 * dil - (dil - 1), step=dil)
                    if g == 0:
                        k.cp('dve', dst, pT_.v(512 + j * 128, 512 + j * 128 + 128))
                    else:
                        k.tt('dve', dst, pT_.v(512 + j * 128, 512 + j * 128 + 128), dst, ALU.add)
                dst = denT.v(i0, i0 + 128 * dil - (dil - 1), p0=0, p1=4, step=dil)
                if g == 0:
                    k.cp('dve', dst, pT_.v(768, 896, p0=0, p1=4))
                else:
                    k.tt('dve', dst, pT_.v(768, 896, p0=0, p1=4), dst, ALU.add)
    ehd_t = fwork.sub(512 + 3 * T + 260, 256)
    k.dma('sp', ehd_t.v(p0=0, p1=4), ehd_d.v())
    k.recip(denT.v(p0=0, p1=4), denT.v(p0=0, p1=4))
    for j in range(2):
        for tb in range(4):
            psb = k.ps[tb % 2].sub(0, 512)
            k.mm(psb.v(), ehd_t.v(j * 128, j * 128 + 128, p0=0, p1=4), denT.v(tb * 512, tb * 512 + 512, p0=0, p1=4))
            k.tt('dve', obT.v(j * T + tb * 512, j * T + tb * 512 + 512), numT.v(j * T + tb * 512, j * T + tb * 512 + 512), psb.v(), ALU.mult)

    wo = AB.sub(b0 + 20480, 8192)
    wpa = AB.sub(b0 + 28672, 4096)
    gat = AB.sub(b0 + 32768, 8192)
    mrg = AB.sub(b0 + 40960, 4096)
    wpb = AB.sub(b0 + 45056, 2048)
    k.dma('pool', wo.v(), wo_d.v())
    k.dma('pool', wpa.v(), wpa_d.v())
    k.dma('pool', wpb.v(), wpb_d.v())
    m1 = fwork.sub(0, 512)
    for tb in range(4):
        for gi_ in range(2):
            src = gates_d.ap[:, gi_ * 8 * T: (gi_ + 1) * 8 * T].rearrange("p (c t) -> p c t", t=T)[:, :, tb * 512:(tb + 1) * 512]
            k.dma('sp', V(gat.v(gi_ * 4096, gi_ * 4096 + 4096, pat="p (c t) -> p c t", t=512).ap, gat.keys(gi_ * 4096, gi_ * 4096 + 4096)),
                  V(src, gates_d.v().keys))
        for cc in range(8):
            psa = k.ps[cc % 2].sub(0, 512)
            psb = k.ps[cc % 2].sub(512, 512)
            for kc in range(4):
                k.mm(psa.v(), wpa.v(kc * 1024 + cc * 128, kc * 1024 + cc * 128 + 128),
                     oaT.v(kc * T + tb * 512, kc * T + tb * 512 + 512), start=(kc == 0), stop=(kc == 3))
            for kc in range(2):
                k.mm(psb.v(), wpb.v(kc * 1024 + cc * 128, kc * 1024 + cc * 128 + 128),
                     obT.v(kc * T + tb * 512, kc * T + tb * 512 + 512), start=(kc == 0), stop=(kc == 1))
            k.tt('dve', m1.v(), psa.v(), gat.v(cc * 512, cc * 512 + 512), ALU.mult)
            k.tt('dve', mrg.v(cc * 512, cc * 512 + 512), psb.v(), gat.v(4096 + cc * 512, 4096 + cc * 512 + 512), ALU.mult)
            k.tt('pool', mrg.v(cc * 512, cc * 512 + 512), mrg.v(cc * 512, cc * 512 + 512), m1.v(), ALU.add)
        for cc in range(8):
            psy = k.ps[2 + cc % 2].sub(0, 512)
            for kc in range(8):
                k.mm(psy.v(), wo.v(kc * 1024 + cc * 128, kc * 1024 + cc * 128 + 128),
                     mrg.v(kc * 512, kc * 512 + 512), start=(kc == 0), stop=(kc == 7))
            xs = xT.v(cc * T + tb * 512, cc * T + tb * 512 + 512)
            k.stt('dve', xs, psy.v(), mod.v(16 + cc, 17 + cc), xs, ALU.mult, ALU.add)

    hT = AB.sub(b0, 8 * T)
    wring = [AB.sub(b0 + 16384 + i * 6144, 6144) for i in range(4)]
    hid = [AB.sub(b0 + 40960 + i * 4096, 4096) for i in range(2)]
    wb2 = AB.sub(b0 + 49152, 1024)
    h32b = fwork.sub(2048, 4096)
    lg = fwork.sub(6144, 36 * 16)
    comb = fwork.sub(6144 + 576, 32 * 16)
    rw = fwork.sub(6144 + 576 + 512, 96)

    def hook(tb, cc):
        if cc is not None:
            return h32b.sub(cc * 512, 512)
        for q in range(4):
            tt_ = tb * 4 + q
            psl = k.ps[2 + q % 2].sub(0, 36)
            for kc in range(8):
                k.mm(psl.v(), h32b.v(kc * 512 + q * 128, kc * 512 + q * 128 + 128), wr.v(kc * 36, kc * 36 + 36),
                     start=(kc == 0), stop=(kc == 7))
            k.tt('dve', lg.v(tt_ * 36, tt_ * 36 + 36), psl.v(), br.v(), ALU.add)
        return None

    rms_mod(k, xT, hT, c, sc2, mod.sub(24, 8), fwork.sub(0, 2048), wb2, h32_hook=hook)

    for tt_ in range(16):
        l1 = lg.v(tt_ * 36, tt_ * 36 + 4)
        l2 = lg.sub(tt_ * 36 + 4, 32)
        r = rw
        m1c = r.v(0, 1); nm1 = r.v(1, 2); e1 = r.v(2, 6); s1 = r.v(6, 7); gval = r.v(7, 8)
        oh = r.sub(8, 4); l2m = r.sub(12, 32); ig = r.sub(44, 8); v1 = r.v(52, 53); mk1 = r.sub(53, 8)
        ig2 = r.sub(61, 8); v2 = r.v(69, 70); mk2 = r.sub(70, 8); dd = r.v(78, 79); w1 = r.v(79, 80); w2 = r.v(80, 81)
        cig = r.sub(81, 8)
        k.red('dve', m1c, l1, ALU.max)
        k.ts('dve', nm1, m1c, -1.0, None, ALU.mult)
        k.act(e1, l1, AF.Exp, bias=nm1, accum=s1)
        k.recip(gval, s1)
        k.ts('dve', oh.v(), l1, m1c, None, ALU.is_equal)
        k.tt('dve', V(l2m.v(0, 32, pat="p (g e) -> p g e", e=8).ap, l2m.keys(0, 32)),
             V(l2.v(0, 32, pat="p (g e) -> p g e", e=8).ap, l2.keys(0, 32)),
             V(oh.v(0, 4).ap.unsqueeze(2).to_broadcast([128, 4, 8]), oh.keys(0, 4)), ALU.mult)
        k.red('dve', ig.v(), V(l2m.v(0, 32, pat="p (g e) -> p e g", e=8).ap, l2m.keys(0, 32)), ALU.add)
        k.red('dve', v1, ig.v(), ALU.max)
        k.ts('dve', mk1.v(), ig.v(), v1, None, ALU.is_equal)
        k.stt('dve', ig2.v(), mk1.v(), -1e30, ig.v(), ALU.mult, ALU.add)
        k.red('dve', v2, ig2.v(), ALU.max)
        k.ts('dve', mk2.v(), ig2.v(), v2, None, ALU.is_equal)
        k.tt('dve', dd, v1, v2, ALU.subtract)
        k.act(w1, dd, AF.Sigmoid)
        k.act(w2, dd, AF.Sigmoid, scale=-1.0)
        k.tt('dve', w1, w1, gval, ALU.mult)
        k.tt('dve', w2, w2, gval, ALU.mult)
        k.ts('dve', cig.v(), mk1.v(), w1, None, ALU.mult)
        k.stt('dve', cig.v(), mk2.v(), w2, cig.v(), ALU.mult, ALU.add)
        k.tt('dve', V(comb.v(tt_ * 32, tt_ * 32 + 32, pat="p (g e) -> p g e", e=8).ap, comb.keys(tt_ * 32, tt_ * 32 + 32)),
             V(oh.v(0, 4).ap.unsqueeze(2).to_broadcast([128, 4, 8]), oh.keys(0, 4)),
             V(cig.v(0, 8).ap.unsqueeze(1).to_broadcast([128, 4, 8]), cig.keys(0, 8)), ALU.mult)
    CTt = fwork.sub(0, 2048)
    for tt_ in range(16):
        pst = k.ps[2 + tt_ % 2].sub(512, 128)
        k.tr(pst.v(p0=0, p1=32), comb.v(tt_ * 32, tt_ * 32 + 32), c['ident_f'].v())
        k.cp('dve', CTt.v(tt_ * 128, tt_ * 128 + 128, p0=0, p1=32), pst.v(p0=0, p1=32))
    selt = fwork.sub(2048, 4096)
    k.dma('sp', selt.v(p0=0, p1=32), sel_d.v())

    G = 2
    sg = [fwork.sub(6144 + i * 512, 512) for i in range(2)]

    def wload(e):
        k.dma('pool', wring[e % 4].v(), V(we_d.ap[e], we_d.v(key=e).keys))

    for e in range(4):
        wload(e)
    hcnt = [0]
    for eg in range(NEXP // G):
        if eg >= 1 and (eg + 1) * G < NEXP:
            for ei in range(G):
                wload((eg + 1) * G + ei)
        for tb in range(4):
            hb = hid[hcnt[0] % 2]
            hcnt[0] += 1
            for ei in range(G):
                e = eg * G + ei
                w = wring[e % 4]
                psc = k.ps[3].sub(512, 512)
                k.mm(psc.v(), selt.v(e * 128, e * 128 + 128, p0=0, p1=32), CTt.v(tb * 512, tb * 512 + 512, p0=0, p1=32))
                for ch in range(2):
                    psg = k.ps[ch].sub(0, 512)
                    psu = k.ps[ch].sub(512, 512)
                    for kc in range(8):
                        k.mm(psg.v(), w.v(kc * 256 + ch * 128, kc * 256 + ch * 128 + 128),
                             hT.v(kc * T + tb * 512, kc * T + tb * 512 + 512), start=(kc == 0), stop=(kc == 7))
                    for kc in range(8):
                        k.mm(psu.v(), w.v(2048 + kc * 256 + ch * 128, 2048 + kc * 256 + ch * 128 + 128),
                             hT.v(kc * T + tb * 512, kc * T + tb * 512 + 512), start=(kc == 0), stop=(kc == 7))
                    s_ = sg[ch]
                    k.act(s_.v(), psg.v(), AF.Silu)
                    k.tt('dve', s_.v(), psu.v(), s_.v(), ALU.mult)
                    k.tt('dve', hb.v((ei * 2 + ch) * 512, (ei * 2 + ch) * 512 + 512), psc.v(), s_.v(), ALU.mult)
            for cc in range(8):
                psy = k.ps[2].sub((cc % 2) * 512, 512)
                for ei in range(G):
                    w = wring[(eg * G + ei) % 4]
                    for ch in range(2):
                        k.mm(psy.v(), w.v(4096 + ch * 1024 + cc * 128, 4096 + ch * 1024 + cc * 128 + 128),
                             hb.v((ei * 2 + ch) * 512, (ei * 2 + ch) * 512 + 512),
                             start=(ei == 0 and ch == 0), stop=(ei == G - 1 and ch == 1))
                xs = xT.v(cc * T + tb * 512, cc * T + tb * 512 + 512)
                k.stt('dve', xs, psy.v(), mod.v(40 + cc, 41 + cc), xs, ALU.mult, ALU.add)
    return


def build_F():
    k = K(nf32=25600, nbf16=51200)
    nc = k.nc
    D = {}

    def ext(name, shape, dt):
        D[name] = k.din(name, shape, dt)

    def itn(name, shape, dt):
        D[name] = DBuf(name, nc.dram_tensor(name, list(shape), dt).ap())

    ext('xT', [128, 8 * T], F32); ext('c_col', [128, 8], F32); ext('posb', [128, T], I32); ext('invf', [128, 1], F32)
    ext('sel', [32, 32 * 128], F32); ext('ehd', [4, 256], F32); ext('idx', [128, 93], I32); ext('vmask', [128, 69], F32)
    for l in range(2):
        ext('w_ada%d' % l, [6, 128, 8 * 1024], F32); ext('b_ada%d' % l, [128, 48], F32); ext('gains%d' % l, [128, 4], F32)
        for n, nc_ in WIN_GROUPS:
            ext('w_%s%d' % (n, l), [128, 8 * nc_], F32)
        ext('lamp%d' % l, [128, 258], F32); ext('subln%d' % l, [128, 128], F32)
        ext('w_pa%d' % l, [128, 4096], F32); ext('w_pb%d' % l, [128, 2048], F32); ext('w_o%d' % l, [128, 8192], F32)
        ext('w_r%d' % l, [128, 288], F32); ext('b_r%d' % l, [128, 36], F32); ext('w_e%d' % l, [NEXP, 128, 6144], F32)
    def pieces(nm, n, ls, as_):
        D[nm + '_loc'] = [DBuf('%s_loc%d' % (nm, i), nc.dram_tensor('%s_loc%d' % (nm, i), ls, BF16).ap()) for i in range(n)]
        D[nm + '_all'] = [DBuf('%s_all%d' % (nm, i), nc.dram_tensor('%s_all%d' % (nm, i), as_, BF16).ap()) for i in range(n)]
    pieces('kTa', 2, [256, 2048], [1024, 2048])
    pieces('va', 4, [T, 129], [SEQ, 129])
    pieces('kTb', 3, [256, 2048], [1024, 2048])
    pieces('vb', 3, [T, 256], [SEQ, 256])
    itn('qTa_s', [128, 4 * T], BF16); itn('qTb_s', [128, 6 * T], BF16)
    itn('gates_s', [128, 16 * T], BF16); itn('mod_s', [128, 48], F32)
    out_d = k.dout('outT', [128, 8 * T], F32)
    c = load_consts(k, True)
    xT = k.fa(8 * T)
    k.idx_t = k.es.enter_context(nc.sbuf_tensor("idx_t", [128, 93], I32))
    idxb = Buf("idx_t", k.idx_t, 4, 0, 93)
    k.dma('sp', idxb.v(), D['idx'].v())
    k.dma('sp', xT.v(), D['xT'].v())
    base = (k.fo, k.bo)
    rg = [[0, 1, 2, 3], [4, 5, 6, 7]]
    for l in range(2):
        k.fo, k.bo = base
        def after_kv():
            for nm, i_ in [('kTa', 0), ('kTa', 1), ('va', 0), ('va', 1), ('va', 2), ('va', 3),
                           ('kTb', 0), ('kTb', 1), ('kTb', 2), ('vb', 0), ('vb', 1), ('vb', 2)]:
                loc, al = D[nm + '_loc'][i_], D[nm + '_all'][i_]
                k.S.add('pool', lambda e, loc=loc, al=al: e.collective_compute(
                    "AllGather", ALU.bypass, replica_groups=rg, ins=[loc.ap.opt()], outs=[al.ap.opt()]),
                    reads=loc.v().keys, writes=al.v().keys, dma=True, cc=True)
        k.after_kv = after_kv
        body_A(k, c, xT, D, l)
        k.fo, k.bo = base
        body_B(k, c, xT, D, l)
    k.dma('sp', out_d.v(), xT.v())
    return k.finalize()


_cache = {}


def _fm(w):
    K_, N = w.shape
    return np.ascontiguousarray(w.reshape(K_ // 128, 128, N).transpose(1, 0, 2).reshape(128, (K_ // 128) * N))


def _index_tables(jr):
    p = np.arange(128)
    idx = np.zeros((128, 93), np.int64)
    vmask = np.zeros((128, 69), np.float32)
    for jp in range(6):
        for seg in range(4):
            rank = [jr - 1, jr, jr, jr + 1][seg]
            half = [1, 0, 1, 0][seg]
            rank = min(max(rank, 0), 3)
            idx[:, jp * 4 + seg] = rank * 512 + ((jp % 2) * 128 + p) * 2 + half
    t = 0
    for g, dil in enumerate([1, 4, 16]):
        for r in range(dil):
            for n in range(16 // dil + 1):
                a_k = 128 * n - 64 + p
                gpos = jr * T + a_k * dil + r
                valid = (gpos >= 0) & (gpos < SEQ)
                gp = np.where(valid, gpos, 0)
                idx[:, 24 + t] = gp
                vmask[:, t] = valid.astype(np.float32)
                t += 1
    assert t == 69
    return idx.astype(np.int32), vmask


def kernel(x, c, positions, w_ada, b_ada, w_in, qn_a, kn_a, lam_q1, lam_k1, lam_q2, lam_k2,
           subln_a, qn_b, kn_b, w_pa, w_pb, w_o, w_r1, b_r1, w_r2, b_r2, w_e_gate, w_e_up, w_e_down):
    x = np.asarray(x, np.float32)
    consts = host_consts()
    invf = (np.float32(10000.0) ** (-np.arange(0, 64, 2, dtype=np.float32) / np.float32(64))).astype(np.float32)
    invf_col = invf[(np.arange(128) % 64) % 32].reshape(128, 1).astype(np.float32)
    cores = list(range(NCORES))
    sel = np.zeros((32, 32 * 128), np.float32)
    for e in range(32):
        sel[e, e * 128:(e + 1) * 128] = 1.0
    ehd = np.zeros((4, 256), np.float32)
    for h in range(4):
        j, half = h // 2, h % 2
        ehd[h, j * 128 + half * 64: j * 128 + half * 64 + 64] = 1.0
    cuts = np.cumsum([0, 512, 512, 512, 768, 768, 768, 1024, 1024])
    names = ['qa', 'ka', 'va', 'qb', 'kb', 'vb', 'ga', 'gb']
    shared = {"consts": consts, "invf": invf_col, "sel": sel, "ehd": ehd}
    pidx = np.arange(128) % 64
    for l in range(2):
        wl = np.asarray(w_in[l], np.float32)
        for i, n in enumerate(names):
            shared["w_%s%d" % (n, l)] = _fm(wl[:, cuts[i]:cuts[i + 1]])
        shared["w_ada%d" % l] = np.ascontiguousarray(
            np.asarray(w_ada[l], np.float32).reshape(8, 128, 6, 1024).transpose(2, 1, 0, 3).reshape(6, 128, 8 * 1024))
        shared["b_ada%d" % l] = np.ascontiguousarray(np.asarray(b_ada[l], np.float32).reshape(48, 128).T)
        shared["gains%d" % l] = np.stack([np.asarray(qn_a[l])[pidx], np.asarray(kn_a[l])[pidx],
                                          np.asarray(qn_b[l])[pidx], np.asarray(kn_b[l])[pidx]], axis=1).astype(np.float32)
        lam_init = 0.8 - 0.6 * math.exp(-0.3 * l)
        lamp = np.concatenate([np.asarray(lam_q1[l]), np.asarray(lam_k1[l]), np.asarray(lam_q2[l]), np.asarray(lam_k2[l]),
                               np.array([lam_init, 1.0 - lam_init])]).astype(np.float32)
        shared["lamp%d" % l] = np.ascontiguousarray(np.broadcast_to(lamp[None, :], (128, 258)))
        shared["subln%d" % l] = np.ascontiguousarray(np.broadcast_to(np.asarray(subln_a[l], np.float32)[:, None], (128, 128)))
        shared["w_r%d" % l] = _fm(np.concatenate([np.asarray(w_r1[l], np.float32), np.asarray(w_r2[l], np.float32)], axis=1))
        shared["b_r%d" % l] = np.ascontiguousarray(np.broadcast_to(
            np.concatenate([np.asarray(b_r1[l]), np.asarray(b_r2[l])]).astype(np.float32)[None, :], (128, 36)))
        we = np.empty((NEXP, 128, 6144), np.float32)
        for e in range(NEXP):
            we[e, :, 0:2048] = _fm(np.asarray(w_e_gate[l, e], np.float32))
            we[e, :, 2048:4096] = _fm(np.asarray(w_e_up[l, e], np.float32))
            we[e, :, 4096:6144] = _fm(np.asarray(w_e_down[l, e], np.float32))
        shared["w_e%d" % l] = we
        shared["w_pa%d" % l] = _fm(np.asarray(w_pa[l], np.float32))
        shared["w_pb%d" % l] = _fm(np.asarray(w_pb[l], np.float32))
        shared["w_o%d" % l] = _fm(np.asarray(w_o[l], np.float32))
    in_maps = []
    for ci in cores:
        b, j = ci // 4, ci % 4
        xs = x[b, j * T:(j + 1) * T, :]
        idx, vmask = _index_tables(j)
        m = dict(shared)
        m["xT"] = np.ascontiguousarray(xs.T.reshape(8, 128, T).transpose(1, 0, 2).reshape(128, 8 * T))
        m["c_col"] = np.ascontiguousarray(np.asarray(c[b], np.float32).reshape(8, 128).T)
        m["posb"] = np.ascontiguousarray(np.broadcast_to(np.asarray(positions[b, j * T:(j + 1) * T], np.int32)[None, :], (128, T)))
        m["idx"] = idx
        m["vmask"] = vmask
        in_maps.append(m)
    if 'F' not in _cache:
        _cache['F'] = build_F()
    res = run_bass_kernel_spmd(_cache['F'], in_maps, core_ids=cores).results
    out = np.empty((2, SEQ, D), np.float32)
    for ci in cores:
        b, j = ci // 4, ci % 4
        o = np.asarray(res[ci]["outT"], np.float32)
        out[b, j * T:(j + 1) * T, :] = o.reshape(128, 8, T).transpose(1, 0, 2).reshape(D, T).T
    return out
```

```python
import math
import numpy as np
from contextlib import ExitStack
import concourse.bass as bass
import concourse.mybir as mybir
from concourse.bass_utils import run_bass_kernel_spmd

F32 = mybir.dt.float32
BF16 = mybir.dt.bfloat16
I32 = mybir.dt.int32
ALU = mybir.AluOpType
AF = mybir.ActivationFunctionType
AX = mybir.AxisListType

ENGS = ['pe', 'act', 'dve', 'pool', 'sp']
CHUNK = 1024

NCORES = 8
T = 2048
SEQ = 8192
D = 1024
EPS = 1e-6
NEG = -30000.0
WIN = 4096
PADW = 1024
NEXP = 32


class Op:
    __slots__ = ('eng', 'fn', 'src', 'pos', 'signal', 'waits', 'know', 'is_dma', 'cnt')


class V:
    __slots__ = ('ap', 'keys')

    def __init__(self, ap, keys):
        self.ap = ap
        self.keys = keys


class Buf:
    def __init__(self, arena_name, handle, esz, off, n):
        self.an = arena_name
        self.t = handle
        self.esz = esz
        self.off = off
        self.n = n

    def keys(self, a, b):
        ch = 2048 if self.an.startswith('ps') else CHUNK
        lo = ((self.off + a) * self.esz) // ch
        hi = ((self.off + b) * self.esz - 1) // ch
        return [(self.an, i) for i in range(lo, hi + 1)]

    def v(self, a=0, b=None, p0=0, p1=128, step=1, pat=None, **kw):
        if b is None:
            b = self.n
        assert 0 <= a < b <= self.n, (a, b, self.n)
        if step == 1:
            ap = self.t[p0:p1, self.off + a:self.off + b]
        else:
            ap = self.t[p0:p1, self.off + a:self.off + b:step]
        if pat is not None:
            ap = ap.rearrange(pat, **kw)
        return V(ap, self.keys(a, b))

    def sub(self, off, n):
        assert off + n <= self.n
        return Buf(self.an, self.t, self.esz, self.off + off, n)


class DBuf:
    def __init__(self, name, ap):
        self.name = name
        self.ap = ap

    def v(self, ap=None, key=None):
        return V(self.ap if ap is None else ap, [(self.name, key)])


class Sched:
    def __init__(self, nc, n_dma_sems=16):
        self.nc = nc
        self.ops = {e: [] for e in ENGS}
        self.ncomp = {e: 0 for e in ENGS}
        self.last_w = {}
        self.readers = {}
        self.known = {e: {} for e in ENGS}
        self.n_dma_sems = n_dma_sems
        self.dma_last = [None] * n_dma_sems
        self.dma_cnt = [0] * n_dma_sems
        self.dma_rr = 0
        self.cc_cnt = 0
        self.cc_last = None

    def _need(self, e, d):
        k = self.known[e]
        if k.get(d.src, -1) >= d.pos:
            return None
        d.signal = True
        for s, p in d.know.items():
            if k.get(s, -1) < p:
                k[s] = p
        return d

    def add(self, eng, fn, reads=(), writes=(), dma=False, cc=False):
        op = Op()
        op.eng = eng
        op.fn = fn
        op.is_dma = dma
        op.signal = dma
        deps = []
        seen = set()
        pr_ = [k for k in reads if isinstance(k[0], str) and k[0].startswith('ps')]
        if pr_:
            writes = list(writes) + pr_
        for k in reads:
            w = self.last_w.get(k)
            if w is not None and id(w) not in seen:
                seen.add(id(w)); deps.append(w)
        for k in writes:
            w = self.last_w.get(k)
            if w is not None and id(w) not in seen:
                seen.add(id(w)); deps.append(w)
            for r in self.readers.get(k, ()):
                if id(r) not in seen:
                    seen.add(id(r)); deps.append(r)
        if cc:
            op.src = ('cc', 0)
            op.pos = self.cc_cnt
            self.cc_cnt += 1
            if self.cc_last is not None and id(self.cc_last) not in seen:
                seen.add(id(self.cc_last)); deps.append(self.cc_last)
            self.cc_last = op
        elif dma:
            slot = self.dma_rr
            self.dma_rr = (self.dma_rr + 1) % self.n_dma_sems
            prev = self.dma_last[slot]
            if prev is not None and id(prev) not in seen:
                seen.add(id(prev)); deps.append(prev)
            op.src = ('dma', slot)
            op.pos = self.dma_cnt[slot]
            self.dma_cnt[slot] += 1
            self.dma_last[slot] = op
        else:
            op.src = eng
            op.pos = self.ncomp[eng]
            self.ncomp[eng] += 1
        waits = []
        deps.sort(key=lambda d: -d.pos)
        for d in deps:
            if (not dma) and (not d.is_dma) and d.src == eng:
                if eng == 'pe':
                    continue
                if op.pos - d.pos > 2:
                    continue
            w = self._need(eng, d)
            if w is not None:
                waits.append(w)
        op.waits = waits
        know = dict(self.known[eng])
        know[op.src] = op.pos
        op.know = know
        for k in reads:
            self.readers.setdefault(k, []).append(op)
        for k in writes:
            self.last_w[k] = op
            self.readers[k] = []
        self.ops[eng].append(op)
        return op

    def finish(self):
        op = Op()
        op.eng = 'sp'; op.fn = None; op.is_dma = False; op.signal = False
        op.src = 'sp'; op.pos = self.ncomp['sp']; self.ncomp['sp'] += 1
        waits = []
        for d in list(self.dma_last) + [self.cc_last]:
            if d is not None:
                w = self._need('sp', d)
                if w is not None:
                    waits.append(w)
        for e in ['pe', 'act', 'dve', 'pool']:
            comp = [o for o in self.ops[e] if not o.is_dma]
            if comp:
                w = self._need('sp', comp[-1])
                if w is not None:
                    waits.append(w)
        op.waits = waits
        op.know = {}
        self.ops['sp'].append(op)

    def emit(self, sems):
        nc = self.nc
        for e in ENGS:
            c = 0
            for o in self.ops[e]:
                if o.is_dma:
                    continue
                if o.signal:
                    c += 1
                o.cnt = c
        engobj = {'pe': nc.tensor, 'act': nc.scalar, 'dve': nc.vector, 'pool': nc.gpsimd, 'sp': nc.sync}

        def run(e):
            eo = engobj[e]
            for o in self.ops[e]:
                for d in o.waits:
                    if d.is_dma and d.src[0] == 'cc':
                        eo.wait_ge(sems[d.src], d.pos + 1)
                    elif d.is_dma:
                        eo.wait_ge(sems[d.src], 16 * (d.pos + 1))
                    else:
                        eo.wait_ge(sems[d.src], d.cnt)
                if o.fn is None:
                    continue
                ins = o.fn(eo)
                if o.is_dma and o.src[0] == 'cc':
                    ins.then_inc(sems[o.src])
                elif o.is_dma:
                    ins.then_inc(sems[o.src], 16)
                elif o.signal:
                    ins.then_inc(sems[e], 1)
        return run


class K:
    def __init__(self, nf32, nbf16):
        self.nc = bass.Bass("TRN2", target_bir_lowering=False)
        self.es = ExitStack()
        nc = self.nc
        self.S = Sched(nc)
        es = self.es
        self.af_t = es.enter_context(nc.sbuf_tensor("arena_f", [128, nf32], F32))
        self.ab_t = es.enter_context(nc.sbuf_tensor("arena_b", [128, nbf16], BF16))
        self.AF_ = Buf("af", self.af_t, 4, 0, nf32)
        self.AB_ = Buf("ab", self.ab_t, 2, 0, nbf16)
        self.ps = []
        for i in range(4):
            t = es.enter_context(nc.psum_tensor("ps%d" % i, [128, 1024], F32))
            self.ps.append(Buf("ps%d" % i, t, 4, 0, 1024))
        self.sems = {}
        for e in ENGS:
            self.sems[e] = es.enter_context(nc.semaphore("s_" + e))
        for i in range(self.S.n_dma_sems):
            self.sems[('dma', i)] = es.enter_context(nc.semaphore("d%d" % i))
        self.sems[('cc', 0)] = es.enter_context(nc.semaphore("ccsem"))
        self.fo = 0
        self.bo = 0
        self.dram = {}

    def din(self, name, shape, dt):
        ap = self.nc.dram_tensor(name, list(shape), dt, kind="ExternalInput").ap()
        d = DBuf(name, ap)
        self.dram[name] = d
        return d

    def dout(self, name, shape, dt):
        ap = self.nc.dram_tensor(name, list(shape), dt, kind="ExternalOutput").ap()
        d = DBuf(name, ap)
        self.dram[name] = d
        return d

    def fa(self, n):
        b = self.AF_.sub(self.fo, n)
        self.fo += n
        return b

    def ba(self, n):
        b = self.AB_.sub(self.bo, n)
        self.bo += n
        return b

    def mm(self, out, lhsT, rhs, start=True, stop=True):
        self.S.add('pe', lambda e: e.matmul(out.ap, lhsT.ap, rhs.ap, start=start, stop=stop),
                   reads=lhsT.keys + rhs.keys, writes=out.keys)

    def tr(self, out, in_, ident):
        self.S.add('pe', lambda e: e.transpose(out.ap, in_.ap, ident.ap),
                   reads=in_.keys + ident.keys, writes=out.keys)

    def act(self, out, in_, func, scale=1.0, bias=0.0, accum=None):
        reads = list(in_.keys)
        kw = {}
        if isinstance(bias, V):
            reads += bias.keys
            kw['bias'] = bias.ap
        else:
            kw['bias'] = float(bias)
        if isinstance(scale, V):
            reads += scale.keys
            kw['scale'] = scale.ap
        else:
            kw['scale'] = float(scale)
        writes = list(out.keys)
        if accum is not None:
            writes += accum.keys
            kw['accum_out'] = accum.ap
        self.S.add('act', lambda e: e.activation(out=out.ap, in_=in_.ap, func=func, **kw),
                   reads=reads, writes=writes)

    def tt(self, eng, out, in0, in1, op):
        self.S.add(eng, lambda e: e.tensor_tensor(out=out.ap, in0=in0.ap, in1=in1.ap, op=op),
                   reads=in0.keys + in1.keys, writes=out.keys)

    def ts(self, eng, out, in0, s1, s2=None, op0=ALU.mult, op1=None):
        reads = list(in0.keys)
        a1 = s1
        a2 = s2
        if isinstance(s1, V):
            reads += s1.keys; a1 = s1.ap
        if isinstance(s2, V):
            reads += s2.keys; a2 = s2.ap
        if op1 is None:
            self.S.add(eng, lambda e: e.tensor_scalar(out=out.ap, in0=in0.ap, scalar1=a1, scalar2=None, op0=op0),
                       reads=reads, writes=out.keys)
        else:
            self.S.add(eng, lambda e: e.tensor_scalar(out=out.ap, in0=in0.ap, scalar1=a1, scalar2=a2, op0=op0, op1=op1),
                       reads=reads, writes=out.keys)

    def stt(self, eng, out, in0, scalar, in1, op0, op1):
        reads = in0.keys + in1.keys
        a = scalar
        if isinstance(scalar, V):
            reads = reads + scalar.keys; a = scalar.ap
        self.S.add(eng, lambda e: e.scalar_tensor_tensor(out=out.ap, in0=in0.ap, scalar=a, in1=in1.ap, op0=op0, op1=op1),
                   reads=reads, writes=out.keys)

    def cp(self, eng, out, in_):
        if eng == 'act':
            self.S.add('act', lambda e: e.copy(out=out.ap, in_=in_.ap), reads=in_.keys, writes=out.keys)
        else:
            self.S.add(eng, lambda e: e.tensor_copy(out=out.ap, in_=in_.ap), reads=in_.keys, writes=out.keys)

    def red(self, eng, out, in_, op, axis=AX.X):
        self.S.add(eng, lambda e: e.tensor_reduce(out=out.ap, in_=in_.ap, axis=axis, op=op),
                   reads=in_.keys, writes=out.keys)

    def recip(self, out, in_):
        self.S.add('dve', lambda e: e.reciprocal(out=out.ap, in_=in_.ap), reads=in_.keys, writes=out.keys)

    def memset(self, eng, out, val):
        self.S.add(eng, lambda e: e.memset(out.ap, val), writes=out.keys)

    def dma(self, eng, out, in_):
        self.S.add(eng, lambda e: e.dma_start(out=out.ap, in_=in_.ap), reads=in_.keys, writes=out.keys, dma=True)

    def finalize(self):
        S = self.S
        S.finish()
        run = S.emit(self.sems)
        with self.nc.Block() as block:
            @block.tensor
            def _(e): run('pe')
            @block.scalar
            def _(e): run('act')
            @block.vector
            def _(e): run('dve')
            @block.gpsimd
            def _(e): run('pool')
            @block.sync
            def _(e): run('sp')
        self.es.close()
        return self.nc


def load_consts(k, need_rope):
    c = {}
    cin = k.din("consts", [128, 5 * 128], F32)
    cf = k.fa(256).sub(0, 128)
    k.dma('sp', cf.v(), V(cin.ap[:, 0:128], cin.v().keys))
    c['ident_f'] = cf
    cb = k.ba(5 * 128)
    k.dma('pool', cb.v(), cin.v())
    c['ident_b'] = cb.sub(0, 128)
    c['onesbd'] = cb.sub(128, 128)
    c['rotT'] = cb.sub(256, 128)
    c['maskA'] = cb.sub(384, 128)
    c['maskB'] = cb.sub(512, 128)
    ones = k.ba(128)
    k.ba(256)
    k.memset('pool', ones.v(), 1.0)
    c['ones_b'] = ones
    return c


def host_consts():
    ident = np.eye(128, dtype=np.float32)
    onesbd = np.zeros((128, 128), np.float32)
    onesbd[:64, :64] = 1.0
    onesbd[64:, 64:] = 1.0
    rotT = np.zeros((128, 128), np.float32)
    for m in range(128):
        j = m % 64
        if j < 32:
            rotT[m + 32, m] = -1.0
        else:
            rotT[m - 32, m] = 1.0
    u = np.arange(128)[:, None]
    a = np.arange(128)[None, :]
    maskA = np.where(u >= a, 0.0, NEG).astype(np.float32)
    maskB = np.where(u <= a, 0.0, NEG).astype(np.float32)
    return np.concatenate([ident, onesbd, rotT, maskA, maskB], axis=1)


TWO_PI = 2.0 * math.pi
C1 = 6.28125
C2 = float(np.float32(TWO_PI - 6.28125))
C3 = float(TWO_PI - 6.28125 - float(np.float32(TWO_PI - 6.28125)))


WIN_GROUPS = [('ka', 512), ('kb', 768), ('va', 512), ('vb', 768), ('qa', 512), ('qb', 768), ('ga', 1024), ('gb', 1024)]


def rms_mod(k, xT, hT, c, scale_col, shift_col, work_f, work_b, h32_hook=None):
    sq = [work_b.sub(i * 512, 512) for i in range(2)]
    sd = work_f.sub(0, 512)
    rs = work_f.sub(512, 512)
    tmp = [work_f.sub(1024 + i * 512, 512) for i in range(2)]
    for tb in range(4):
        pss = k.ps[tb % 2].sub(0, 512)
        for cc in range(8):
            s = sq[cc % 2]
            k.act(s.v(), xT.v(cc * T + tb * 512, cc * T + tb * 512 + 512), AF.Square)
            k.mm(pss.v(), c['ones_b'].v(), s.v(), start=(cc == 0), stop=(cc == 7))
        k.act(sd.v(), pss.v(), AF.Sqrt, scale=1.0 / D, bias=EPS)
        k.recip(rs.v(), sd.v())
        for cc in range(8):
            t = tmp[cc % 2]
            k.tt('pool', t.v(), xT.v(cc * T + tb * 512, cc * T + tb * 512 + 512), rs.v(), ALU.mult)
            if h32_hook is not None:
                h32 = h32_hook(tb, cc)
                k.ts('dve', h32.v(), t.v(), scale_col.v(cc, cc + 1), shift_col.v(cc, cc + 1), ALU.mult, ALU.add)
                k.cp('pool', hT.v(cc * T + tb * 512, cc * T + tb * 512 + 512), h32.v())
            else:
                k.ts('dve', hT.v(cc * T + tb * 512, cc * T + tb * 512 + 512), t.v(),
                     scale_col.v(cc, cc + 1), shift_col.v(cc, cc + 1), ALU.mult, ALU.add)
        if h32_hook is not None:
            h32_hook(tb, None)


def body_A(k, c, xT, D, l):
    stg = 99
    ccol_d = D['c_col']
    pos_d = D['posb']
    invf_d = D['invf']
    wada_d = D['w_ada%d' % l]
    bada_d = D['b_ada%d' % l]
    gains_d = D['gains%d' % l]
    wg_d = {n: D['w_%s%d' % (n, l)] for n, nc_ in WIN_GROUPS}
    kTa_o = D['kTa_loc']
    kTb_o = D['kTb_loc']
    qTa_o = D['qTa_s']
    qTb_o = D['qTb_s']
    va_o = D['va_loc']
    vb_o = D['vb_loc']
    gates_o = D['gates_s']
    mod_o = D['mod_s']

    cosT = k.fa(T)
    sinT = k.fa(T)
    small = k.fa(256)
    work_f = k.fa(4096)
    hT = k.ba(8 * T)
    wring = [k.ba(8192) for _ in range(3)]
    work_b = k.ba(2048)
    stage = [k.ba(2048) for _ in range(2)]
    vst = [k.ba(1024) for _ in range(2)]

    ccol = small.sub(0, 8)
    cact = k.ba(8)
    bada = small.sub(8, 48)
    mod = small.sub(56, 48)
    gains = small.sub(104, 4)
    invf = small.sub(108, 1)

    k.dma('sp', ccol.v(), ccol_d.v())
    k.dma('sp', bada.v(), bada_d.v())
    k.dma('sp', gains.v(), gains_d.v())
    k.dma('sp', invf.v(), invf_d.v())

    ang = work_f.sub(0, T)
    kk = work_f.sub(T, T)
    posi_v = V(kk.v().ap.bitcast(I32), kk.v().keys)
    k.dma('sp', posi_v, pos_d.v())
    k.cp('dve', ang.v(), posi_v)
    k.ts('dve', ang.v(), ang.v(), invf.v(), None, ALU.mult)
    k.ts('dve', kk.v(), ang.v(), 1.0 / TWO_PI, 12582912.0, ALU.mult, ALU.add)
    k.ts('dve', kk.v(), kk.v(), 12582912.0, None, ALU.subtract)
    k.stt('dve', ang.v(), kk.v(), -C1, ang.v(), ALU.mult, ALU.add)
    k.stt('dve', ang.v(), kk.v(), -C2, ang.v(), ALU.mult, ALU.add)
    k.stt('dve', ang.v(), kk.v(), -C3, ang.v(), ALU.mult, ALU.add)
    k.ts('dve', ang.v(), ang.v(), math.pi, -math.pi, ALU.min, ALU.max)
    k.act(sinT.v(), ang.v(), AF.Sin)
    k.act(kk.v(), ang.v(), AF.Sin, scale=0.5)
    k.tt('dve', kk.v(), kk.v(), kk.v(), ALU.mult)
    k.ts('dve', cosT.v(), kk.v(), -2.0, 1.0, ALU.mult, ALU.add)

    k.act(cact.v(), ccol.v(), AF.Silu)
    psm = k.ps[3].sub(0, 48)
    for s in range(6):
        wb = wring[s % 3]
        k.dma('pool', wb.v(), V(wada_d.ap[s], wada_d.v(key=s).keys))
        for cc in range(8):
            for kc in range(8):
                k.mm(psm.v(s * 8 + cc, s * 8 + cc + 1),
                     wb.v(kc * 1024 + cc * 128, kc * 1024 + cc * 128 + 128),
                     cact.v(kc, kc + 1), start=(kc == 0), stop=(kc == 7))
    k.tt('dve', mod.v(), psm.v(), bada.v(), ALU.add)
    k.dma('sp', mod_o.v(), mod.v())
    sc1 = small.sub(152, 8)
    k.ts('dve', sc1.v(), mod.v(8, 16), 1.0, None, ALU.add)

    rms_mod(k, xT, hT, c, sc1, mod.sub(0, 8), work_f, work_b)

    raw = [work_f.sub(i * 512, 512) for i in range(2)]
    rst = [work_f.sub(1024 + i * 512, 512) for i in range(2)]
    t1 = [work_f.sub(2048 + i * 512, 512) for i in range(2)]
    t2 = [work_f.sub(3072 + i * 512, 512) for i in range(2)]
    sqb = [work_b.sub(i * 512, 512) for i in range(2)]
    qnb = [work_b.sub(1024 + i * 512, 512) for i in range(2)]
    cnt = [0]

    def qk_block(psb, gcol, outv, tb):
        i = cnt[0] % 2
        cnt[0] += 1
        k.act(sqb[i].v(), psb.v(), AF.Square)
        k.ts('dve', raw[i].v(), psb.v(), gcol, None, ALU.mult)
        ps2 = k.ps[2].sub(i * 512, 512)
        k.mm(ps2.v(), c['onesbd'].v(), sqb[i].v())
        k.act(rst[i].v(), ps2.v(), AF.Sqrt, scale=1.0 / 64, bias=EPS)
        k.recip(rst[i].v(), rst[i].v())
        k.tt('pool', qnb[i].v(), raw[i].v(), rst[i].v(), ALU.mult)
        ps3 = k.ps[3].sub(i * 512, 512)
        k.mm(ps3.v(), c['rotT'].v(), qnb[i].v())
        k.tt('pool', t1[i].v(), qnb[i].v(), cosT.v(tb * 512, tb * 512 + 512), ALU.mult)
        k.tt('dve', t2[i].v(), ps3.v(), sinT.v(tb * 512, tb * 512 + 512), ALU.mult)
        k.tt('pool', outv, t1[i].v(), t2[i].v(), ALU.add)

    pcnt = [0]
    def wload_g(gj):
        nm_, nc__ = WIN_GROUPS[gj]
        k.dma('pool', wring[gj % 3].v(0, 8 * nc__), wg_d[nm_].v())

    wload_g(0)
    wload_g(1)
    for gi, (name, ncol) in enumerate(WIN_GROUPS):
        wb = wring[gi % 3]
        if name == 'qb':
            k.after_kv()
        if gi + 2 < len(WIN_GROUPS):
            wload_g(gi + 2)
        if name in ('ka', 'kb', 'qa', 'qb'):
            npair = ncol // 128
            gidx = {'qa': 0, 'ka': 1, 'qb': 2, 'kb': 3}[name]
            od = {'ka': kTa_o, 'kb': kTb_o, 'qa': qTa_o, 'qb': qTb_o}[name]
            for pr in range(npair):
                st = stage[pr % 2]
                for tb in range(4):
                    psb = k.ps[pcnt[0] % 2].sub(512 * ((pcnt[0] // 2) % 2), 512)
                    pcnt[0] += 1
                    for kc in range(8):
                        k.mm(psb.v(), wb.v(kc * ncol + pr * 128, kc * ncol + pr * 128 + 128),
                             hT.v(kc * T + tb * 512, kc * T + tb * 512 + 512), start=(kc == 0), stop=(kc == 7))
                    qk_block(psb, gains.v(gidx, gidx + 1), st.v(tb * 512, tb * 512 + 512), tb)
                if name in ('ka', 'kb'):
                    odp = od[pr // 2]
                    k.dma('sp', V(odp.ap[(pr % 2) * 128:(pr % 2 + 1) * 128, :], odp.v().keys), st.v())
                else:
                    k.dma('sp', V(od.ap[:, pr * T:(pr + 1) * T], od.v().keys), st.v())
        elif name in ('va', 'vb'):
            nh_, dh_, d_ = (4, 129, 128) if name == 'va' else (12, 64, 64)
            od = va_o if name == 'va' else vb_o
            w_ = nh_ * dh_
            for i in range(2):
                k.memset('pool', vst[i].v(0, w_), 1.0)
            for tt_ in range(16):
                st = vst[tt_ % 2]
                for n0 in range(0, ncol, 512):
                    nn = min(512, ncol - n0)
                    psb = k.ps[pcnt[0] % 2].sub(512 * ((pcnt[0] // 2) % 2), 512)
                    pcnt[0] += 1
                    for kc in range(8):
                        k.mm(psb.v(0, nn), hT.v(kc * T + tt_ * 128, kc * T + tt_ * 128 + 128),
                             wb.v(kc * ncol + n0, kc * ncol + n0 + nn), start=(kc == 0), stop=(kc == 7))
                    h0 = n0 // d_
                    nhh = nn // d_
                    outv = V(st.v(h0 * dh_, (h0 + nhh) * dh_, pat="p (h c) -> p h c", c=dh_).ap[:, :, 0:d_],
                             st.keys(h0 * dh_, (h0 + nhh) * dh_))
                    inv = V(psb.v(0, nn, pat="p (h c) -> p h c", c=d_).ap, psb.keys(0, nn))
                    k.cp('act', outv, inv)
                if name == 'va':
                    for h4 in range(4):
                        k.dma('sp', V(od[h4].ap[tt_ * 128:(tt_ + 1) * 128, :], od[h4].v().keys), st.v(h4 * 129, h4 * 129 + 129))
                else:
                    for g3 in range(3):
                        k.dma('sp', V(od[g3].ap[tt_ * 128:(tt_ + 1) * 128, :], od[g3].v().keys), st.v(g3 * 256, g3 * 256 + 256))
        else:
            gi_ = 0 if name == 'ga' else 1
            for cc in range(8):
                st = stage[cc % 2]
                for tb in range(4):
                    psb = k.ps[pcnt[0] % 2].sub(512 * ((pcnt[0] // 2) % 2), 512)
                    pcnt[0] += 1
                    for kc in range(8):
                        k.mm(psb.v(), wb.v(kc * ncol + cc * 128, kc * ncol + cc * 128 + 128),
                             hT.v(kc * T + tb * 512, kc * T + tb * 512 + 512), start=(kc == 0), stop=(kc == 7))
                    k.act(st.v(tb * 512, tb * 512 + 512), psb.v(), AF.Sigmoid)
                o0 = (gi_ * 8 + cc) * T
                k.dma('sp', V(gates_o.ap[:, o0:o0 + T], gates_o.v().keys), st.v())
    return


def body_B(k, c, xT, D, l):
    mod_d = D['mod_s']
    qTa_d = D['qTa_s']
    qTb_d = D['qTb_s']
    kTa_d = D['kTa_all']
    va_d = D['va_all']
    kTb_d = D['kTb_all']
    vb_d = D['vb_all']
    gates_d = D['gates_s']
    lam_d = D['lamp%d' % l]
    subln_d = D['subln%d' % l]
    wpa_d = D['w_pa%d' % l]
    wpb_d = D['w_pb%d' % l]
    wo_d = D['w_o%d' % l]
    wr_d = D['w_r%d' % l]
    br_d = D['b_r%d' % l]
    sel_d = D['sel']
    ehd_d = D['ehd']
    we_d = D['w_e%d' % l]

    small = k.fa(1024)
    fwork = k.fa(25600 - k.fo)
    mod = small.sub(0, 48)
    lamp = small.sub(48, 258)
    subln = small.sub(320, 128)
    br = small.sub(448, 36)
    sc2 = small.sub(484, 8)
    lamcol = small.sub(492, 4)
    tmp64 = small.sub(512, 128)
    wr = small.sub(640, 288)

    k.dma('sp', mod.v(), mod_d.v())
    k.dma('sp', lamp.v(), lam_d.v())
    k.dma('sp', subln.v(), subln_d.v())
    k.dma('sp', br.v(), br_d.v())
    k.dma('sp', wr.v(), wr_d.v())

    k.tt('dve', tmp64.v(0, 64), lamp.v(0, 64), lamp.v(64, 128), ALU.mult)
    k.tt('dve', tmp64.v(64, 128), lamp.v(128, 192), lamp.v(192, 256), ALU.mult)
    k.red('dve', lamcol.v(0, 2), tmp64.v(0, 128, pat="p (a w) -> p a w", w=64), ALU.add)
    k.act(lamcol.v(0, 2), lamcol.v(0, 2), AF.Exp)
    k.tt('dve', lamcol.v(3, 4), lamcol.v(0, 1), lamcol.v(1, 2), ALU.subtract)
    k.tt('dve', lamcol.v(0, 1), lamcol.v(3, 4), lamp.v(256, 257), ALU.add)
    k.ts('dve', lamcol.v(1, 2), lamcol.v(0, 1), -1.0, None, ALU.mult)
    k.ts('dve', sc2.v(), mod.v(32, 40), 1.0, None, ALU.add)

    AB = k.AB_
    b0 = k.bo
    qTa = AB.sub(b0 + 0, 8192)
    oaT = AB.sub(b0 + 8192, 8192)
    obT = AB.sub(b0 + 16384, 4096)
    qTb = AB.sub(b0 + 20480, 12288)
    kring = [AB.sub(b0 + 32768 + i * 2048, 2048) for i in range(3)]
    vring = [AB.sub(b0 + 38912 + i * 2048, 2048) for i in range(3)]
    PT = [AB.sub(b0 + 45104 + i * 1024, 1024) for i in range(2)]

    k.dma('sp', qTa.v(), qTa_d.v())
    PT4 = [AB.sub(b0 + 45056 + i * 1024, 1024) for i in range(4)]
    tsum = AB.sub(b0 + 49152, 1024)
    acc = fwork.sub(1024, 1024)
    dsb = fwork.sub(2048, 512)
    rsb = fwork.sub(2560, 512)
    r2 = fwork.sub(3072, 512)
    slcol = fwork.sub(0, 1)
    k.tt('dve', slcol.v(), subln.v(0, 1), lamp.v(257, 258), ALU.mult)
    ld = [0]
    for h in range(4):
        for qb in range(4):
            OT = k.ps[2]

            SB = [0, 1, 3]

            def qk(kt, kbuf):
                pss = k.ps[SB[kt % 3]]
                for m in range(2):
                    k.mm(pss.v(m * 512, m * 512 + 512),
                         kbuf.v((kt % 16) * 128, (kt % 16) * 128 + 128, p0=m * 64, p1=m * 64 + 64),
                         qTa.v(h * T + qb * 512, h * T + qb * 512 + 512, p0=m * 64, p1=m * 64 + 64))

            bufs = {}

            def load(ch):
                i = ld[0] % 3
                ld[0] += 1
                kb_, vb_ = kring[i], vring[i]
                k.dma('sp', kb_.v(), V(kTa_d[h // 2].ap[ch * 256 + (h % 2) * 128:ch * 256 + (h % 2) * 128 + 128, :], kTa_d[h // 2].v().keys))
                src = va_d[h].ap[ch * 2048:(ch + 1) * 2048, 0:128].rearrange("(t p) c -> p t c", p=128)
                k.dma('sp', V(vb_.v(0, 2048, pat="p (t c) -> p t c", c=128).ap, vb_.keys(0, 2048)), V(src, va_d[h].v().keys))
                bufs[ch] = (kb_, vb_)

            load(0)
            load(1)
            qk(0, bufs[0][0])
            qk(1, bufs[0][0])
            for kt in range(64):
                ch = kt // 16
                if kt % 16 == 0 and ch + 2 < 4:
                    load(ch + 2)
                if kt + 2 < 64:
                    qk(kt + 2, bufs[(kt + 2) // 16][0])
                pt = PT4[kt % 4]
                k.act(pt.v(), k.ps[SB[kt % 3]].v(), AF.Exp, scale=0.125)
                vb_ = bufs[ch][1]
                for m in range(2):
                    k.mm(OT.v(m * 512, m * 512 + 512), vb_.v((kt % 16) * 128, (kt % 16) * 128 + 128),
                         pt.v(m * 512, m * 512 + 512), start=(kt == 0), stop=(kt == 63))
                if kt % 4 == 1:
                    k.tt('dve', tsum.v(), PT4[(kt - 1) % 4].v(), pt.v(), ALU.add)
                elif kt % 4 == 3:
                    k.tt('dve', tsum.v(), tsum.v(), PT4[(kt - 1) % 4].v(), ALU.add)
                    k.tt('dve', tsum.v(), tsum.v(), pt.v(), ALU.add)
                    if kt == 3:
                        k.cp('dve', acc.v(), tsum.v())
                    else:
                        k.tt('dve', acc.v(), acc.v(), tsum.v(), ALU.add)
            k.cp('dve', tsum.v(), acc.v())
            for m in range(2):
                psd = k.ps[0].sub(m * 512, 512)
                k.mm(psd.v(), c['ones_b'].v(), tsum.v(m * 512, m * 512 + 512))
            k.recip(rsb.v(), k.ps[0].v(0, 512))
            k.recip(r2.v(), k.ps[0].v(512, 1024))
            k.tt('dve', dsb.v(), OT.v(0, 512), rsb.v(), ALU.mult)
            k.tt('dve', r2.v(), OT.v(512, 1024), r2.v(), ALU.mult)
            k.stt('dve', dsb.v(), r2.v(), lamcol.v(1, 2), dsb.v(), ALU.mult, ALU.add)
            sqA = PT4[0].sub(0, 512)
            k.act(sqA.v(), dsb.v(), AF.Square)
            pss_ = k.ps[1].sub(0, 512)
            k.mm(pss_.v(), c['ones_b'].v(), sqA.v())
            k.act(rsb.v(), pss_.v(), AF.Sqrt, scale=1.0 / 128, bias=EPS)
            k.recip(rsb.v(), rsb.v())
            k.stt('dve', oaT.v(h * T + qb * 512, h * T + qb * 512 + 512), dsb.v(), slcol.v(), rsb.v(), ALU.mult, ALU.mult)

    k.dma('sp', qTb.v(), qTb_d.v())
    kbb = [AB.sub(b0 + i * 4096, 4096) for i in range(2)]
    vtr = [AB.sub(b0 + 32768 + i * 260, 260) for i in range(8)]
    numT = fwork.sub(512, 2 * T)
    denT = fwork.sub(512 + 2 * T, T)
    Osb = fwork.sub(512 + 3 * T, 260)
    vld = [0]
    vtile_no = [0]
    U32 = mybir.dt.uint32
    idx_t = k.idx_t
    kidx = Buf("idx_t", idx_t, 4, 0, 24)
    vidx = Buf("idx_t", idx_t, 4, 24, 69)
    vmask = fwork.sub(7200, 69)
    k.dma('sp', vmask.v(), D['vmask'].v())
    kTb_view = [d_.ap.rearrange("r (h c) -> (r h) c", h=2) for d_ in kTb_d]
    vstg = [AB.sub(b0 + 32768 + 8 * 260 + i * 256, 256) for i in range(4)]

    def igather(outv, src_ap, src_d, idxv):
        k.S.add('pool', lambda e: e.indirect_dma_start(out=outv.ap, out_offset=None, in_=src_ap,
                                                       in_offset=bass.IndirectOffsetOnAxis(ap=idxv.ap.bitcast(U32), axis=0)),
                reads=src_d.v().keys + idxv.keys, writes=outv.keys, dma=True)
    for g, dil in enumerate([1, 4, 16]):
        for j in range(2):
            for seg in range(4):
                col = (g * 2 + j) * 4 + seg
                igather(kbb[j].v(seg * 1024, seg * 1024 + 1024), kTb_view[g], kTb_d[g], kidx.v(col, col + 1))
        ntile = 16 // dil
        for r in range(dil):
            vt = {}

            def vload(n):
                i = vld[0] % 8
                vld[0] += 1
                tno = vtile_no[0]
                vtile_no[0] += 1
                vs_ = vstg[tno % 4]
                igather(vs_.v(), vb_d[g].ap, vb_d[g], vidx.v(tno, tno + 1))
                k.ts('dve', V(vtr[i].v(0, 260, pat="p (h c) -> p h c", c=65).ap[:, :, 0:64], vtr[i].keys(0, 260)),
                     V(vs_.v(0, 256, pat="p (h c) -> p h c", c=64).ap, vs_.keys(0, 256)), vmask.v(tno, tno + 1), None, ALU.mult)
                k.ts('dve', V(vtr[i].v(0, 260, pat="p (h c) -> p h c", c=65).ap[:, :, 64], vtr[i].keys(0, 260)),
                     c['ones_b'].v(0, 4), vmask.v(tno, tno + 1), None, ALU.mult)
                vt[n] = vtr[i]

            vload(0)
            for m in range(ntile):
                vload(m + 1)
                pss = k.ps[m % 2]
                i0 = 128 * m * dil + r
                for hh in range(4):
                    j, half = hh // 2, hh % 2
                    for ab in range(2):
                        n = m + ab
                        w0 = PADW + (128 * n - 64) * dil + r
                        col = (hh * 2 + ab) * 128
                        k.mm(pss.v(col, col + 128), c['ident_b'].v(), c['maskA' if ab == 0 else 'maskB'].v(), start=True, stop=False)
                        k.mm(pss.v(col, col + 128),
                             kbb[j].v(w0, w0 + 128 * dil - (dil - 1), p0=half * 64, p1=half * 64 + 64, step=dil),
                             qTb.v((g * 2 + j) * T + i0, (g * 2 + j) * T + i0 + 128 * dil - (dil - 1), p0=half * 64, p1=half * 64 + 64, step=dil),
                             start=False, stop=True)
                pt = PT[m % 2]
                k.act(pt.v(), pss.v(), AF.Exp, scale=0.125)
                Ops = k.ps[2 + (m % 2)].sub(0, 260)
                for hh in range(4):
                    for ab in range(2):
                        col = (hh * 2 + ab) * 128
                        k.mm(Ops.v(hh * 65, hh * 65 + 65), pt.v(col, col + 128), vt[m + ab].v(hh * 65, hh * 65 + 65),
                             start=(ab == 0), stop=(ab == 1))
                k.cp('dve', V(Osb.v(0, 256, pat="p (h c) -> p h c", c=64).ap, Osb.keys(0, 256)),
                     V(Ops.v(0, 260, pat="p (h c) -> p h c", c=65).ap[:, :, 0:64], Ops.keys(0, 260)))
                k.cp('dve', Osb.v(256, 260), V(Ops.v(0, 260, pat="p (h c) -> p h c", c=65).ap[:, :, 64], Ops.keys(0, 260)))
                pT_ = k.ps[2 + (m % 2)]
                for j in range(2):
                    k.tr(pT_.v(512 + j * 128, 512 + j * 128 + 128), Osb.v(j * 128, j * 128 + 128), c['ident_f'].v())
                k.tr(pT_.v(768, 896, p0=0, p1=4), Osb.v(256, 260), c['ident_f'].v())
                for j in range(2):
                    dst = numT.v(j * T + i0, j * T + i0 + 128 * dil - (dil - 1), step=dil)
                    if g == 0:
                        k.cp('dve', dst, pT_.v(512 + j * 128, 512 + j * 128 + 128))
                    else:
                        k.tt('dve', dst, pT_.v(512 + j * 128, 512 + j * 128 + 128), dst, ALU.add)
                dst = denT.v(i0, i0 + 128 * dil - (dil - 1), p0=0, p1=4, step=dil)
                if g == 0:
                    k.cp('dve', dst, pT_.v(768, 896, p0=0, p1=4))
                else:
                    k.tt('dve', dst, pT_.v(768, 896, p0=0, p1=4), dst, ALU.add)
    ehd_t = fwork.sub(512 + 3 * T + 260, 256)
    k.dma('sp', ehd_t.v(p0=0, p1=4), ehd_d.v())
    k.recip(denT.v(p0=0, p1=4), denT.v(p0=0, p1=4))
    for j in range(2):
        for tb in range(4):
            psb = k.ps[tb % 2].sub(0, 512)
            k.mm(psb.v(), ehd_t.v(j * 128, j * 128 + 128, p0=0, p1=4), denT.v(tb * 512, tb * 512 + 512, p0=0, p1=4))
            k.tt('dve', obT.v(j * T + tb * 512, j * T + tb * 512 + 512), numT.v(j * T + tb * 512, j * T + tb * 512 + 512), psb.v(), ALU.mult)

    wo = AB.sub(b0 + 20480, 8192)
    wpa = AB.sub(b0 + 28672, 4096)
    gat = AB.sub(b0 + 32768, 8192)
    mrg = AB.sub(b0 + 40960, 4096)
    wpb = AB.sub(b0 + 45056, 2048)
    k.dma('pool', wo.v(), wo_d.v())
    k.dma('pool', wpa.v(), wpa_d.v())
    k.dma('pool', wpb.v(), wpb_d.v())
    m1 = fwork.sub(0, 512)
    for tb in range(4):
        for gi_ in range(2):
            src = gates_d.ap[:, gi_ * 8 * T: (gi_ + 1) * 8 * T].rearrange("p (c t) -> p c t", t=T)[:, :, tb * 512:(tb + 1) * 512]
            k.dma('sp', V(gat.v(gi_ * 4096, gi_ * 4096 + 4096, pat="p (c t) -> p c t", t=512).ap, gat.keys(gi_ * 4096, gi_ * 4096 + 4096)),
                  V(src, gates_d.v().keys))
        for cc in range(8):
            psa = k.ps[cc % 2].sub(0, 512)
            psb = k.ps[cc % 2].sub(512, 512)
            for kc in range(4):
                k.mm(psa.v(), wpa.v(kc * 1024 + cc * 128, kc * 1024 + cc * 128 + 128),
                     oaT.v(kc * T + tb * 512, kc * T + tb * 512 + 512), start=(kc == 0), stop=(kc == 3))
            for kc in range(2):
                k.mm(psb.v(), wpb.v(kc * 1024 + cc * 128, kc * 1024 + cc * 128 + 128),
                     obT.v(kc * T + tb * 512, kc * T + tb * 512 + 512), start=(kc == 0), stop=(kc == 1))
            k.tt('dve', m1.v(), psa.v(), gat.v(cc * 512, cc * 512 + 512), ALU.mult)
            k.tt('dve', mrg.v(cc * 512, cc * 512 + 512), psb.v(), gat.v(4096 + cc * 512, 4096 + cc * 512 + 512), ALU.mult)
            k.tt('pool', mrg.v(cc * 512, cc * 512 + 512), mrg.v(cc * 512, cc * 512 + 512), m1.v(), ALU.add)
        for cc in range(8):
            psy = k.ps[2 + cc % 2].sub(0, 512)
            for kc in range(8):
                k.mm(psy.v(), wo.v(kc * 1024 + cc * 128, kc * 1024 + cc * 128 + 128),
                     mrg.v(kc * 512, kc * 512 + 512), start=(kc == 0), stop=(kc == 7))
            xs = xT.v(cc * T + tb * 512, cc * T + tb * 512 + 512)
            k.stt('dve', xs, psy.v(), mod.v(16 + cc, 17 + cc), xs, ALU.mult, ALU.add)

    hT = AB.sub(b0, 8 * T)
    wring = [AB.sub(b0 + 16384 + i * 6144, 6144) for i in range(4)]
    hid = [AB.sub(b0 + 40960 + i * 4096, 4096) for i in range(2)]
    wb2 = AB.sub(b0 + 49152, 1024)
    h32b = fwork.sub(2048, 4096)
    lg = fwork.sub(6144, 36 * 16)
    comb = fwork.sub(6144 + 576, 32 * 16)
    rw = fwork.sub(6144 + 576 + 512, 96)

    def hook(tb, cc):
        if cc is not None:
            return h32b.sub(cc * 512, 512)
        for q in range(4):
            tt_ = tb * 4 + q
            psl = k.ps[2 + q % 2].sub(0, 36)
            for kc in range(8):
                k.mm(psl.v(), h32b.v(kc * 512 + q * 128, kc * 512 + q * 128 + 128), wr.v(kc * 36, kc * 36 + 36),
                     start=(kc == 0), stop=(kc == 7))
            k.tt('dve', lg.v(tt_ * 36, tt_ * 36 + 36), psl.v(), br.v(), ALU.add)
        return None

    rms_mod(k, xT, hT, c, sc2, mod.sub(24, 8), fwork.sub(0, 2048), wb2, h32_hook=hook)

    for tt_ in range(16):
        l1 = lg.v(tt_ * 36, tt_ * 36 + 4)
        l2 = lg.sub(tt_ * 36 + 4, 32)
        r = rw
        m1c = r.v(0, 1); nm1 = r.v(1, 2); e1 = r.v(2, 6); s1 = r.v(6, 7); gval = r.v(7, 8)
        oh = r.sub(8, 4); l2m = r.sub(12, 32); ig = r.sub(44, 8); v1 = r.v(52, 53); mk1 = r.sub(53, 8)
        ig2 = r.sub(61, 8); v2 = r.v(69, 70); mk2 = r.sub(70, 8); dd = r.v(78, 79); w1 = r.v(79, 80); w2 = r.v(80, 81)
        cig = r.sub(81, 8)
        k.red('dve', m1c, l1, ALU.max)
        k.ts('dve', nm1, m1c, -1.0, None, ALU.mult)
        k.act(e1, l1, AF.Exp, bias=nm1, accum=s1)
        k.recip(gval, s1)
        k.ts('dve', oh.v(), l1, m1c, None, ALU.is_equal)
        k.tt('dve', V(l2m.v(0, 32, pat="p (g e) -> p g e", e=8).ap, l2m.keys(0, 32)),
             V(l2.v(0, 32, pat="p (g e) -> p g e", e=8).ap, l2.keys(0, 32)),
             V(oh.v(0, 4).ap.unsqueeze(2).to_broadcast([128, 4, 8]), oh.keys(0, 4)), ALU.mult)
        k.red('dve', ig.v(), V(l2m.v(0, 32, pat="p (g e) -> p e g", e=8).ap, l2m.keys(0, 32)), ALU.add)
        k.red('dve', v1, ig.v(), ALU.max)
        k.ts('dve', mk1.v(), ig.v(), v1, None, ALU.is_equal)
        k.stt('dve', ig2.v(), mk1.v(), -1e30, ig.v(), ALU.mult, ALU.add)
        k.red('dve', v2, ig2.v(), ALU.max)
        k.ts('dve', mk2.v(), ig2.v(), v2, None, ALU.is_equal)
        k.tt('dve', dd, v1, v2, ALU.subtract)
        k.act(w1, dd, AF.Sigmoid)
        k.act(w2, dd, AF.Sigmoid, scale=-1.0)
        k.tt('dve', w1, w1, gval, ALU.mult)
        k.tt('dve', w2, w2, gval, ALU.mult)
        k.ts('dve', cig.v(), mk1.v(), w1, None, ALU.mult)
        k.stt('dve', cig.v(), mk2.v(), w2, cig.v(), ALU.mult, ALU.add)
        k.tt('dve', V(comb.v(tt_ * 32, tt_ * 32 + 32, pat="p (g e) -> p g e", e=8).ap, comb.keys(tt_ * 32, tt_ * 32 + 32)),
             V(oh.v(0, 4).ap.unsqueeze(2).to_broadcast([128, 4, 8]), oh.keys(0, 4)),
             V(cig.v(0, 8).ap.unsqueeze(1).to_broadcast([128, 4, 8]), cig.keys(0, 8)), ALU.mult)
    CTt = fwork.sub(0, 2048)
    for tt_ in range(16):
        pst = k.ps[2 + tt_ % 2].sub(512, 128)
        k.tr(pst.v(p0=0, p1=32), comb.v(tt_ * 32, tt_ * 32 + 32), c['ident_f'].v())
        k.cp('dve', CTt.v(tt_ * 128, tt_ * 128 + 128, p0=0, p1=32), pst.v(p0=0, p1=32))
    selt = fwork.sub(2048, 4096)
    k.dma('sp', selt.v(p0=0, p1=32), sel_d.v())

    G = 2
    sg = [fwork.sub(6144 + i * 512, 512) for i in range(2)]

    def wload(e):
        k.dma('pool', wring[e % 4].v(), V(we_d.ap[e], we_d.v(key=e).keys))

    for e in range(4):
        wload(e)
    hcnt = [0]
    for eg in range(NEXP // G):
        if eg >= 1 and (eg + 1) * G < NEXP:
            for ei in range(G):
                wload((eg + 1) * G + ei)
        for tb in range(4):
            hb = hid[hcnt[0] % 2]
            hcnt[0] += 1
            for ei in range(G):
                e = eg * G + ei
                w = wring[e % 4]
                psc = k.ps[3].sub(512, 512)
                k.mm(psc.v(), selt.v(e * 128, e * 128 + 128, p0=0, p1=32), CTt.v(tb * 512, tb * 512 + 512, p0=0, p1=32))
                for ch in range(2):
                    psg = k.ps[ch].sub(0, 512)
                    psu = k.ps[ch].sub(512, 512)
                    for kc in range(8):
                        k.mm(psg.v(), w.v(kc * 256 + ch * 128, kc * 256 + ch * 128 + 128),
                             hT.v(kc * T + tb * 512, kc * T + tb * 512 + 512), start=(kc == 0), stop=(kc == 7))
                    for kc in range(8):
                        k.mm(psu.v(), w.v(2048 + kc * 256 + ch * 128, 2048 + kc * 256 + ch * 128 + 128),
                             hT.v(kc * T + tb * 512, kc * T + tb * 512 + 512), start=(kc == 0), stop=(kc == 7))
                    s_ = sg[ch]
                    k.act(s_.v(), psg.v(), AF.Silu)
                    k.tt('dve', s_.v(), psu.v(), s_.v(), ALU.mult)
                    k.tt('dve', hb.v((ei * 2 + ch) * 512, (ei * 2 + ch) * 512 + 512), psc.v(), s_.v(), ALU.mult)
            for cc in range(8):
                psy = k.ps[2].sub((cc % 2) * 512, 512)
                for ei in range(G):
                    w = wring[(eg * G + ei) % 4]
                    for ch in range(2):
                        k.mm(psy.v(), w.v(4096 + ch * 1024 + cc * 128, 4096 + ch * 1024 + cc * 128 + 128),
                             hb.v((ei * 2 + ch) * 512, (ei * 2 + ch) * 512 + 512),
                             start=(ei == 0 and ch == 0), stop=(ei == G - 1 and ch == 1))
                xs = xT.v(cc * T + tb * 512, cc * T + tb * 512 + 512)
                k.stt('dve', xs, psy.v(), mod.v(40 + cc, 41 + cc), xs, ALU.mult, ALU.add)
    return


def build_F():
    k = K(nf32=25600, nbf16=51200)
    nc = k.nc
    D = {}

    def ext(name, shape, dt):
        D[name] = k.din(name, shape, dt)

    def itn(name, shape, dt):
        D[name] = DBuf(name, nc.dram_tensor(name, list(shape), dt).ap())

    ext('xT', [128, 8 * T], F32); ext('c_col', [128, 8], F32); ext('posb', [128, T], I32); ext('invf', [128, 1], F32)
    ext('sel', [32, 32 * 128], F32); ext('ehd', [4, 256], F32); ext('idx', [128, 93], I32); ext('vmask', [128, 69], F32)
    for l in range(2):
        ext('w_ada%d' % l, [6, 128, 8 * 1024], F32); ext('b_ada%d' % l, [128, 48], F32); ext('gains%d' % l, [128, 4], F32)
        for n, nc_ in WIN_GROUPS:
            ext('w_%s%d' % (n, l), [128, 8 * nc_], F32)
        ext('lamp%d' % l, [128, 258], F32); ext('subln%d' % l, [128, 128], F32)
        ext('w_pa%d' % l, [128, 4096], F32); ext('w_pb%d' % l, [128, 2048], F32); ext('w_o%d' % l, [128, 8192], F32)
        ext('w_r%d' % l, [128, 288], F32); ext('b_r%d' % l, [128, 36], F32); ext('w_e%d' % l, [NEXP, 128, 6144], F32)
    def pieces(nm, n, ls, as_):
        D[nm + '_loc'] = [DBuf('%s_loc%d' % (nm, i), nc.dram_tensor('%s_loc%d' % (nm, i), ls, BF16).ap()) for i in range(n)]
        D[nm + '_all'] = [DBuf('%s_all%d' % (nm, i), nc.dram_tensor('%s_all%d' % (nm, i), as_, BF16).ap()) for i in range(n)]
    pieces('kTa', 2, [256, 2048], [1024, 2048])
    pieces('va', 4, [T, 129], [SEQ, 129])
    pieces('kTb', 3, [256, 2048], [1024, 2048])
    pieces('vb', 3, [T, 256], [SEQ, 256])
    itn('qTa_s', [128, 4 * T], BF16); itn('qTb_s', [128, 6 * T], BF16)
    itn('gates_s', [128, 16 * T], BF16); itn('mod_s', [128, 48], F32)
    out_d = k.dout('outT', [128, 8 * T], F32)
    c = load_consts(k, True)
    xT = k.fa(8 * T)
    k.idx_t = k.es.enter_context(nc.sbuf_tensor("idx_t", [128, 93], I32))
    idxb = Buf("idx_t", k.idx_t, 4, 0, 93)
    k.dma('sp', idxb.v(), D['idx'].v())
    k.dma('sp', xT.v(), D['xT'].v())
    base = (k.fo, k.bo)
    rg = [[0, 1, 2, 3], [4, 5, 6, 7]]
    for l in range(2):
        k.fo, k.bo = base
        def after_kv():
            for nm, i_ in [('kTa', 0), ('kTa', 1), ('va', 0), ('va', 1), ('va', 2), ('va', 3),
                           ('kTb', 0), ('kTb', 1), ('kTb', 2), ('vb', 0), ('vb', 1), ('vb', 2)]:
                loc, al = D[nm + '_loc'][i_], D[nm + '_all'][i_]
                k.S.add('pool', lambda e, loc=loc, al=al: e.collective_compute(
                    "AllGather", ALU.bypass, replica_groups=rg, ins=[loc.ap.opt()], outs=[al.ap.opt()]),
                    reads=loc.v().keys, writes=al.v().keys, dma=True, cc=True)
        k.after_kv = after_kv
        body_A(k, c, xT, D, l)
        k.fo, k.bo = base
        body_B(k, c, xT, D, l)
    k.dma('sp', out_d.v(), xT.v())
    return k.finalize()


_cache = {}


def _fm(w):
    K_, N = w.shape
    return np.ascontiguousarray(w.reshape(K_ // 128, 128, N).transpose(1, 0, 2).reshape(128, (K_ // 128) * N))


def _index_tables(jr):
    p = np.arange(128)
    idx = np.zeros((128, 93), np.int64)
    vmask = np.zeros((128, 69), np.float32)
    for jp in range(6):
        for seg in range(4):
            rank = [jr - 1, jr, jr, jr + 1][seg]
            half = [1, 0, 1, 0][seg]
            rank = min(max(rank, 0), 3)
            idx[:, jp * 4 + seg] = rank * 512 + ((jp % 2) * 128 + p) * 2 + half
    t = 0
    for g, dil in enumerate([1, 4, 16]):
        for r in range(dil):
            for n in range(16 // dil + 1):
                a_k = 128 * n - 64 + p
                gpos = jr * T + a_k * dil + r
                valid = (gpos >= 0) & (gpos < SEQ)
                gp = np.where(valid, gpos, 0)
                idx[:, 24 + t] = gp
                vmask[:, t] = valid.astype(np.float32)
                t += 1
    assert t == 69
    return idx.astype(np.int32), vmask


def kernel(x, c, positions, w_ada, b_ada, w_in, qn_a, kn_a, lam_q1, lam_k1, lam_q2, lam_k2,
           subln_a, qn_b, kn_b, w_pa, w_pb, w_o, w_r1, b_r1, w_r2, b_r2, w_e_gate, w_e_up, w_e_down):
    x = np.asarray(x, np.float32)
    consts = host_consts()
    invf = (np.float32(10000.0) ** (-np.arange(0, 64, 2, dtype=np.float32) / np.float32(64))).astype(np.float32)
    invf_col = invf[(np.arange(128) % 64) % 32].reshape(128, 1).astype(np.float32)
    cores = list(range(NCORES))
    sel = np.zeros((32, 32 * 128), np.float32)
    for e in range(32):
        sel[e, e * 128:(e + 1) * 128] = 1.0
    ehd = np.zeros((4, 256), np.float32)
    for h in range(4):
        j, half = h // 2, h % 2
        ehd[h, j * 128 + half * 64: j * 128 + half * 64 + 64] = 1.0
    cuts = np.cumsum([0, 512, 512, 512, 768, 768, 768, 1024, 1024])
    names = ['qa', 'ka', 'va', 'qb', 'kb', 'vb', 'ga', 'gb']
    shared = {"consts": consts, "invf": invf_col, "sel": sel, "ehd": ehd}
    pidx = np.arange(128) % 64
    for l in range(2):
        wl = np.asarray(w_in[l], np.float32)
        for i, n in enumerate(names):
            shared["w_%s%d" % (n, l)] = _fm(wl[:, cuts[i]:cuts[i + 1]])
        shared["w_ada%d" % l] = np.ascontiguousarray(
            np.asarray(w_ada[l], np.float32).reshape(8, 128, 6, 1024).transpose(2, 1, 0, 3).reshape(6, 128, 8 * 1024))
        shared["b_ada%d" % l] = np.ascontiguousarray(np.asarray(b_ada[l], np.float32).reshape(48, 128).T)
        shared["gains%d" % l] = np.stack([np.asarray(qn_a[l])[pidx], np.asarray(kn_a[l])[pidx],
                                          np.asarray(qn_b[l])[pidx], np.asarray(kn_b[l])[pidx]], axis=1).astype(np.float32)
        lam_init = 0.8 - 0.6 * math.exp(-0.3 * l)
        lamp = np.concatenate([np.asarray(lam_q1[l]), np.asarray(lam_k1[l]), np.asarray(lam_q2[l]), np.asarray(lam_k2[l]),
                               np.array([lam_init, 1.0 - lam_init])]).astype(np.float32)
        shared["lamp%d" % l] = np.ascontiguousarray(np.broadcast_to(lamp[None, :], (128, 258)))
        shared["subln%d" % l] = np.ascontiguousarray(np.broadcast_to(np.asarray(subln_a[l], np.float32)[:, None], (128, 128)))
        shared["w_r%d" % l] = _fm(np.concatenate([np.asarray(w_r1[l], np.float32), np.asarray(w_r2[l], np.float32)], axis=1))
        shared["b_r%d" % l] = np.ascontiguousarray(np.broadcast_to(
            np.concatenate([np.asarray(b_r1[l]), np.asarray(b_r2[l])]).astype(np.float32)[None, :], (128, 36)))
        we = np.empty((NEXP, 128, 6144), np.float32)
        for e in range(NEXP):
            we[e, :, 0:2048] = _fm(np.asarray(w_e_gate[l, e], np.float32))
            we[e, :, 2048:4096] = _fm(np.asarray(w_e_up[l, e], np.float32))
            we[e, :, 4096:6144] = _fm(np.asarray(w_e_down[l, e], np.float32))
        shared["w_e%d" % l] = we
        shared["w_pa%d" % l] = _fm(np.asarray(w_pa[l], np.float32))
        shared["w_pb%d" % l] = _fm(np.asarray(w_pb[l], np.float32))
        shared["w_o%d" % l] = _fm(np.asarray(w_o[l], np.float32))
    in_maps = []
    for ci in cores:
        b, j = ci // 4, ci % 4
        xs = x[b, j * T:(j + 1) * T, :]
        idx, vmask = _index_tables(j)
        m = dict(shared)
        m["xT"] = np.ascontiguousarray(xs.T.reshape(8, 128, T).transpose(1, 0, 2).reshape(128, 8 * T))
        m["c_col"] = np.ascontiguousarray(np.asarray(c[b], np.float32).reshape(8, 128).T)
        m["posb"] = np.ascontiguousarray(np.broadcast_to(np.asarray(positions[b, j * T:(j + 1) * T], np.int32)[None, :], (128, T)))
        m["idx"] = idx
        m["vmask"] = vmask
        in_maps.append(m)
    if 'F' not in _cache:
        _cache['F'] = build_F()
    res = run_bass_kernel_spmd(_cache['F'], in_maps, core_ids=cores).results
    out = np.empty((2, SEQ, D), np.float32)
    for ci in cores:
        b, j = ci // 4, ci % 4
        o = np.asarray(res[ci]["outT"], np.float32)
        out[b, j * T:(j + 1) * T, :] = o.reshape(128, 8, T).transpose(1, 0, 2).reshape(D, T).T
    return out
```

```python
import math
import numpy as np
from contextlib import ExitStack
import concourse.bass as bass
import concourse.mybir as mybir
from concourse.bass_utils import run_bass_kernel_spmd

F32 = mybir.dt.float32
BF16 = mybir.dt.bfloat16
I32 = mybir.dt.int32
ALU = mybir.AluOpType
AF = mybir.ActivationFunctionType
AX = mybir.AxisListType

ENGS = ['pe', 'act', 'dve', 'pool', 'sp']
CHUNK = 1024

NCORES = 8
T = 2048
SEQ = 8192
D = 1024
EPS = 1e-6
NEG = -30000.0
WIN = 4096
PADW = 1024
NEXP = 32


class Op:
    __slots__ = ('eng', 'fn', 'src', 'pos', 'signal', 'waits', 'know', 'is_dma', 'cnt')


class V:
    __slots__ = ('ap', 'keys')

    def __init__(self, ap, keys):
        self.ap = ap
        self.keys = keys


class Buf:
    def __init__(self, arena_name, handle, esz, off, n):
        self.an = arena_name
        self.t = handle
        self.esz = esz
        self.off = off
        self.n = n

    def keys(self, a, b):
        ch = 2048 if self.an.startswith('ps') else CHUNK
        lo = ((self.off + a) * self.esz) // ch
        hi = ((self.off + b) * self.esz - 1) // ch
        return [(self.an, i) for i in range(lo, hi + 1)]

    def v(self, a=0, b=None, p0=0, p1=128, step=1, pat=None, **kw):
        if b is None:
            b = self.n
        assert 0 <= a < b <= self.n, (a, b, self.n)
        if step == 1:
            ap = self.t[p0:p1, self.off + a:self.off + b]
        else:
            ap = self.t[p0:p1, self.off + a:self.off + b:step]
        if pat is not None:
            ap = ap.rearrange(pat, **kw)
        return V(ap, self.keys(a, b))

    def sub(self, off, n):
        assert off + n <= self.n
        return Buf(self.an, self.t, self.esz, self.off + off, n)


class DBuf:
    def __init__(self, name, ap):
        self.name = name
        self.ap = ap

    def v(self, ap=None, key=None):
        return V(self.ap if ap is None else ap, [(self.name, key)])


class Sched:
    def __init__(self, nc, n_dma_sems=16):
        self.nc = nc
        self.ops = {e: [] for e in ENGS}
        self.ncomp = {e: 0 for e in ENGS}
        self.last_w = {}
        self.readers = {}
        self.known = {e: {} for e in ENGS}
        self.n_dma_sems = n_dma_sems
        self.dma_last = [None] * n_dma_sems
        self.dma_cnt = [0] * n_dma_sems
        self.dma_rr = 0
        self.cc_cnt = 0
        self.cc_last = None

    def _need(self, e, d):
        k = self.known[e]
        if k.get(d.src, -1) >= d.pos:
            return None
        d.signal = True
        for s, p in d.know.items():
            if k.get(s, -1) < p:
                k[s] = p
        return d

    def add(self, eng, fn, reads=(), writes=(), dma=False, cc=False):
        op = Op()
        op.eng = eng
        op.fn = fn
        op.is_dma = dma
        op.signal = dma
        deps = []
        seen = set()
        pr_ = [k for k in reads if isinstance(k[0], str) and k[0].startswith('ps')]
        if pr_:
            writes = list(writes) + pr_
        for k in reads:
            w = self.last_w.get(k)
            if w is not None and id(w) not in seen:
                seen.add(id(w)); deps.append(w)
        for k in writes:
            w = self.last_w.get(k)
            if w is not None and id(w) not in seen:
                seen.add(id(w)); deps.append(w)
            for r in self.readers.get(k, ()):
                if id(r) not in seen:
                    seen.add(id(r)); deps.append(r)
        if cc:
            op.src = ('cc', 0)
            op.pos = self.cc_cnt
            self.cc_cnt += 1
            if self.cc_last is not None and id(self.cc_last) not in seen:
                seen.add(id(self.cc_last)); deps.append(self.cc_last)
            self.cc_last = op
        elif dma:
            slot = self.dma_rr
            self.dma_rr = (self.dma_rr + 1) % self.n_dma_sems
            prev = self.dma_last[slot]
            if prev is not None and id(prev) not in seen:
                seen.add(id(prev)); deps.append(prev)
            op.src = ('dma', slot)
            op.pos = self.dma_cnt[slot]
            self.dma_cnt[slot] += 1
            self.dma_last[slot] = op
        else:
            op.src = eng
            op.pos = self.ncomp[eng]
            self.ncomp[eng] += 1
        waits = []
        deps.sort(key=lambda d: -d.pos)
        for d in deps:
            if (not dma) and (not d.is_dma) and d.src == eng:
                if eng == 'pe':
                    continue
                if op.pos - d.pos > 2:
                    continue
            w = self._need(eng, d)
            if w is not None:
                waits.append(w)
        op.waits = waits
        know = dict(self.known[eng])
        know[op.src] = op.pos
        op.know = know
        for k in reads:
            self.readers.setdefault(k, []).append(op)
        for k in writes:
            self.last_w[k] = op
            self.readers[k] = []
        self.ops[eng].append(op)
        return op

    def finish(self):
        op = Op()
        op.eng = 'sp'; op.fn = None; op.is_dma = False; op.signal = False
        op.src = 'sp'; op.pos = self.ncomp['sp']; self.ncomp['sp'] += 1
        waits = []
        for d in list(self.dma_last) + [self.cc_last]:
            if d is not None:
                w = self._need('sp', d)
                if w is not None:
                    waits.append(w)
        for e in ['pe', 'act', 'dve', 'pool']:
            comp = [o for o in self.ops[e] if not o.is_dma]
            if comp:
                w = self._need('sp', comp[-1])
                if w is not None:
                    waits.append(w)
        op.waits = waits
        op.know = {}
        self.ops['sp'].append(op)

    def emit(self, sems):
        nc = self.nc
        for e in ENGS:
            c = 0
            for o in self.ops[e]:
                if o.is_dma:
                    continue
                if o.signal:
                    c += 1
                o.cnt = c
        engobj = {'pe': nc.tensor, 'act': nc.scalar, 'dve': nc.vector, 'pool': nc.gpsimd, 'sp': nc.sync}

        def run(e):
            eo = engobj[e]
            for o in self.ops[e]:
                for d in o.waits:
                    if d.is_dma and d.src[0] == 'cc':
                        eo.wait_ge(sems[d.src], d.pos + 1)
                    elif d.is_dma:
                        eo.wait_ge(sems[d.src], 16 * (d.pos + 1))
                    else:
                        eo.wait_ge(sems[d.src], d.cnt)
                if o.fn is None:
                    continue
                ins = o.fn(eo)
                if o.is_dma and o.src[0] == 'cc':
                    ins.then_inc(sems[o.src])
                elif o.is_dma:
                    ins.then_inc(sems[o.src], 16)
                elif o.signal:
                    ins.then_inc(sems[e], 1)
        return run


class K:
    def __init__(self, nf32, nbf16):
        self.nc = bass.Bass("TRN2", target_bir_lowering=False)
        self.es = ExitStack()
        nc = self.nc
        self.S = Sched(nc)
        es = self.es
        self.af_t = es.enter_context(nc.sbuf_tensor("arena_f", [128, nf32], F32))
        self.ab_t = es.enter_context(nc.sbuf_tensor("arena_b", [128, nbf16], BF16))
        self.AF_ = Buf("af", self.af_t, 4, 0, nf32)
        self.AB_ = Buf("ab", self.ab_t, 2, 0, nbf16)
        self.ps = []
        for i in range(4):
            t = es.enter_context(nc.psum_tensor("ps%d" % i, [128, 1024], F32))
            self.ps.append(Buf("ps%d" % i, t, 4, 0, 1024))
        self.sems = {}
        for e in ENGS:
            self.sems[e] = es.enter_context(nc.semaphore("s_" + e))
        for i in range(self.S.n_dma_sems):
            self.sems[('dma', i)] = es.enter_context(nc.semaphore("d%d" % i))
        self.sems[('cc', 0)] = es.enter_context(nc.semaphore("ccsem"))
        self.fo = 0
        self.bo = 0
        self.dram = {}

    def din(self, name, shape, dt):
        ap = self.nc.dram_tensor(name, list(shape), dt, kind="ExternalInput").ap()
        d = DBuf(name, ap)
        self.dram[name] = d
        return d

    def dout(self, name, shape, dt):
        ap = self.nc.dram_tensor(name, list(shape), dt, kind="ExternalOutput").ap()
        d = DBuf(name, ap)
        self.dram[name] = d
        return d

    def fa(self, n):
        b = self.AF_.sub(self.fo, n)
        self.fo += n
        return b

    def ba(self, n):
        b = self.AB_.sub(self.bo, n)
        self.bo += n
        return b

    def mm(self, out, lhsT, rhs, start=True, stop=True):
        self.S.add('pe', lambda e: e.matmul(out.ap, lhsT.ap, rhs.ap, start=start, stop=stop),
                   reads=lhsT.keys + rhs.keys, writes=out.keys)

    def tr(self, out, in_, ident):
        self.S.add('pe', lambda e: e.transpose(out.ap, in_.ap, ident.ap),
                   reads=in_.keys + ident.keys, writes=out.keys)

    def act(self, out, in_, func, scale=1.0, bias=0.0, accum=None):
        reads = list(in_.keys)
        kw = {}
        if isinstance(bias, V):
            reads += bias.keys
            kw['bias'] = bias.ap
        else:
            kw['bias'] = float(bias)
        if isinstance(scale, V):
            reads += scale.keys
            kw['scale'] = scale.ap
        else:
            kw['scale'] = float(scale)
        writes = list(out.keys)
        if accum is not None:
            writes += accum.keys
            kw['accum_out'] = accum.ap
        self.S.add('act', lambda e: e.activation(out=out.ap, in_=in_.ap, func=func, **kw),
                   reads=reads, writes=writes)

    def tt(self, eng, out, in0, in1, op):
        self.S.add(eng, lambda e: e.tensor_tensor(out=out.ap, in0=in0.ap, in1=in1.ap, op=op),
                   reads=in0.keys + in1.keys, writes=out.keys)

    def ts(self, eng, out, in0, s1, s2=None, op0=ALU.mult, op1=None):
        reads = list(in0.keys)
        a1 = s1
        a2 = s2
        if isinstance(s1, V):
            reads += s1.keys; a1 = s1.ap
        if isinstance(s2, V):
            reads += s2.keys; a2 = s2.ap
        if op1 is None:
            self.S.add(eng, lambda e: e.tensor_scalar(out=out.ap, in0=in0.ap, scalar1=a1, scalar2=None, op0=op0),
                       reads=reads, writes=out.keys)
        else:
            self.S.add(eng, lambda e: e.tensor_scalar(out=out.ap, in0=in0.ap, scalar1=a1, scalar2=a2, op0=op0, op1=op1),
                       reads=reads, writes=out.keys)

    def stt(self, eng, out, in0, scalar, in1, op0, op1):
        reads = in0.keys + in1.keys
        a = scalar
        if isinstance(scalar, V):
            reads = reads + scalar.keys; a = scalar.ap
        self.S.add(eng, lambda e: e.scalar_tensor_tensor(out=out.ap, in0=in0.ap, scalar=a, in1=in1.ap, op0=op0, op1=op1),
                   reads=reads, writes=out.keys)

    def cp(self, eng, out, in_):
        if eng == 'act':
            self.S.add('act', lambda e: e.copy(out=out.ap, in_=in_.ap), reads=in_.keys, writes=out.keys)
        else:
            self.S.add(eng, lambda e: e.tensor_copy(out=out.ap, in_=in_.ap), reads=in_.keys, writes=out.keys)

    def red(self, eng, out, in_, op, axis=AX.X):
        self.S.add(eng, lambda e: e.tensor_reduce(out=out.ap, in_=in_.ap, axis=axis, op=op),
                   reads=in_.keys, writes=out.keys)

    def recip(self, out, in_):
        self.S.add('dve', lambda e: e.reciprocal(out=out.ap, in_=in_.ap), reads=in_.keys, writes=out.keys)

    def memset(self, eng, out, val):
        self.S.add(eng, lambda e: e.memset(out.ap, val), writes=out.keys)

    def dma(self, eng, out, in_):
        self.S.add(eng, lambda e: e.dma_start(out=out.ap, in_=in_.ap), reads=in_.keys, writes=out.keys, dma=True)

    def finalize(self):
        S = self.S
        S.finish()
        run = S.emit(self.sems)
        with self.nc.Block() as block:
            @block.tensor
            def _(e): run('pe')
            @block.scalar
            def _(e): run('act')
            @block.vector
            def _(e): run('dve')
            @block.gpsimd
            def _(e): run('pool')
            @block.sync
            def _(e): run('sp')
        self.es.close()
        return self.nc


def load_consts(k, need_rope):
    c = {}
    cin = k.din("consts", [128, 5 * 128], F32)
    cf = k.fa(256).sub(0, 128)
    k.dma('sp', cf.v(), V(cin.ap[:, 0:128], cin.v().keys))
    c['ident_f'] = cf
    cb = k.ba(5 * 128)
    k.dma('pool', cb.v(), cin.v())
    c['ident_b'] = cb.sub(0, 128)
    c['onesbd'] = cb.sub(128, 128)
    c['rotT'] = cb.sub(256, 128)
    c['maskA'] = cb.sub(384, 128)
    c['maskB'] = cb.sub(512, 128)
    ones = k.ba(128)
    k.ba(256)
    k.memset('pool', ones.v(), 1.0)
    c['ones_b'] = ones
    return c


def host_consts():
    ident = np.eye(128, dtype=np.float32)
    onesbd = np.zeros((128, 128), np.float32)
    onesbd[:64, :64] = 1.0
    onesbd[64:, 64:] = 1.0
    rotT = np.zeros((128, 128), np.float32)
    for m in range(128):
        j = m % 64
        if j < 32:
            rotT[m + 32, m] = -1.0
        else:
            rotT[m - 32, m] = 1.0
    u = np.arange(128)[:, None]
    a = np.arange(128)[None, :]
    maskA = np.where(u >= a, 0.0, NEG).astype(np.float32)
    maskB = np.where(u <= a, 0.0, NEG).astype(np.float32)
    return np.concatenate([ident, onesbd, rotT, maskA, maskB], axis=1)


TWO_PI = 2.0 * math.pi
C1 = 6.28125
C2 = float(np.float32(TWO_PI - 6.28125))
C3 = float(TWO_PI - 6.28125 - float(np.float32(TWO_PI - 6.28125)))


WIN_GROUPS = [('ka', 512), ('kb', 768), ('va', 512), ('vb', 768), ('qa', 512), ('qb', 768), ('ga', 1024), ('gb', 1024)]


def rms_mod(k, xT, hT, c, scale_col, shift_col, work_f, work_b, h32_hook=None):
    sq = [work_b.sub(i * 512, 512) for i in range(2)]
    sd = work_f.sub(0, 512)
    rs = work_f.sub(512, 512)
    tmp = [work_f.sub(1024 + i * 512, 512) for i in range(2)]
    for tb in range(4):
        pss = k.ps[tb % 2].sub(0, 512)
        for cc in range(8):
            s = sq[cc % 2]
            k.act(s.v(), xT.v(cc * T + tb * 512, cc * T + tb * 512 + 512), AF.Square)
            k.mm(pss.v(), c['ones_b'].v(), s.v(), start=(cc == 0), stop=(cc == 7))
        k.act(sd.v(), pss.v(), AF.Sqrt, scale=1.0 / D, bias=EPS)
        k.recip(rs.v(), sd.v())
        for cc in range(8):
            t = tmp[cc % 2]
            k.tt('pool', t.v(), xT.v(cc * T + tb * 512, cc * T + tb * 512 + 512), rs.v(), ALU.mult)
            if h32_hook is not None:
                h32 = h32_hook(tb, cc)
                k.ts('dve', h32.v(), t.v(), scale_col.v(cc, cc + 1), shift_col.v(cc, cc + 1), ALU.mult, ALU.add)
                k.cp('pool', hT.v(cc * T + tb * 512, cc * T + tb * 512 + 512), h32.v())
            else:
                k.ts('dve', hT.v(cc * T + tb * 512, cc * T + tb * 512 + 512), t.v(),
                     scale_col.v(cc, cc + 1), shift_col.v(cc, cc + 1), ALU.mult, ALU.add)
        if h32_hook is not None:
            h32_hook(tb, None)


def body_A(k, c, xT, D, l):
    stg = 99
    ccol_d = D['c_col']
    pos_d = D['posb']
    invf_d = D['invf']
    wada_d = D['w_ada%d' % l]
    bada_d = D['b_ada%d' % l]
    gains_d = D['gains%d' % l]
    wg_d = {n: D['w_%s%d' % (n, l)] for n, nc_ in WIN_GROUPS}
    kTa_o = D['kTa_loc']
    kTb_o = D['kTb_loc']
    qTa_o = D['qTa_s']
    qTb_o = D['qTb_s']
    va_o = D['va_loc']
    vb_o = D['vb_loc']
    gates_o = D['gates_s']
    mod_o = D['mod_s']

    cosT = k.fa(T)
    sinT = k.fa(T)
    small = k.fa(256)
    work_f = k.fa(4096)
    hT = k.ba(8 * T)
    wring = [k.ba(8192) for _ in range(3)]
    work_b = k.ba(2048)
    stage = [k.ba(2048) for _ in range(2)]
    vst = [k.ba(1024) for _ in range(2)]

    ccol = small.sub(0, 8)
    cact = k.ba(8)
    bada = small.sub(8, 48)
    mod = small.sub(56, 48)
    gains = small.sub(104, 4)
    invf = small.sub(108, 1)

    k.dma('sp', ccol.v(), ccol_d.v())
    k.dma('sp', bada.v(), bada_d.v())
    k.dma('sp', gains.v(), gains_d.v())
    k.dma('sp', invf.v(), invf_d.v())

    ang = work_f.sub(0, T)
    kk = work_f.sub(T, T)
    posi_v = V(kk.v().ap.bitcast(I32), kk.v().keys)
    k.dma('sp', posi_v, pos_d.v())
    k.cp('dve', ang.v(), posi_v)
    k.ts('dve', ang.v(), ang.v(), invf.v(), None, ALU.mult)
    k.ts('dve', kk.v(), ang.v(), 1.0 / TWO_PI, 12582912.0, ALU.mult, ALU.add)
    k.ts('dve', kk.v(), kk.v(), 12582912.0, None, ALU.subtract)
    k.stt('dve', ang.v(), kk.v(), -C1, ang.v(), ALU.mult, ALU.add)
    k.stt('dve', ang.v(), kk.v(), -C2, ang.v(), ALU.mult, ALU.add)
    k.stt('dve', ang.v(), kk.v(), -C3, ang.v(), ALU.mult, ALU.add)
    k.ts('dve', ang.v(), ang.v(), math.pi, -math.pi, ALU.min, ALU.max)
    k.act(sinT.v(), ang.v(), AF.Sin)
    k.act(kk.v(), ang.v(), AF.Sin, scale=0.5)
    k.tt('dve', kk.v(), kk.v(), kk.v(), ALU.mult)
    k.ts('dve', cosT.v(), kk.v(), -2.0, 1.0, ALU.mult, ALU.add)

    k.act(cact.v(), ccol.v(), AF.Silu)
    psm = k.ps[3].sub(0, 48)
    for s in range(6):
        wb = wring[s % 3]
        k.dma('pool', wb.v(), V(wada_d.ap[s], wada_d.v(key=s).keys))
        for cc in range(8):
            for kc in range(8):
                k.mm(psm.v(s * 8 + cc, s * 8 + cc + 1),
                     wb.v(kc * 1024 + cc * 128, kc * 1024 + cc * 128 + 128),
                     cact.v(kc, kc + 1), start=(kc == 0), stop=(kc == 7))
    k.tt('dve', mod.v(), psm.v(), bada.v(), ALU.add)
    k.dma('sp', mod_o.v(), mod.v())
    sc1 = small.sub(152, 8)
    k.ts('dve', sc1.v(), mod.v(8, 16), 1.0, None, ALU.add)

    rms_mod(k, xT, hT, c, sc1, mod.sub(0, 8), work_f, work_b)

    raw = [work_f.sub(i * 512, 512) for i in range(2)]
    rst = [work_f.sub(1024 + i * 512, 512) for i in range(2)]
    t1 = [work_f.sub(2048 + i * 512, 512) for i in range(2)]
    t2 = [work_f.sub(3072 + i * 512, 512) for i in range(2)]
    sqb = [work_b.sub(i * 512, 512) for i in range(2)]
    qnb = [work_b.sub(1024 + i * 512, 512) for i in range(2)]
    cnt = [0]

    def qk_block(psb, gcol, outv, tb):
        i = cnt[0] % 2
        cnt[0] += 1
        k.act(sqb[i].v(), psb.v(), AF.Square)
        k.ts('dve', raw[i].v(), psb.v(), gcol, None, ALU.mult)
        ps2 = k.ps[2].sub(i * 512, 512)
        k.mm(ps2.v(), c['onesbd'].v(), sqb[i].v())
        k.act(rst[i].v(), ps2.v(), AF.Sqrt, scale=1.0 / 64, bias=EPS)
        k.recip(rst[i].v(), rst[i].v())
        k.tt('pool', qnb[i].v(), raw[i].v(), rst[i].v(), ALU.mult)
        ps3 = k.ps[3].sub(i * 512, 512)
        k.mm(ps3.v(), c['rotT'].v(), qnb[i].v())
        k.tt('pool', t1[i].v(), qnb[i].v(), cosT.v(tb * 512, tb * 512 + 512), ALU.mult)
        k.tt('dve', t2[i].v(), ps3.v(), sinT.v(tb * 512, tb * 512 + 512), ALU.mult)
        k.tt('pool', outv, t1[i].v(), t2[i].v(), ALU.add)

    pcnt = [0]
    def wload_g(gj):
        nm_, nc__ = WIN_GROUPS[gj]
        k.dma('pool', wring[gj % 3].v(0, 8 * nc__), wg_d[nm_].v())

    wload_g(0)
    wload_g(1)
    for gi, (name, ncol) in enumerate(WIN_GROUPS):
        wb = wring[gi % 3]
        if gi + 2 < len(WIN_GROUPS):
            wload_g(gi + 2)
        if name in ('ka', 'kb', 'qa', 'qb'):
            npair = ncol // 128
            gidx = {'qa': 0, 'ka': 1, 'qb': 2, 'kb': 3}[name]
            od = {'ka': kTa_o, 'kb': kTb_o, 'qa': qTa_o, 'qb': qTb_o}[name]
            for pr in range(npair):
                st = stage[pr % 2]
                for tb in range(4):
                    psb = k.ps[pcnt[0] % 2].sub(512 * ((pcnt[0] // 2) % 2), 512)
                    pcnt[0] += 1
                    for kc in range(8):
                        k.mm(psb.v(), wb.v(kc * ncol + pr * 128, kc * ncol + pr * 128 + 128),
                             hT.v(kc * T + tb * 512, kc * T + tb * 512 + 512), start=(kc == 0), stop=(kc == 7))
                    qk_block(psb, gains.v(gidx, gidx + 1), st.v(tb * 512, tb * 512 + 512), tb)
                if name in ('ka', 'kb'):
                    odp = od[pr // 2]
                    k.dma('sp', V(odp.ap[(pr % 2) * 128:(pr % 2 + 1) * 128, :], odp.v().keys), st.v())
                else:
                    k.dma('sp', V(od.ap[:, pr * T:(pr + 1) * T], od.v().keys), st.v())
        elif name in ('va', 'vb'):
            nh_, dh_, d_ = (4, 129, 128) if name == 'va' else (12, 64, 64)
            od = va_o if name == 'va' else vb_o
            w_ = nh_ * dh_
            for i in range(2):
                k.memset('pool', vst[i].v(0, w_), 1.0)
            for tt_ in range(16):
                st = vst[tt_ % 2]
                for n0 in range(0, ncol, 512):
                    nn = min(512, ncol - n0)
                    psb = k.ps[pcnt[0] % 2].sub(512 * ((pcnt[0] // 2) % 2), 512)
                    pcnt[0] += 1
                    for kc in range(8):
                        k.mm(psb.v(0, nn), hT.v(kc * T + tt_ * 128, kc * T + tt_ * 128 + 128),
                             wb.v(kc * ncol + n0, kc * ncol + n0 + nn), start=(kc == 0), stop=(kc == 7))
                    h0 = n0 // d_
                    nhh = nn // d_
                    outv = V(st.v(h0 * dh_, (h0 + nhh) * dh_, pat="p (h c) -> p h c", c=dh_).ap[:, :, 0:d_],
                             st.keys(h0 * dh_, (h0 + nhh) * dh_))
                    inv = V(psb.v(0, nn, pat="p (h c) -> p h c", c=d_).ap, psb.keys(0, nn))
                    k.cp('act', outv, inv)
                if name == 'va':
                    for h4 in range(4):
                        k.dma('sp', V(od[h4].ap[tt_ * 128:(tt_ + 1) * 128, :], od[h4].v().keys), st.v(h4 * 129, h4 * 129 + 129))
                else:
                    for g3 in range(3):
                        k.dma('sp', V(od[g3].ap[tt_ * 128:(tt_ + 1) * 128, :], od[g3].v().keys), st.v(g3 * 256, g3 * 256 + 256))
        else:
            gi_ = 0 if name == 'ga' else 1
            for cc in range(8):
                st = stage[cc % 2]
                for tb in range(4):
                    psb = k.ps[pcnt[0] % 2].sub(512 * ((pcnt[0] // 2) % 2), 512)
                    pcnt[0] += 1
                    for kc in range(8):
                        k.mm(psb.v(), wb.v(kc * ncol + cc * 128, kc * ncol + cc * 128 + 128),
                             hT.v(kc * T + tb * 512, kc * T + tb * 512 + 512), start=(kc == 0), stop=(kc == 7))
                    k.act(st.v(tb * 512, tb * 512 + 512), psb.v(), AF.Sigmoid)
                o0 = (gi_ * 8 + cc) * T
                k.dma('sp', V(gates_o.ap[:, o0:o0 + T], gates_o.v().keys), st.v())
    return


def body_B(k, c, xT, D, l):
    mod_d = D['mod_s']
    qTa_d = D['qTa_s']
    qTb_d = D['qTb_s']
    kTa_d = D['kTa_all']
    va_d = D['va_all']
    kTb_d = D['kTb_all']
    vb_d = D['vb_all']
    gates_d = D['gates_s']
    lam_d = D['lamp%d' % l]
    subln_d = D['subln%d' % l]
    wpa_d = D['w_pa%d' % l]
    wpb_d = D['w_pb%d' % l]
    wo_d = D['w_o%d' % l]
    wr_d = D['w_r%d' % l]
    br_d = D['b_r%d' % l]
    sel_d = D['sel']
    ehd_d = D['ehd']
    we_d = D['w_e%d' % l]

    small = k.fa(1024)
    fwork = k.fa(25600 - k.fo)
    mod = small.sub(0, 48)
    lamp = small.sub(48, 258)
    subln = small.sub(320, 128)
    br = small.sub(448, 36)
    sc2 = small.sub(484, 8)
    lamcol = small.sub(492, 4)
    tmp64 = small.sub(512, 128)
    wr = small.sub(640, 288)

    k.dma('sp', mod.v(), mod_d.v())
    k.dma('sp', lamp.v(), lam_d.v())
    k.dma('sp', subln.v(), subln_d.v())
    k.dma('sp', br.v(), br_d.v())
    k.dma('sp', wr.v(), wr_d.v())

    k.tt('dve', tmp64.v(0, 64), lamp.v(0, 64), lamp.v(64, 128), ALU.mult)
    k.tt('dve', tmp64.v(64, 128), lamp.v(128, 192), lamp.v(192, 256), ALU.mult)
    k.red('dve', lamcol.v(0, 2), tmp64.v(0, 128, pat="p (a w) -> p a w", w=64), ALU.add)
    k.act(lamcol.v(0, 2), lamcol.v(0, 2), AF.Exp)
    k.tt('dve', lamcol.v(3, 4), lamcol.v(0, 1), lamcol.v(1, 2), ALU.subtract)
    k.tt('dve', lamcol.v(0, 1), lamcol.v(3, 4), lamp.v(256, 257), ALU.add)
    k.ts('dve', lamcol.v(1, 2), lamcol.v(0, 1), -1.0, None, ALU.mult)
    k.ts('dve', sc2.v(), mod.v(32, 40), 1.0, None, ALU.add)

    AB = k.AB_
    b0 = k.bo
    qTa = AB.sub(b0 + 0, 8192)
    oaT = AB.sub(b0 + 8192, 8192)
    obT = AB.sub(b0 + 16384, 4096)
    qTb = AB.sub(b0 + 20480, 12288)
    kring = [AB.sub(b0 + 32768 + i * 2048, 2048) for i in range(3)]
    vring = [AB.sub(b0 + 38912 + i * 2048, 2048) for i in range(3)]
    PT = [AB.sub(b0 + 45104 + i * 1024, 1024) for i in range(2)]

    k.dma('sp', qTa.v(), qTa_d.v())
    PT4 = [AB.sub(b0 + 45056 + i * 1024, 1024) for i in range(4)]
    tsum = AB.sub(b0 + 49152, 1024)
    acc = fwork.sub(1024, 1024)
    dsb = fwork.sub(2048, 512)
    rsb = fwork.sub(2560, 512)
    r2 = fwork.sub(3072, 512)
    slcol = fwork.sub(0, 1)
    k.tt('dve', slcol.v(), subln.v(0, 1), lamp.v(257, 258), ALU.mult)
    ld = [0]
    for h in range(4):
        for qb in range(4):
            OT = k.ps[2]

            SB = [0, 1, 3]

            def qk(kt, kbuf):
                pss = k.ps[SB[kt % 3]]
                for m in range(2):
                    k.mm(pss.v(m * 512, m * 512 + 512),
                         kbuf.v((kt % 16) * 128, (kt % 16) * 128 + 128, p0=m * 64, p1=m * 64 + 64),
                         qTa.v(h * T + qb * 512, h * T + qb * 512 + 512, p0=m * 64, p1=m * 64 + 64))

            bufs = {}

            def load(ch):
                i = ld[0] % 3
                ld[0] += 1
                kb_, vb_ = kring[i], vring[i]
                k.dma('sp', kb_.v(), V(kTa_d[h // 2].ap[ch * 256 + (h % 2) * 128:ch * 256 + (h % 2) * 128 + 128, :], kTa_d[h // 2].v().keys))
                src = va_d[h].ap[ch * 2048:(ch + 1) * 2048, 0:128].rearrange("(t p) c -> p t c", p=128)
                k.dma('sp', V(vb_.v(0, 2048, pat="p (t c) -> p t c", c=128).ap, vb_.keys(0, 2048)), V(src, va_d[h].v().keys))
                bufs[ch] = (kb_, vb_)

            load(0)
            load(1)
            qk(0, bufs[0][0])
            qk(1, bufs[0][0])
            for kt in range(64):
                ch = kt // 16
                if kt % 16 == 0 and ch + 2 < 4:
                    load(ch + 2)
                if kt + 2 < 64:
                    qk(kt + 2, bufs[(kt + 2) // 16][0])
                pt = PT4[kt % 4]
                k.act(pt.v(), k.ps[SB[kt % 3]].v(), AF.Exp, scale=0.125)
                vb_ = bufs[ch][1]
                for m in range(2):
                    k.mm(OT.v(m * 512, m * 512 + 512), vb_.v((kt % 16) * 128, (kt % 16) * 128 + 128),
                         pt.v(m * 512, m * 512 + 512), start=(kt == 0), stop=(kt == 63))
                if kt % 4 == 1:
                    k.tt('dve', tsum.v(), PT4[(kt - 1) % 4].v(), pt.v(), ALU.add)
                elif kt % 4 == 3:
                    k.tt('dve', tsum.v(), tsum.v(), PT4[(kt - 1) % 4].v(), ALU.add)
                    k.tt('dve', tsum.v(), tsum.v(), pt.v(), ALU.add)
                    if kt == 3:
                        k.cp('dve', acc.v(), tsum.v())
                    else:
                        k.tt('dve', acc.v(), acc.v(), tsum.v(), ALU.add)
            k.cp('dve', tsum.v(), acc.v())
            for m in range(2):
                psd = k.ps[0].sub(m * 512, 512)
                k.mm(psd.v(), c['ones_b'].v(), tsum.v(m * 512, m * 512 + 512))
            k.recip(rsb.v(), k.ps[0].v(0, 512))
            k.recip(r2.v(), k.ps[0].v(512, 1024))
            k.tt('dve', dsb.v(), OT.v(0, 512), rsb.v(), ALU.mult)
            k.tt('dve', r2.v(), OT.v(512, 1024), r2.v(), ALU.mult)
            k.stt('dve', dsb.v(), r2.v(), lamcol.v(1, 2), dsb.v(), ALU.mult, ALU.add)
            sqA = PT4[0].sub(0, 512)
            k.act(sqA.v(), dsb.v(), AF.Square)
            pss_ = k.ps[1].sub(0, 512)
            k.mm(pss_.v(), c['ones_b'].v(), sqA.v())
            k.act(rsb.v(), pss_.v(), AF.Sqrt, scale=1.0 / 128, bias=EPS)
            k.recip(rsb.v(), rsb.v())
            k.stt('dve', oaT.v(h * T + qb * 512, h * T + qb * 512 + 512), dsb.v(), slcol.v(), rsb.v(), ALU.mult, ALU.mult)

    k.dma('sp', qTb.v(), qTb_d.v())
    kbb = [AB.sub(b0 + i * 4096, 4096) for i in range(2)]
    vtr = [AB.sub(b0 + 32768 + i * 260, 260) for i in range(8)]
    numT = fwork.sub(512, 2 * T)
    denT = fwork.sub(512 + 2 * T, T)
    Osb = fwork.sub(512 + 3 * T, 260)
    vld = [0]
    vtile_no = [0]
    U32 = mybir.dt.uint32
    idx_t = k.idx_t
    kidx = Buf("idx_t", idx_t, 4, 0, 24)
    vidx = Buf("idx_t", idx_t, 4, 24, 69)
    vmask = fwork.sub(7200, 69)
    k.dma('sp', vmask.v(), D['vmask'].v())
    kTb_view = [d_.ap.rearrange("r (h c) -> (r h) c", h=2) for d_ in kTb_d]
    vstg = [AB.sub(b0 + 32768 + 8 * 260 + i * 256, 256) for i in range(4)]

    def igather(outv, src_ap, src_d, idxv):
        k.S.add('pool', lambda e: e.indirect_dma_start(out=outv.ap, out_offset=None, in_=src_ap,
                                                       in_offset=bass.IndirectOffsetOnAxis(ap=idxv.ap.bitcast(U32), axis=0)),
                reads=src_d.v().keys + idxv.keys, writes=outv.keys, dma=True)
    for g, dil in enumerate([1, 4, 16]):
        for j in range(2):
            for seg in range(4):
                col = (g * 2 + j) * 4 + seg
                igather(kbb[j].v(seg * 1024, seg * 1024 + 1024), kTb_view[g], kTb_d[g], kidx.v(col, col + 1))
        ntile = 16 // dil
        for r in range(dil):
            vt = {}

            def vload(n):
                i = vld[0] % 8
                vld[0] += 1
                tno = vtile_no[0]
                vtile_no[0] += 1
                vs_ = vstg[tno % 4]
                igather(vs_.v(), vb_d[g].ap, vb_d[g], vidx.v(tno, tno + 1))
                k.ts('dve', V(vtr[i].v(0, 260, pat="p (h c) -> p h c", c=65).ap[:, :, 0:64], vtr[i].keys(0, 260)),
                     V(vs_.v(0, 256, pat="p (h c) -> p h c", c=64).ap, vs_.keys(0, 256)), vmask.v(tno, tno + 1), None, ALU.mult)
                k.ts('dve', V(vtr[i].v(0, 260, pat="p (h c) -> p h c", c=65).ap[:, :, 64], vtr[i].keys(0, 260)),
                     c['ones_b'].v(0, 4), vmask.v(tno, tno + 1), None, ALU.mult)
                vt[n] = vtr[i]

            vload(0)
            for m in range(ntile):
                vload(m + 1)
                pss = k.ps[m % 2]
                i0 = 128 * m * dil + r
                for hh in range(4):
                    j, half = hh // 2, hh % 2
                    for ab in range(2):
                        n = m + ab
                        w0 = PADW + (128 * n - 64) * dil + r
                        col = (hh * 2 + ab) * 128
                        k.mm(pss.v(col, col + 128), c['ident_b'].v(), c['maskA' if ab == 0 else 'maskB'].v(), start=True, stop=False)
                        k.mm(pss.v(col, col + 128),
                             kbb[j].v(w0, w0 + 128 * dil - (dil - 1), p0=half * 64, p1=half * 64 + 64, step=dil),
                             qTb.v((g * 2 + j) * T + i0, (g * 2 + j) * T + i0 + 128 * dil - (dil - 1), p0=half * 64, p1=half * 64 + 64, step=dil),
                             start=False, stop=True)
                pt = PT[m % 2]
                k.act(pt.v(), pss.v(), AF.Exp, scale=0.125)
                Ops = k.ps[2 + (m % 2)].sub(0, 260)
                for hh in range(4):
                    for ab in range(2):
                        col = (hh * 2 + ab) * 128
                        k.mm(Ops.v(hh * 65, hh * 65 + 65), pt.v(col, col + 128), vt[m + ab].v(hh * 65, hh * 65 + 65),
                             start=(ab == 0), stop=(ab == 1))
                k.cp('dve', V(Osb.v(0, 256, pat="p (h c) -> p h c", c=64).ap, Osb.keys(0, 256)),
                     V(Ops.v(0, 260, pat="p (h c) -> p h c", c=65).ap[:, :, 0:64], Ops.keys(0, 260)))
                k.cp('dve', Osb.v(256, 260), V(Ops.v(0, 260, pat="p (h c) -> p h c", c=65).ap[:, :, 64], Ops.keys(0, 260)))
                pT_ = k.ps[2 + (m % 2)]
                for j in range(2):
                    k.tr(pT_.v(512 + j * 128, 512 + j * 128 + 128), Osb.v(j * 128, j * 128 + 128), c['ident_f'].v())
                k.tr(pT_.v(768, 896, p0=0, p1=4), Osb.v(256, 260), c['ident_f'].v())
                for j in range(2):
                    dst = numT.v(j * T + i0, j * T + i0 + 128 * dil - (dil - 1), step=dil)
                    if g == 0:
                        k.cp('dve', dst, pT_.v(512 + j * 128, 512 + j * 128 + 128))
                    else:
                        k.tt('dve', dst, pT_.v(512 + j * 128, 512 + j * 128 + 128), dst, ALU.add)
                dst = denT.v(i0, i0 + 128 * dil - (dil - 1), p0=0, p1=4, step=dil)
                if g == 0:
                    k.cp('dve', dst, pT_.v(768, 896, p0=0, p1=4))
                else:
                    k.tt('dve', dst, pT_.v(768, 896, p0=0, p1=4), dst, ALU.add)
    ehd_t = fwork.sub(512 + 3 * T + 260, 256)
    k.dma('sp', ehd_t.v(p0=0, p1=4), ehd_d.v())
    k.recip(denT.v(p0=0, p1=4), denT.v(p0=0, p1=4))
    for j in range(2):
        for tb in range(4):
            psb = k.ps[tb % 2].sub(0, 512)
            k.mm(psb.v(), ehd_t.v(j * 128, j * 128 + 128, p0=0, p1=4), denT.v(tb * 512, tb * 512 + 512, p0=0, p1=4))
            k.tt('dve', obT.v(j * T + tb * 512, j * T + tb * 512 + 512), numT.v(j * T + tb * 512, j * T + tb * 512 + 512), psb.v(), ALU.mult)

    wo = AB.sub(b0 + 20480, 8192)
    wpa = AB.sub(b0 + 28672, 4096)
    gat = AB.sub(b0 + 32768, 8192)
    mrg = AB.sub(b0 + 40960, 4096)
    wpb = AB.sub(b0 + 45056, 2048)
    k.dma('pool', wo.v(), wo_d.v())
    k.dma('pool', wpa.v(), wpa_d.v())
    k.dma('pool', wpb.v(), wpb_d.v())
    m1 = fwork.sub(0, 512)
    for tb in range(4):
        for gi_ in range(2):
            src = gates_d.ap[:, gi_ * 8 * T: (gi_ + 1) * 8 * T].rearrange("p (c t) -> p c t", t=T)[:, :, tb * 512:(tb + 1) * 512]
            k.dma('sp', V(gat.v(gi_ * 4096, gi_ * 4096 + 4096, pat="p (c t) -> p c t", t=512).ap, gat.keys(gi_ * 4096, gi_ * 4096 + 4096)),
                  V(src, gates_d.v().keys))
        for cc in range(8):
            psa = k.ps[cc % 2].sub(0, 512)
            psb = k.ps[cc % 2].sub(512, 512)
            for kc in range(4):
                k.mm(psa.v(), wpa.v(kc * 1024 + cc * 128, kc * 1024 + cc * 128 + 128),
                     oaT.v(kc * T + tb * 512, kc * T + tb * 512 + 512), start=(kc == 0), stop=(kc == 3))
            for kc in range(2):
                k.mm(psb.v(), wpb.v(kc * 1024 + cc * 128, kc * 1024 + cc * 128 + 128),
                     obT.v(kc * T + tb * 512, kc * T + tb * 512 + 512), start=(kc == 0), stop=(kc == 1))
            k.tt('dve', m1.v(), psa.v(), gat.v(cc * 512, cc * 512 + 512), ALU.mult)
            k.tt('dve', mrg.v(cc * 512, cc * 512 + 512), psb.v(), gat.v(4096 + cc * 512, 4096 + cc * 512 + 512), ALU.mult)
            k.tt('pool', mrg.v(cc * 512, cc * 512 + 512), mrg.v(cc * 512, cc * 512 + 512), m1.v(), ALU.add)
        for cc in range(8):
            psy = k.ps[2 + cc % 2].sub(0, 512)
            for kc in range(8):
                k.mm(psy.v(), wo.v(kc * 1024 + cc * 128, kc * 1024 + cc * 128 + 128),
                     mrg.v(kc * 512, kc * 512 + 512), start=(kc == 0), stop=(kc == 7))
            xs = xT.v(cc * T + tb * 512, cc * T + tb * 512 + 512)
            k.stt('dve', xs, psy.v(), mod.v(16 + cc, 17 + cc), xs, ALU.mult, ALU.add)

    hT = AB.sub(b0, 8 * T)
    wring = [AB.sub(b0 + 16384 + i * 6144, 6144) for i in range(4)]
    hid = [AB.sub(b0 + 40960 + i * 4096, 4096) for i in range(2)]
    wb2 = AB.sub(b0 + 49152, 1024)
    h32b = fwork.sub(2048, 4096)
    lg = fwork.sub(6144, 36 * 16)
    comb = fwork.sub(6144 + 576, 32 * 16)
    rw = fwork.sub(6144 + 576 + 512, 96)

    def hook(tb, cc):
        if cc is not None:
            return h32b.sub(cc * 512, 512)
        for q in range(4):
            tt_ = tb * 4 + q
            psl = k.ps[2 + q % 2].sub(0, 36)
            for kc in range(8):
                k.mm(psl.v(), h32b.v(kc * 512 + q * 128, kc * 512 + q * 128 + 128), wr.v(kc * 36, kc * 36 + 36),
                     start=(kc == 0), stop=(kc == 7))
            k.tt('dve', lg.v(tt_ * 36, tt_ * 36 + 36), psl.v(), br.v(), ALU.add)
        return None

    rms_mod(k, xT, hT, c, sc2, mod.sub(24, 8), fwork.sub(0, 2048), wb2, h32_hook=hook)

    for tt_ in range(16):
        l1 = lg.v(tt_ * 36, tt_ * 36 + 4)
        l2 = lg.sub(tt_ * 36 + 4, 32)
        r = rw
        m1c = r.v(0, 1); nm1 = r.v(1, 2); e1 = r.v(2, 6); s1 = r.v(6, 7); gval = r.v(7, 8)
        oh = r.sub(8, 4); l2m = r.sub(12, 32); ig = r.sub(44, 8); v1 = r.v(52, 53); mk1 = r.sub(53, 8)
        ig2 = r.sub(61, 8); v2 = r.v(69, 70); mk2 = r.sub(70, 8); dd = r.v(78, 79); w1 = r.v(79, 80); w2 = r.v(80, 81)
        cig = r.sub(81, 8)
        k.red('dve', m1c, l1, ALU.max)
        k.ts('dve', nm1, m1c, -1.0, None, ALU.mult)
        k.act(e1, l1, AF.Exp, bias=nm1, accum=s1)
        k.recip(gval, s1)
        k.ts('dve', oh.v(), l1, m1c, None, ALU.is_equal)
        k.tt('dve', V(l2m.v(0, 32, pat="p (g e) -> p g e", e=8).ap, l2m.keys(0, 32)),
             V(l2.v(0, 32, pat="p (g e) -> p g e", e=8).ap, l2.keys(0, 32)),
             V(oh.v(0, 4).ap.unsqueeze(2).to_broadcast([128, 4, 8]), oh.keys(0, 4)), ALU.mult)
        k.red('dve', ig.v(), V(l2m.v(0, 32, pat="p (g e) -> p e g", e=8).ap, l2m.keys(0, 32)), ALU.add)
        k.red('dve', v1, ig.v(), ALU.max)
        k.ts('dve', mk1.v(), ig.v(), v1, None, ALU.is_equal)
        k.stt('dve', ig2.v(), mk1.v(), -1e30, ig.v(), ALU.mult, ALU.add)
        k.red('dve', v2, ig2.v(), ALU.max)
        k.ts('dve', mk2.v(), ig2.v(), v2, None, ALU.is_equal)
        k.tt('dve', dd, v1, v2, ALU.subtract)
        k.act(w1, dd, AF.Sigmoid)
        k.act(w2, dd, AF.Sigmoid, scale=-1.0)
        k.tt('dve', w1, w1, gval, ALU.mult)
        k.tt('dve', w2, w2, gval, ALU.mult)
        k.ts('dve', cig.v(), mk1.v(), w1, None, ALU.mult)
        k.stt('dve', cig.v(), mk2.v(), w2, cig.v(), ALU.mult, ALU.add)
        k.tt('dve', V(comb.v(tt_ * 32, tt_ * 32 + 32, pat="p (g e) -> p g e", e=8).ap, comb.keys(tt_ * 32, tt_ * 32 + 32)),
             V(oh.v(0, 4).ap.unsqueeze(2).to_broadcast([128, 4, 8]), oh.keys(0, 4)),
             V(cig.v(0, 8).ap.unsqueeze(1).to_broadcast([128, 4, 8]), cig.keys(0, 8)), ALU.mult)
    CTt = fwork.sub(0, 2048)
    for tt_ in range(16):
        pst = k.ps[2 + tt_ % 2].sub(512, 128)
        k.tr(pst.v(p0=0, p1=32), comb.v(tt_ * 32, tt_ * 32 + 32), c['ident_f'].v())
        k.cp('dve', CTt.v(tt_ * 128, tt_ * 128 + 128, p0=0, p1=32), pst.v(p0=0, p1=32))
    selt = fwork.sub(2048, 4096)
    k.dma('sp', selt.v(p0=0, p1=32), sel_d.v())

    G = 2
    sg = [fwork.sub(6144 + i * 512, 512) for i in range(2)]

    def wload(e):
        k.dma('pool', wring[e % 4].v(), V(we_d.ap[e], we_d.v(key=e).keys))

    for e in range(4):
        wload(e)
    hcnt = [0]
    for eg in range(NEXP // G):
        if eg >= 1 and (eg + 1) * G < NEXP:
            for ei in range(G):
                wload((eg + 1) * G + ei)
        for tb in range(4):
            hb = hid[hcnt[0] % 2]
            hcnt[0] += 1
            for ei in range(G):
                e = eg * G + ei
                w = wring[e % 4]
                psc = k.ps[3].sub(512, 512)
                k.mm(psc.v(), selt.v(e * 128, e * 128 + 128, p0=0, p1=32), CTt.v(tb * 512, tb * 512 + 512, p0=0, p1=32))
                for ch in range(2):
                    psg = k.ps[ch].sub(0, 512)
                    psu = k.ps[ch].sub(512, 512)
                    for kc in range(8):
                        k.mm(psg.v(), w.v(kc * 256 + ch * 128, kc * 256 + ch * 128 + 128),
                             hT.v(kc * T + tb * 512, kc * T + tb * 512 + 512), start=(kc == 0), stop=(kc == 7))
                    for kc in range(8):
                        k.mm(psu.v(), w.v(2048 + kc * 256 + ch * 128, 2048 + kc * 256 + ch * 128 + 128),
                             hT.v(kc * T + tb * 512, kc * T + tb * 512 + 512), start=(kc == 0), stop=(kc == 7))
                    s_ = sg[ch]
                    k.act(s_.v(), psg.v(), AF.Silu)
                    k.tt('dve', s_.v(), psu.v(), s_.v(), ALU.mult)
                    k.tt('dve', hb.v((ei * 2 + ch) * 512, (ei * 2 + ch) * 512 + 512), psc.v(), s_.v(), ALU.mult)
            for cc in range(8):
                psy = k.ps[2].sub((cc % 2) * 512, 512)
                for ei in range(G):
                    w = wring[(eg * G + ei) % 4]
                    for ch in range(2):
                        k.mm(psy.v(), w.v(4096 + ch * 1024 + cc * 128, 4096 + ch * 1024 + cc * 128 + 128),
                             hb.v((ei * 2 + ch) * 512, (ei * 2 + ch) * 512 + 512),
                             start=(ei == 0 and ch == 0), stop=(ei == G - 1 and ch == 1))
                xs = xT.v(cc * T + tb * 512, cc * T + tb * 512 + 512)
                k.stt('dve', xs, psy.v(), mod.v(40 + cc, 41 + cc), xs, ALU.mult, ALU.add)
    return


def build_F():
    k = K(nf32=25600, nbf16=51200)
    nc = k.nc
    D = {}

    def ext(name, shape, dt):
        D[name] = k.din(name, shape, dt)

    def itn(name, shape, dt):
        D[name] = DBuf(name, nc.dram_tensor(name, list(shape), dt).ap())

    ext('xT', [128, 8 * T], F32); ext('c_col', [128, 8], F32); ext('posb', [128, T], I32); ext('invf', [128, 1], F32)
    ext('sel', [32, 32 * 128], F32); ext('ehd', [4, 256], F32); ext('idx', [128, 93], I32); ext('vmask', [128, 69], F32)
    for l in range(2):
        ext('w_ada%d' % l, [6, 128, 8 * 1024], F32); ext('b_ada%d' % l, [128, 48], F32); ext('gains%d' % l, [128, 4], F32)
        for n, nc_ in WIN_GROUPS:
            ext('w_%s%d' % (n, l), [128, 8 * nc_], F32)
        ext('lamp%d' % l, [128, 258], F32); ext('subln%d' % l, [128, 128], F32)
        ext('w_pa%d' % l, [128, 4096], F32); ext('w_pb%d' % l, [128, 2048], F32); ext('w_o%d' % l, [128, 8192], F32)
        ext('w_r%d' % l, [128, 288], F32); ext('b_r%d' % l, [128, 36], F32); ext('w_e%d' % l, [NEXP, 128, 6144], F32)
    def pieces(nm, n, ls, as_):
        D[nm + '_loc'] = [DBuf('%s_loc%d' % (nm, i), nc.dram_tensor('%s_loc%d' % (nm, i), ls, BF16).ap()) for i in range(n)]
        D[nm + '_all'] = [DBuf('%s_all%d' % (nm, i), nc.dram_tensor('%s_all%d' % (nm, i), as_, BF16).ap()) for i in range(n)]
    pieces('kTa', 2, [256, 2048], [1024, 2048])
    pieces('va', 4, [T, 129], [SEQ, 129])
    pieces('kTb', 3, [256, 2048], [1024, 2048])
    pieces('vb', 3, [T, 256], [SEQ, 256])
    itn('qTa_s', [128, 4 * T], BF16); itn('qTb_s', [128, 6 * T], BF16)
    itn('gates_s', [128, 16 * T], BF16); itn('mod_s', [128, 48], F32)
    out_d = k.dout('outT', [128, 8 * T], F32)
    c = load_consts(k, True)
    xT = k.fa(8 * T)
    k.idx_t = k.es.enter_context(nc.sbuf_tensor("idx_t", [128, 93], I32))
    idxb = Buf("idx_t", k.idx_t, 4, 0, 93)
    k.dma('sp', idxb.v(), D['idx'].v())
    k.dma('sp', xT.v(), D['xT'].v())
    base = (k.fo, k.bo)
    rg = [[0, 1, 2, 3], [4, 5, 6, 7]]
    for l in range(2):
        k.fo, k.bo = base
        def after_kv():
            for nm, i_ in [('kTa', 0), ('va', 0), ('va', 1), ('kTa', 1), ('va', 2), ('va', 3),
                           ('kTb', 0), ('kTb', 1), ('kTb', 2), ('vb', 0), ('vb', 1), ('vb', 2)]:
                loc, al = D[nm + '_loc'][i_], D[nm + '_all'][i_]
                k.S.add('pool', lambda e, loc=loc, al=al: e.collective_compute(
                    "AllGather", ALU.bypass, replica_groups=rg, ins=[loc.ap.opt()], outs=[al.ap.opt()]),
                    reads=loc.v().keys, writes=al.v().keys, dma=True, cc=True)
        k.after_kv = after_kv
        body_A(k, c, xT, D, l)
        after_kv()
        k.fo, k.bo = base
        body_B(k, c, xT, D, l)
    k.dma('sp', out_d.v(), xT.v())
    return k.finalize()


_cache = {}


def _fm(w):
    K_, N = w.shape
    return np.ascontiguousarray(w.reshape(K_ // 128, 128, N).transpose(1, 0, 2).reshape(128, (K_ // 128) * N))


def _index_tables(jr):
    p = np.arange(128)
    idx = np.zeros((128, 93), np.int64)
    vmask = np.zeros((128, 69), np.float32)
    for jp in range(6):
        for seg in range(4):
            rank = [jr - 1, jr, jr, jr + 1][seg]
            half = [1, 0, 1, 0][seg]
            rank = min(max(rank, 0), 3)
            idx[:, jp * 4 + seg] = rank * 512 + ((jp % 2) * 128 + p) * 2 + half
    t = 0
    for g, dil in enumerate([1, 4, 16]):
        for r in range(dil):
            for n in range(16 // dil + 1):
                a_k = 128 * n - 64 + p
                gpos = jr * T + a_k * dil + r
                valid = (gpos >= 0) & (gpos < SEQ)
                gp = np.where(valid, gpos, 0)
                idx[:, 24 + t] = gp
                vmask[:, t] = valid.astype(np.float32)
                t += 1
    assert t == 69
    return idx.astype(np.int32), vmask


def kernel(x, c, positions, w_ada, b_ada, w_in, qn_a, kn_a, lam_q1, lam_k1, lam_q2, lam_k2,
           subln_a, qn_b, kn_b, w_pa, w_pb, w_o, w_r1, b_r1, w_r2, b_r2, w_e_gate, w_e_up, w_e_down):
    x = np.asarray(x, np.float32)
    consts = host_consts()
    invf = (np.float32(10000.0) ** (-np.arange(0, 64, 2, dtype=np.float32) / np.float32(64))).astype(np.float32)
    invf_col = invf[(np.arange(128) % 64) % 32].reshape(128, 1).astype(np.float32)
    cores = list(range(NCORES))
    sel = np.zeros((32, 32 * 128), np.float32)
    for e in range(32):
        sel[e, e * 128:(e + 1) * 128] = 1.0
    ehd = np.zeros((4, 256), np.float32)
    for h in range(4):
        j, half = h // 2, h % 2
        ehd[h, j * 128 + half * 64: j * 128 + half * 64 + 64] = 1.0
    cuts = np.cumsum([0, 512, 512, 512, 768, 768, 768, 1024, 1024])
    names = ['qa', 'ka', 'va', 'qb', 'kb', 'vb', 'ga', 'gb']
    shared = {"consts": consts, "invf": invf_col, "sel": sel, "ehd": ehd}
    pidx = np.arange(128) % 64
    for l in range(2):
        wl = np.asarray(w_in[l], np.float32)
        for i, n in enumerate(names):
            shared["w_%s%d" % (n, l)] = _fm(wl[:, cuts[i]:cuts[i + 1]])
        shared["w_ada%d" % l] = np.ascontiguousarray(
            np.asarray(w_ada[l], np.float32).reshape(8, 128, 6, 1024).transpose(2, 1, 0, 3).reshape(6, 128, 8 * 1024))
        shared["b_ada%d" % l] = np.ascontiguousarray(np.asarray(b_ada[l], np.float32).reshape(48, 128).T)
        shared["gains%d" % l] = np.stack([np.asarray(qn_a[l])[pidx], np.asarray(kn_a[l])[pidx],
                                          np.asarray(qn_b[l])[pidx], np.asarray(kn_b[l])[pidx]], axis=1).astype(np.float32)
        lam_init = 0.8 - 0.6 * math.exp(-0.3 * l)
        lamp = np.concatenate([np.asarray(lam_q1[l]), np.asarray(lam_k1[l]), np.asarray(lam_q2[l]), np.asarray(lam_k2[l]),
                               np.array([lam_init, 1.0 - lam_init])]).astype(np.float32)
        shared["lamp%d" % l] = np.ascontiguousarray(np.broadcast_to(lamp[None, :], (128, 258)))
        shared["subln%d" % l] = np.ascontiguousarray(np.broadcast_to(np.asarray(subln_a[l], np.float32)[:, None], (128, 128)))
        shared["w_r%d" % l] = _fm(np.concatenate([np.asarray(w_r1[l], np.float32), np.asarray(w_r2[l], np.float32)], axis=1))
        shared["b_r%d" % l] = np.ascontiguousarray(np.broadcast_to(
            np.concatenate([np.asarray(b_r1[l]), np.asarray(b_r2[l])]).astype(np.float32)[None, :], (128, 36)))
        we = np.empty((NEXP, 128, 6144), np.float32)
        for e in range(NEXP):
            we[e, :, 0:2048] = _fm(np.asarray(w_e_gate[l, e], np.float32))
            we[e, :, 2048:4096] = _fm(np.asarray(w_e_up[l, e], np.float32))
            we[e, :, 4096:6144] = _fm(np.asarray(w_e_down[l, e], np.float32))
        shared["w_e%d" % l] = we
        shared["w_pa%d" % l] = _fm(np.asarray(w_pa[l], np.float32))
        shared["w_pb%d" % l] = _fm(np.asarray(w_pb[l], np.float32))
        shared["w_o%d" % l] = _fm(np.asarray(w_o[l], np.float32))
    in_maps = []
    for ci in cores:
        b, j = ci // 4, ci % 4
        xs = x[b, j * T:(j + 1) * T, :]
        idx, vmask = _index_tables(j)
        m = dict(shared)
        m["xT"] = np.ascontiguousarray(xs.T.reshape(8, 128, T).transpose(1, 0, 2).reshape(128, 8 * T))
        m["c_col"] = np.ascontiguousarray(np.asarray(c[b], np.float32).reshape(8, 128).T)
        m["posb"] = np.ascontiguousarray(np.broadcast_to(np.asarray(positions[b, j * T:(j + 1) * T], np.int32)[None, :], (128, T)))
        m["idx"] = idx
        m["vmask"] = vmask
        in_maps.append(m)
    if 'F' not in _cache:
        _cache['F'] = build_F()
    res = run_bass_kernel_spmd(_cache['F'], in_maps, core_ids=cores).results
    out = np.empty((2, SEQ, D), np.float32)
    for ci in cores:
        b, j = ci // 4, ci % 4
        o = np.asarray(res[ci]["outT"], np.float32)
        out[b, j * T:(j + 1) * T, :] = o.reshape(128, 8, T).transpose(1, 0, 2).reshape(D, T).T
    return out
```

```python
import math
import numpy as np
from contextlib import ExitStack
import concourse.bass as bass
import concourse.mybir as mybir
from concourse.bass_utils import run_bass_kernel_spmd

F32 = mybir.dt.float32
BF16 = mybir.dt.bfloat16
I32 = mybir.dt.int32
ALU = mybir.AluOpType
AF = mybir.ActivationFunctionType
AX = mybir.AxisListType

ENGS = ['pe', 'act', 'dve', 'pool', 'sp']
CHUNK = 1024

NCORES = 8
T = 2048
SEQ = 8192
D = 1024
EPS = 1e-6
NEG = -30000.0
WIN = 4096
PADW = 1024
NEXP = 32


class Op:
    __slots__ = ('eng', 'fn', 'src', 'pos', 'signal', 'waits', 'know', 'is_dma', 'cnt')


class V:
    __slots__ = ('ap', 'keys')

    def __init__(self, ap, keys):
        self.ap = ap
        self.keys = keys


class Buf:
    def __init__(self, arena_name, handle, esz, off, n):
        self.an = arena_name
        self.t = handle
        self.esz = esz
        self.off = off
        self.n = n

    def keys(self, a, b):
        ch = 2048 if self.an.startswith('ps') else CHUNK
        lo = ((self.off + a) * self.esz) // ch
        hi = ((self.off + b) * self.esz - 1) // ch
        return [(self.an, i) for i in range(lo, hi + 1)]

    def v(self, a=0, b=None, p0=0, p1=128, step=1, pat=None, **kw):
        if b is None:
            b = self.n
        assert 0 <= a < b <= self.n, (a, b, self.n)
        if step == 1:
            ap = self.t[p0:p1, self.off + a:self.off + b]
        else:
            ap = self.t[p0:p1, self.off + a:self.off + b:step]
        if pat is not None:
            ap = ap.rearrange(pat, **kw)
        return V(ap, self.keys(a, b))

    def sub(self, off, n):
        assert off + n <= self.n
        return Buf(self.an, self.t, self.esz, self.off + off, n)


class DBuf:
    def __init__(self, name, ap):
        self.name = name
        self.ap = ap

    def v(self, ap=None, key=None):
        return V(self.ap if ap is None else ap, [(self.name, key)])


class Sched:
    def __init__(self, nc, n_dma_sems=16):
        self.nc = nc
        self.ops = {e: [] for e in ENGS}
        self.ncomp = {e: 0 for e in ENGS}
        self.last_w = {}
        self.readers = {}
        self.known = {e: {} for e in ENGS}
        self.n_dma_sems = n_dma_sems
        self.dma_last = [None] * n_dma_sems
        self.dma_cnt = [0] * n_dma_sems
        self.dma_rr = 0
        self.cc_cnt = 0
        self.cc_last = None

    def _need(self, e, d):
        k = self.known[e]
        if k.get(d.src, -1) >= d.pos:
            return None
        d.signal = True
        for s, p in d.know.items():
            if k.get(s, -1) < p:
                k[s] = p
        return d

    def add(self, eng, fn, reads=(), writes=(), dma=False, cc=False):
        op = Op()
        op.eng = eng
        op.fn = fn
        op.is_dma = dma
        op.signal = dma
        deps = []
        seen = set()
        pr_ = [k for k in reads if isinstance(k[0], str) and k[0].startswith('ps')]
        if pr_:
            writes = list(writes) + pr_
        for k in reads:
            w = self.last_w.get(k)
            if w is not None and id(w) not in seen:
                seen.add(id(w)); deps.append(w)
        for k in writes:
            w = self.last_w.get(k)
            if w is not None and id(w) not in seen:
                seen.add(id(w)); deps.append(w)
            for r in self.readers.get(k, ()):
                if id(r) not in seen:
                    seen.add(id(r)); deps.append(r)
        if cc:
            op.src = ('cc', 0)
            op.pos = self.cc_cnt
            self.cc_cnt += 1
            if self.cc_last is not None and id(self.cc_last) not in seen:
                seen.add(id(self.cc_last)); deps.append(self.cc_last)
            self.cc_last = op
        elif dma:
            slot = self.dma_rr
            self.dma_rr = (self.dma_rr + 1) % self.n_dma_sems
            prev = self.dma_last[slot]
            if prev is not None and id(prev) not in seen:
                seen.add(id(prev)); deps.append(prev)
            op.src = ('dma', slot)
            op.pos = self.dma_cnt[slot]
            self.dma_cnt[slot] += 1
            self.dma_last[slot] = op
        else:
            op.src = eng
            op.pos = self.ncomp[eng]
            self.ncomp[eng] += 1
        waits = []
        deps.sort(key=lambda d: -d.pos)
        for d in deps:
            if (not dma) and (not d.is_dma) and d.src == eng:
                if eng == 'pe':
                    continue
                if op.pos - d.pos > 2:
                    continue
            w = self._need(eng, d)
            if w is not None:
                waits.append(w)
        op.waits = waits
        know = dict(self.known[eng])
        know[op.src] = op.pos
        op.know = know
        for k in reads:
            self.readers.setdefault(k, []).append(op)
        for k in writes:
            self.last_w[k] = op
            self.readers[k] = []
        self.ops[eng].append(op)
        return op

    def finish(self):
        op = Op()
        op.eng = 'sp'; op.fn = None; op.is_dma = False; op.signal = False
        op.src = 'sp'; op.pos = self.ncomp['sp']; self.ncomp['sp'] += 1
        waits = []
        for d in list(self.dma_last) + [self.cc_last]:
            if d is not None:
                w = self._need('sp', d)
                if w is not None:
                    waits.append(w)
        for e in ['pe', 'act', 'dve', 'pool']:
            comp = [o for o in self.ops[e] if not o.is_dma]
            if comp:
                w = self._need('sp', comp[-1])
                if w is not None:
                    waits.append(w)
        op.waits = waits
        op.know = {}
        self.ops['sp'].append(op)

    def emit(self, sems):
        nc = self.nc
        for e in ENGS:
            c = 0
            for o in self.ops[e]:
                if o.is_dma:
                    continue
                if o.signal:
                    c += 1
                o.cnt = c
        engobj = {'pe': nc.tensor, 'act': nc.scalar, 'dve': nc.vector, 'pool': nc.gpsimd, 'sp': nc.sync}

        def run(e):
            eo = engobj[e]
            for o in self.ops[e]:
                for d in o.waits:
                    if d.is_dma and d.src[0] == 'cc':
                        eo.wait_ge(sems[d.src], d.pos + 1)
                    elif d.is_dma:
                        eo.wait_ge(sems[d.src], 16 * (d.pos + 1))
                    else:
                        eo.wait_ge(sems[d.src], d.cnt)
                if o.fn is None:
                    continue
                ins = o.fn(eo)
                if o.is_dma and o.src[0] == 'cc':
                    ins.then_inc(sems[o.src])
                elif o.is_dma:
                    ins.then_inc(sems[o.src], 16)
                elif o.signal:
                    ins.then_inc(sems[e], 1)
        return run


class K:
    def __init__(self, nf32, nbf16):
        self.nc = bass.Bass("TRN2", target_bir_lowering=False)
        self.es = ExitStack()
        nc = self.nc
        self.S = Sched(nc)
        es = self.es
        self.af_t = es.enter_context(nc.sbuf_tensor("arena_f", [128, nf32], F32))
        self.ab_t = es.enter_context(nc.sbuf_tensor("arena_b", [128, nbf16], BF16))
        self.AF_ = Buf("af", self.af_t, 4, 0, nf32)
        self.AB_ = Buf("ab", self.ab_t, 2, 0, nbf16)
        self.ps = []
        for i in range(4):
            t = es.enter_context(nc.psum_tensor("ps%d" % i, [128, 1024], F32))
            self.ps.append(Buf("ps%d" % i, t, 4, 0, 1024))
        self.sems = {}
        for e in ENGS:
            self.sems[e] = es.enter_context(nc.semaphore("s_" + e))
        for i in range(self.S.n_dma_sems):
            self.sems[('dma', i)] = es.enter_context(nc.semaphore("d%d" % i))
        self.sems[('cc', 0)] = es.enter_context(nc.semaphore("ccsem"))
        self.fo = 0
        self.bo = 0
        self.dram = {}

    def din(self, name, shape, dt):
        ap = self.nc.dram_tensor(name, list(shape), dt, kind="ExternalInput").ap()
        d = DBuf(name, ap)
        self.dram[name] = d
        return d

    def dout(self, name, shape, dt):
        ap = self.nc.dram_tensor(name, list(shape), dt, kind="ExternalOutput").ap()
        d = DBuf(name, ap)
        self.dram[name] = d
        return d

    def fa(self, n):
        b = self.AF_.sub(self.fo, n)
        self.fo += n
        return b

    def ba(self, n):
        b = self.AB_.sub(self.bo, n)
        self.bo += n
        return b

    def mm(self, out, lhsT, rhs, start=True, stop=True):
        self.S.add('pe', lambda e: e.matmul(out.ap, lhsT.ap, rhs.ap, start=start, stop=stop),
                   reads=lhsT.keys + rhs.keys, writes=out.keys)

    def tr(self, out, in_, ident):
        self.S.add('pe', lambda e: e.transpose(out.ap, in_.ap, ident.ap),
                   reads=in_.keys + ident.keys, writes=out.keys)

    def act(self, out, in_, func, scale=1.0, bias=0.0, accum=None):
        reads = list(in_.keys)
        kw = {}
        if isinstance(bias, V):
            reads += bias.keys
            kw['bias'] = bias.ap
        else:
            kw['bias'] = float(bias)
        if isinstance(scale, V):
            reads += scale.keys
            kw['scale'] = scale.ap
        else:
            kw['scale'] = float(scale)
        writes = list(out.keys)
        if accum is not None:
            writes += accum.keys
            kw['accum_out'] = accum.ap
        self.S.add('act', lambda e: e.activation(out=out.ap, in_=in_.ap, func=func, **kw),
                   reads=reads, writes=writes)

    def tt(self, eng, out, in0, in1, op):
        self.S.add(eng, lambda e: e.tensor_tensor(out=out.ap, in0=in0.ap, in1=in1.ap, op=op),
                   reads=in0.keys + in1.keys, writes=out.keys)

    def ts(self, eng, out, in0, s1, s2=None, op0=ALU.mult, op1=None):
        reads = list(in0.keys)
        a1 = s1
        a2 = s2
        if isinstance(s1, V):
            reads += s1.keys; a1 = s1.ap
        if isinstance(s2, V):
            reads += s2.keys; a2 = s2.ap
        if op1 is None:
            self.S.add(eng, lambda e: e.tensor_scalar(out=out.ap, in0=in0.ap, scalar1=a1, scalar2=None, op0=op0),
                       reads=reads, writes=out.keys)
        else:
            self.S.add(eng, lambda e: e.tensor_scalar(out=out.ap, in0=in0.ap, scalar1=a1, scalar2=a2, op0=op0, op1=op1),
                       reads=reads, writes=out.keys)

    def stt(self, eng, out, in0, scalar, in1, op0, op1):
        reads = in0.keys + in1.keys
        a = scalar
        if isinstance(scalar, V):
            reads = reads + scalar.keys; a = scalar.ap
        self.S.add(eng, lambda e: e.scalar_tensor_tensor(out=out.ap, in0=in0.ap, scalar=a, in1=in1.ap, op0=op0, op1=op1),
                   reads=reads, writes=out.keys)

    def cp(self, eng, out, in_):
        if eng == 'act':
            self.S.add('act', lambda e: e.copy(out=out.ap, in_=in_.ap), reads=in_.keys, writes=out.keys)
        else:
            self.S.add(eng, lambda e: e.tensor_copy(out=out.ap, in_=in_.ap), reads=in_.keys, writes=out.keys)

    def red(self, eng, out, in_, op, axis=AX.X):
        self.S.add(eng, lambda e: e.tensor_reduce(out=out.ap, in_=in_.ap, axis=axis, op=op),
                   reads=in_.keys, writes=out.keys)

    def recip(self, out, in_):
        self.S.add('dve', lambda e: e.reciprocal(out=out.ap, in_=in_.ap), reads=in_.keys, writes=out.keys)

    def memset(self, eng, out, val):
        self.S.add(eng, lambda e: e.memset(out.ap, val), writes=out.keys)

    def dma(self, eng, out, in_):
        self.S.add(eng, lambda e: e.dma_start(out=out.ap, in_=in_.ap), reads=in_.keys, writes=out.keys, dma=True)

    def finalize(self):
        S = self.S
        S.finish()
        run = S.emit(self.sems)
        with self.nc.Block() as block:
            @block.tensor
            def _(e): run('pe')
            @block.scalar
            def _(e): run('act')
            @block.vector
            def _(e): run('dve')
            @block.gpsimd
            def _(e): run('pool')
            @block.sync
            def _(e): run('sp')
        self.es.close()
        return self.nc


def load_consts(k, need_rope):
    c = {}
    cin = k.din("consts", [128, 5 * 128], F32)
    cf = k.fa(256).sub(0, 128)
    k.dma('sp', cf.v(), V(cin.ap[:, 0:128], cin.v().keys))
    c['ident_f'] = cf
    cb = k.ba(5 * 128)
    k.dma('pool', cb.v(), cin.v())
    c['ident_b'] = cb.sub(0, 128)
    c['onesbd'] = cb.sub(128, 128)
    c['rotT'] = cb.sub(256, 128)
    c['maskA'] = cb.sub(384, 128)
    c['maskB'] = cb.sub(512, 128)
    ones = k.ba(128)
    k.ba(256)
    k.memset('pool', ones.v(), 1.0)
    c['ones_b'] = ones
    return c


def host_consts():
    ident = np.eye(128, dtype=np.float32)
    onesbd = np.zeros((128, 128), np.float32)
    onesbd[:64, :64] = 1.0
    onesbd[64:, 64:] = 1.0
    rotT = np.zeros((128, 128), np.float32)
    for m in range(128):
        j = m % 64
        if j < 32:
            rotT[m + 32, m] = -1.0
        else:
            rotT[m - 32, m] = 1.0
    u = np.arange(128)[:, None]
    a = np.arange(128)[None, :]
    maskA = np.where(u >= a, 0.0, NEG).astype(np.float32)
    maskB = np.where(u <= a, 0.0, NEG).astype(np.float32)
    return np.concatenate([ident, onesbd, rotT, maskA, maskB], axis=1)


TWO_PI = 2.0 * math.pi
C1 = 6.28125
C2 = float(np.float32(TWO_PI - 6.28125))
C3 = float(TWO_PI - 6.28125 - float(np.float32(TWO_PI - 6.28125)))


WIN_GROUPS = [('ka', 512), ('kb', 768), ('va', 512), ('vb', 768), ('qa', 512), ('qb', 768), ('ga', 1024), ('gb', 1024)]


def rms_mod(k, xT, hT, c, scale_col, shift_col, work_f, work_b, h32_hook=None):
    sq = [work_b.sub(i * 512, 512) for i in range(2)]
    sd = work_f.sub(0, 512)
    rs = work_f.sub(512, 512)
    tmp = [work_f.sub(1024 + i * 512, 512) for i in range(2)]
    for tb in range(4):
        pss = k.ps[tb % 2].sub(0, 512)
        for cc in range(8):
            s = sq[cc % 2]
            k.act(s.v(), xT.v(cc * T + tb * 512, cc * T + tb * 512 + 512), AF.Square)
            k.mm(pss.v(), c['ones_b'].v(), s.v(), start=(cc == 0), stop=(cc == 7))
        k.act(sd.v(), pss.v(), AF.Sqrt, scale=1.0 / D, bias=EPS)
        k.recip(rs.v(), sd.v())
        for cc in range(8):
            t = tmp[cc % 2]
            k.tt('pool', t.v(), xT.v(cc * T + tb * 512, cc * T + tb * 512 + 512), rs.v(), ALU.mult)
            if h32_hook is not None:
                h32 = h32_hook(tb, cc)
                k.ts('dve', h32.v(), t.v(), scale_col.v(cc, cc + 1), shift_col.v(cc, cc + 1), ALU.mult, ALU.add)
                k.cp('pool', hT.v(cc * T + tb * 512, cc * T + tb * 512 + 512), h32.v())
            else:
                k.ts('dve', hT.v(cc * T + tb * 512, cc * T + tb * 512 + 512), t.v(),
                     scale_col.v(cc, cc + 1), shift_col.v(cc, cc + 1), ALU.mult, ALU.add)
        if h32_hook is not None:
            h32_hook(tb, None)


def body_A(k, c, xT, D, l):
    stg = 99
    ccol_d = D['c_col']
    pos_d = D['posb']
    invf_d = D['invf']
    wada_d = D['w_ada%d' % l]
    bada_d = D['b_ada%d' % l]
    gains_d = D['gains%d' % l]
    wg_d = {n: D['w_%s%d' % (n, l)] for n, nc_ in WIN_GROUPS}
    kTa_o = D['kTa_loc']
    kTb_o = D['kTb_loc']
    qTa_o = D['qTa_s']
    qTb_o = D['qTb_s']
    va_o = D['va_loc']
    vb_o = D['vb_loc']
    gates_o = D['gates_s']
    mod_o = D['mod_s']

    cosT = k.fa(T)
    sinT = k.fa(T)
    small = k.fa(256)
    work_f = k.fa(4096)
    hT = k.ba(8 * T)
    wring = [k.ba(8192) for _ in range(3)]
    work_b = k.ba(2048)
    stage = [k.ba(2048) for _ in range(2)]
    vst = [k.ba(1024) for _ in range(2)]

    ccol = small.sub(0, 8)
    cact = k.ba(8)
    bada = small.sub(8, 48)
    mod = small.sub(56, 48)
    gains = small.sub(104, 4)
    invf = small.sub(108, 1)

    k.dma('sp', ccol.v(), ccol_d.v())
    k.dma('sp', bada.v(), bada_d.v())
    k.dma('sp', gains.v(), gains_d.v())
    k.dma('sp', invf.v(), invf_d.v())

    ang = work_f.sub(0, T)
    kk = work_f.sub(T, T)
    posi_v = V(kk.v().ap.bitcast(I32), kk.v().keys)
    k.dma('sp', posi_v, pos_d.v())
    k.cp('dve', ang.v(), posi_v)
    k.ts('dve', ang.v(), ang.v(), invf.v(), None, ALU.mult)
    k.ts('dve', kk.v(), ang.v(), 1.0 / TWO_PI, 12582912.0, ALU.mult, ALU.add)
    k.ts('dve', kk.v(), kk.v(), 12582912.0, None, ALU.subtract)
    k.stt('dve', ang.v(), kk.v(), -C1, ang.v(), ALU.mult, ALU.add)
    k.stt('dve', ang.v(), kk.v(), -C2, ang.v(), ALU.mult, ALU.add)
    k.stt('dve', ang.v(), kk.v(), -C3, ang.v(), ALU.mult, ALU.add)
    k.ts('dve', ang.v(), ang.v(), math.pi, -math.pi, ALU.min, ALU.max)
    k.act(sinT.v(), ang.v(), AF.Sin)
    k.act(kk.v(), ang.v(), AF.Sin, scale=0.5)
    k.tt('dve', kk.v(), kk.v(), kk.v(), ALU.mult)
    k.ts('dve', cosT.v(), kk.v(), -2.0, 1.0, ALU.mult, ALU.add)

    k.act(cact.v(), ccol.v(), AF.Silu)
    psm = k.ps[3].sub(0, 48)
    for s in range(6):
        wb = wring[s % 3]
        k.dma('pool', wb.v(), V(wada_d.ap[s], wada_d.v(key=s).keys))
        for cc in range(8):
            for kc in range(8):
                k.mm(psm.v(s * 8 + cc, s * 8 + cc + 1),
                     wb.v(kc * 1024 + cc * 128, kc * 1024 + cc * 128 + 128),
                     cact.v(kc, kc + 1), start=(kc == 0), stop=(kc == 7))
    k.tt('dve', mod.v(), psm.v(), bada.v(), ALU.add)
    k.dma('sp', mod_o.v(), mod.v())
    sc1 = small.sub(152, 8)
    k.ts('dve', sc1.v(), mod.v(8, 16), 1.0, None, ALU.add)

    rms_mod(k, xT, hT, c, sc1, mod.sub(0, 8), work_f, work_b)

    raw = [work_f.sub(i * 512, 512) for i in range(2)]
    rst = [work_f.sub(1024 + i * 512, 512) for i in range(2)]
    t1 = [work_f.sub(2048 + i * 512, 512) for i in range(2)]
    t2 = [work_f.sub(3072 + i * 512, 512) for i in range(2)]
    sqb = [work_b.sub(i * 512, 512) for i in range(2)]
    qnb = [work_b.sub(1024 + i * 512, 512) for i in range(2)]
    cnt = [0]

    def qk_block(psb, gcol, outv, tb):
        i = cnt[0] % 2
        cnt[0] += 1
        k.act(sqb[i].v(), psb.v(), AF.Square)
        k.ts('dve', raw[i].v(), psb.v(), gcol, None, ALU.mult)
        ps2 = k.ps[2].sub(i * 512, 512)
        k.mm(ps2.v(), c['onesbd'].v(), sqb[i].v())
        k.act(rst[i].v(), ps2.v(), AF.Ln, scale=1.0 / 64, bias=EPS)
        k.act(rst[i].v(), rst[i].v(), AF.Exp, scale=-0.5)
        k.tt('pool', qnb[i].v(), raw[i].v(), rst[i].v(), ALU.mult)
        ps3 = k.ps[3].sub(i * 512, 512)
        k.mm(ps3.v(), c['rotT'].v(), qnb[i].v())
        k.tt('dve', t1[i].v(), qnb[i].v(), cosT.v(tb * 512, tb * 512 + 512), ALU.mult)
        k.tt('dve', t2[i].v(), ps3.v(), sinT.v(tb * 512, tb * 512 + 512), ALU.mult)
        k.tt('pool', outv, t1[i].v(), t2[i].v(), ALU.add)

    pcnt = [0]
    def wload_g(gj):
        nm_, nc__ = WIN_GROUPS[gj]
        k.dma('pool', wring[gj % 3].v(0, 8 * nc__), wg_d[nm_].v())

    wload_g(0)
    wload_g(1)
    for gi, (name, ncol) in enumerate(WIN_GROUPS):
        wb = wring[gi % 3]
        if gi + 2 < len(WIN_GROUPS):
            wload_g(gi + 2)
        if name in ('ka', 'kb', 'qa', 'qb'):
            npair = ncol // 128
            gidx = {'qa': 0, 'ka': 1, 'qb': 2, 'kb': 3}[name]
            od = {'ka': kTa_o, 'kb': kTb_o, 'qa': qTa_o, 'qb': qTb_o}[name]
            for pr in range(npair):
                st = stage[pr % 2]
                for tb in range(4):
                    psb = k.ps[pcnt[0] % 2].sub(512 * ((pcnt[0] // 2) % 2), 512)
                    pcnt[0] += 1
                    for kc in range(8):
                        k.mm(psb.v(), wb.v(kc * ncol + pr * 128, kc * ncol + pr * 128 + 128),
                             hT.v(kc * T + tb * 512, kc * T + tb * 512 + 512), start=(kc == 0), stop=(kc == 7))
                    qk_block(psb, gains.v(gidx, gidx + 1), st.v(tb * 512, tb * 512 + 512), tb)
                if name in ('ka', 'kb'):
                    odp = od[pr // 2]
                    k.dma('sp', V(odp.ap[(pr % 2) * 128:(pr % 2 + 1) * 128, :], odp.v().keys), st.v())
                else:
                    k.dma('sp', V(od.ap[:, pr * T:(pr + 1) * T], od.v().keys), st.v())
        elif name in ('va', 'vb'):
            nh_, dh_, d_ = (4, 129, 128) if name == 'va' else (12, 64, 64)
            od = va_o if name == 'va' else vb_o
            w_ = nh_ * dh_
            for i in range(2):
                k.memset('pool', vst[i].v(0, w_), 1.0)
            for tt_ in range(16):
                st = vst[tt_ % 2]
                for n0 in range(0, ncol, 512):
                    nn = min(512, ncol - n0)
                    psb = k.ps[pcnt[0] % 2].sub(512 * ((pcnt[0] // 2) % 2), 512)
                    pcnt[0] += 1
                    for kc in range(8):
                        k.mm(psb.v(0, nn), hT.v(kc * T + tt_ * 128, kc * T + tt_ * 128 + 128),
                             wb.v(kc * ncol + n0, kc * ncol + n0 + nn), start=(kc == 0), stop=(kc == 7))
                    h0 = n0 // d_
                    nhh = nn // d_
                    outv = V(st.v(h0 * dh_, (h0 + nhh) * dh_, pat="p (h c) -> p h c", c=dh_).ap[:, :, 0:d_],
                             st.keys(h0 * dh_, (h0 + nhh) * dh_))
                    inv = V(psb.v(0, nn, pat="p (h c) -> p h c", c=d_).ap, psb.keys(0, nn))
                    k.cp('act', outv, inv)
                if name == 'va':
                    for h4 in range(4):
                        k.dma('sp', V(od[h4].ap[tt_ * 128:(tt_ + 1) * 128, :], od[h4].v().keys), st.v(h4 * 129, h4 * 129 + 129))
                else:
                    for g3 in range(3):
                        k.dma('sp', V(od[g3].ap[tt_ * 128:(tt_ + 1) * 128, :], od[g3].v().keys), st.v(g3 * 256, g3 * 256 + 256))
        else:
            gi_ = 0 if name == 'ga' else 1
            for cc in range(8):
                st = stage[cc % 2]
                for tb in range(4):
                    psb = k.ps[pcnt[0] % 2].sub(512 * ((pcnt[0] // 2) % 2), 512)
                    pcnt[0] += 1
                    for kc in range(8):
                        k.mm(psb.v(), wb.v(kc * ncol + cc * 128, kc * ncol + cc * 128 + 128),
                             hT.v(kc * T + tb * 512, kc * T + tb * 512 + 512), start=(kc == 0), stop=(kc == 7))
                    k.act(st.v(tb * 512, tb * 512 + 512), psb.v(), AF.Sigmoid)
                o0 = (gi_ * 8 + cc) * T
                k.dma('sp', V(gates_o.ap[:, o0:o0 + T], gates_o.v().keys), st.v())
    return


def body_B(k, c, xT, D, l):
    mod_d = D['mod_s']
    qTa_d = D['qTa_s']
    qTb_d = D['qTb_s']
    kTa_d = D['kTa_all']
    va_d = D['va_all']
    kTb_d = D['kTb_all']
    vb_d = D['vb_all']
    gates_d = D['gates_s']
    lam_d = D['lamp%d' % l]
    subln_d = D['subln%d' % l]
    wpa_d = D['w_pa%d' % l]
    wpb_d = D['w_pb%d' % l]
    wo_d = D['w_o%d' % l]
    wr_d = D['w_r%d' % l]
    br_d = D['b_r%d' % l]
    sel_d = D['sel']
    ehd_d = D['ehd']
    we_d = D['w_e%d' % l]

    small = k.fa(1024)
    fwork = k.fa(25600 - k.fo)
    mod = small.sub(0, 48)
    lamp = small.sub(48, 258)
    subln = small.sub(320, 128)
    br = small.sub(448, 36)
    sc2 = small.sub(484, 8)
    lamcol = small.sub(492, 4)
    tmp64 = small.sub(512, 128)
    wr = small.sub(640, 288)

    k.dma('sp', mod.v(), mod_d.v())
    k.dma('sp', lamp.v(), lam_d.v())
    k.dma('sp', subln.v(), subln_d.v())
    k.dma('sp', br.v(), br_d.v())
    k.dma('sp', wr.v(), wr_d.v())

    k.tt('dve', tmp64.v(0, 64), lamp.v(0, 64), lamp.v(64, 128), ALU.mult)
    k.tt('dve', tmp64.v(64, 128), lamp.v(128, 192), lamp.v(192, 256), ALU.mult)
    k.red('dve', lamcol.v(0, 2), tmp64.v(0, 128, pat="p (a w) -> p a w", w=64), ALU.add)
    k.act(lamcol.v(0, 2), lamcol.v(0, 2), AF.Exp)
    k.tt('dve', lamcol.v(3, 4), lamcol.v(0, 1), lamcol.v(1, 2), ALU.subtract)
    k.tt('dve', lamcol.v(0, 1), lamcol.v(3, 4), lamp.v(256, 257), ALU.add)
    k.ts('dve', lamcol.v(1, 2), lamcol.v(0, 1), -1.0, None, ALU.mult)
    k.ts('dve', sc2.v(), mod.v(32, 40), 1.0, None, ALU.add)

    AB = k.AB_
    b0 = k.bo
    qTa = AB.sub(b0 + 0, 8192)
    oaT = AB.sub(b0 + 8192, 8192)
    obT = AB.sub(b0 + 16384, 4096)
    qTb = AB.sub(b0 + 20480, 12288)
    kring = [AB.sub(b0 + 32768 + i * 2048, 2048) for i in range(3)]
    vring = [AB.sub(b0 + 38912 + i * 2048, 2048) for i in range(3)]
    PT = [AB.sub(b0 + 45104 + i * 1024, 1024) for i in range(2)]

    k.dma('sp', qTa.v(), qTa_d.v())
    PT4 = [AB.sub(b0 + 45056 + i * 1024, 1024) for i in range(4)]
    tsum = AB.sub(b0 + 49152, 1024)
    acc = fwork.sub(1024, 1024)
    dsb = fwork.sub(2048, 512)
    rsb = fwork.sub(2560, 512)
    r2 = fwork.sub(3072, 512)
    slcol = fwork.sub(0, 1)
    k.tt('dve', slcol.v(), subln.v(0, 1), lamp.v(257, 258), ALU.mult)
    ld = [0]
    for h in range(4):
        for qb in range(4):
            OT = k.ps[2]

            SB = [0, 1, 3]

            def qk(kt, kbuf):
                pss = k.ps[SB[kt % 3]]
                for m in range(2):
                    k.mm(pss.v(m * 512, m * 512 + 512),
                         kbuf.v((kt % 16) * 128, (kt % 16) * 128 + 128, p0=m * 64, p1=m * 64 + 64),
                         qTa.v(h * T + qb * 512, h * T + qb * 512 + 512, p0=m * 64, p1=m * 64 + 64))

            bufs = {}

            def load(ch):
                i = ld[0] % 3
                ld[0] += 1
                kb_, vb_ = kring[i], vring[i]
                k.dma('sp', kb_.v(), V(kTa_d[h // 2].ap[ch * 256 + (h % 2) * 128:ch * 256 + (h % 2) * 128 + 128, :], kTa_d[h // 2].v().keys))
                src = va_d[h].ap[ch * 2048:(ch + 1) * 2048, 0:128].rearrange("(t p) c -> p t c", p=128)
                k.dma('sp', V(vb_.v(0, 2048, pat="p (t c) -> p t c", c=128).ap, vb_.keys(0, 2048)), V(src, va_d[h].v().keys))
                bufs[ch] = (kb_, vb_)

            load(0)
            load(1)
            qk(0, bufs[0][0])
            qk(1, bufs[0][0])
            for kt in range(64):
                ch = kt // 16
                if kt % 16 == 0 and ch + 2 < 4:
                    load(ch + 2)
                if kt + 2 < 64:
                    qk(kt + 2, bufs[(kt + 2) // 16][0])
                pt = PT4[kt % 4]
                k.act(pt.v(), k.ps[SB[kt % 3]].v(), AF.Exp, scale=0.125)
                vb_ = bufs[ch][1]
                for m in range(2):
                    k.mm(OT.v(m * 512, m * 512 + 512), vb_.v((kt % 16) * 128, (kt % 16) * 128 + 128),
                         pt.v(m * 512, m * 512 + 512), start=(kt == 0), stop=(kt == 63))
                if kt % 4 == 1:
                    k.tt('dve', tsum.v(), PT4[(kt - 1) % 4].v(), pt.v(), ALU.add)
                elif kt % 4 == 3:
                    k.tt('dve', tsum.v(), tsum.v(), PT4[(kt - 1) % 4].v(), ALU.add)
                    k.tt('dve', tsum.v(), tsum.v(), pt.v(), ALU.add)
                    if kt == 3:
                        k.cp('dve', acc.v(), tsum.v())
                    else:
                        k.tt('dve', acc.v(), acc.v(), tsum.v(), ALU.add)
            k.cp('dve', tsum.v(), acc.v())
            for m in range(2):
                psd = k.ps[0].sub(m * 512, 512)
                k.mm(psd.v(), c['ones_b'].v(), tsum.v(m * 512, m * 512 + 512))
            k.recip(rsb.v(), k.ps[0].v(0, 512))
            k.recip(r2.v(), k.ps[0].v(512, 1024))
            k.tt('dve', dsb.v(), OT.v(0, 512), rsb.v(), ALU.mult)
            k.tt('dve', r2.v(), OT.v(512, 1024), r2.v(), ALU.mult)
            k.stt('dve', dsb.v(), r2.v(), lamcol.v(1, 2), dsb.v(), ALU.mult, ALU.add)
            sqA = PT4[0].sub(0, 512)
            k.act(sqA.v(), dsb.v(), AF.Square)
            pss_ = k.ps[1].sub(0, 512)
            k.mm(pss_.v(), c['ones_b'].v(), sqA.v())
            k.act(rsb.v(), pss_.v(), AF.Sqrt, scale=1.0 / 128, bias=EPS)
            k.recip(rsb.v(), rsb.v())
            k.stt('dve', oaT.v(h * T + qb * 512, h * T + qb * 512 + 512), dsb.v(), slcol.v(), rsb.v(), ALU.mult, ALU.mult)

    k.dma('sp', qTb.v(), qTb_d.v())
    kbb = [AB.sub(b0 + i * 4096, 4096) for i in range(2)]
    vtr = [AB.sub(b0 + 32768 + i * 260, 260) for i in range(8)]
    numT = fwork.sub(512, 2 * T)
    denT = fwork.sub(512 + 2 * T, T)
    Osb = fwork.sub(512 + 3 * T, 260)
    vld = [0]
    vtile_no = [0]
    U32 = mybir.dt.uint32
    idx_t = k.idx_t
    kidx = Buf("idx_t", idx_t, 4, 0, 24)
    vidx = Buf("idx_t", idx_t, 4, 24, 69)
    vmask = fwork.sub(7200, 69)
    k.dma('sp', vmask.v(), D['vmask'].v())
    kTb_view = [d_.ap.rearrange("r (h c) -> (r h) c", h=2) for d_ in kTb_d]
    vstg = [AB.sub(b0 + 32768 + 8 * 260 + i * 256, 256) for i in range(4)]

    def igather(outv, src_ap, src_d, idxv):
        k.S.add('pool', lambda e: e.indirect_dma_start(out=outv.ap, out_offset=None, in_=src_ap,
                                                       in_offset=bass.IndirectOffsetOnAxis(ap=idxv.ap.bitcast(U32), axis=0)),
                reads=src_d.v().keys + idxv.keys, writes=outv.keys, dma=True)
    for g, dil in enumerate([1, 4, 16]):
        for j in range(2):
            for seg in range(4):
                col = (g * 2 + j) * 4 + seg
                igather(kbb[j].v(seg * 1024, seg * 1024 + 1024), kTb_view[g], kTb_d[g], kidx.v(col, col + 1))
        ntile = 16 // dil
        for r in range(dil):
            vt = {}

            def vload(n):
                i = vld[0] % 8
                vld[0] += 1
                tno = vtile_no[0]
                vtile_no[0] += 1
                vs_ = vstg[tno % 4]
                igather(vs_.v(), vb_d[g].ap, vb_d[g], vidx.v(tno, tno + 1))
                k.ts('dve', V(vtr[i].v(0, 260, pat="p (h c) -> p h c", c=65).ap[:, :, 0:64], vtr[i].keys(0, 260)),
                     V(vs_.v(0, 256, pat="p (h c) -> p h c", c=64).ap, vs_.keys(0, 256)), vmask.v(tno, tno + 1), None, ALU.mult)
                k.ts('dve', V(vtr[i].v(0, 260, pat="p (h c) -> p h c", c=65).ap[:, :, 64], vtr[i].keys(0, 260)),
                     c['ones_b'].v(0, 4), vmask.v(tno, tno + 1), None, ALU.mult)
                vt[n] = vtr[i]

            vload(0)
            for m in range(ntile):
                vload(m + 1)
                pss = k.ps[m % 2]
                i0 = 128 * m * dil + r
                for hh in range(4):
                    j, half = hh // 2, hh % 2
                    for ab in range(2):
                        n = m + ab
                        w0 = PADW + (128 * n - 64) * dil + r
                        col = (hh * 2 + ab) * 128
                        k.mm(pss.v(col, col + 128), c['ident_b'].v(), c['maskA' if ab == 0 else 'maskB'].v(), start=True, stop=False)
                        k.mm(pss.v(col, col + 128),
                             kbb[j].v(w0, w0 + 128 * dil - (dil - 1), p0=half * 64, p1=half * 64 + 64, step=dil),
                             qTb.v((g * 2 + j) * T + i0, (g * 2 + j) * T + i0 + 128 * dil - (dil - 1), p0=half * 64, p1=half * 64 + 64, step=dil),
                             start=False, stop=True)
                pt = PT[m % 2]
                k.act(pt.v(), pss.v(), AF.Exp, scale=0.125)
                Ops = k.ps[2 + (m % 2)].sub(0, 260)
                for hh in range(4):
                    for ab in range(2):
                        col = (hh * 2 + ab) * 128
                        k.mm(Ops.v(hh * 65, hh * 65 + 65), pt.v(col, col + 128), vt[m + ab].v(hh * 65, hh * 65 + 65),
                             start=(ab == 0), stop=(ab == 1))
                k.cp('dve', V(Osb.v(0, 256, pat="p (h c) -> p h c", c=64).ap, Osb.keys(0, 256)),
                     V(Ops.v(0, 260, pat="p (h c) -> p h c", c=65).ap[:, :, 0:64], Ops.keys(0, 260)))
                k.cp('dve', Osb.v(256, 260), V(Ops.v(0, 260, pat="p (h c) -> p h c", c=65).ap[:, :, 64], Ops.keys(0, 260)))
                pT_ = k.ps[2 + (m % 2)]
                for j in range(2):
                    k.tr(pT_.v(512 + j * 128, 512 + j * 128 + 128), Osb.v(j * 128, j * 128 + 128), c['ident_f'].v())
                k.tr(pT_.v(768, 896, p0=0, p1=4), Osb.v(256, 260), c['ident_f'].v())
                for j in range(2):
                    dst = numT.v(j * T + i0, j * T + i0 + 128 * dil - (dil - 1), step=dil)
                    if g == 0:
                        k.cp('dve', dst, pT_.v(512 + j * 128, 512 + j * 128 + 128))
                    else:
                        k.tt('dve', dst, pT_.v(512 + j * 128, 512 + j * 128 + 128), dst, ALU.add)
                dst = denT.v(i0, i0 + 128 * dil - (dil - 1), p0=0, p1=4, step=dil)
                if g == 0:
                    k.cp('dve', dst, pT_.v(768, 896, p0=0, p1=4))
                else:
                    k.tt('dve', dst, pT_.v(768, 896, p0=0, p1=4), dst, ALU.add)
    ehd_t = fwork.sub(512 + 3 * T + 260, 256)
    k.dma('sp', ehd_t.v(p0=0, p1=4), ehd_d.v())
    k.recip(denT.v(p0=0, p1=4), denT.v(p0=0, p1=4))
    for j in range(2):
        for tb in range(4):
            psb = k.ps[tb % 2].sub(0, 512)
            k.mm(psb.v(), ehd_t.v(j * 128, j * 128 + 128, p0=0, p1=4), denT.v(tb * 512, tb * 512 + 512, p0=0, p1=4))
            k.tt('dve', obT.v(j * T + tb * 512, j * T + tb * 512 + 512), numT.v(j * T + tb * 512, j * T + tb * 512 + 512), psb.v(), ALU.mult)

    wo = AB.sub(b0 + 20480, 8192)
    wpa = AB.sub(b0 + 28672, 4096)
    gat = AB.sub(b0 + 32768, 8192)
    mrg = AB.sub(b0 + 40960, 4096)
    wpb = AB.sub(b0 + 45056, 2048)
    k.dma('pool', wo.v(), wo_d.v())
    k.dma('pool', wpa.v(), wpa_d.v())
    k.dma('pool', wpb.v(), wpb_d.v())
    m1 = fwork.sub(0, 512)
    for tb in range(4):
        for gi_ in range(2):
            src = gates_d.ap[:, gi_ * 8 * T: (gi_ + 1) * 8 * T].rearrange("p (c t) -> p c t", t=T)[:, :, tb * 512:(tb + 1) * 512]
            k.dma('sp', V(gat.v(gi_ * 4096, gi_ * 4096 + 4096, pat="p (c t) -> p c t", t=512).ap, gat.keys(gi_ * 4096, gi_ * 4096 + 4096)),
                  V(src, gates_d.v().keys))
        for cc in range(8):
            psa = k.ps[cc % 2].sub(0, 512)
            psb = k.ps[cc % 2].sub(512, 512)
            for kc in range(4):
                k.mm(psa.v(), wpa.v(kc * 1024 + cc * 128, kc * 1024 + cc * 128 + 128),
                     oaT.v(kc * T + tb * 512, kc * T + tb * 512 + 512), start=(kc == 0), stop=(kc == 3))
            for kc in range(2):
                k.mm(psb.v(), wpb.v(kc * 1024 + cc * 128, kc * 1024 + cc * 128 + 128),
                     obT.v(kc * T + tb * 512, kc * T + tb * 512 + 512), start=(kc == 0), stop=(kc == 1))
            k.tt('dve', m1.v(), psa.v(), gat.v(cc * 512, cc * 512 + 512), ALU.mult)
            k.tt('dve', mrg.v(cc * 512, cc * 512 + 512), psb.v(), gat.v(4096 + cc * 512, 4096 + cc * 512 + 512), ALU.mult)
            k.tt('pool', mrg.v(cc * 512, cc * 512 + 512), mrg.v(cc * 512, cc * 512 + 512), m1.v(), ALU.add)
        for cc in range(8):
            psy = k.ps[2 + cc % 2].sub(0, 512)
            for kc in range(8):
                k.mm(psy.v(), wo.v(kc * 1024 + cc * 128, kc * 1024 + cc * 128 + 128),
                     mrg.v(kc * 512, kc * 512 + 512), start=(kc == 0), stop=(kc == 7))
            xs = xT.v(cc * T + tb * 512, cc * T + tb * 512 + 512)
            k.stt('dve', xs, psy.v(), mod.v(16 + cc, 17 + cc), xs, ALU.mult, ALU.add)

    hT = AB.sub(b0, 8 * T)
    wring = [AB.sub(b0 + 16384 + i * 6144, 6144) for i in range(4)]
    hid = [AB.sub(b0 + 40960 + i * 4096, 4096) for i in range(2)]
    wb2 = AB.sub(b0 + 49152, 1024)
    h32b = fwork.sub(2048, 4096)
    lg = fwork.sub(6144, 36 * 16)
    comb = fwork.sub(6144 + 576, 32 * 16)
    rw = fwork.sub(6144 + 576 + 512, 96)

    def hook(tb, cc):
        if cc is not None:
            return h32b.sub(cc * 512, 512)
        for q in range(4):
            tt_ = tb * 4 + q
            psl = k.ps[2 + q % 2].sub(0, 36)
            for kc in range(8):
                k.mm(psl.v(), h32b.v(kc * 512 + q * 128, kc * 512 + q * 128 + 128), wr.v(kc * 36, kc * 36 + 36),
                     start=(kc == 0), stop=(kc == 7))
            k.tt('dve', lg.v(tt_ * 36, tt_ * 36 + 36), psl.v(), br.v(), ALU.add)
        return None

    rms_mod(k, xT, hT, c, sc2, mod.sub(24, 8), fwork.sub(0, 2048), wb2, h32_hook=hook)

    for tt_ in range(16):
        l1 = lg.v(tt_ * 36, tt_ * 36 + 4)
        l2 = lg.sub(tt_ * 36 + 4, 32)
        r = rw
        m1c = r.v(0, 1); nm1 = r.v(1, 2); e1 = r.v(2, 6); s1 = r.v(6, 7); gval = r.v(7, 8)
        oh = r.sub(8, 4); l2m = r.sub(12, 32); ig = r.sub(44, 8); v1 = r.v(52, 53); mk1 = r.sub(53, 8)
        ig2 = r.sub(61, 8); v2 = r.v(69, 70); mk2 = r.sub(70, 8); dd = r.v(78, 79); w1 = r.v(79, 80); w2 = r.v(80, 81)
        cig = r.sub(81, 8)
        k.red('dve', m1c, l1, ALU.max)
        k.ts('dve', nm1, m1c, -1.0, None, ALU.mult)
        k.act(e1, l1, AF.Exp, bias=nm1, accum=s1)
        k.recip(gval, s1)
        k.ts('dve', oh.v(), l1, m1c, None, ALU.is_equal)
        k.tt('dve', V(l2m.v(0, 32, pat="p (g e) -> p g e", e=8).ap, l2m.keys(0, 32)),
             V(l2.v(0, 32, pat="p (g e) -> p g e", e=8).ap, l2.keys(0, 32)),
             V(oh.v(0, 4).ap.unsqueeze(2).to_broadcast([128, 4, 8]), oh.keys(0, 4)), ALU.mult)
        k.red('dve', ig.v(), V(l2m.v(0, 32, pat="p (g e) -> p e g", e=8).ap, l2m.keys(0, 32)), ALU.add)
        k.red('dve', v1, ig.v(), ALU.max)
        k.ts('dve', mk1.v(), ig.v(), v1, None, ALU.is_equal)
        k.stt('dve', ig2.v(), mk1.v(), -1e30, ig.v(), ALU.mult, ALU.add)
        k.red('dve', v2, ig2.v(), ALU.max)
        k.ts('dve', mk2.v(), ig2.v(), v2, None, ALU.is_equal)
        k.tt('dve', dd, v1, v2, ALU.subtract)
        k.act(w1, dd, AF.Sigmoid)
        k.act(w2, dd, AF.Sigmoid, scale=-1.0)
        k.tt('dve', w1, w1, gval, ALU.mult)
        k.tt('dve', w2, w2, gval, ALU.mult)
        k.ts('dve', cig.v(), mk1.v(), w1, None, ALU.mult)
        k.stt('dve', cig.v(), mk2.v(), w2, cig.v(), ALU.mult, ALU.add)
        k.tt('dve', V(comb.v(tt_ * 32, tt_ * 32 + 32, pat="p (g e) -> p g e", e=8).ap, comb.keys(tt_ * 32, tt_ * 32 + 32)),
             V(oh.v(0, 4).ap.unsqueeze(2).to_broadcast([128, 4, 8]), oh.keys(0, 4)),
             V(cig.v(0, 8).ap.unsqueeze(1).to_broadcast([128, 4, 8]), cig.keys(0, 8)), ALU.mult)
    CTt = fwork.sub(0, 2048)
    for tt_ in range(16):
        pst = k.ps[2 + tt_ % 2].sub(512, 128)
        k.tr(pst.v(p0=0, p1=32), comb.v(tt_ * 32, tt_ * 32 + 32), c['ident_f'].v())
        k.cp('dve', CTt.v(tt_ * 128, tt_ * 128 + 128, p0=0, p1=32), pst.v(p0=0, p1=32))
    selt = fwork.sub(2048, 4096)
    k.dma('sp', selt.v(p0=0, p1=32), sel_d.v())

    G = 2
    sg = [fwork.sub(6144 + i * 512, 512) for i in range(2)]

    def wload(e):
        k.dma('pool', wring[e % 4].v(), V(we_d.ap[e], we_d.v(key=e).keys))

    for e in range(4):
        wload(e)
    hcnt = [0]
    for eg in range(NEXP // G):
        if eg >= 1 and (eg + 1) * G < NEXP:
            for ei in range(G):
                wload((eg + 1) * G + ei)
        for tb in range(4):
            hb = hid[hcnt[0] % 2]
            hcnt[0] += 1
            for ei in range(G):
                e = eg * G + ei
                w = wring[e % 4]
                psc = k.ps[3].sub(512, 512)
                k.mm(psc.v(), selt.v(e * 128, e * 128 + 128, p0=0, p1=32), CTt.v(tb * 512, tb * 512 + 512, p0=0, p1=32))
                for ch in range(2):
                    psg = k.ps[ch].sub(0, 512)
                    psu = k.ps[ch].sub(512, 512)
                    for kc in range(8):
                        k.mm(psg.v(), w.v(kc * 256 + ch * 128, kc * 256 + ch * 128 + 128),
                             hT.v(kc * T + tb * 512, kc * T + tb * 512 + 512), start=(kc == 0), stop=(kc == 7))
                    for kc in range(8):
                        k.mm(psu.v(), w.v(2048 + kc * 256 + ch * 128, 2048 + kc * 256 + ch * 128 + 128),
                             hT.v(kc * T + tb * 512, kc * T + tb * 512 + 512), start=(kc == 0), stop=(kc == 7))
                    s_ = sg[ch]
                    k.act(s_.v(), psg.v(), AF.Silu)
                    k.tt('dve', s_.v(), psu.v(), s_.v(), ALU.mult)
                    k.tt('dve', hb.v((ei * 2 + ch) * 512, (ei * 2 + ch) * 512 + 512), psc.v(), s_.v(), ALU.mult)
            for cc in range(8):
                psy = k.ps[2].sub((cc % 2) * 512, 512)
                for ei in range(G):
                    w = wring[(eg * G + ei) % 4]
                    for ch in range(2):
                        k.mm(psy.v(), w.v(4096 + ch * 1024 + cc * 128, 4096 + ch * 1024 + cc * 128 + 128),
                             hb.v((ei * 2 + ch) * 512, (ei * 2 + ch) * 512 + 512),
                             start=(ei == 0 and ch == 0), stop=(ei == G - 1 and ch == 1))
                xs = xT.v(cc * T + tb * 512, cc * T + tb * 512 + 512)
                k.stt('dve', xs, psy.v(), mod.v(40 + cc, 41 + cc), xs, ALU.mult, ALU.add)
    return


def build_F():
    k = K(nf32=25600, nbf16=51200)
    nc = k.nc
    D = {}

    def ext(name, shape, dt):
        D[name] = k.din(name, shape, dt)

    def itn(name, shape, dt):
        D[name] = DBuf(name, nc.dram_tensor(name, list(shape), dt).ap())

    ext('xT', [128, 8 * T], F32); ext('c_col', [128, 8], F32); ext('posb', [128, T], I32); ext('invf', [128, 1], F32)
    ext('sel', [32, 32 * 128], F32); ext('ehd', [4, 256], F32); ext('idx', [128, 93], I32); ext('vmask', [128, 69], F32)
    for l in range(2):
        ext('w_ada%d' % l, [6, 128, 8 * 1024], F32); ext('b_ada%d' % l, [128, 48], F32); ext('gains%d' % l, [128, 4], F32)
        for n, nc_ in WIN_GROUPS:
            ext('w_%s%d' % (n, l), [128, 8 * nc_], F32)
        ext('lamp%d' % l, [128, 258], F32); ext('subln%d' % l, [128, 128], F32)
        ext('w_pa%d' % l, [128, 4096], F32); ext('w_pb%d' % l, [128, 2048], F32); ext('w_o%d' % l, [128, 8192], F32)
        ext('w_r%d' % l, [128, 288], F32); ext('b_r%d' % l, [128, 36], F32); ext('w_e%d' % l, [NEXP, 128, 6144], F32)
    def pieces(nm, n, ls, as_):
        D[nm + '_loc'] = [DBuf('%s_loc%d' % (nm, i), nc.dram_tensor('%s_loc%d' % (nm, i), ls, BF16).ap()) for i in range(n)]
        D[nm + '_all'] = [DBuf('%s_all%d' % (nm, i), nc.dram_tensor('%s_all%d' % (nm, i), as_, BF16).ap()) for i in range(n)]
    pieces('kTa', 2, [256, 2048], [1024, 2048])
    pieces('va', 4, [T, 129], [SEQ, 129])
    pieces('kTb', 3, [256, 2048], [1024, 2048])
    pieces('vb', 3, [T, 256], [SEQ, 256])
    itn('qTa_s', [128, 4 * T], BF16); itn('qTb_s', [128, 6 * T], BF16)
    itn('gates_s', [128, 16 * T], BF16); itn('mod_s', [128, 48], F32)
    out_d = k.dout('outT', [128, 8 * T], F32)
    c = load_consts(k, True)
    xT = k.fa(8 * T)
    k.idx_t = k.es.enter_context(nc.sbuf_tensor("idx_t", [128, 93], I32))
    idxb = Buf("idx_t", k.idx_t, 4, 0, 93)
    k.dma('sp', idxb.v(), D['idx'].v())
    k.dma('sp', xT.v(), D['xT'].v())
    base = (k.fo, k.bo)
    rg = [[0, 1, 2, 3], [4, 5, 6, 7]]
    for l in range(2):
        k.fo, k.bo = base
        def after_kv():
            for nm, i_ in [('kTa', 0), ('va', 0), ('va', 1), ('kTa', 1), ('va', 2), ('va', 3),
                           ('kTb', 0), ('kTb', 1), ('kTb', 2), ('vb', 0), ('vb', 1), ('vb', 2)]:
                loc, al = D[nm + '_loc'][i_], D[nm + '_all'][i_]
                k.S.add('pool', lambda e, loc=loc, al=al: e.collective_compute(
                    "AllGather", ALU.bypass, replica_groups=rg, ins=[loc.ap.opt()], outs=[al.ap.opt()]),
                    reads=loc.v().keys, writes=al.v().keys, dma=True, cc=True)
        k.after_kv = after_kv
        body_A(k, c, xT, D, l)
        after_kv()
        k.fo, k.bo = base
        body_B(k, c, xT, D, l)
    k.dma('sp', out_d.v(), xT.v())
    return k.finalize()


_cache = {}


def _fm(w):
    K_, N = w.shape
    return np.ascontiguousarray(w.reshape(K_ // 128, 128, N).transpose(1, 0, 2).reshape(128, (K_ // 128) * N))


def _index_tables(jr):
    p = np.arange(128)
    idx = np.zeros((128, 93), np.int64)
    vmask = np.zeros((128, 69), np.float32)
    for jp in range(6):
        for seg in range(4):
            rank = [jr - 1, jr, jr, jr + 1][seg]
            half = [1, 0, 1, 0][seg]
            rank = min(max(rank, 0), 3)
            idx[:, jp * 4 + seg] = rank * 512 + ((jp % 2) * 128 + p) * 2 + half
    t = 0
    for g, dil in enumerate([1, 4, 16]):
        for r in range(dil):
            for n in range(16 // dil + 1):
                a_k = 128 * n - 64 + p
                gpos = jr * T + a_k * dil + r
                valid = (gpos >= 0) & (gpos < SEQ)
                gp = np.where(valid, gpos, 0)
                idx[:, 24 + t] = gp
                vmask[:, t] = valid.astype(np.float32)
                t += 1
    assert t == 69
    return idx.astype(np.int32), vmask


def kernel(x, c, positions, w_ada, b_ada, w_in, qn_a, kn_a, lam_q1, lam_k1, lam_q2, lam_k2,
           subln_a, qn_b, kn_b, w_pa, w_pb, w_o, w_r1, b_r1, w_r2, b_r2, w_e_gate, w_e_up, w_e_down):
    x = np.asarray(x, np.float32)
    consts = host_consts()
    invf = (np.float32(10000.0) ** (-np.arange(0, 64, 2, dtype=np.float32) / np.float32(64))).astype(np.float32)
    invf_col = invf[(np.arange(128) % 64) % 32].reshape(128, 1).astype(np.float32)
    cores = list(range(NCORES))
    sel = np.zeros((32, 32 * 128), np.float32)
    for e in range(32):
        sel[e, e * 128:(e + 1) * 128] = 1.0
    ehd = np.zeros((4, 256), np.float32)
    for h in range(4):
        j, half = h // 2, h % 2
        ehd[h, j * 128 + half * 64: j * 128 + half * 64 + 64] = 1.0
    cuts = np.cumsum([0, 512, 512, 512, 768, 768, 768, 1024, 1024])
    names = ['qa', 'ka', 'va', 'qb', 'kb', 'vb', 'ga', 'gb']
    shared = {"consts": consts, "invf": invf_col, "sel": sel, "ehd": ehd}
    pidx = np.arange(128) % 64
    for l in range(2):
        wl = np.asarray(w_in[l], np.float32)
        for i, n in enumerate(names):
            shared["w_%s%d" % (n, l)] = _fm(wl[:, cuts[i]:cuts[i + 1]])
        shared["w_ada%d" % l] = np.ascontiguousarray(
            np.asarray(w_ada[l], np.float32).reshape(8, 128, 6, 1024).transpose(2, 1, 0, 3).reshape(6, 128, 8 * 1024))
        shared["b_ada%d" % l] = np.ascontiguousarray(np.asarray(b_ada[l], np.float32).reshape(48, 128).T)
        shared["gains%d" % l] = np.stack([np.asarray(qn_a[l])[pidx], np.asarray(kn_a[l])[pidx],
                                          np.asarray(qn_b[l])[pidx], np.asarray(kn_b[l])[pidx]], axis=1).astype(np.float32)
        lam_init = 0.8 - 0.6 * math.exp(-0.3 * l)
        lamp = np.concatenate([np.asarray(lam_q1[l]), np.asarray(lam_k1[l]), np.asarray(lam_q2[l]), np.asarray(lam_k2[l]),
                               np.array([lam_init, 1.0 - lam_init])]).astype(np.float32)
        shared["lamp%d" % l] = np.ascontiguousarray(np.broadcast_to(lamp[None, :], (128, 258)))
        shared["subln%d" % l] = np.ascontiguousarray(np.broadcast_to(np.asarray(subln_a[l], np.float32)[:, None], (128, 128)))
        shared["w_r%d" % l] = _fm(np.concatenate([np.asarray(w_r1[l], np.float32), np.asarray(w_r2[l], np.float32)], axis=1))
        shared["b_r%d" % l] = np.ascontiguousarray(np.broadcast_to(
            np.concatenate([np.asarray(b_r1[l]), np.asarray(b_r2[l])]).astype(np.float32)[None, :], (128, 36)))
        we = np.empty((NEXP, 128, 6144), np.float32)
        for e in range(NEXP):
            we[e, :, 0:2048] = _fm(np.asarray(w_e_gate[l, e], np.float32))
            we[e, :, 2048:4096] = _fm(np.asarray(w_e_up[l, e], np.float32))
            we[e, :, 4096:6144] = _fm(np.asarray(w_e_down[l, e], np.float32))
        shared["w_e%d" % l] = we
        shared["w_pa%d" % l] = _fm(np.asarray(w_pa[l], np.float32))
        shared["w_pb%d" % l] = _fm(np.asarray(w_pb[l], np.float32))
        shared["w_o%d" % l] = _fm(np.asarray(w_o[l], np.float32))
    in_maps = []
    for ci in cores:
        b, j = ci // 4, ci % 4
        xs = x[b, j * T:(j + 1) * T, :]
        idx, vmask = _index_tables(j)
        m = dict(shared)
        m["xT"] = np.ascontiguousarray(xs.T.reshape(8, 128, T).transpose(1, 0, 2).reshape(128, 8 * T))
        m["c_col"] = np.ascontiguousarray(np.asarray(c[b], np.float32).reshape(8, 128).T)
        m["posb"] = np.ascontiguousarray(np.broadcast_to(np.asarray(positions[b, j * T:(j + 1) * T], np.int32)[None, :], (128, T)))
        m["idx"] = idx
        m["vmask"] = vmask
        in_maps.append(m)
    if 'F' not in _cache:
        _cache['F'] = build_F()
    res = run_bass_kernel_spmd(_cache['F'], in_maps, core_ids=cores).results
    out = np.empty((2, SEQ, D), np.float32)
    for ci in cores:
        b, j = ci // 4, ci % 4
        o = np.asarray(res[ci]["outT"], np.float32)
        out[b, j * T:(j + 1) * T, :] = o.reshape(128, 8, T).transpose(1, 0, 2).reshape(D, T).T
    return out
```

```python
import math
import numpy as np
from contextlib import ExitStack
import concourse.bass as bass
import concourse.mybir as mybir
from concourse.bass_utils import run_bass_kernel_spmd

F32 = mybir.dt.float32
BF16 = mybir.dt.bfloat16
I32 = mybir.dt.int32
ALU = mybir.AluOpType
AF = mybir.ActivationFunctionType
AX = mybir.AxisListType

ENGS = ['pe', 'act', 'dve', 'pool', 'sp']
CHUNK = 1024

NCORES = 8
T = 2048
SEQ = 8192
D = 1024
EPS = 1e-6
NEG = -30000.0
WIN = 4096
PADW = 1024
NEXP = 32


class Op:
    __slots__ = ('eng', 'fn', 'src', 'pos', 'signal', 'waits', 'know', 'is_dma', 'cnt')


class V:
    __slots__ = ('ap', 'keys')

    def __init__(self, ap, keys):
        self.ap = ap
        self.keys = keys


class Buf:
    def __init__(self, arena_name, handle, esz, off, n):
        self.an = arena_name
        self.t = handle
        self.esz = esz
        self.off = off
        self.n = n

    def keys(self, a, b):
        ch = 2048 if self.an.startswith('ps') else CHUNK
        lo = ((self.off + a) * self.esz) // ch
        hi = ((self.off + b) * self.esz - 1) // ch
        return [(self.an, i) for i in range(lo, hi + 1)]

    def v(self, a=0, b=None, p0=0, p1=128, step=1, pat=None, **kw):
        if b is None:
            b = self.n
        assert 0 <= a < b <= self.n, (a, b, self.n)
        if step == 1:
            ap = self.t[p0:p1, self.off + a:self.off + b]
        else:
            ap = self.t[p0:p1, self.off + a:self.off + b:step]
        if pat is not None:
            ap = ap.rearrange(pat, **kw)
        return V(ap, self.keys(a, b))

    def sub(self, off, n):
        assert off + n <= self.n
        return Buf(self.an, self.t, self.esz, self.off + off, n)


class DBuf:
    def __init__(self, name, ap):
        self.name = name
        self.ap = ap

    def v(self, ap=None, key=None):
        return V(self.ap if ap is None else ap, [(self.name, key)])


class Sched:
    def __init__(self, nc, n_dma_sems=16):
        self.nc = nc
        self.ops = {e: [] for e in ENGS}
        self.ncomp = {e: 0 for e in ENGS}
        self.last_w = {}
        self.readers = {}
        self.known = {e: {} for e in ENGS}
        self.n_dma_sems = n_dma_sems
        self.dma_last = [None] * n_dma_sems
        self.dma_cnt = [0] * n_dma_sems
        self.dma_rr = 0
        self.dma_rr_sw = 0
        self.cc_cnt = 0
        self.cc_last = None

    def _need(self, e, d):
        k = self.known[e]
        if k.get(d.src, -1) >= d.pos:
            return None
        d.signal = True
        for s, p in d.know.items():
            if k.get(s, -1) < p:
                k[s] = p
        return d

    def add(self, eng, fn, reads=(), writes=(), dma=False, cc=False):
        op = Op()
        op.eng = eng
        op.fn = fn
        op.is_dma = dma
        op.signal = dma
        deps = []
        seen = set()
        pr_ = [k for k in reads if isinstance(k[0], str) and k[0].startswith('ps')]
        if pr_:
            writes = list(writes) + pr_
        for k in reads:
            w = self.last_w.get(k)
            if w is not None and id(w) not in seen:
                seen.add(id(w)); deps.append(w)
        for k in writes:
            w = self.last_w.get(k)
            if w is not None and id(w) not in seen:
                seen.add(id(w)); deps.append(w)
            for r in self.readers.get(k, ()):
                if id(r) not in seen:
                    seen.add(id(r)); deps.append(r)
        if cc:
            op.src = ('cc', 0)
            op.pos = self.cc_cnt
            self.cc_cnt += 1
            if self.cc_last is not None and id(self.cc_last) not in seen:
                seen.add(id(self.cc_last)); deps.append(self.cc_last)
            self.cc_last = op
        elif dma:
            half = self.n_dma_sems // 2
            if eng == 'pool':
                slot = half + self.dma_rr_sw
                self.dma_rr_sw = (self.dma_rr_sw + 1) % (self.n_dma_sems - half)
            else:
                slot = self.dma_rr
                self.dma_rr = (self.dma_rr + 1) % half
            prev = self.dma_last[slot]
            if prev is not None and id(prev) not in seen:
                seen.add(id(prev)); deps.append(prev)
            op.src = ('dma', slot)
            op.pos = self.dma_cnt[slot]
            self.dma_cnt[slot] += 1
            self.dma_last[slot] = op
        else:
            op.src = eng
            op.pos = self.ncomp[eng]
            self.ncomp[eng] += 1
        waits = []
        deps.sort(key=lambda d: -d.pos)
        for d in deps:
            if (not dma) and (not d.is_dma) and d.src == eng:
                if eng == 'pe':
                    continue
                if op.pos - d.pos > 2:
                    continue
            w = self._need(eng, d)
            if w is not None:
                waits.append(w)
        op.waits = waits
        know = dict(self.known[eng])
        know[op.src] = op.pos
        op.know = know
        for k in reads:
            self.readers.setdefault(k, []).append(op)
        for k in writes:
            self.last_w[k] = op
            self.readers[k] = []
        self.ops[eng].append(op)
        return op

    def finish(self):
        op = Op()
        op.eng = 'sp'; op.fn = None; op.is_dma = False; op.signal = False
        op.src = 'sp'; op.pos = self.ncomp['sp']; self.ncomp['sp'] += 1
        waits = []
        for d in list(self.dma_last) + [self.cc_last]:
            if d is not None:
                w = self._need('sp', d)
                if w is not None:
                    waits.append(w)
        for e in ['pe', 'act', 'dve', 'pool']:
            comp = [o for o in self.ops[e] if not o.is_dma]
            if comp:
                w = self._need('sp', comp[-1])
                if w is not None:
                    waits.append(w)
        op.waits = waits
        op.know = {}
        self.ops['sp'].append(op)

    def emit(self, sems):
        nc = self.nc
        for e in ENGS:
            c = 0
            for o in self.ops[e]:
                if o.is_dma:
                    continue
                if o.signal:
                    c += 1
                o.cnt = c
        engobj = {'pe': nc.tensor, 'act': nc.scalar, 'dve': nc.vector, 'pool': nc.gpsimd, 'sp': nc.sync}

        def run(e):
            eo = engobj[e]
            for o in self.ops[e]:
                for d in o.waits:
                    if d.is_dma and d.src[0] == 'cc':
                        eo.wait_ge(sems[d.src], d.pos + 1)
                    elif d.is_dma:
                        eo.wait_ge(sems[d.src], 16 * (d.pos + 1))
                    else:
                        eo.wait_ge(sems[d.src], d.cnt)
                if o.fn is None:
                    continue
                ins = o.fn(eo)
                if o.is_dma and o.src[0] == 'cc':
                    ins.then_inc(sems[o.src])
                elif o.is_dma:
                    ins.then_inc(sems[o.src], 16)
                elif o.signal:
                    ins.then_inc(sems[e], 1)
        return run


class K:
    def __init__(self, nf32, nbf16):
        self.nc = bass.Bass("TRN2", target_bir_lowering=False)
        self.es = ExitStack()
        nc = self.nc
        self.S = Sched(nc)
        es = self.es
        self.af_t = es.enter_context(nc.sbuf_tensor("arena_f", [128, nf32], F32))
        self.ab_t = es.enter_context(nc.sbuf_tensor("arena_b", [128, nbf16], BF16))
        self.AF_ = Buf("af", self.af_t, 4, 0, nf32)
        self.AB_ = Buf("ab", self.ab_t, 2, 0, nbf16)
        self.ps = []
        for i in range(4):
            t = es.enter_context(nc.psum_tensor("ps%d" % i, [128, 1024], F32))
            self.ps.append(Buf("ps%d" % i, t, 4, 0, 1024))
        self.sems = {}
        for e in ENGS:
            self.sems[e] = es.enter_context(nc.semaphore("s_" + e))
        for i in range(self.S.n_dma_sems):
            self.sems[('dma', i)] = es.enter_context(nc.semaphore("d%d" % i))
        self.sems[('cc', 0)] = es.enter_context(nc.semaphore("ccsem"))
        self.fo = 0
        self.bo = 0
        self.dram = {}

    def din(self, name, shape, dt):
        ap = self.nc.dram_tensor(name, list(shape), dt, kind="ExternalInput").ap()
        d = DBuf(name, ap)
        self.dram[name] = d
        return d

    def dout(self, name, shape, dt):
        ap = self.nc.dram_tensor(name, list(shape), dt, kind="ExternalOutput").ap()
        d = DBuf(name, ap)
        self.dram[name] = d
        return d

    def fa(self, n):
        b = self.AF_.sub(self.fo, n)
        self.fo += n
        return b

    def ba(self, n):
        b = self.AB_.sub(self.bo, n)
        self.bo += n
        return b

    def mm(self, out, lhsT, rhs, start=True, stop=True):
        self.S.add('pe', lambda e: e.matmul(out.ap, lhsT.ap, rhs.ap, start=start, stop=stop),
                   reads=lhsT.keys + rhs.keys, writes=out.keys)

    def tr(self, out, in_, ident):
        self.S.add('pe', lambda e: e.transpose(out.ap, in_.ap, ident.ap),
                   reads=in_.keys + ident.keys, writes=out.keys)

    def act(self, out, in_, func, scale=1.0, bias=0.0, accum=None):
        reads = list(in_.keys)
        kw = {}
        if isinstance(bias, V):
            reads += bias.keys
            kw['bias'] = bias.ap
        else:
            kw['bias'] = float(bias)
        if isinstance(scale, V):
            reads += scale.keys
            kw['scale'] = scale.ap
        else:
            kw['scale'] = float(scale)
        writes = list(out.keys)
        if accum is not None:
            writes += accum.keys
            kw['accum_out'] = accum.ap
        self.S.add('act', lambda e: e.activation(out=out.ap, in_=in_.ap, func=func, **kw),
                   reads=reads, writes=writes)

    def tt(self, eng, out, in0, in1, op):
        self.S.add(eng, lambda e: e.tensor_tensor(out=out.ap, in0=in0.ap, in1=in1.ap, op=op),
                   reads=in0.keys + in1.keys, writes=out.keys)

    def ts(self, eng, out, in0, s1, s2=None, op0=ALU.mult, op1=None):
        reads = list(in0.keys)
        a1 = s1
        a2 = s2
        if isinstance(s1, V):
            reads += s1.keys; a1 = s1.ap
        if isinstance(s2, V):
            reads += s2.keys; a2 = s2.ap
        if op1 is None:
            self.S.add(eng, lambda e: e.tensor_scalar(out=out.ap, in0=in0.ap, scalar1=a1, scalar2=None, op0=op0),
                       reads=reads, writes=out.keys)
        else:
            self.S.add(eng, lambda e: e.tensor_scalar(out=out.ap, in0=in0.ap, scalar1=a1, scalar2=a2, op0=op0, op1=op1),
                       reads=reads, writes=out.keys)

    def stt(self, eng, out, in0, scalar, in1, op0, op1):
        reads = in0.keys + in1.keys
        a = scalar
        if isinstance(scalar, V):
            reads = reads + scalar.keys; a = scalar.ap
        self.S.add(eng, lambda e: e.scalar_tensor_tensor(out=out.ap, in0=in0.ap, scalar=a, in1=in1.ap, op0=op0, op1=op1),
                   reads=reads, writes=out.keys)

    def cp(self, eng, out, in_):
        if eng == 'act':
            self.S.add('act', lambda e: e.copy(out=out.ap, in_=in_.ap), reads=in_.keys, writes=out.keys)
        else:
            self.S.add(eng, lambda e: e.tensor_copy(out=out.ap, in_=in_.ap), reads=in_.keys, writes=out.keys)

    def red(self, eng, out, in_, op, axis=AX.X):
        self.S.add(eng, lambda e: e.tensor_reduce(out=out.ap, in_=in_.ap, axis=axis, op=op),
                   reads=in_.keys, writes=out.keys)

    def recip(self, out, in_):
        self.S.add('dve', lambda e: e.reciprocal(out=out.ap, in_=in_.ap), reads=in_.keys, writes=out.keys)

    def memset(self, eng, out, val):
        self.S.add(eng, lambda e: e.memset(out.ap, val), writes=out.keys)

    def dma(self, eng, out, in_):
        self.S.add(eng, lambda e: e.dma_start(out=out.ap, in_=in_.ap), reads=in_.keys, writes=out.keys, dma=True)

    def finalize(self):
        S = self.S
        S.finish()
        run = S.emit(self.sems)
        with self.nc.Block() as block:
            @block.tensor
            def _(e): run('pe')
            @block.scalar
            def _(e): run('act')
            @block.vector
            def _(e): run('dve')
            @block.gpsimd
            def _(e): run('pool')
            @block.sync
            def _(e): run('sp')
        self.es.close()
        return self.nc


def load_consts(k, need_rope):
    c = {}
    cin = k.din("consts", [128, 5 * 128], F32)
    cf = k.fa(256).sub(0, 128)
    k.dma('sp', cf.v(), V(cin.ap[:, 0:128], cin.v().keys))
    c['ident_f'] = cf
    cb = k.ba(5 * 128)
    k.dma('pool', cb.v(), cin.v())
    c['ident_b'] = cb.sub(0, 128)
    c['onesbd'] = cb.sub(128, 128)
    c['rotT'] = cb.sub(256, 128)
    c['maskA'] = cb.sub(384, 128)
    c['maskB'] = cb.sub(512, 128)
    ones = k.ba(128)
    k.ba(256)
    k.memset('pool', ones.v(), 1.0)
    c['ones_b'] = ones
    return c


def host_consts():
    ident = np.eye(128, dtype=np.float32)
    onesbd = np.zeros((128, 128), np.float32)
    onesbd[:64, :64] = 1.0
    onesbd[64:, 64:] = 1.0
    rotT = np.zeros((128, 128), np.float32)
    for m in range(128):
        j = m % 64
        if j < 32:
            rotT[m + 32, m] = -1.0
        else:
            rotT[m - 32, m] = 1.0
    u = np.arange(128)[:, None]
    a = np.arange(128)[None, :]
    maskA = np.where(u >= a, 0.0, NEG).astype(np.float32)
    maskB = np.where(u <= a, 0.0, NEG).astype(np.float32)
    return np.concatenate([ident, onesbd, rotT, maskA, maskB], axis=1)


TWO_PI = 2.0 * math.pi
C1 = 6.28125
C2 = float(np.float32(TWO_PI - 6.28125))
C3 = float(TWO_PI - 6.28125 - float(np.float32(TWO_PI - 6.28125)))


WIN_GROUPS = [('ka', 512), ('kb', 768), ('va', 512), ('vb', 768), ('qa', 512), ('qb', 768), ('ga', 1024), ('gb', 1024)]


def rms_mod(k, xT, hT, c, scale_col, shift_col, work_f, work_b, h32_hook=None):
    sq = [work_b.sub(i * 512, 512) for i in range(2)]
    sd = work_f.sub(0, 512)
    rs = work_f.sub(512, 512)
    tmp = [work_f.sub(1024 + i * 512, 512) for i in range(2)]
    for tb in range(4):
        pss = k.ps[tb % 2].sub(0, 512)
        for cc in range(8):
            s = sq[cc % 2]
            k.act(s.v(), xT.v(cc * T + tb * 512, cc * T + tb * 512 + 512), AF.Square)
            k.mm(pss.v(), c['ones_b'].v(), s.v(), start=(cc == 0), stop=(cc == 7))
        k.act(sd.v(), pss.v(), AF.Sqrt, scale=1.0 / D, bias=EPS)
        k.recip(rs.v(), sd.v())
        for cc in range(8):
            t = tmp[cc % 2]
            k.tt('pool', t.v(), xT.v(cc * T + tb * 512, cc * T + tb * 512 + 512), rs.v(), ALU.mult)
            if h32_hook is not None:
                h32 = h32_hook(tb, cc)
                k.ts('dve', h32.v(), t.v(), scale_col.v(cc, cc + 1), shift_col.v(cc, cc + 1), ALU.mult, ALU.add)
                k.cp('pool', hT.v(cc * T + tb * 512, cc * T + tb * 512 + 512), h32.v())
            else:
                k.ts('dve', hT.v(cc * T + tb * 512, cc * T + tb * 512 + 512), t.v(),
                     scale_col.v(cc, cc + 1), shift_col.v(cc, cc + 1), ALU.mult, ALU.add)
        if h32_hook is not None:
            h32_hook(tb, None)


def body_A(k, c, xT, D, l):
    stg = 99
    ccol_d = D['c_col']
    pos_d = D['posb']
    invf_d = D['invf']
    wada_d = D['w_ada%d' % l]
    bada_d = D['b_ada%d' % l]
    gains_d = D['gains%d' % l]
    wg_d = {n: D['w_%s%d' % (n, l)] for n, nc_ in WIN_GROUPS}
    kTa_o = D['kTa_loc']
    kTb_o = D['kTb_loc']
    qTa_o = D['qTa_s']
    qTb_o = D['qTb_s']
    va_o = D['va_loc']
    vb_o = D['vb_loc']
    gates_o = D['gates_s']
    mod_o = D['mod_s']

    cosT = k.fa(T)
    sinT = k.fa(T)
    small = k.fa(256)
    work_f = k.fa(4096)
    hT = k.ba(8 * T)
    wring = [k.ba(8192) for _ in range(3)]
    work_b = k.ba(2048)
    stage = [k.ba(2048) for _ in range(2)]
    vst = [k.ba(1024) for _ in range(2)]

    ccol = small.sub(0, 8)
    cact = k.ba(8)
    bada = small.sub(8, 48)
    mod = small.sub(56, 48)
    gains = small.sub(104, 4)
    invf = small.sub(108, 1)

    k.dma('sp', ccol.v(), ccol_d.v())
    k.dma('sp', bada.v(), bada_d.v())
    k.dma('sp', gains.v(), gains_d.v())
    k.dma('sp', invf.v(), invf_d.v())

    ang = work_f.sub(0, T)
    kk = work_f.sub(T, T)
    posi_v = V(kk.v().ap.bitcast(I32), kk.v().keys)
    k.dma('sp', posi_v, pos_d.v())
    k.cp('dve', ang.v(), posi_v)
    k.ts('dve', ang.v(), ang.v(), invf.v(), None, ALU.mult)
    k.ts('dve', kk.v(), ang.v(), 1.0 / TWO_PI, 12582912.0, ALU.mult, ALU.add)
    k.ts('dve', kk.v(), kk.v(), 12582912.0, None, ALU.subtract)
    k.stt('dve', ang.v(), kk.v(), -C1, ang.v(), ALU.mult, ALU.add)
    k.stt('dve', ang.v(), kk.v(), -C2, ang.v(), ALU.mult, ALU.add)
    k.stt('dve', ang.v(), kk.v(), -C3, ang.v(), ALU.mult, ALU.add)
    k.ts('dve', ang.v(), ang.v(), math.pi, -math.pi, ALU.min, ALU.max)
    k.act(sinT.v(), ang.v(), AF.Sin)
    k.act(kk.v(), ang.v(), AF.Sin, scale=0.5)
    k.tt('dve', kk.v(), kk.v(), kk.v(), ALU.mult)
    k.ts('dve', cosT.v(), kk.v(), -2.0, 1.0, ALU.mult, ALU.add)

    k.act(cact.v(), ccol.v(), AF.Silu)
    psm = k.ps[3].sub(0, 48)
    for s in range(6):
        wb = wring[s % 3]
        k.dma('pool', wb.v(), V(wada_d.ap[s], wada_d.v(key=s).keys))
        for cc in range(8):
            for kc in range(8):
                k.mm(psm.v(s * 8 + cc, s * 8 + cc + 1),
                     wb.v(kc * 1024 + cc * 128, kc * 1024 + cc * 128 + 128),
                     cact.v(kc, kc + 1), start=(kc == 0), stop=(kc == 7))
    k.tt('dve', mod.v(), psm.v(), bada.v(), ALU.add)
    k.dma('sp', mod_o.v(), mod.v())
    sc1 = small.sub(152, 8)
    k.ts('dve', sc1.v(), mod.v(8, 16), 1.0, None, ALU.add)

    rms_mod(k, xT, hT, c, sc1, mod.sub(0, 8), work_f, work_b)

    raw = [work_f.sub(i * 512, 512) for i in range(2)]
    rst = [work_f.sub(1024 + i * 512, 512) for i in range(2)]
    t1 = [work_f.sub(2048 + i * 512, 512) for i in range(2)]
    t2 = [work_f.sub(3072 + i * 512, 512) for i in range(2)]
    sqb = [work_b.sub(i * 512, 512) for i in range(2)]
    qnb = [work_b.sub(1024 + i * 512, 512) for i in range(2)]
    cnt = [0]

    def qk_block(psb, gcol, outv, tb):
        i = cnt[0] % 2
        cnt[0] += 1
        k.act(sqb[i].v(), psb.v(), AF.Square)
        k.ts('dve', raw[i].v(), psb.v(), gcol, None, ALU.mult)
        ps2 = k.ps[2].sub(i * 512, 512)
        k.mm(ps2.v(), c['onesbd'].v(), sqb[i].v())
        k.act(rst[i].v(), ps2.v(), AF.Ln, scale=1.0 / 64, bias=EPS)
        k.act(rst[i].v(), rst[i].v(), AF.Exp, scale=-0.5)
        k.tt('pool', qnb[i].v(), raw[i].v(), rst[i].v(), ALU.mult)
        ps3 = k.ps[3].sub(i * 512, 512)
        k.mm(ps3.v(), c['rotT'].v(), qnb[i].v())
        k.tt('dve', t1[i].v(), qnb[i].v(), cosT.v(tb * 512, tb * 512 + 512), ALU.mult)
        k.tt('dve', t2[i].v(), ps3.v(), sinT.v(tb * 512, tb * 512 + 512), ALU.mult)
        k.tt('pool', outv, t1[i].v(), t2[i].v(), ALU.add)

    pcnt = [0]
    def wload_g(gj):
        nm_, nc__ = WIN_GROUPS[gj]
        k.dma('pool', wring[gj % 3].v(0, 8 * nc__), wg_d[nm_].v())

    wload_g(0)
    wload_g(1)
    for gi, (name, ncol) in enumerate(WIN_GROUPS):
        wb = wring[gi % 3]
        if gi + 2 < len(WIN_GROUPS):
            wload_g(gi + 2)
        if name in ('ka', 'kb', 'qa', 'qb'):
            npair = ncol // 128
            gidx = {'qa': 0, 'ka': 1, 'qb': 2, 'kb': 3}[name]
            od = {'ka': kTa_o, 'kb': kTb_o, 'qa': qTa_o, 'qb': qTb_o}[name]
            for pr in range(npair):
                st = stage[pr % 2]
                for tb in range(4):
                    psb = k.ps[pcnt[0] % 2].sub(512 * ((pcnt[0] // 2) % 2), 512)
                    pcnt[0] += 1
                    for kc in range(8):
                        k.mm(psb.v(), wb.v(kc * ncol + pr * 128, kc * ncol + pr * 128 + 128),
                             hT.v(kc * T + tb * 512, kc * T + tb * 512 + 512), start=(kc == 0), stop=(kc == 7))
                    qk_block(psb, gains.v(gidx, gidx + 1), st.v(tb * 512, tb * 512 + 512), tb)
                if name in ('ka', 'kb'):
                    odp = od[pr // 2]
                    k.dma('sp', V(odp.ap[(pr % 2) * 128:(pr % 2 + 1) * 128, :], odp.v().keys), st.v())
                else:
                    k.dma('sp', V(od.ap[:, pr * T:(pr + 1) * T], od.v().keys), st.v())
        elif name in ('va', 'vb'):
            nh_, dh_, d_ = (4, 129, 128) if name == 'va' else (12, 64, 64)
            od = va_o if name == 'va' else vb_o
            w_ = nh_ * dh_
            for i in range(2):
                k.memset('pool', vst[i].v(0, w_), 1.0)
            for tt_ in range(16):
                st = vst[tt_ % 2]
                for n0 in range(0, ncol, 512):
                    nn = min(512, ncol - n0)
                    psb = k.ps[pcnt[0] % 2].sub(512 * ((pcnt[0] // 2) % 2), 512)
                    pcnt[0] += 1
                    for kc in range(8):
                        k.mm(psb.v(0, nn), hT.v(kc * T + tt_ * 128, kc * T + tt_ * 128 + 128),
                             wb.v(kc * ncol + n0, kc * ncol + n0 + nn), start=(kc == 0), stop=(kc == 7))
                    h0 = n0 // d_
                    nhh = nn // d_
                    outv = V(st.v(h0 * dh_, (h0 + nhh) * dh_, pat="p (h c) -> p h c", c=dh_).ap[:, :, 0:d_],
                             st.keys(h0 * dh_, (h0 + nhh) * dh_))
                    inv = V(psb.v(0, nn, pat="p (h c) -> p h c", c=d_).ap, psb.keys(0, nn))
                    k.cp('act', outv, inv)
                if name == 'va':
                    for h4 in range(4):
                        k.dma('sp', V(od[h4].ap[tt_ * 128:(tt_ + 1) * 128, :], od[h4].v().keys), st.v(h4 * 129, h4 * 129 + 129))
                else:
                    for g3 in range(3):
                        k.dma('sp', V(od[g3].ap[tt_ * 128:(tt_ + 1) * 128, :], od[g3].v().keys), st.v(g3 * 256, g3 * 256 + 256))
        else:
            gi_ = 0 if name == 'ga' else 1
            for cc in range(8):
                st = stage[cc % 2]
                for tb in range(4):
                    psb = k.ps[pcnt[0] % 2].sub(512 * ((pcnt[0] // 2) % 2), 512)
                    pcnt[0] += 1
                    for kc in range(8):
                        k.mm(psb.v(), wb.v(kc * ncol + cc * 128, kc * ncol + cc * 128 + 128),
                             hT.v(kc * T + tb * 512, kc * T + tb * 512 + 512), start=(kc == 0), stop=(kc == 7))
                    k.act(st.v(tb * 512, tb * 512 + 512), psb.v(), AF.Sigmoid)
                o0 = (gi_ * 8 + cc) * T
                k.dma('sp', V(gates_o.ap[:, o0:o0 + T], gates_o.v().keys), st.v())
    return


def body_B(k, c, xT, D, l):
    mod_d = D['mod_s']
    qTa_d = D['qTa_s']
    qTb_d = D['qTb_s']
    kTa_d = D['kTa_all']
    va_d = D['va_all']
    kTb_d = D['kTb_all']
    vb_d = D['vb_all']
    gates_d = D['gates_s']
    lam_d = D['lamp%d' % l]
    subln_d = D['subln%d' % l]
    wpa_d = D['w_pa%d' % l]
    wpb_d = D['w_pb%d' % l]
    wo_d = D['w_o%d' % l]
    wr_d = D['w_r%d' % l]
    br_d = D['b_r%d' % l]
    sel_d = D['sel']
    ehd_d = D['ehd']
    we_d = D['w_e%d' % l]

    small = k.fa(1024)
    fwork = k.fa(25600 - k.fo)
    mod = small.sub(0, 48)
    lamp = small.sub(48, 258)
    subln = small.sub(320, 128)
    br = small.sub(448, 36)
    sc2 = small.sub(484, 8)
    lamcol = small.sub(492, 4)
    tmp64 = small.sub(512, 128)
    wr = small.sub(640, 288)

    k.dma('sp', mod.v(), mod_d.v())
    k.dma('sp', lamp.v(), lam_d.v())
    k.dma('sp', subln.v(), subln_d.v())
    k.dma('sp', br.v(), br_d.v())
    k.dma('sp', wr.v(), wr_d.v())

    k.tt('dve', tmp64.v(0, 64), lamp.v(0, 64), lamp.v(64, 128), ALU.mult)
    k.tt('dve', tmp64.v(64, 128), lamp.v(128, 192), lamp.v(192, 256), ALU.mult)
    k.red('dve', lamcol.v(0, 2), tmp64.v(0, 128, pat="p (a w) -> p a w", w=64), ALU.add)
    k.act(lamcol.v(0, 2), lamcol.v(0, 2), AF.Exp)
    k.tt('dve', lamcol.v(3, 4), lamcol.v(0, 1), lamcol.v(1, 2), ALU.subtract)
    k.tt('dve', lamcol.v(0, 1), lamcol.v(3, 4), lamp.v(256, 257), ALU.add)
    k.ts('dve', lamcol.v(1, 2), lamcol.v(0, 1), -1.0, None, ALU.mult)
    k.ts('dve', sc2.v(), mod.v(32, 40), 1.0, None, ALU.add)

    AB = k.AB_
    b0 = k.bo
    qTa = AB.sub(b0 + 0, 8192)
    oaT = AB.sub(b0 + 8192, 8192)
    obT = AB.sub(b0 + 16384, 4096)
    qTb = AB.sub(b0 + 20480, 12288)
    kring = [AB.sub(b0 + 32768 + i * 2048, 2048) for i in range(3)]
    vring = [AB.sub(b0 + 38912 + i * 2048, 2048) for i in range(3)]
    PT = [AB.sub(b0 + 45104 + i * 1024, 1024) for i in range(2)]

    k.dma('sp', qTa.v(), qTa_d.v())
    PT4 = [AB.sub(b0 + 45056 + i * 1024, 1024) for i in range(4)]
    tsum = AB.sub(b0 + 49152, 1024)
    acc = fwork.sub(1024, 1024)
    dsb = fwork.sub(2048, 512)
    rsb = fwork.sub(2560, 512)
    r2 = fwork.sub(3072, 512)
    slcol = fwork.sub(0, 1)
    k.tt('dve', slcol.v(), subln.v(0, 1), lamp.v(257, 258), ALU.mult)
    ld = [0]
    for h in range(4):
        for qb in range(4):
            OT = k.ps[2]

            SB = [0, 1, 3]

            def qk(kt, kbuf):
                pss = k.ps[SB[kt % 3]]
                for m in range(2):
                    k.mm(pss.v(m * 512, m * 512 + 512),
                         kbuf.v((kt % 16) * 128, (kt % 16) * 128 + 128, p0=m * 64, p1=m * 64 + 64),
                         qTa.v(h * T + qb * 512, h * T + qb * 512 + 512, p0=m * 64, p1=m * 64 + 64))

            bufs = {}

            def load(ch):
                i = ld[0] % 3
                ld[0] += 1
                kb_, vb_ = kring[i], vring[i]
                k.dma('sp', kb_.v(), V(kTa_d[h // 2].ap[ch * 256 + (h % 2) * 128:ch * 256 + (h % 2) * 128 + 128, :], kTa_d[h // 2].v().keys))
                src = va_d[h].ap[ch * 2048:(ch + 1) * 2048, 0:128].rearrange("(t p) c -> p t c", p=128)
                k.dma('sp', V(vb_.v(0, 2048, pat="p (t c) -> p t c", c=128).ap, vb_.keys(0, 2048)), V(src, va_d[h].v().keys))
                bufs[ch] = (kb_, vb_)

            load(0)
            load(1)
            qk(0, bufs[0][0])
            qk(1, bufs[0][0])
            for kt in range(64):
                ch = kt // 16
                if kt % 16 == 0 and ch + 2 < 4:
                    load(ch + 2)
                if kt + 2 < 64:
                    qk(kt + 2, bufs[(kt + 2) // 16][0])
                pt = PT4[kt % 4]
                k.act(pt.v(), k.ps[SB[kt % 3]].v(), AF.Exp, scale=0.125)
                vb_ = bufs[ch][1]
                for m in range(2):
                    k.mm(OT.v(m * 512, m * 512 + 512), vb_.v((kt % 16) * 128, (kt % 16) * 128 + 128),
                         pt.v(m * 512, m * 512 + 512), start=(kt == 0), stop=(kt == 63))
                if kt % 4 == 1:
                    k.tt('dve', tsum.v(), PT4[(kt - 1) % 4].v(), pt.v(), ALU.add)
                elif kt % 4 == 3:
                    k.tt('dve', tsum.v(), tsum.v(), PT4[(kt - 1) % 4].v(), ALU.add)
                    k.tt('dve', tsum.v(), tsum.v(), pt.v(), ALU.add)
                    if kt == 3:
                        k.cp('dve', acc.v(), tsum.v())
                    else:
                        k.tt('dve', acc.v(), acc.v(), tsum.v(), ALU.add)
            k.cp('dve', tsum.v(), acc.v())
            for m in range(2):
                psd = k.ps[0].sub(m * 512, 512)
                k.mm(psd.v(), c['ones_b'].v(), tsum.v(m * 512, m * 512 + 512))
            k.recip(rsb.v(), k.ps[0].v(0, 512))
            k.recip(r2.v(), k.ps[0].v(512, 1024))
            k.tt('dve', dsb.v(), OT.v(0, 512), rsb.v(), ALU.mult)
            k.tt('dve', r2.v(), OT.v(512, 1024), r2.v(), ALU.mult)
            k.stt('dve', dsb.v(), r2.v(), lamcol.v(1, 2), dsb.v(), ALU.mult, ALU.add)
            sqA = PT4[0].sub(0, 512)
            k.act(sqA.v(), dsb.v(), AF.Square)
            pss_ = k.ps[1].sub(0, 512)
            k.mm(pss_.v(), c['ones_b'].v(), sqA.v())
            k.act(rsb.v(), pss_.v(), AF.Sqrt, scale=1.0 / 128, bias=EPS)
            k.recip(rsb.v(), rsb.v())
            k.stt('dve', oaT.v(h * T + qb * 512, h * T + qb * 512 + 512), dsb.v(), slcol.v(), rsb.v(), ALU.mult, ALU.mult)

    k.dma('sp', qTb.v(), qTb_d.v())
    kbb = [AB.sub(b0 + i * 4096, 4096) for i in range(2)]
    vtr = [AB.sub(b0 + 32768 + i * 260, 260) for i in range(8)]
    numT = fwork.sub(512, 2 * T)
    denT = fwork.sub(512 + 2 * T, T)
    Osb = fwork.sub(512 + 3 * T, 260)
    vld = [0]
    vtile_no = [0]
    U32 = mybir.dt.uint32
    idx_t = k.idx_t
    kidx = Buf("idx_t", idx_t, 4, 0, 24)
    vidx = Buf("idx_t", idx_t, 4, 24, 69)
    vmask = fwork.sub(7200, 69)
    k.dma('sp', vmask.v(), D['vmask'].v())
    kTb_view = [d_.ap.rearrange("r (h c) -> (r h) c", h=2) for d_ in kTb_d]
    vstg = [AB.sub(b0 + 32768 + 8 * 260 + i * 256, 256) for i in range(4)]

    def igather(outv, src_ap, src_d, idxv):
        k.S.add('pool', lambda e: e.indirect_dma_start(out=outv.ap, out_offset=None, in_=src_ap,
                                                       in_offset=bass.IndirectOffsetOnAxis(ap=idxv.ap.bitcast(U32), axis=0)),
                reads=src_d.v().keys + idxv.keys, writes=outv.keys, dma=True)
    for g, dil in enumerate([1, 4, 16]):
        for j in range(2):
            for seg in range(4):
                col = (g * 2 + j) * 4 + seg
                igather(kbb[j].v(seg * 1024, seg * 1024 + 1024), kTb_view[g], kTb_d[g], kidx.v(col, col + 1))
        ntile = 16 // dil
        for r in range(dil):
            vt = {}

            def vload(n):
                i = vld[0] % 8
                vld[0] += 1
                tno = vtile_no[0]
                vtile_no[0] += 1
                vs_ = vstg[tno % 4]
                igather(vs_.v(), vb_d[g].ap, vb_d[g], vidx.v(tno, tno + 1))
                k.ts('dve', V(vtr[i].v(0, 260, pat="p (h c) -> p h c", c=65).ap[:, :, 0:64], vtr[i].keys(0, 260)),
                     V(vs_.v(0, 256, pat="p (h c) -> p h c", c=64).ap, vs_.keys(0, 256)), vmask.v(tno, tno + 1), None, ALU.mult)
                k.ts('dve', V(vtr[i].v(0, 260, pat="p (h c) -> p h c", c=65).ap[:, :, 64], vtr[i].keys(0, 260)),
                     c['ones_b'].v(0, 4), vmask.v(tno, tno + 1), None, ALU.mult)
                vt[n] = vtr[i]

            vload(0)
            for m in range(ntile):
                vload(m + 1)
                pss = k.ps[m % 2]
                i0 = 128 * m * dil + r
                for hh in range(4):
                    j, half = hh // 2, hh % 2
                    for ab in range(2):
                        n = m + ab
                        w0 = PADW + (128 * n - 64) * dil + r
                        col = (hh * 2 + ab) * 128
                        k.mm(pss.v(col, col + 128), c['ident_b'].v(), c['maskA' if ab == 0 else 'maskB'].v(), start=True, stop=False)
                        k.mm(pss.v(col, col + 128),
                             kbb[j].v(w0, w0 + 128 * dil - (dil - 1), p0=half * 64, p1=half * 64 + 64, step=dil),
                             qTb.v((g * 2 + j) * T + i0, (g * 2 + j) * T + i0 + 128 * dil - (dil - 1), p0=half * 64, p1=half * 64 + 64, step=dil),
                             start=False, stop=True)
                pt = PT[m % 2]
                k.act(pt.v(), pss.v(), AF.Exp, scale=0.125)
                Ops = k.ps[2 + (m % 2)].sub(0, 260)
                for hh in range(4):
                    for ab in range(2):
                        col = (hh * 2 + ab) * 128
                        k.mm(Ops.v(hh * 65, hh * 65 + 65), pt.v(col, col + 128), vt[m + ab].v(hh * 65, hh * 65 + 65),
                             start=(ab == 0), stop=(ab == 1))
                k.cp('dve', V(Osb.v(0, 256, pat="p (h c) -> p h c", c=64).ap, Osb.keys(0, 256)),
                     V(Ops.v(0, 260, pat="p (h c) -> p h c", c=65).ap[:, :, 0:64], Ops.keys(0, 260)))
                k.cp('dve', Osb.v(256, 260), V(Ops.v(0, 260, pat="p (h c) -> p h c", c=65).ap[:, :, 64], Ops.keys(0, 260)))
                pT_ = k.ps[2 + (m % 2)]
                for j in range(2):
                    k.tr(pT_.v(512 + j * 128, 512 + j * 128 + 128), Osb.v(j * 128, j * 128 + 128), c['ident_f'].v())
                k.tr(pT_.v(768, 896, p0=0, p1=4), Osb.v(256, 260), c['ident_f'].v())
                for j in range(2):
                    dst = numT.v(j * T + i0, j * T + i0 + 128 * dil - (dil - 1), step=dil)
                    if g == 0:
                        k.cp('dve', dst, pT_.v(512 + j * 128, 512 + j * 128 + 128))
                    else:
                        k.tt('dve', dst, pT_.v(512 + j * 128, 512 + j * 128 + 128), dst, ALU.add)
                dst = denT.v(i0, i0 + 128 * dil - (dil - 1), p0=0, p1=4, step=dil)
                if g == 0:
                    k.cp('dve', dst, pT_.v(768, 896, p0=0, p1=4))
                else:
                    k.tt('dve', dst, pT_.v(768, 896, p0=0, p1=4), dst, ALU.add)
    ehd_t = fwork.sub(512 + 3 * T + 260, 256)
    k.dma('sp', ehd_t.v(p0=0, p1=4), ehd_d.v())
    k.recip(denT.v(p0=0, p1=4), denT.v(p0=0, p1=4))
    for j in range(2):
        for tb in range(4):
            psb = k.ps[tb % 2].sub(0, 512)
            k.mm(psb.v(), ehd_t.v(j * 128, j * 128 + 128, p0=0, p1=4), denT.v(tb * 512, tb * 512 + 512, p0=0, p1=4))
            k.tt('dve', obT.v(j * T + tb * 512, j * T + tb * 512 + 512), numT.v(j * T + tb * 512, j * T + tb * 512 + 512), psb.v(), ALU.mult)

    wo = AB.sub(b0 + 20480, 8192)
    wpa = AB.sub(b0 + 28672, 4096)
    gat = AB.sub(b0 + 32768, 8192)
    mrg = AB.sub(b0 + 40960, 4096)
    wpb = AB.sub(b0 + 45056, 2048)
    k.dma('pool', wo.v(), wo_d.v())
    k.dma('pool', wpa.v(), wpa_d.v())
    k.dma('pool', wpb.v(), wpb_d.v())
    m1 = fwork.sub(0, 512)
    for tb in range(4):
        for gi_ in range(2):
            src = gates_d.ap[:, gi_ * 8 * T: (gi_ + 1) * 8 * T].rearrange("p (c t) -> p c t", t=T)[:, :, tb * 512:(tb + 1) * 512]
            k.dma('sp', V(gat.v(gi_ * 4096, gi_ * 4096 + 4096, pat="p (c t) -> p c t", t=512).ap, gat.keys(gi_ * 4096, gi_ * 4096 + 4096)),
                  V(src, gates_d.v().keys))
        for cc in range(8):
            psa = k.ps[cc % 2].sub(0, 512)
            psb = k.ps[cc % 2].sub(512, 512)
            for kc in range(4):
                k.mm(psa.v(), wpa.v(kc * 1024 + cc * 128, kc * 1024 + cc * 128 + 128),
                     oaT.v(kc * T + tb * 512, kc * T + tb * 512 + 512), start=(kc == 0), stop=(kc == 3))
            for kc in range(2):
                k.mm(psb.v(), wpb.v(kc * 1024 + cc * 128, kc * 1024 + cc * 128 + 128),
                     obT.v(kc * T + tb * 512, kc * T + tb * 512 + 512), start=(kc == 0), stop=(kc == 1))
            k.tt('dve', m1.v(), psa.v(), gat.v(cc * 512, cc * 512 + 512), ALU.mult)
            k.tt('dve', mrg.v(cc * 512, cc * 512 + 512), psb.v(), gat.v(4096 + cc * 512, 4096 + cc * 512 + 512), ALU.mult)
            k.tt('pool', mrg.v(cc * 512, cc * 512 + 512), mrg.v(cc * 512, cc * 512 + 512), m1.v(), ALU.add)
        for cc in range(8):
            psy = k.ps[2 + cc % 2].sub(0, 512)
            for kc in range(8):
                k.mm(psy.v(), wo.v(kc * 1024 + cc * 128, kc * 1024 + cc * 128 + 128),
                     mrg.v(kc * 512, kc * 512 + 512), start=(kc == 0), stop=(kc == 7))
            xs = xT.v(cc * T + tb * 512, cc * T + tb * 512 + 512)
            k.stt('dve', xs, psy.v(), mod.v(16 + cc, 17 + cc), xs, ALU.mult, ALU.add)

    hT = AB.sub(b0, 8 * T)
    wring = [AB.sub(b0 + 16384 + i * 6144, 6144) for i in range(4)]
    hid = [AB.sub(b0 + 40960 + i * 4096, 4096) for i in range(2)]
    wb2 = AB.sub(b0 + 49152, 1024)
    h32b = fwork.sub(2048, 4096)
    lg = fwork.sub(6144, 36 * 16)
    comb = fwork.sub(6144 + 576, 32 * 16)
    rw = fwork.sub(6144 + 576 + 512, 96)

    def hook(tb, cc):
        if cc is not None:
            return h32b.sub(cc * 512, 512)
        for q in range(4):
            tt_ = tb * 4 + q
            psl = k.ps[2 + q % 2].sub(0, 36)
            for kc in range(8):
                k.mm(psl.v(), h32b.v(kc * 512 + q * 128, kc * 512 + q * 128 + 128), wr.v(kc * 36, kc * 36 + 36),
                     start=(kc == 0), stop=(kc == 7))
            k.tt('dve', lg.v(tt_ * 36, tt_ * 36 + 36), psl.v(), br.v(), ALU.add)
        return None

    rms_mod(k, xT, hT, c, sc2, mod.sub(24, 8), fwork.sub(0, 2048), wb2, h32_hook=hook)

    Rb = fwork.sub(0, 2048)
    m1 = Rb.sub(0, 16); e1 = Rb.sub(16, 64); s1 = Rb.sub(80, 16); gval = Rb.sub(96, 16); oh = Rb.sub(112, 64)
    ig = Rb.sub(176, 128); tmpg = Rb.sub(304, 128); v1 = Rb.sub(432, 16); mk1 = Rb.sub(448, 128); ig2 = Rb.sub(576, 128)
    v2 = Rb.sub(704, 16); mk2 = Rb.sub(720, 128); dd = Rb.sub(848, 16); ee = Rb.sub(864, 16); w1 = Rb.sub(880, 16)
    w2 = Rb.sub(896, 16); cig = Rb.sub(912, 128); tmp2 = Rb.sub(1040, 128)

    def v3(buf, n):
        return V(buf.v(0, 16 * n, pat="p (t e) -> p t e", e=n).ap, buf.keys(0, 16 * n))

    def bc(buf, n):
        return V(buf.v(0, 16).ap.unsqueeze(2).to_broadcast([128, 16, n]), buf.keys(0, 16))

    def lgv(a_, b_):
        return V(lg.v(0, 576, pat="p (t e) -> p t e", e=36).ap[:, :, a_:b_], lg.keys(0, 576))

    def oh_g(g):
        return V(oh.v(0, 64, pat="p (t e) -> p t e", e=4).ap[:, :, g:g + 1].to_broadcast([128, 16, 8]), oh.keys(0, 64))

    def comb_g(g):
        return V(comb.v(0, 512, pat="p (t g e) -> p t g e", g=4, e=8).ap[:, :, g, :], comb.keys(0, 512))

    k.red('dve', m1.v(), lgv(0, 4), ALU.max)
    k.tt('dve', v3(e1, 4), lgv(0, 4), bc(m1, 4), ALU.subtract)
    k.act(e1.v(), e1.v(), AF.Exp)
    k.red('dve', s1.v(), v3(e1, 4), ALU.add)
    k.recip(gval.v(), s1.v())
    k.tt('dve', v3(oh, 4), lgv(0, 4), bc(m1, 4), ALU.is_equal)
    for g in range(4):
        dst = ig if g == 0 else tmpg
        k.tt('dve', v3(dst, 8), lgv(4 + 8 * g, 12 + 8 * g), oh_g(g), ALU.mult)
        if g > 0:
            k.tt('dve', ig.v(), ig.v(), tmpg.v(), ALU.add)
    k.red('dve', v1.v(), v3(ig, 8), ALU.max)
    k.tt('dve', v3(mk1, 8), v3(ig, 8), bc(v1, 8), ALU.is_equal)
    k.stt('dve', ig2.v(), mk1.v(), -1e30, ig.v(), ALU.mult, ALU.add)
    k.red('dve', v2.v(), v3(ig2, 8), ALU.max)
    k.tt('dve', v3(mk2, 8), v3(ig2, 8), bc(v2, 8), ALU.is_equal)
    k.tt('dve', dd.v(), v1.v(), v2.v(), ALU.subtract)
    k.act(ee.v(), dd.v(), AF.Exp, scale=-1.0)
    k.ts('dve', w1.v(), ee.v(), 1.0, None, ALU.add)
    k.recip(w1.v(), w1.v())
    k.tt('dve', w2.v(), ee.v(), w1.v(), ALU.mult)
    k.tt('dve', w1.v(), w1.v(), gval.v(), ALU.mult)
    k.tt('dve', w2.v(), w2.v(), gval.v(), ALU.mult)
    k.tt('dve', v3(cig, 8), v3(mk1, 8), bc(w1, 8), ALU.mult)
    k.tt('dve', v3(tmp2, 8), v3(mk2, 8), bc(w2, 8), ALU.mult)
    k.tt('dve', cig.v(), cig.v(), tmp2.v(), ALU.add)
    for g in range(4):
        k.tt('dve', comb_g(g), v3(cig, 8), oh_g(g), ALU.mult)
    CTt = fwork.sub(0, 2048)
    for tt_ in range(16):
        pst = k.ps[2 + tt_ % 2].sub(512, 128)
        k.tr(pst.v(p0=0, p1=32), comb.v(tt_ * 32, tt_ * 32 + 32), c['ident_f'].v())
        k.cp('dve', CTt.v(tt_ * 128, tt_ * 128 + 128, p0=0, p1=32), pst.v(p0=0, p1=32))
    selt = fwork.sub(2048, 4096)
    k.dma('sp', selt.v(p0=0, p1=32), sel_d.v())

    G = 2
    sg = [fwork.sub(6144 + i * 512, 512) for i in range(2)]

    def wload(e):
        k.dma('pool', wring[e % 4].v(), V(we_d.ap[e], we_d.v(key=e).keys))

    for e in range(4):
        wload(e)
    hcnt = [0]
    for eg in range(NEXP // G):
        if eg >= 1 and (eg + 1) * G < NEXP:
            for ei in range(G):
                wload((eg + 1) * G + ei)
        for tb in range(4):
            hb = hid[hcnt[0] % 2]
            hcnt[0] += 1
            for ei in range(G):
                e = eg * G + ei
                w = wring[e % 4]
                psc = k.ps[3].sub(512, 512)
                k.mm(psc.v(), selt.v(e * 128, e * 128 + 128, p0=0, p1=32), CTt.v(tb * 512, tb * 512 + 512, p0=0, p1=32))
                for ch in range(2):
                    psg = k.ps[ch].sub(0, 512)
                    psu = k.ps[ch].sub(512, 512)
                    for kc in range(8):
                        k.mm(psg.v(), w.v(kc * 256 + ch * 128, kc * 256 + ch * 128 + 128),
                             hT.v(kc * T + tb * 512, kc * T + tb * 512 + 512), start=(kc == 0), stop=(kc == 7))
                    for kc in range(8):
                        k.mm(psu.v(), w.v(2048 + kc * 256 + ch * 128, 2048 + kc * 256 + ch * 128 + 128),
                             hT.v(kc * T + tb * 512, kc * T + tb * 512 + 512), start=(kc == 0), stop=(kc == 7))
                    s_ = sg[ch]
                    k.act(s_.v(), psg.v(), AF.Silu)
                    k.tt('dve', s_.v(), psu.v(), s_.v(), ALU.mult)
                    k.tt('dve', hb.v((ei * 2 + ch) * 512, (ei * 2 + ch) * 512 + 512), psc.v(), s_.v(), ALU.mult)
            for cc in range(8):
                psy = k.ps[2].sub((cc % 2) * 512, 512)
                for ei in range(G):
                    w = wring[(eg * G + ei) % 4]
                    for ch in range(2):
                        k.mm(psy.v(), w.v(4096 + ch * 1024 + cc * 128, 4096 + ch * 1024 + cc * 128 + 128),
                             hb.v((ei * 2 + ch) * 512, (ei * 2 + ch) * 512 + 512),
                             start=(ei == 0 and ch == 0), stop=(ei == G - 1 and ch == 1))
                xs = xT.v(cc * T + tb * 512, cc * T + tb * 512 + 512)
                k.stt('dve', xs, psy.v(), mod.v(40 + cc, 41 + cc), xs, ALU.mult, ALU.add)
    return


def build_F():
    k = K(nf32=25600, nbf16=51200)
    nc = k.nc
    D = {}

    def ext(name, shape, dt):
        D[name] = k.din(name, shape, dt)

    def itn(name, shape, dt):
        D[name] = DBuf(name, nc.dram_tensor(name, list(shape), dt).ap())

    ext('xT', [128, 8 * T], F32); ext('c_col', [128, 8], F32); ext('posb', [128, T], I32); ext('invf', [128, 1], F32)
    ext('sel', [32, 32 * 128], F32); ext('ehd', [4, 256], F32); ext('idx', [128, 93], I32); ext('vmask', [128, 69], F32)
    for l in range(2):
        ext('w_ada%d' % l, [6, 128, 8 * 1024], F32); ext('b_ada%d' % l, [128, 48], F32); ext('gains%d' % l, [128, 4], F32)
        for n, nc_ in WIN_GROUPS:
            ext('w_%s%d' % (n, l), [128, 8 * nc_], F32)
        ext('lamp%d' % l, [128, 258], F32); ext('subln%d' % l, [128, 128], F32)
        ext('w_pa%d' % l, [128, 4096], F32); ext('w_pb%d' % l, [128, 2048], F32); ext('w_o%d' % l, [128, 8192], F32)
        ext('w_r%d' % l, [128, 288], F32); ext('b_r%d' % l, [128, 36], F32); ext('w_e%d' % l, [NEXP, 128, 6144], F32)
    def pieces(nm, n, ls, as_):
        D[nm + '_loc'] = [DBuf('%s_loc%d' % (nm, i), nc.dram_tensor('%s_loc%d' % (nm, i), ls, BF16).ap()) for i in range(n)]
        D[nm + '_all'] = [DBuf('%s_all%d' % (nm, i), nc.dram_tensor('%s_all%d' % (nm, i), as_, BF16).ap()) for i in range(n)]
    pieces('kTa', 2, [256, 2048], [1024, 2048])
    pieces('va', 4, [T, 129], [SEQ, 129])
    pieces('kTb', 3, [256, 2048], [1024, 2048])
    pieces('vb', 3, [T, 256], [SEQ, 256])
    itn('qTa_s', [128, 4 * T], BF16); itn('qTb_s', [128, 6 * T], BF16)
    itn('gates_s', [128, 16 * T], BF16); itn('mod_s', [128, 48], F32)
    out_d = k.dout('outT', [128, 8 * T], F32)
    c = load_consts(k, True)
    xT = k.fa(8 * T)
    k.idx_t = k.es.enter_context(nc.sbuf_tensor("idx_t", [128, 93], I32))
    idxb = Buf("idx_t", k.idx_t, 4, 0, 93)
    k.dma('sp', idxb.v(), D['idx'].v())
    k.dma('sp', xT.v(), D['xT'].v())
    base = (k.fo, k.bo)
    rg = [[0, 1, 2, 3], [4, 5, 6, 7]]
    for l in range(2):
        k.fo, k.bo = base
        def after_kv():
            for nm, i_ in [('kTa', 0), ('va', 0), ('va', 1), ('kTa', 1), ('va', 2), ('va', 3),
                           ('kTb', 0), ('kTb', 1), ('kTb', 2), ('vb', 0), ('vb', 1), ('vb', 2)]:
                loc, al = D[nm + '_loc'][i_], D[nm + '_all'][i_]
                k.S.add('pool', lambda e, loc=loc, al=al: e.collective_compute(
                    "AllGather", ALU.bypass, replica_groups=rg, ins=[loc.ap.opt()], outs=[al.ap.opt()]),
                    reads=loc.v().keys, writes=al.v().keys, dma=True, cc=True)
        k.after_kv = after_kv
        body_A(k, c, xT, D, l)
        after_kv()
        k.fo, k.bo = base
        body_B(k, c, xT, D, l)
    k.dma('sp', out_d.v(), xT.v())
    return k.finalize()


_cache = {}


def _fm(w):
    K_, N = w.shape
    return np.ascontiguousarray(w.reshape(K_ // 128, 128, N).transpose(1, 0, 2).reshape(128, (K_ // 128) * N))


def _index_tables(jr):
    p = np.arange(128)
    idx = np.zeros((128, 93), np.int64)
    vmask = np.zeros((128, 69), np.float32)
    for jp in range(6):
        for seg in range(4):
            rank = [jr - 1, jr, jr, jr + 1][seg]
            half = [1, 0, 1, 0][seg]
            rank = min(max(rank, 0), 3)
            idx[:, jp * 4 + seg] = rank * 512 + ((jp % 2) * 128 + p) * 2 + half
    t = 0
    for g, dil in enumerate([1, 4, 16]):
        for r in range(dil):
            for n in range(16 // dil + 1):
                a_k = 128 * n - 64 + p
                gpos = jr * T + a_k * dil + r
                valid = (gpos >= 0) & (gpos < SEQ)
                gp = np.where(valid, gpos, 0)
                idx[:, 24 + t] = gp
                vmask[:, t] = valid.astype(np.float32)
                t += 1
    assert t == 69
    return idx.astype(np.int32), vmask


def kernel(x, c, positions, w_ada, b_ada, w_in, qn_a, kn_a, lam_q1, lam_k1, lam_q2, lam_k2,
           subln_a, qn_b, kn_b, w_pa, w_pb, w_o, w_r1, b_r1, w_r2, b_r2, w_e_gate, w_e_up, w_e_down):
    x = np.asarray(x, np.float32)
    consts = host_consts()
    invf = (np.float32(10000.0) ** (-np.arange(0, 64, 2, dtype=np.float32) / np.float32(64))).astype(np.float32)
    invf_col = invf[(np.arange(128) % 64) % 32].reshape(128, 1).astype(np.float32)
    cores = list(range(NCORES))
    sel = np.zeros((32, 32 * 128), np.float32)
    for e in range(32):
        sel[e, e * 128:(e + 1) * 128] = 1.0
    ehd = np.zeros((4, 256), np.float32)
    for h in range(4):
        j, half = h // 2, h % 2
        ehd[h, j * 128 + half * 64: j * 128 + half * 64 + 64] = 1.0
    cuts = np.cumsum([0, 512, 512, 512, 768, 768, 768, 1024, 1024])
    names = ['qa', 'ka', 'va', 'qb', 'kb', 'vb', 'ga', 'gb']
    shared = {"consts": consts, "invf": invf_col, "sel": sel, "ehd": ehd}
    pidx = np.arange(128) % 64
    for l in range(2):
        wl = np.asarray(w_in[l], np.float32)
        for i, n in enumerate(names):
            shared["w_%s%d" % (n, l)] = _fm(wl[:, cuts[i]:cuts[i + 1]])
        shared["w_ada%d" % l] = np.ascontiguousarray(
            np.asarray(w_ada[l], np.float32).reshape(8, 128, 6, 1024).transpose(2, 1, 0, 3).reshape(6, 128, 8 * 1024))
        shared["b_ada%d" % l] = np.ascontiguousarray(np.asarray(b_ada[l], np.float32).reshape(48, 128).T)
        shared["gains%d" % l] = np.stack([np.asarray(qn_a[l])[pidx], np.asarray(kn_a[l])[pidx],
                                          np.asarray(qn_b[l])[pidx], np.asarray(kn_b[l])[pidx]], axis=1).astype(np.float32)
        lam_init = 0.8 - 0.6 * math.exp(-0.3 * l)
        lamp = np.concatenate([np.asarray(lam_q1[l]), np.asarray(lam_k1[l]), np.asarray(lam_q2[l]), np.asarray(lam_k2[l]),
                               np.array([lam_init, 1.0 - lam_init])]).astype(np.float32)
        shared["lamp%d" % l] = np.ascontiguousarray(np.broadcast_to(lamp[None, :], (128, 258)))
        shared["subln%d" % l] = np.ascontiguousarray(np.broadcast_to(np.asarray(subln_a[l], np.float32)[:, None], (128, 128)))
        shared["w_r%d" % l] = _fm(np.concatenate([np.asarray(w_r1[l], np.float32), np.asarray(w_r2[l], np.float32)], axis=1))
        shared["b_r%d" % l] = np.ascontiguousarray(np.broadcast_to(
            np.concatenate([np.asarray(b_r1[l]), np.asarray(b_r2[l])]).astype(np.float32)[None, :], (128, 36)))
        we = np.empty((NEXP, 128, 6144), np.float32)
        for e in range(NEXP):
            we[e, :, 0:2048] = _fm(np.asarray(w_e_gate[l, e], np.float32))
            we[e, :, 2048:4096] = _fm(np.asarray(w_e_up[l, e], np.float32))
            we[e, :, 4096:6144] = _fm(np.asarray(w_e_down[l, e], np.float32))
        shared["w_e%d" % l] = we
        shared["w_pa%d" % l] = _fm(np.asarray(w_pa[l], np.float32))
        shared["w_pb%d" % l] = _fm(np.asarray(w_pb[l], np.float32))
        shared["w_o%d" % l] = _fm(np.asarray(w_o[l], np.float32))
    in_maps = []
    for ci in cores:
        b, j = ci // 4, ci % 4
        xs = x[b, j * T:(j + 1) * T, :]
        idx, vmask = _index_tables(j)
        m = dict(shared)
        m["xT"] = np.ascontiguousarray(xs.T.reshape(8, 128, T).transpose(1, 0, 2).reshape(128, 8 * T))
        m["c_col"] = np.ascontiguousarray(np.asarray(c[b], np.float32).reshape(8, 128).T)
        m["posb"] = np.ascontiguousarray(np.broadcast_to(np.asarray(positions[b, j * T:(j + 1) * T], np.int32)[None, :], (128, T)))
        m["idx"] = idx
        m["vmask"] = vmask
        in_maps.append(m)
    if 'F' not in _cache:
        _cache['F'] = build_F()
    res = run_bass_kernel_spmd(_cache['F'], in_maps, core_ids=cores).results
    out = np.empty((2, SEQ, D), np.float32)
    for ci in cores:
        b, j = ci // 4, ci % 4
        o = np.asarray(res[ci]["outT"], np.float32)
        out[b, j * T:(j + 1) * T, :] = o.reshape(128, 8, T).transpose(1, 0, 2).reshape(D, T).T
    return out
```

```python
import math
import numpy as np
from contextlib import ExitStack
import concourse.bass as bass
import concourse.mybir as mybir
from concourse.bass_utils import run_bass_kernel_spmd

F32 = mybir.dt.float32
BF16 = mybir.dt.bfloat16
I32 = mybir.dt.int32
ALU = mybir.AluOpType
AF = mybir.ActivationFunctionType
AX = mybir.AxisListType

ENGS = ['pe', 'act', 'dve', 'pool', 'sp']
CHUNK = 1024

NCORES = 8
T = 2048
SEQ = 8192
D = 1024
EPS = 1e-6
NEG = -30000.0
WIN = 4096
PADW = 1024
NEXP = 32


class Op:
    __slots__ = ('eng', 'fn', 'src', 'pos', 'signal', 'waits', 'know', 'is_dma', 'cnt')


class V:
    __slots__ = ('ap', 'keys')

    def __init__(self, ap, keys):
        self.ap = ap
        self.keys = keys


class Buf:
    def __init__(self, arena_name, handle, esz, off, n):
        self.an = arena_name
        self.t = handle
        self.esz = esz
        self.off = off
        self.n = n

    def keys(self, a, b):
        ch = 2048 if self.an.startswith('ps') else CHUNK
        lo = ((self.off + a) * self.esz) // ch
        hi = ((self.off + b) * self.esz - 1) // ch
        return [(self.an, i) for i in range(lo, hi + 1)]

    def v(self, a=0, b=None, p0=0, p1=128, step=1, pat=None, **kw):
        if b is None:
            b = self.n
        assert 0 <= a < b <= self.n, (a, b, self.n)
        if step == 1:
            ap = self.t[p0:p1, self.off + a:self.off + b]
        else:
            ap = self.t[p0:p1, self.off + a:self.off + b:step]
        if pat is not None:
            ap = ap.rearrange(pat, **kw)
        return V(ap, self.keys(a, b))

    def sub(self, off, n):
        assert off + n <= self.n
        return Buf(self.an, self.t, self.esz, self.off + off, n)


class DBuf:
    def __init__(self, name, ap):
        self.name = name
        self.ap = ap

    def v(self, ap=None, key=None):
        return V(self.ap if ap is None else ap, [(self.name, key)])


class Sched:
    def __init__(self, nc, n_dma_sems=32):
        self.nc = nc
        self.ops = {e: [] for e in ENGS}
        self.ncomp = {e: 0 for e in ENGS}
        self.last_w = {}
        self.readers = {}
        self.known = {e: {} for e in ENGS}
        self.n_dma_sems = n_dma_sems
        self.dma_last = [None] * n_dma_sems
        self.dma_cnt = [0] * n_dma_sems
        self.dma_rr = 0
        self.dma_rr_sw = 0
        self.cc_cnt = 0
        self.cc_last = None

    def _need(self, e, d):
        k = self.known[e]
        if k.get(d.src, -1) >= d.pos:
            return None
        d.signal = True
        for s, p in d.know.items():
            if k.get(s, -1) < p:
                k[s] = p
        return d

    def add(self, eng, fn, reads=(), writes=(), dma=False, cc=False):
        op = Op()
        op.eng = eng
        op.fn = fn
        op.is_dma = dma
        op.signal = dma
        deps = []
        seen = set()
        pr_ = [k for k in reads if isinstance(k[0], str) and k[0].startswith('ps')]
        if pr_:
            writes = list(writes) + pr_
        for k in reads:
            w = self.last_w.get(k)
            if w is not None and id(w) not in seen:
                seen.add(id(w)); deps.append(w)
        for k in writes:
            w = self.last_w.get(k)
            if w is not None and id(w) not in seen:
                seen.add(id(w)); deps.append(w)
            for r in self.readers.get(k, ()):
                if id(r) not in seen:
                    seen.add(id(r)); deps.append(r)
        if cc:
            op.src = ('cc', 0)
            op.pos = self.cc_cnt
            self.cc_cnt += 1
            if self.cc_last is not None and id(self.cc_last) not in seen:
                seen.add(id(self.cc_last)); deps.append(self.cc_last)
            self.cc_last = op
        elif dma:
            half = self.n_dma_sems // 2
            if eng == 'pool':
                slot = half + self.dma_rr_sw
                self.dma_rr_sw = (self.dma_rr_sw + 1) % (self.n_dma_sems - half)
            else:
                slot = self.dma_rr
                self.dma_rr = (self.dma_rr + 1) % half
            prev = self.dma_last[slot]
            if prev is not None and id(prev) not in seen:
                seen.add(id(prev)); deps.append(prev)
            op.src = ('dma', slot)
            op.pos = self.dma_cnt[slot]
            self.dma_cnt[slot] += 1
            self.dma_last[slot] = op
        else:
            op.src = eng
            op.pos = self.ncomp[eng]
            self.ncomp[eng] += 1
        waits = []
        deps.sort(key=lambda d: -d.pos)
        for d in deps:
            if (not dma) and (not d.is_dma) and d.src == eng:
                if eng == 'pe':
                    continue
                if op.pos - d.pos > 2:
                    continue
            w = self._need(eng, d)
            if w is not None:
                waits.append(w)
        op.waits = waits
        know = dict(self.known[eng])
        know[op.src] = op.pos
        op.know = know
        for k in reads:
            self.readers.setdefault(k, []).append(op)
        for k in writes:
            self.last_w[k] = op
            self.readers[k] = []
        self.ops[eng].append(op)
        return op

    def finish(self):
        op = Op()
        op.eng = 'sp'; op.fn = None; op.is_dma = False; op.signal = False
        op.src = 'sp'; op.pos = self.ncomp['sp']; self.ncomp['sp'] += 1
        waits = []
        for d in list(self.dma_last) + [self.cc_last]:
            if d is not None:
                w = self._need('sp', d)
                if w is not None:
                    waits.append(w)
        for e in ['pe', 'act', 'dve', 'pool']:
            comp = [o for o in self.ops[e] if not o.is_dma]
            if comp:
                w = self._need('sp', comp[-1])
                if w is not None:
                    waits.append(w)
        op.waits = waits
        op.know = {}
        self.ops['sp'].append(op)

    def emit(self, sems):
        nc = self.nc
        for e in ENGS:
            c = 0
            for o in self.ops[e]:
                if o.is_dma:
                    continue
                if o.signal:
                    c += 1
                o.cnt = c
        engobj = {'pe': nc.tensor, 'act': nc.scalar, 'dve': nc.vector, 'pool': nc.gpsimd, 'sp': nc.sync}

        def run(e):
            eo = engobj[e]
            for o in self.ops[e]:
                for d in o.waits:
                    if d.is_dma and d.src[0] == 'cc':
                        eo.wait_ge(sems[d.src], d.pos + 1)
                    elif d.is_dma:
                        eo.wait_ge(sems[d.src], 16 * (d.pos + 1))
                    else:
                        eo.wait_ge(sems[d.src], d.cnt)
                if o.fn is None:
                    continue
                ins = o.fn(eo)
                if o.is_dma and o.src[0] == 'cc':
                    ins.then_inc(sems[o.src])
                elif o.is_dma:
                    ins.then_inc(sems[o.src], 16)
                elif o.signal:
                    ins.then_inc(sems[e], 1)
        return run


class K:
    def __init__(self, nf32, nbf16):
        self.nc = bass.Bass("TRN2", target_bir_lowering=False)
        self.es = ExitStack()
        nc = self.nc
        self.S = Sched(nc)
        es = self.es
        self.af_t = es.enter_context(nc.sbuf_tensor("arena_f", [128, nf32], F32))
        self.ab_t = es.enter_context(nc.sbuf_tensor("arena_b", [128, nbf16], BF16))
        self.AF_ = Buf("af", self.af_t, 4, 0, nf32)
        self.AB_ = Buf("ab", self.ab_t, 2, 0, nbf16)
        self.ps = []
        for i in range(4):
            t = es.enter_context(nc.psum_tensor("ps%d" % i, [128, 1024], F32))
            self.ps.append(Buf("ps%d" % i, t, 4, 0, 1024))
        self.sems = {}
        for e in ENGS:
            self.sems[e] = es.enter_context(nc.semaphore("s_" + e))
        for i in range(self.S.n_dma_sems):
            self.sems[('dma', i)] = es.enter_context(nc.semaphore("d%d" % i))
        self.sems[('cc', 0)] = es.enter_context(nc.semaphore("ccsem"))
        self.fo = 0
        self.bo = 0
        self.dram = {}

    def din(self, name, shape, dt):
        ap = self.nc.dram_tensor(name, list(shape), dt, kind="ExternalInput").ap()
        d = DBuf(name, ap)
        self.dram[name] = d
        return d

    def dout(self, name, shape, dt):
        ap = self.nc.dram_tensor(name, list(shape), dt, kind="ExternalOutput").ap()
        d = DBuf(name, ap)
        self.dram[name] = d
        return d

    def fa(self, n):
        b = self.AF_.sub(self.fo, n)
        self.fo += n
        return b

    def ba(self, n):
        b = self.AB_.sub(self.bo, n)
        self.bo += n
        return b

    def mm(self, out, lhsT, rhs, start=True, stop=True):
        self.S.add('pe', lambda e: e.matmul(out.ap, lhsT.ap, rhs.ap, start=start, stop=stop),
                   reads=lhsT.keys + rhs.keys, writes=out.keys)

    def tr(self, out, in_, ident):
        self.S.add('pe', lambda e: e.transpose(out.ap, in_.ap, ident.ap),
                   reads=in_.keys + ident.keys, writes=out.keys)

    def act(self, out, in_, func, scale=1.0, bias=0.0, accum=None):
        reads = list(in_.keys)
        kw = {}
        if isinstance(bias, V):
            reads += bias.keys
            kw['bias'] = bias.ap
        else:
            kw['bias'] = float(bias)
        if isinstance(scale, V):
            reads += scale.keys
            kw['scale'] = scale.ap
        else:
            kw['scale'] = float(scale)
        writes = list(out.keys)
        if accum is not None:
            writes += accum.keys
            kw['accum_out'] = accum.ap
        self.S.add('act', lambda e: e.activation(out=out.ap, in_=in_.ap, func=func, **kw),
                   reads=reads, writes=writes)

    def tt(self, eng, out, in0, in1, op):
        self.S.add(eng, lambda e: e.tensor_tensor(out=out.ap, in0=in0.ap, in1=in1.ap, op=op),
                   reads=in0.keys + in1.keys, writes=out.keys)

    def ts(self, eng, out, in0, s1, s2=None, op0=ALU.mult, op1=None):
        reads = list(in0.keys)
        a1 = s1
        a2 = s2
        if isinstance(s1, V):
            reads += s1.keys; a1 = s1.ap
        if isinstance(s2, V):
            reads += s2.keys; a2 = s2.ap
        if op1 is None:
            self.S.add(eng, lambda e: e.tensor_scalar(out=out.ap, in0=in0.ap, scalar1=a1, scalar2=None, op0=op0),
                       reads=reads, writes=out.keys)
        else:
            self.S.add(eng, lambda e: e.tensor_scalar(out=out.ap, in0=in0.ap, scalar1=a1, scalar2=a2, op0=op0, op1=op1),
                       reads=reads, writes=out.keys)

    def stt(self, eng, out, in0, scalar, in1, op0, op1):
        reads = in0.keys + in1.keys
        a = scalar
        if isinstance(scalar, V):
            reads = reads + scalar.keys; a = scalar.ap
        self.S.add(eng, lambda e: e.scalar_tensor_tensor(out=out.ap, in0=in0.ap, scalar=a, in1=in1.ap, op0=op0, op1=op1),
                   reads=reads, writes=out.keys)

    def cp(self, eng, out, in_):
        if eng == 'act':
            self.S.add('act', lambda e: e.copy(out=out.ap, in_=in_.ap), reads=in_.keys, writes=out.keys)
        else:
            self.S.add(eng, lambda e: e.tensor_copy(out=out.ap, in_=in_.ap), reads=in_.keys, writes=out.keys)

    def red(self, eng, out, in_, op, axis=AX.X):
        self.S.add(eng, lambda e: e.tensor_reduce(out=out.ap, in_=in_.ap, axis=axis, op=op),
                   reads=in_.keys, writes=out.keys)

    def recip(self, out, in_):
        self.S.add('dve', lambda e: e.reciprocal(out=out.ap, in_=in_.ap), reads=in_.keys, writes=out.keys)

    def memset(self, eng, out, val):
        self.S.add(eng, lambda e: e.memset(out.ap, val), writes=out.keys)

    def dma(self, eng, out, in_):
        self.S.add(eng, lambda e: e.dma_start(out=out.ap, in_=in_.ap), reads=in_.keys, writes=out.keys, dma=True)

    def finalize(self):
        S = self.S
        S.finish()
        run = S.emit(self.sems)
        with self.nc.Block() as block:
            @block.tensor
            def _(e): run('pe')
            @block.scalar
            def _(e): run('act')
            @block.vector
            def _(e): run('dve')
            @block.gpsimd
            def _(e): run('pool')
            @block.sync
            def _(e): run('sp')
        self.es.close()
        return self.nc


def load_consts(k, need_rope):
    c = {}
    cin = k.din("consts", [128, 5 * 128], F32)
    cf = k.fa(256).sub(0, 128)
    k.dma('sp', cf.v(), V(cin.ap[:, 0:128], cin.v().keys))
    c['ident_f'] = cf
    cb = k.ba(5 * 128)
    k.dma('pool', cb.v(), cin.v())
    c['ident_b'] = cb.sub(0, 128)
    c['onesbd'] = cb.sub(128, 128)
    c['rotT'] = cb.sub(256, 128)
    c['maskA'] = cb.sub(384, 128)
    c['maskB'] = cb.sub(512, 128)
    ones = k.ba(128)
    k.ba(256)
    k.memset('pool', ones.v(), 1.0)
    c['ones_b'] = ones
    return c


def host_consts():
    ident = np.eye(128, dtype=np.float32)
    onesbd = np.zeros((128, 128), np.float32)
    onesbd[:64, :64] = 1.0
    onesbd[64:, 64:] = 1.0
    rotT = np.zeros((128, 128), np.float32)
    for m in range(128):
        j = m % 64
        if j < 32:
            rotT[m + 32, m] = -1.0
        else:
            rotT[m - 32, m] = 1.0
    u = np.arange(128)[:, None]
    a = np.arange(128)[None, :]
    maskA = np.where(u >= a, 0.0, NEG).astype(np.float32)
    maskB = np.where(u <= a, 0.0, NEG).astype(np.float32)
    return np.concatenate([ident, onesbd, rotT, maskA, maskB], axis=1)


TWO_PI = 2.0 * math.pi
C1 = 6.28125
C2 = float(np.float32(TWO_PI - 6.28125))
C3 = float(TWO_PI - 6.28125 - float(np.float32(TWO_PI - 6.28125)))


WIN_GROUPS = [('ka', 512), ('kb', 768), ('va', 512), ('vb', 768), ('qa', 512), ('qb', 768), ('ga', 1024), ('gb', 1024)]


def rms_mod(k, xT, hT, c, scale_col, shift_col, work_f, work_b, h32_hook=None):
    sq = [work_b.sub(i * 512, 512) for i in range(2)]
    sd = work_f.sub(0, 512)
    rs = work_f.sub(512, 512)
    tmp = [work_f.sub(1024 + i * 512, 512) for i in range(2)]
    for tb in range(4):
        pss = k.ps[tb % 2].sub(0, 512)
        for cc in range(8):
            s = sq[cc % 2]
            k.act(s.v(), xT.v(cc * T + tb * 512, cc * T + tb * 512 + 512), AF.Square)
            k.mm(pss.v(), c['ones_b'].v(), s.v(), start=(cc == 0), stop=(cc == 7))
        k.act(sd.v(), pss.v(), AF.Sqrt, scale=1.0 / D, bias=EPS)
        k.recip(rs.v(), sd.v())
        for cc in range(8):
            t = tmp[cc % 2]
            k.tt('pool', t.v(), xT.v(cc * T + tb * 512, cc * T + tb * 512 + 512), rs.v(), ALU.mult)
            if h32_hook is not None:
                h32 = h32_hook(tb, cc)
                k.ts('dve', h32.v(), t.v(), scale_col.v(cc, cc + 1), shift_col.v(cc, cc + 1), ALU.mult, ALU.add)
                k.cp('pool', hT.v(cc * T + tb * 512, cc * T + tb * 512 + 512), h32.v())
            else:
                k.ts('dve', hT.v(cc * T + tb * 512, cc * T + tb * 512 + 512), t.v(),
                     scale_col.v(cc, cc + 1), shift_col.v(cc, cc + 1), ALU.mult, ALU.add)
        if h32_hook is not None:
            h32_hook(tb, None)


def body_A(k, c, xT, D, l):
    stg = 99
    ccol_d = D['c_col']
    pos_d = D['posb']
    invf_d = D['invf']
    wada_d = D['w_ada%d' % l]
    bada_d = D['b_ada%d' % l]
    gains_d = D['gains%d' % l]
    wg_d = {n: D['w_%s%d' % (n, l)] for n, nc_ in WIN_GROUPS}
    kTa_o = D['kTa_loc']
    kTb_o = D['kTb_loc']
    qTa_o = D['qTa_s']
    qTb_o = D['qTb_s']
    va_o = D['va_loc']
    vb_o = D['vb_loc']
    gates_o = D['gates_s']
    mod_o = D['mod_s']

    cosT = k.fa(T)
    sinT = k.fa(T)
    small = k.fa(256)
    work_f = k.fa(4096)
    hT = k.ba(8 * T)
    wring = [k.ba(8192) for _ in range(3)]
    work_b = k.ba(2048)
    stage = [k.ba(2048) for _ in range(2)]
    vst = [k.ba(1024) for _ in range(2)]

    ccol = small.sub(0, 8)
    cact = k.ba(8)
    bada = small.sub(8, 48)
    mod = small.sub(56, 48)
    gains = small.sub(104, 4)
    invf = small.sub(108, 1)

    k.dma('sp', ccol.v(), ccol_d.v())
    k.dma('sp', bada.v(), bada_d.v())
    k.dma('sp', gains.v(), gains_d.v())
    k.dma('sp', invf.v(), invf_d.v())

    ang = work_f.sub(0, T)
    kk = work_f.sub(T, T)
    posi_v = V(kk.v().ap.bitcast(I32), kk.v().keys)
    k.dma('sp', posi_v, pos_d.v())
    k.cp('dve', ang.v(), posi_v)
    k.ts('dve', ang.v(), ang.v(), invf.v(), None, ALU.mult)
    k.ts('dve', kk.v(), ang.v(), 1.0 / TWO_PI, 12582912.0, ALU.mult, ALU.add)
    k.ts('dve', kk.v(), kk.v(), 12582912.0, None, ALU.subtract)
    k.stt('dve', ang.v(), kk.v(), -C1, ang.v(), ALU.mult, ALU.add)
    k.stt('dve', ang.v(), kk.v(), -C2, ang.v(), ALU.mult, ALU.add)
    k.stt('dve', ang.v(), kk.v(), -C3, ang.v(), ALU.mult, ALU.add)
    k.ts('dve', ang.v(), ang.v(), math.pi, -math.pi, ALU.min, ALU.max)
    k.act(sinT.v(), ang.v(), AF.Sin)
    k.act(kk.v(), ang.v(), AF.Sin, scale=0.5)
    k.tt('dve', kk.v(), kk.v(), kk.v(), ALU.mult)
    k.ts('dve', cosT.v(), kk.v(), -2.0, 1.0, ALU.mult, ALU.add)

    k.act(cact.v(), ccol.v(), AF.Silu)
    psm = k.ps[3].sub(0, 48)
    for s in range(6):
        wb = wring[s % 3]
        k.dma('pool', wb.v(), V(wada_d.ap[s], wada_d.v(key=s).keys))
        for cc in range(8):
            for kc in range(8):
                k.mm(psm.v(s * 8 + cc, s * 8 + cc + 1),
                     wb.v(kc * 1024 + cc * 128, kc * 1024 + cc * 128 + 128),
                     cact.v(kc, kc + 1), start=(kc == 0), stop=(kc == 7))
    k.tt('dve', mod.v(), psm.v(), bada.v(), ALU.add)
    k.dma('sp', mod_o.v(), mod.v())
    sc1 = small.sub(152, 8)
    k.ts('dve', sc1.v(), mod.v(8, 16), 1.0, None, ALU.add)

    rms_mod(k, xT, hT, c, sc1, mod.sub(0, 8), work_f, work_b)

    raw = [work_f.sub(i * 512, 512) for i in range(2)]
    rst = [work_f.sub(1024 + i * 512, 512) for i in range(2)]
    t1 = [work_f.sub(2048 + i * 512, 512) for i in range(2)]
    t2 = [work_f.sub(3072 + i * 512, 512) for i in range(2)]
    sqb = [work_b.sub(i * 512, 512) for i in range(2)]
    qnb = [work_b.sub(1024 + i * 512, 512) for i in range(2)]
    cnt = [0]

    def qk_block(psb, gcol, outv, tb):
        i = cnt[0] % 2
        cnt[0] += 1
        k.act(sqb[i].v(), psb.v(), AF.Square)
        k.ts('dve', raw[i].v(), psb.v(), gcol, None, ALU.mult)
        ps2 = k.ps[2].sub(i * 512, 512)
        k.mm(ps2.v(), c['onesbd'].v(), sqb[i].v())
        k.act(rst[i].v(), ps2.v(), AF.Ln, scale=1.0 / 64, bias=EPS)
        k.act(rst[i].v(), rst[i].v(), AF.Exp, scale=-0.5)
        k.tt('pool', qnb[i].v(), raw[i].v(), rst[i].v(), ALU.mult)
        ps3 = k.ps[3].sub(i * 512, 512)
        k.mm(ps3.v(), c['rotT'].v(), qnb[i].v())
        k.tt('dve', t1[i].v(), qnb[i].v(), cosT.v(tb * 512, tb * 512 + 512), ALU.mult)
        k.tt('dve', t2[i].v(), ps3.v(), sinT.v(tb * 512, tb * 512 + 512), ALU.mult)
        k.tt('pool', outv, t1[i].v(), t2[i].v(), ALU.add)

    pcnt = [0]
    def wload_g(gj):
        nm_, nc__ = WIN_GROUPS[gj]
        k.dma('pool', wring[gj % 3].v(0, 8 * nc__), wg_d[nm_].v())

    wload_g(0)
    wload_g(1)
    for gi, (name, ncol) in enumerate(WIN_GROUPS):
        wb = wring[gi % 3]
        if gi + 2 < len(WIN_GROUPS):
            wload_g(gi + 2)
        if name in ('ka', 'kb', 'qa', 'qb'):
            npair = ncol // 128
            gidx = {'qa': 0, 'ka': 1, 'qb': 2, 'kb': 3}[name]
            od = {'ka': kTa_o, 'kb': kTb_o, 'qa': qTa_o, 'qb': qTb_o}[name]
            for pr in range(npair):
                st = stage[pr % 2]
                for tb in range(4):
                    psb = k.ps[pcnt[0] % 2].sub(512 * ((pcnt[0] // 2) % 2), 512)
                    pcnt[0] += 1
                    for kc in range(8):
                        k.mm(psb.v(), wb.v(kc * ncol + pr * 128, kc * ncol + pr * 128 + 128),
                             hT.v(kc * T + tb * 512, kc * T + tb * 512 + 512), start=(kc == 0), stop=(kc == 7))
                    qk_block(psb, gains.v(gidx, gidx + 1), st.v(tb * 512, tb * 512 + 512), tb)
                if name in ('ka', 'kb'):
                    odp = od[pr // 2]
                    k.dma('sp', V(odp.ap[(pr % 2) * 128:(pr % 2 + 1) * 128, :], odp.v().keys), st.v())
                else:
                    k.dma('sp', V(od.ap[:, pr * T:(pr + 1) * T], od.v().keys), st.v())
        elif name in ('va', 'vb'):
            nh_, dh_, d_ = (4, 129, 128) if name == 'va' else (12, 64, 64)
            od = va_o if name == 'va' else vb_o
            w_ = nh_ * dh_
            for i in range(2):
                k.memset('pool', vst[i].v(0, w_), 1.0)
            for tt_ in range(16):
                st = vst[tt_ % 2]
                for n0 in range(0, ncol, 512):
                    nn = min(512, ncol - n0)
                    psb = k.ps[pcnt[0] % 2].sub(512 * ((pcnt[0] // 2) % 2), 512)
                    pcnt[0] += 1
                    for kc in range(8):
                        k.mm(psb.v(0, nn), hT.v(kc * T + tt_ * 128, kc * T + tt_ * 128 + 128),
                             wb.v(kc * ncol + n0, kc * ncol + n0 + nn), start=(kc == 0), stop=(kc == 7))
                    h0 = n0 // d_
                    nhh = nn // d_
                    outv = V(st.v(h0 * dh_, (h0 + nhh) * dh_, pat="p (h c) -> p h c", c=dh_).ap[:, :, 0:d_],
                             st.keys(h0 * dh_, (h0 + nhh) * dh_))
                    inv = V(psb.v(0, nn, pat="p (h c) -> p h c", c=d_).ap, psb.keys(0, nn))
                    k.cp('act', outv, inv)
                if name == 'va':
                    for h4 in range(4):
                        k.dma('sp', V(od[h4].ap[tt_ * 128:(tt_ + 1) * 128, :], od[h4].v().keys), st.v(h4 * 129, h4 * 129 + 129))
                else:
                    for g3 in range(3):
                        k.dma('sp', V(od[g3].ap[tt_ * 128:(tt_ + 1) * 128, :], od[g3].v().keys), st.v(g3 * 256, g3 * 256 + 256))
        else:
            gi_ = 0 if name == 'ga' else 1
            for cc in range(8):
                st = stage[cc % 2]
                for tb in range(4):
                    psb = k.ps[pcnt[0] % 2].sub(512 * ((pcnt[0] // 2) % 2), 512)
                    pcnt[0] += 1
                    for kc in range(8):
                        k.mm(psb.v(), wb.v(kc * ncol + cc * 128, kc * ncol + cc * 128 + 128),
                             hT.v(kc * T + tb * 512, kc * T + tb * 512 + 512), start=(kc == 0), stop=(kc == 7))
                    k.act(st.v(tb * 512, tb * 512 + 512), psb.v(), AF.Sigmoid)
                o0 = (gi_ * 8 + cc) * T
                k.dma('sp', V(gates_o.ap[:, o0:o0 + T], gates_o.v().keys), st.v())
    return


def body_B(k, c, xT, D, l):
    mod_d = D['mod_s']
    qTa_d = D['qTa_s']
    qTb_d = D['qTb_s']
    kTa_d = D['kTa_all']
    va_d = D['va_all']
    kTb_d = D['kTb_all']
    vb_d = D['vb_all']
    gates_d = D['gates_s']
    lam_d = D['lamp%d' % l]
    subln_d = D['subln%d' % l]
    wpa_d = D['w_pa%d' % l]
    wpb_d = D['w_pb%d' % l]
    wo_d = D['w_o%d' % l]
    wr_d = D['w_r%d' % l]
    br_d = D['b_r%d' % l]
    sel_d = D['sel']
    ehd_d = D['ehd']
    we_d = D['w_e%d' % l]

    small = k.fa(1024)
    fwork = k.fa(25600 - k.fo)
    mod = small.sub(0, 48)
    lamp = small.sub(48, 258)
    subln = small.sub(320, 128)
    br = small.sub(448, 36)
    sc2 = small.sub(484, 8)
    lamcol = small.sub(492, 4)
    tmp64 = small.sub(512, 128)
    wr = small.sub(640, 288)

    k.dma('sp', mod.v(), mod_d.v())
    k.dma('sp', lamp.v(), lam_d.v())
    k.dma('sp', subln.v(), subln_d.v())
    k.dma('sp', br.v(), br_d.v())
    k.dma('sp', wr.v(), wr_d.v())

    k.tt('dve', tmp64.v(0, 64), lamp.v(0, 64), lamp.v(64, 128), ALU.mult)
    k.tt('dve', tmp64.v(64, 128), lamp.v(128, 192), lamp.v(192, 256), ALU.mult)
    k.red('dve', lamcol.v(0, 2), tmp64.v(0, 128, pat="p (a w) -> p a w", w=64), ALU.add)
    k.act(lamcol.v(0, 2), lamcol.v(0, 2), AF.Exp)
    k.tt('dve', lamcol.v(3, 4), lamcol.v(0, 1), lamcol.v(1, 2), ALU.subtract)
    k.tt('dve', lamcol.v(0, 1), lamcol.v(3, 4), lamp.v(256, 257), ALU.add)
    k.ts('dve', lamcol.v(1, 2), lamcol.v(0, 1), -1.0, None, ALU.mult)
    k.ts('dve', sc2.v(), mod.v(32, 40), 1.0, None, ALU.add)

    AB = k.AB_
    b0 = k.bo
    qTa = AB.sub(b0 + 0, 8192)
    oaT = AB.sub(b0 + 8192, 8192)
    obT = AB.sub(b0 + 16384, 4096)
    qTb = AB.sub(b0 + 20480, 12288)
    kring = [AB.sub(b0 + 32768 + i * 2048, 2048) for i in range(3)]
    vring = [AB.sub(b0 + 38912 + i * 2048, 2048) for i in range(3)]
    PT = [AB.sub(b0 + 45104 + i * 1024, 1024) for i in range(2)]

    k.dma('sp', qTa.v(), qTa_d.v())
    PT4 = [AB.sub(b0 + 45056 + i * 1024, 1024) for i in range(4)]
    tsum = AB.sub(b0 + 49152, 1024)
    acc = fwork.sub(1024, 1024)
    dsb = fwork.sub(2048, 512)
    rsb = fwork.sub(2560, 512)
    r2 = fwork.sub(3072, 512)
    slcol = fwork.sub(0, 1)
    k.tt('dve', slcol.v(), subln.v(0, 1), lamp.v(257, 258), ALU.mult)
    ld = [0]
    for h in range(4):
        for qb in range(4):
            OT = k.ps[2]

            SB = [0, 1, 3]

            def qk(kt, kbuf):
                pss = k.ps[SB[kt % 3]]
                for m in range(2):
                    k.mm(pss.v(m * 512, m * 512 + 512),
                         kbuf.v((kt % 16) * 128, (kt % 16) * 128 + 128, p0=m * 64, p1=m * 64 + 64),
                         qTa.v(h * T + qb * 512, h * T + qb * 512 + 512, p0=m * 64, p1=m * 64 + 64))

            bufs = {}

            def load(ch):
                i = ld[0] % 3
                ld[0] += 1
                kb_, vb_ = kring[i], vring[i]
                k.dma('sp', kb_.v(), V(kTa_d[h // 2].ap[ch * 256 + (h % 2) * 128:ch * 256 + (h % 2) * 128 + 128, :], kTa_d[h // 2].v().keys))
                src = va_d[h].ap[ch * 2048:(ch + 1) * 2048, 0:128].rearrange("(t p) c -> p t c", p=128)
                k.dma('sp', V(vb_.v(0, 2048, pat="p (t c) -> p t c", c=128).ap, vb_.keys(0, 2048)), V(src, va_d[h].v().keys))
                bufs[ch] = (kb_, vb_)

            load(0)
            load(1)
            qk(0, bufs[0][0])
            qk(1, bufs[0][0])
            for kt in range(64):
                ch = kt // 16
                if kt % 16 == 0 and ch + 2 < 4:
                    load(ch + 2)
                if kt + 2 < 64:
                    qk(kt + 2, bufs[(kt + 2) // 16][0])
                pt = PT4[kt % 4]
                k.act(pt.v(), k.ps[SB[kt % 3]].v(), AF.Exp, scale=0.125)
                vb_ = bufs[ch][1]
                for m in range(2):
                    k.mm(OT.v(m * 512, m * 512 + 512), vb_.v((kt % 16) * 128, (kt % 16) * 128 + 128),
                         pt.v(m * 512, m * 512 + 512), start=(kt == 0), stop=(kt == 63))
                if kt % 4 == 1:
                    k.tt('dve', tsum.v(), PT4[(kt - 1) % 4].v(), pt.v(), ALU.add)
                elif kt % 4 == 3:
                    k.tt('dve', tsum.v(), tsum.v(), PT4[(kt - 1) % 4].v(), ALU.add)
                    k.tt('dve', tsum.v(), tsum.v(), pt.v(), ALU.add)
                    if kt == 3:
                        k.cp('dve', acc.v(), tsum.v())
                    else:
                        k.tt('dve', acc.v(), acc.v(), tsum.v(), ALU.add)
            k.cp('dve', tsum.v(), acc.v())
            for m in range(2):
                psd = k.ps[0].sub(m * 512, 512)
                k.mm(psd.v(), c['ones_b'].v(), tsum.v(m * 512, m * 512 + 512))
            k.recip(rsb.v(), k.ps[0].v(0, 512))
            k.recip(r2.v(), k.ps[0].v(512, 1024))
            k.tt('dve', dsb.v(), OT.v(0, 512), rsb.v(), ALU.mult)
            k.tt('dve', r2.v(), OT.v(512, 1024), r2.v(), ALU.mult)
            k.stt('dve', dsb.v(), r2.v(), lamcol.v(1, 2), dsb.v(), ALU.mult, ALU.add)
            sqA = PT4[0].sub(0, 512)
            k.act(sqA.v(), dsb.v(), AF.Square)
            pss_ = k.ps[1].sub(0, 512)
            k.mm(pss_.v(), c['ones_b'].v(), sqA.v())
            k.act(rsb.v(), pss_.v(), AF.Sqrt, scale=1.0 / 128, bias=EPS)
            k.recip(rsb.v(), rsb.v())
            k.stt('dve', oaT.v(h * T + qb * 512, h * T + qb * 512 + 512), dsb.v(), slcol.v(), rsb.v(), ALU.mult, ALU.mult)

    k.dma('sp', qTb.v(), qTb_d.v())
    kbb = [AB.sub(b0 + i * 4096, 4096) for i in range(2)]
    vtr = [AB.sub(b0 + 32768 + i * 260, 260) for i in range(8)]
    numT = fwork.sub(512, 2 * T)
    denT = fwork.sub(512 + 2 * T, T)
    Osb = fwork.sub(512 + 3 * T, 260)
    vld = [0]
    vtile_no = [0]
    U32 = mybir.dt.uint32
    idx_t = k.idx_t
    kidx = Buf("idx_t", idx_t, 4, 0, 24)
    vidx = Buf("idx_t", idx_t, 4, 24, 69)
    vmask = fwork.sub(7200, 69)
    k.dma('sp', vmask.v(), D['vmask'].v())
    kTb_view = [d_.ap.rearrange("r (h c) -> (r h) c", h=2) for d_ in kTb_d]
    vstg = [AB.sub(b0 + 32768 + 8 * 260 + i * 256, 256) for i in range(4)]

    def igather(outv, src_ap, src_d, idxv):
        k.S.add('pool', lambda e: e.indirect_dma_start(out=outv.ap, out_offset=None, in_=src_ap,
                                                       in_offset=bass.IndirectOffsetOnAxis(ap=idxv.ap.bitcast(U32), axis=0)),
                reads=src_d.v().keys + idxv.keys, writes=outv.keys, dma=True)
    for g, dil in enumerate([1, 4, 16]):
        for j in range(2):
            for seg in range(4):
                col = (g * 2 + j) * 4 + seg
                igather(kbb[j].v(seg * 1024, seg * 1024 + 1024), kTb_view[g], kTb_d[g], kidx.v(col, col + 1))
        ntile = 16 // dil
        for r in range(dil):
            vt = {}

            def vload(n):
                i = vld[0] % 8
                vld[0] += 1
                tno = vtile_no[0]
                vtile_no[0] += 1
                vs_ = vstg[tno % 4]
                igather(vs_.v(), vb_d[g].ap, vb_d[g], vidx.v(tno, tno + 1))
                k.ts('dve', V(vtr[i].v(0, 260, pat="p (h c) -> p h c", c=65).ap[:, :, 0:64], vtr[i].keys(0, 260)),
                     V(vs_.v(0, 256, pat="p (h c) -> p h c", c=64).ap, vs_.keys(0, 256)), vmask.v(tno, tno + 1), None, ALU.mult)
                k.ts('dve', V(vtr[i].v(0, 260, pat="p (h c) -> p h c", c=65).ap[:, :, 64], vtr[i].keys(0, 260)),
                     c['ones_b'].v(0, 4), vmask.v(tno, tno + 1), None, ALU.mult)
                vt[n] = vtr[i]

            vload(0)
            for m in range(ntile):
                vload(m + 1)
                pss = k.ps[m % 2]
                i0 = 128 * m * dil + r
                for hh in range(4):
                    j, half = hh // 2, hh % 2
                    for ab in range(2):
                        n = m + ab
                        w0 = PADW + (128 * n - 64) * dil + r
                        col = (hh * 2 + ab) * 128
                        k.mm(pss.v(col, col + 128), c['ident_b'].v(), c['maskA' if ab == 0 else 'maskB'].v(), start=True, stop=False)
                        k.mm(pss.v(col, col + 128),
                             kbb[j].v(w0, w0 + 128 * dil - (dil - 1), p0=half * 64, p1=half * 64 + 64, step=dil),
                             qTb.v((g * 2 + j) * T + i0, (g * 2 + j) * T + i0 + 128 * dil - (dil - 1), p0=half * 64, p1=half * 64 + 64, step=dil),
                             start=False, stop=True)
                pt = PT[m % 2]
                k.act(pt.v(), pss.v(), AF.Exp, scale=0.125)
                Ops = k.ps[2 + (m % 2)].sub(0, 260)
                for hh in range(4):
                    for ab in range(2):
                        col = (hh * 2 + ab) * 128
                        k.mm(Ops.v(hh * 65, hh * 65 + 65), pt.v(col, col + 128), vt[m + ab].v(hh * 65, hh * 65 + 65),
                             start=(ab == 0), stop=(ab == 1))
                k.cp('dve', V(Osb.v(0, 256, pat="p (h c) -> p h c", c=64).ap, Osb.keys(0, 256)),
                     V(Ops.v(0, 260, pat="p (h c) -> p h c", c=65).ap[:, :, 0:64], Ops.keys(0, 260)))
                k.cp('dve', Osb.v(256, 260), V(Ops.v(0, 260, pat="p (h c) -> p h c", c=65).ap[:, :, 64], Ops.keys(0, 260)))
                pT_ = k.ps[2 + (m % 2)]
                for j in range(2):
                    k.tr(pT_.v(512 + j * 128, 512 + j * 128 + 128), Osb.v(j * 128, j * 128 + 128), c['ident_f'].v())
                k.tr(pT_.v(768, 896, p0=0, p1=4), Osb.v(256, 260), c['ident_f'].v())
                for j in range(2):
                    dst = numT.v(j * T + i0, j * T + i0 + 128 * dil - (dil - 1), step=dil)
                    if g == 0:
                        k.cp('dve', dst, pT_.v(512 + j * 128, 512 + j * 128 + 128))
                    else:
                        k.tt('dve', dst, pT_.v(512 + j * 128, 512 + j * 128 + 128), dst, ALU.add)
                dst = denT.v(i0, i0 + 128 * dil - (dil - 1), p0=0, p1=4, step=dil)
                if g == 0:
                    k.cp('dve', dst, pT_.v(768, 896, p0=0, p1=4))
                else:
                    k.tt('dve', dst, pT_.v(768, 896, p0=0, p1=4), dst, ALU.add)
    ehd_t = fwork.sub(512 + 3 * T + 260, 256)
    k.dma('sp', ehd_t.v(p0=0, p1=4), ehd_d.v())
    k.recip(denT.v(p0=0, p1=4), denT.v(p0=0, p1=4))
    for j in range(2):
        for tb in range(4):
            psb = k.ps[tb % 2].sub(0, 512)
            k.mm(psb.v(), ehd_t.v(j * 128, j * 128 + 128, p0=0, p1=4), denT.v(tb * 512, tb * 512 + 512, p0=0, p1=4))
            k.tt('dve', obT.v(j * T + tb * 512, j * T + tb * 512 + 512), numT.v(j * T + tb * 512, j * T + tb * 512 + 512), psb.v(), ALU.mult)

    wo = AB.sub(b0 + 20480, 8192)
    wpa = AB.sub(b0 + 28672, 4096)
    gat = AB.sub(b0 + 32768, 8192)
    mrg = AB.sub(b0 + 40960, 4096)
    wpb = AB.sub(b0 + 45056, 2048)
    k.dma('pool', wo.v(), wo_d.v())
    k.dma('pool', wpa.v(), wpa_d.v())
    k.dma('pool', wpb.v(), wpb_d.v())
    m1 = fwork.sub(0, 512)
    for tb in range(4):
        for gi_ in range(2):
            src = gates_d.ap[:, gi_ * 8 * T: (gi_ + 1) * 8 * T].rearrange("p (c t) -> p c t", t=T)[:, :, tb * 512:(tb + 1) * 512]
            k.dma('sp', V(gat.v(gi_ * 4096, gi_ * 4096 + 4096, pat="p (c t) -> p c t", t=512).ap, gat.keys(gi_ * 4096, gi_ * 4096 + 4096)),
                  V(src, gates_d.v().keys))
        for cc in range(8):
            psa = k.ps[cc % 2].sub(0, 512)
            psb = k.ps[cc % 2].sub(512, 512)
            for kc in range(4):
                k.mm(psa.v(), wpa.v(kc * 1024 + cc * 128, kc * 1024 + cc * 128 + 128),
                     oaT.v(kc * T + tb * 512, kc * T + tb * 512 + 512), start=(kc == 0), stop=(kc == 3))
            for kc in range(2):
                k.mm(psb.v(), wpb.v(kc * 1024 + cc * 128, kc * 1024 + cc * 128 + 128),
                     obT.v(kc * T + tb * 512, kc * T + tb * 512 + 512), start=(kc == 0), stop=(kc == 1))
            k.tt('dve', m1.v(), psa.v(), gat.v(cc * 512, cc * 512 + 512), ALU.mult)
            k.tt('dve', mrg.v(cc * 512, cc * 512 + 512), psb.v(), gat.v(4096 + cc * 512, 4096 + cc * 512 + 512), ALU.mult)
            k.tt('pool', mrg.v(cc * 512, cc * 512 + 512), mrg.v(cc * 512, cc * 512 + 512), m1.v(), ALU.add)
        for cc in range(8):
            psy = k.ps[2 + cc % 2].sub(0, 512)
            for kc in range(8):
                k.mm(psy.v(), wo.v(kc * 1024 + cc * 128, kc * 1024 + cc * 128 + 128),
                     mrg.v(kc * 512, kc * 512 + 512), start=(kc == 0), stop=(kc == 7))
            xs = xT.v(cc * T + tb * 512, cc * T + tb * 512 + 512)
            k.stt('dve', xs, psy.v(), mod.v(16 + cc, 17 + cc), xs, ALU.mult, ALU.add)

    hT = AB.sub(b0, 8 * T)
    wring = [AB.sub(b0 + 16384 + i * 6144, 6144) for i in range(4)]
    hid = [AB.sub(b0 + 40960 + i * 4096, 4096) for i in range(2)]
    wb2 = AB.sub(b0 + 49152, 1024)
    h32b = fwork.sub(2048, 4096)
    lg = fwork.sub(6144, 36 * 16)
    comb = fwork.sub(6144 + 576, 32 * 16)
    rw = fwork.sub(6144 + 576 + 512, 96)

    def hook(tb, cc):
        if cc is not None:
            return h32b.sub(cc * 512, 512)
        for q in range(4):
            tt_ = tb * 4 + q
            psl = k.ps[2 + q % 2].sub(0, 36)
            for kc in range(8):
                k.mm(psl.v(), h32b.v(kc * 512 + q * 128, kc * 512 + q * 128 + 128), wr.v(kc * 36, kc * 36 + 36),
                     start=(kc == 0), stop=(kc == 7))
            k.tt('dve', lg.v(tt_ * 36, tt_ * 36 + 36), psl.v(), br.v(), ALU.add)
        return None

    rms_mod(k, xT, hT, c, sc2, mod.sub(24, 8), fwork.sub(0, 2048), wb2, h32_hook=hook)

    Rb = fwork.sub(0, 2048)
    m1 = Rb.sub(0, 16); e1 = Rb.sub(16, 64); s1 = Rb.sub(80, 16); gval = Rb.sub(96, 16); oh = Rb.sub(112, 64)
    ig = Rb.sub(176, 128); tmpg = Rb.sub(304, 128); v1 = Rb.sub(432, 16); mk1 = Rb.sub(448, 128); ig2 = Rb.sub(576, 128)
    v2 = Rb.sub(704, 16); mk2 = Rb.sub(720, 128); dd = Rb.sub(848, 16); ee = Rb.sub(864, 16); w1 = Rb.sub(880, 16)
    w2 = Rb.sub(896, 16); cig = Rb.sub(912, 128); tmp2 = Rb.sub(1040, 128)

    def v3(buf, n):
        return V(buf.v(0, 16 * n, pat="p (t e) -> p t e", e=n).ap, buf.keys(0, 16 * n))

    def bc(buf, n):
        return V(buf.v(0, 16).ap.unsqueeze(2).to_broadcast([128, 16, n]), buf.keys(0, 16))

    def lgv(a_, b_):
        return V(lg.v(0, 576, pat="p (t e) -> p t e", e=36).ap[:, :, a_:b_], lg.keys(0, 576))

    def oh_g(g):
        return V(oh.v(0, 64, pat="p (t e) -> p t e", e=4).ap[:, :, g:g + 1].to_broadcast([128, 16, 8]), oh.keys(0, 64))

    def comb_g(g):
        return V(comb.v(0, 512, pat="p (t g e) -> p t g e", g=4, e=8).ap[:, :, g, :], comb.keys(0, 512))

    k.red('dve', m1.v(), lgv(0, 4), ALU.max)
    k.tt('dve', v3(e1, 4), lgv(0, 4), bc(m1, 4), ALU.subtract)
    k.act(e1.v(), e1.v(), AF.Exp)
    k.red('dve', s1.v(), v3(e1, 4), ALU.add)
    k.recip(gval.v(), s1.v())
    k.tt('dve', v3(oh, 4), lgv(0, 4), bc(m1, 4), ALU.is_equal)
    for g in range(4):
        dst = ig if g == 0 else tmpg
        k.tt('dve', v3(dst, 8), lgv(4 + 8 * g, 12 + 8 * g), oh_g(g), ALU.mult)
        if g > 0:
            k.tt('dve', ig.v(), ig.v(), tmpg.v(), ALU.add)
    k.red('dve', v1.v(), v3(ig, 8), ALU.max)
    k.tt('dve', v3(mk1, 8), v3(ig, 8), bc(v1, 8), ALU.is_equal)
    k.stt('dve', ig2.v(), mk1.v(), -1e30, ig.v(), ALU.mult, ALU.add)
    k.red('dve', v2.v(), v3(ig2, 8), ALU.max)
    k.tt('dve', v3(mk2, 8), v3(ig2, 8), bc(v2, 8), ALU.is_equal)
    k.tt('dve', dd.v(), v1.v(), v2.v(), ALU.subtract)
    k.act(ee.v(), dd.v(), AF.Exp, scale=-1.0)
    k.ts('dve', w1.v(), ee.v(), 1.0, None, ALU.add)
    k.recip(w1.v(), w1.v())
    k.tt('dve', w2.v(), ee.v(), w1.v(), ALU.mult)
    k.tt('dve', w1.v(), w1.v(), gval.v(), ALU.mult)
    k.tt('dve', w2.v(), w2.v(), gval.v(), ALU.mult)
    k.tt('dve', v3(cig, 8), v3(mk1, 8), bc(w1, 8), ALU.mult)
    k.tt('dve', v3(tmp2, 8), v3(mk2, 8), bc(w2, 8), ALU.mult)
    k.tt('dve', cig.v(), cig.v(), tmp2.v(), ALU.add)
    for g in range(4):
        k.tt('dve', comb_g(g), v3(cig, 8), oh_g(g), ALU.mult)
    CTt = fwork.sub(0, 2048)
    for tt_ in range(16):
        pst = k.ps[2 + tt_ % 2].sub(512, 128)
        k.tr(pst.v(p0=0, p1=32), comb.v(tt_ * 32, tt_ * 32 + 32), c['ident_f'].v())
        k.cp('dve', CTt.v(tt_ * 128, tt_ * 128 + 128, p0=0, p1=32), pst.v(p0=0, p1=32))
    selt = fwork.sub(2048, 4096)
    k.dma('sp', selt.v(p0=0, p1=32), sel_d.v())

    G = 2
    sg = [fwork.sub(6144 + i * 512, 512) for i in range(2)]

    def wload(e):
        k.dma('pool', wring[e % 4].v(), V(we_d.ap[e], we_d.v(key=e).keys))

    for e in range(4):
        wload(e)
    hcnt = [0]
    for eg in range(NEXP // G):
        if eg >= 1 and (eg + 1) * G < NEXP:
            for ei in range(G):
                wload((eg + 1) * G + ei)
        for tb in range(4):
            hb = hid[hcnt[0] % 2]
            hcnt[0] += 1
            for ei in range(G):
                e = eg * G + ei
                w = wring[e % 4]
                psc = k.ps[3].sub(512, 512)
                k.mm(psc.v(), selt.v(e * 128, e * 128 + 128, p0=0, p1=32), CTt.v(tb * 512, tb * 512 + 512, p0=0, p1=32))
                for ch in range(2):
                    psg = k.ps[ch].sub(0, 512)
                    psu = k.ps[ch].sub(512, 512)
                    for kc in range(8):
                        k.mm(psg.v(), w.v(kc * 256 + ch * 128, kc * 256 + ch * 128 + 128),
                             hT.v(kc * T + tb * 512, kc * T + tb * 512 + 512), start=(kc == 0), stop=(kc == 7))
                    for kc in range(8):
                        k.mm(psu.v(), w.v(2048 + kc * 256 + ch * 128, 2048 + kc * 256 + ch * 128 + 128),
                             hT.v(kc * T + tb * 512, kc * T + tb * 512 + 512), start=(kc == 0), stop=(kc == 7))
                    s_ = sg[ch]
                    k.act(s_.v(), psg.v(), AF.Silu)
                    k.tt('dve', s_.v(), psu.v(), s_.v(), ALU.mult)
                    k.tt('dve', hb.v((ei * 2 + ch) * 512, (ei * 2 + ch) * 512 + 512), psc.v(), s_.v(), ALU.mult)
            for cc in range(8):
                psy = k.ps[2].sub((cc % 2) * 512, 512)
                for ei in range(G):
                    w = wring[(eg * G + ei) % 4]
                    for ch in range(2):
                        k.mm(psy.v(), w.v(4096 + ch * 1024 + cc * 128, 4096 + ch * 1024 + cc * 128 + 128),
                             hb.v((ei * 2 + ch) * 512, (ei * 2 + ch) * 512 + 512),
                             start=(ei == 0 and ch == 0), stop=(ei == G - 1 and ch == 1))
                xs = xT.v(cc * T + tb * 512, cc * T + tb * 512 + 512)
                k.stt('dve', xs, psy.v(), mod.v(40 + cc, 41 + cc), xs, ALU.mult, ALU.add)
    return


def build_F():
    k = K(nf32=25600, nbf16=51200)
    nc = k.nc
    D = {}

    def ext(name, shape, dt):
        D[name] = k.din(name, shape, dt)

    def itn(name, shape, dt):
        D[name] = DBuf(name, nc.dram_tensor(name, list(shape), dt).ap())

    ext('xT', [128, 8 * T], F32); ext('c_col', [128, 8], F32); ext('posb', [128, T], I32); ext('invf', [128, 1], F32)
    ext('sel', [32, 32 * 128], F32); ext('ehd', [4, 256], F32); ext('idx', [128, 93], I32); ext('vmask', [128, 69], F32)
    for l in range(2):
        ext('w_ada%d' % l, [6, 128, 8 * 1024], F32); ext('b_ada%d' % l, [128, 48], F32); ext('gains%d' % l, [128, 4], F32)
        for n, nc_ in WIN_GROUPS:
            ext('w_%s%d' % (n, l), [128, 8 * nc_], F32)
        ext('lamp%d' % l, [128, 258], F32); ext('subln%d' % l, [128, 128], F32)
        ext('w_pa%d' % l, [128, 4096], F32); ext('w_pb%d' % l, [128, 2048], F32); ext('w_o%d' % l, [128, 8192], F32)
        ext('w_r%d' % l, [128, 288], F32); ext('b_r%d' % l, [128, 36], F32); ext('w_e%d' % l, [NEXP, 128, 6144], F32)
    def pieces(nm, n, ls, as_):
        D[nm + '_loc'] = [DBuf('%s_loc%d' % (nm, i), nc.dram_tensor('%s_loc%d' % (nm, i), ls, BF16).ap()) for i in range(n)]
        D[nm + '_all'] = [DBuf('%s_all%d' % (nm, i), nc.dram_tensor('%s_all%d' % (nm, i), as_, BF16).ap()) for i in range(n)]
    pieces('kTa', 2, [256, 2048], [1024, 2048])
    pieces('va', 4, [T, 129], [SEQ, 129])
    pieces('kTb', 3, [256, 2048], [1024, 2048])
    pieces('vb', 3, [T, 256], [SEQ, 256])
    itn('qTa_s', [128, 4 * T], BF16); itn('qTb_s', [128, 6 * T], BF16)
    itn('gates_s', [128, 16 * T], BF16); itn('mod_s', [128, 48], F32)
    out_d = k.dout('outT', [128, 8 * T], F32)
    c = load_consts(k, True)
    xT = k.fa(8 * T)
    k.idx_t = k.es.enter_context(nc.sbuf_tensor("idx_t", [128, 93], I32))
    idxb = Buf("idx_t", k.idx_t, 4, 0, 93)
    k.dma('sp', idxb.v(), D['idx'].v())
    k.dma('sp', xT.v(), D['xT'].v())
    base = (k.fo, k.bo)
    rg = [[0, 1, 2, 3], [4, 5, 6, 7]]
    for l in range(2):
        k.fo, k.bo = base
        def after_kv():
            for nm, i_ in [('kTa', 0), ('va', 0), ('va', 1), ('kTa', 1), ('va', 2), ('va', 3),
                           ('kTb', 0), ('kTb', 1), ('kTb', 2), ('vb', 0), ('vb', 1), ('vb', 2)]:
                loc, al = D[nm + '_loc'][i_], D[nm + '_all'][i_]
                k.S.add('pool', lambda e, loc=loc, al=al: e.collective_compute(
                    "AllGather", ALU.bypass, replica_groups=rg, ins=[loc.ap.opt()], outs=[al.ap.opt()]),
                    reads=loc.v().keys, writes=al.v().keys, dma=True, cc=True)
        k.after_kv = after_kv
        body_A(k, c, xT, D, l)
        after_kv()
        k.fo, k.bo = base
        body_B(k, c, xT, D, l)
    k.dma('sp', out_d.v(), xT.v())
    return k.finalize()


_cache = {}


def _fm(w):
    K_, N = w.shape
    return np.ascontiguousarray(w.reshape(K_ // 128, 128, N).transpose(1, 0, 2).reshape(128, (K_ // 128) * N))


def _index_tables(jr):
    p = np.arange(128)
    idx = np.zeros((128, 93), np.int64)
    vmask = np.zeros((128, 69), np.float32)
    for jp in range(6):
        for seg in range(4):
            rank = [jr - 1, jr, jr, jr + 1][seg]
            half = [1, 0, 1, 0][seg]
            rank = min(max(rank, 0), 3)
            idx[:, jp * 4 + seg] = rank * 512 + ((jp % 2) * 128 + p) * 2 + half
    t = 0
    for g, dil in enumerate([1, 4, 16]):
        for r in range(dil):
            for n in range(16 // dil + 1):
                a_k = 128 * n - 64 + p
                gpos = jr * T + a_k * dil + r
                valid = (gpos >= 0) & (gpos < SEQ)
                gp = np.where(valid, gpos, 0)
                idx[:, 24 + t] = gp
                vmask[:, t] = valid.astype(np.float32)
                t += 1
    assert t == 69
    return idx.astype(np.int32), vmask


def kernel(x, c, positions, w_ada, b_ada, w_in, qn_a, kn_a, lam_q1, lam_k1, lam_q2, lam_k2,
           subln_a, qn_b, kn_b, w_pa, w_pb, w_o, w_r1, b_r1, w_r2, b_r2, w_e_gate, w_e_up, w_e_down):
    x = np.asarray(x, np.float32)
    consts = host_consts()
    invf = (np.float32(10000.0) ** (-np.arange(0, 64, 2, dtype=np.float32) / np.float32(64))).astype(np.float32)
    invf_col = invf[(np.arange(128) % 64) % 32].reshape(128, 1).astype(np.float32)
    cores = list(range(NCORES))
    sel = np.zeros((32, 32 * 128), np.float32)
    for e in range(32):
        sel[e, e * 128:(e + 1) * 128] = 1.0
    ehd = np.zeros((4, 256), np.float32)
    for h in range(4):
        j, half = h // 2, h % 2
        ehd[h, j * 128 + half * 64: j * 128 + half * 64 + 64] = 1.0
    cuts = np.cumsum([0, 512, 512, 512, 768, 768, 768, 1024, 1024])
    names = ['qa', 'ka', 'va', 'qb', 'kb', 'vb', 'ga', 'gb']
    shared = {"consts": consts, "invf": invf_col, "sel": sel, "ehd": ehd}
    pidx = np.arange(128) % 64
    for l in range(2):
        wl = np.asarray(w_in[l], np.float32)
        for i, n in enumerate(names):
            shared["w_%s%d" % (n, l)] = _fm(wl[:, cuts[i]:cuts[i + 1]])
        shared["w_ada%d" % l] = np.ascontiguousarray(
            np.asarray(w_ada[l], np.float32).reshape(8, 128, 6, 1024).transpose(2, 1, 0, 3).reshape(6, 128, 8 * 1024))
        shared["b_ada%d" % l] = np.ascontiguousarray(np.asarray(b_ada[l], np.float32).reshape(48, 128).T)
        shared["gains%d" % l] = np.stack([np.asarray(qn_a[l])[pidx], np.asarray(kn_a[l])[pidx],
                                          np.asarray(qn_b[l])[pidx], np.asarray(kn_b[l])[pidx]], axis=1).astype(np.float32)
        lam_init = 0.8 - 0.6 * math.exp(-0.3 * l)
        lamp = np.concatenate([np.asarray(lam_q1[l]), np.asarray(lam_k1[l]), np.asarray(lam_q2[l]), np.asarray(lam_k2[l]),
                               np.array([lam_init, 1.0 - lam_init])]).astype(np.float32)
        shared["lamp%d" % l] = np.ascontiguousarray(np.broadcast_to(lamp[None, :], (128, 258)))
        shared["subln%d" % l] = np.ascontiguousarray(np.broadcast_to(np.asarray(subln_a[l], np.float32)[:, None], (128, 128)))
        shared["w_r%d" % l] = _fm(np.concatenate([np.asarray(w_r1[l], np.float32), np.asarray(w_r2[l], np.float32)], axis=1))
        shared["b_r%d" % l] = np.ascontiguousarray(np.broadcast_to(
            np.concatenate([np.asarray(b_r1[l]), np.asarray(b_r2[l])]).astype(np.float32)[None, :], (128, 36)))
        we = np.empty((NEXP, 128, 6144), np.float32)
        for e in range(NEXP):
            we[e, :, 0:2048] = _fm(np.asarray(w_e_gate[l, e], np.float32))
            we[e, :, 2048:4096] = _fm(np.asarray(w_e_up[l, e], np.float32))
            we[e, :, 4096:6144] = _fm(np.asarray(w_e_down[l, e], np.float32))
        shared["w_e%d" % l] = we
        shared["w_pa%d" % l] = _fm(np.asarray(w_pa[l], np.float32))
        shared["w_pb%d" % l] = _fm(np.asarray(w_pb[l], np.float32))
        shared["w_o%d" % l] = _fm(np.asarray(w_o[l], np.float32))
    in_maps = []
    for ci in cores:
        b, j = ci // 4, ci % 4
        xs = x[b, j * T:(j + 1) * T, :]
        idx, vmask = _index_tables(j)
        m = dict(shared)
        m["xT"] = np.ascontiguousarray(xs.T.reshape(8, 128, T).transpose(1, 0, 2).reshape(128, 8 * T))
        m["c_col"] = np.ascontiguousarray(np.asarray(c[b], np.float32).reshape(8, 128).T)
        m["posb"] = np.ascontiguousarray(np.broadcast_to(np.asarray(positions[b, j * T:(j + 1) * T], np.int32)[None, :], (128, T)))
        m["idx"] = idx
        m["vmask"] = vmask
        in_maps.append(m)
    if 'F' not in _cache:
        _cache['F'] = build_F()
    res = run_bass_kernel_spmd(_cache['F'], in_maps, core_ids=cores).results
    out = np.empty((2, SEQ, D), np.float32)
    for ci in cores:
        b, j = ci // 4, ci % 4
        o = np.asarray(res[ci]["outT"], np.float32)
        out[b, j * T:(j + 1) * T, :] = o.reshape(128, 8, T).transpose(1, 0, 2).reshape(D, T).T
    return out
```

```python
import math
import numpy as np
from contextlib import ExitStack
import concourse.bass as bass
import concourse.mybir as mybir
from concourse.bass_utils import run_bass_kernel_spmd

F32 = mybir.dt.float32
BF16 = mybir.dt.bfloat16
I32 = mybir.dt.int32
ALU = mybir.AluOpType
AF = mybir.ActivationFunctionType
AX = mybir.AxisListType

ENGS = ['pe', 'act', 'dve', 'pool', 'sp']
CHUNK = 1024

NCORES = 8
T = 2048
SEQ = 8192
D = 1024
EPS = 1e-6
NEG = -30000.0
WIN = 4096
PADW = 1024
NEXP = 32


class Op:
    __slots__ = ('eng', 'fn', 'src', 'pos', 'signal', 'waits', 'know', 'is_dma', 'cnt')


class V:
    __slots__ = ('ap', 'keys')

    def __init__(self, ap, keys):
        self.ap = ap
        self.keys = keys


class Buf:
    def __init__(self, arena_name, handle, esz, off, n):
        self.an = arena_name
        self.t = handle
        self.esz = esz
        self.off = off
        self.n = n

    def keys(self, a, b):
        ch = 2048 if self.an.startswith('ps') else CHUNK
        lo = ((self.off + a) * self.esz) // ch
        hi = ((self.off + b) * self.esz - 1) // ch
        return [(self.an, i) for i in range(lo, hi + 1)]

    def v(self, a=0, b=None, p0=0, p1=128, step=1, pat=None, **kw):
        if b is None:
            b = self.n
        assert 0 <= a < b <= self.n, (a, b, self.n)
        if step == 1:
            ap = self.t[p0:p1, self.off + a:self.off + b]
        else:
            ap = self.t[p0:p1, self.off + a:self.off + b:step]
        if pat is not None:
            ap = ap.rearrange(pat, **kw)
        return V(ap, self.keys(a, b))

    def sub(self, off, n):
        assert off + n <= self.n
        return Buf(self.an, self.t, self.esz, self.off + off, n)


class DBuf:
    def __init__(self, name, ap):
        self.name = name
        self.ap = ap

    def v(self, ap=None, key=None):
        return V(self.ap if ap is None else ap, [(self.name, key)])


class Sched:
    def __init__(self, nc, n_dma_sems=32):
        self.nc = nc
        self.ops = {e: [] for e in ENGS}
        self.ncomp = {e: 0 for e in ENGS}
        self.last_w = {}
        self.readers = {}
        self.known = {e: {} for e in ENGS}
        self.n_dma_sems = n_dma_sems
        self.dma_last = [None] * n_dma_sems
        self.dma_cnt = [0] * n_dma_sems
        self.dma_rr = 0
        self.dma_rr_sw = 0
        self.cc_cnt = 0
        self.cc_last = None

    def _need(self, e, d):
        k = self.known[e]
        if k.get(d.src, -1) >= d.pos:
            return None
        d.signal = True
        for s, p in d.know.items():
            if k.get(s, -1) < p:
                k[s] = p
        return d

    def add(self, eng, fn, reads=(), writes=(), dma=False, cc=False):
        op = Op()
        op.eng = eng
        op.fn = fn
        op.is_dma = dma
        op.signal = dma
        deps = []
        seen = set()
        pr_ = [k for k in reads if isinstance(k[0], str) and k[0].startswith('ps')]
        if pr_:
            writes = list(writes) + pr_
        for k in reads:
            w = self.last_w.get(k)
            if w is not None and id(w) not in seen:
                seen.add(id(w)); deps.append(w)
        for k in writes:
            w = self.last_w.get(k)
            if w is not None and id(w) not in seen:
                seen.add(id(w)); deps.append(w)
            for r in self.readers.get(k, ()):
                if id(r) not in seen:
                    seen.add(id(r)); deps.append(r)
        if cc:
            op.src = ('cc', 0)
            op.pos = self.cc_cnt
            self.cc_cnt += 1
            if self.cc_last is not None and id(self.cc_last) not in seen:
                seen.add(id(self.cc_last)); deps.append(self.cc_last)
            self.cc_last = op
        elif dma:
            half = self.n_dma_sems // 2
            if eng == 'pool':
                slot = half + self.dma_rr_sw
                self.dma_rr_sw = (self.dma_rr_sw + 1) % (self.n_dma_sems - half)
            else:
                slot = self.dma_rr
                self.dma_rr = (self.dma_rr + 1) % half
            prev = self.dma_last[slot]
            if prev is not None and id(prev) not in seen:
                seen.add(id(prev)); deps.append(prev)
            op.src = ('dma', slot)
            op.pos = self.dma_cnt[slot]
            self.dma_cnt[slot] += 1
            self.dma_last[slot] = op
        else:
            op.src = eng
            op.pos = self.ncomp[eng]
            self.ncomp[eng] += 1
        waits = []
        deps.sort(key=lambda d: -d.pos)
        for d in deps:
            if (not dma) and (not d.is_dma) and d.src == eng:
                if eng == 'pe':
                    continue
                if op.pos - d.pos > 2:
                    continue
            w = self._need(eng, d)
            if w is not None:
                waits.append(w)
        op.waits = waits
        know = dict(self.known[eng])
        know[op.src] = op.pos
        op.know = know
        for k in reads:
            self.readers.setdefault(k, []).append(op)
        for k in writes:
            self.last_w[k] = op
            self.readers[k] = []
        self.ops[eng].append(op)
        return op

    def finish(self):
        op = Op()
        op.eng = 'sp'; op.fn = None; op.is_dma = False; op.signal = False
        op.src = 'sp'; op.pos = self.ncomp['sp']; self.ncomp['sp'] += 1
        waits = []
        for d in list(self.dma_last) + [self.cc_last]:
            if d is not None:
                w = self._need('sp', d)
                if w is not None:
                    waits.append(w)
        for e in ['pe', 'act', 'dve', 'pool']:
            comp = [o for o in self.ops[e] if not o.is_dma]
            if comp:
                w = self._need('sp', comp[-1])
                if w is not None:
                    waits.append(w)
        op.waits = waits
        op.know = {}
        self.ops['sp'].append(op)

    def emit(self, sems):
        nc = self.nc
        for e in ENGS:
            c = 0
            for o in self.ops[e]:
                if o.is_dma:
                    continue
                if o.signal:
                    c += 1
                o.cnt = c
        engobj = {'pe': nc.tensor, 'act': nc.scalar, 'dve': nc.vector, 'pool': nc.gpsimd, 'sp': nc.sync}

        def run(e):
            eo = engobj[e]
            for o in self.ops[e]:
                for d in o.waits:
                    if d.is_dma and d.src[0] == 'cc':
                        eo.wait_ge(sems[d.src], d.pos + 1)
                    elif d.is_dma:
                        eo.wait_ge(sems[d.src], 16 * (d.pos + 1))
                    else:
                        eo.wait_ge(sems[d.src], d.cnt)
                if o.fn is None:
                    continue
                ins = o.fn(eo)
                if o.is_dma and o.src[0] == 'cc':
                    ins.then_inc(sems[o.src])
                elif o.is_dma:
                    ins.then_inc(sems[o.src], 16)
                elif o.signal:
                    ins.then_inc(sems[e], 1)
        return run


class K:
    def __init__(self, nf32, nbf16):
        self.nc = bass.Bass("TRN2", target_bir_lowering=False)
        self.es = ExitStack()
        nc = self.nc
        self.S = Sched(nc)
        es = self.es
        self.af_t = es.enter_context(nc.sbuf_tensor("arena_f", [128, nf32], F32))
        self.ab_t = es.enter_context(nc.sbuf_tensor("arena_b", [128, nbf16], BF16))
        self.AF_ = Buf("af", self.af_t, 4, 0, nf32)
        self.AB_ = Buf("ab", self.ab_t, 2, 0, nbf16)
        self.ps = []
        for i in range(4):
            t = es.enter_context(nc.psum_tensor("ps%d" % i, [128, 1024], F32))
            self.ps.append(Buf("ps%d" % i, t, 4, 0, 1024))
        self.sems = {}
        for e in ENGS:
            self.sems[e] = es.enter_context(nc.semaphore("s_" + e))
        for i in range(self.S.n_dma_sems):
            self.sems[('dma', i)] = es.enter_context(nc.semaphore("d%d" % i))
        self.sems[('cc', 0)] = es.enter_context(nc.semaphore("ccsem"))
        self.fo = 0
        self.bo = 0
        self.dram = {}

    def din(self, name, shape, dt):
        ap = self.nc.dram_tensor(name, list(shape), dt, kind="ExternalInput").ap()
        d = DBuf(name, ap)
        self.dram[name] = d
        return d

    def dout(self, name, shape, dt):
        ap = self.nc.dram_tensor(name, list(shape), dt, kind="ExternalOutput").ap()
        d = DBuf(name, ap)
        self.dram[name] = d
        return d

    def fa(self, n):
        b = self.AF_.sub(self.fo, n)
        self.fo += n
        return b

    def ba(self, n):
        b = self.AB_.sub(self.bo, n)
        self.bo += n
        return b

    def mm(self, out, lhsT, rhs, start=True, stop=True):
        self.S.add('pe', lambda e: e.matmul(out.ap, lhsT.ap, rhs.ap, start=start, stop=stop),
                   reads=lhsT.keys + rhs.keys, writes=out.keys)

    def tr(self, out, in_, ident):
        self.S.add('pe', lambda e: e.transpose(out.ap, in_.ap, ident.ap),
                   reads=in_.keys + ident.keys, writes=out.keys)

    def act(self, out, in_, func, scale=1.0, bias=0.0, accum=None):
        reads = list(in_.keys)
        kw = {}
        if isinstance(bias, V):
            reads += bias.keys
            kw['bias'] = bias.ap
        else:
            kw['bias'] = float(bias)
        if isinstance(scale, V):
            reads += scale.keys
            kw['scale'] = scale.ap
        else:
            kw['scale'] = float(scale)
        writes = list(out.keys)
        if accum is not None:
            writes += accum.keys
            kw['accum_out'] = accum.ap
        self.S.add('act', lambda e: e.activation(out=out.ap, in_=in_.ap, func=func, **kw),
                   reads=reads, writes=writes)

    def tt(self, eng, out, in0, in1, op):
        self.S.add(eng, lambda e: e.tensor_tensor(out=out.ap, in0=in0.ap, in1=in1.ap, op=op),
                   reads=in0.keys + in1.keys, writes=out.keys)

    def ts(self, eng, out, in0, s1, s2=None, op0=ALU.mult, op1=None):
        reads = list(in0.keys)
        a1 = s1
        a2 = s2
        if isinstance(s1, V):
            reads += s1.keys; a1 = s1.ap
        if isinstance(s2, V):
            reads += s2.keys; a2 = s2.ap
        if op1 is None:
            self.S.add(eng, lambda e: e.tensor_scalar(out=out.ap, in0=in0.ap, scalar1=a1, scalar2=None, op0=op0),
                       reads=reads, writes=out.keys)
        else:
            self.S.add(eng, lambda e: e.tensor_scalar(out=out.ap, in0=in0.ap, scalar1=a1, scalar2=a2, op0=op0, op1=op1),
                       reads=reads, writes=out.keys)

    def stt(self, eng, out, in0, scalar, in1, op0, op1):
        reads = in0.keys + in1.keys
        a = scalar
        if isinstance(scalar, V):
            reads = reads + scalar.keys; a = scalar.ap
        self.S.add(eng, lambda e: e.scalar_tensor_tensor(out=out.ap, in0=in0.ap, scalar=a, in1=in1.ap, op0=op0, op1=op1),
                   reads=reads, writes=out.keys)

    def cp(self, eng, out, in_):
        if eng == 'act':
            self.S.add('act', lambda e: e.copy(out=out.ap, in_=in_.ap), reads=in_.keys, writes=out.keys)
        else:
            self.S.add(eng, lambda e: e.tensor_copy(out=out.ap, in_=in_.ap), reads=in_.keys, writes=out.keys)

    def red(self, eng, out, in_, op, axis=AX.X):
        self.S.add(eng, lambda e: e.tensor_reduce(out=out.ap, in_=in_.ap, axis=axis, op=op),
                   reads=in_.keys, writes=out.keys)

    def recip(self, out, in_):
        self.S.add('dve', lambda e: e.reciprocal(out=out.ap, in_=in_.ap), reads=in_.keys, writes=out.keys)

    def memset(self, eng, out, val):
        self.S.add(eng, lambda e: e.memset(out.ap, val), writes=out.keys)

    def dma(self, eng, out, in_):
        self.S.add(eng, lambda e: e.dma_start(out=out.ap, in_=in_.ap), reads=in_.keys, writes=out.keys, dma=True)

    def finalize(self):
        S = self.S
        S.finish()
        run = S.emit(self.sems)
        with self.nc.Block() as block:
            @block.tensor
            def _(e): run('pe')
            @block.scalar
            def _(e): run('act')
            @block.vector
            def _(e): run('dve')
            @block.gpsimd
            def _(e): run('pool')
            @block.sync
            def _(e): run('sp')
        self.es.close()
        return self.nc


def load_consts(k, need_rope):
    c = {}
    cin = k.din("consts", [128, 5 * 128], F32)
    cf = k.fa(256).sub(0, 128)
    k.dma('sp', cf.v(), V(cin.ap[:, 0:128], cin.v().keys))
    c['ident_f'] = cf
    cb = k.ba(5 * 128)
    k.dma('pool', cb.v(), cin.v())
    c['ident_b'] = cb.sub(0, 128)
    c['onesbd'] = cb.sub(128, 128)
    c['rotT'] = cb.sub(256, 128)
    c['maskA'] = cb.sub(384, 128)
    c['maskB'] = cb.sub(512, 128)
    ones = k.ba(128)
    k.ba(256)
    k.memset('pool', ones.v(), 1.0)
    c['ones_b'] = ones
    return c


def host_consts():
    ident = np.eye(128, dtype=np.float32)
    onesbd = np.zeros((128, 128), np.float32)
    onesbd[:64, :64] = 1.0
    onesbd[64:, 64:] = 1.0
    rotT = np.zeros((128, 128), np.float32)
    for m in range(128):
        j = m % 64
        if j < 32:
            rotT[m + 32, m] = -1.0
        else:
            rotT[m - 32, m] = 1.0
    u = np.arange(128)[:, None]
    a = np.arange(128)[None, :]
    maskA = np.where(u >= a, 0.0, NEG).astype(np.float32)
    maskB = np.where(u <= a, 0.0, NEG).astype(np.float32)
    return np.concatenate([ident, onesbd, rotT, maskA, maskB], axis=1)


TWO_PI = 2.0 * math.pi
C1 = 6.28125
C2 = float(np.float32(TWO_PI - 6.28125))
C3 = float(TWO_PI - 6.28125 - float(np.float32(TWO_PI - 6.28125)))


WIN_GROUPS = [('ka', 512), ('kb', 768), ('va', 512), ('vb', 768), ('qa', 512), ('qb', 768), ('ga', 1024), ('gb', 1024)]


def rms_mod(k, xT, hT, c, scale_col, shift_col, work_f, work_b, h32_hook=None):
    sq = [work_b.sub(i * 512, 512) for i in range(2)]
    sd = work_f.sub(0, 512)
    rs = work_f.sub(512, 512)
    tmp = [work_f.sub(1024 + i * 512, 512) for i in range(2)]
    for tb in range(4):
        pss = k.ps[tb % 2].sub(0, 512)
        for cc in range(8):
            s = sq[cc % 2]
            k.act(s.v(), xT.v(cc * T + tb * 512, cc * T + tb * 512 + 512), AF.Square)
            k.mm(pss.v(), c['ones_b'].v(), s.v(), start=(cc == 0), stop=(cc == 7))
        k.act(sd.v(), pss.v(), AF.Ln, scale=1.0 / D, bias=EPS)
        k.act(rs.v(), sd.v(), AF.Exp, scale=-0.5)
        for cc in range(8):
            t = tmp[cc % 2]
            k.tt('pool', t.v(), xT.v(cc * T + tb * 512, cc * T + tb * 512 + 512), rs.v(), ALU.mult)
            if h32_hook is not None:
                h32 = h32_hook(tb, cc)
                k.ts('dve', h32.v(), t.v(), scale_col.v(cc, cc + 1), shift_col.v(cc, cc + 1), ALU.mult, ALU.add)
                k.cp('act', hT.v(cc * T + tb * 512, cc * T + tb * 512 + 512), h32.v())
            else:
                k.ts('dve', hT.v(cc * T + tb * 512, cc * T + tb * 512 + 512), t.v(),
                     scale_col.v(cc, cc + 1), shift_col.v(cc, cc + 1), ALU.mult, ALU.add)
        if h32_hook is not None:
            h32_hook(tb, None)


def body_A(k, c, xT, D, l):
    stg = 99
    ccol_d = D['c_col']
    pos_d = D['posb']
    invf_d = D['invf']
    wada_d = D['w_ada%d' % l]
    bada_d = D['b_ada%d' % l]
    gains_d = D['gains%d' % l]
    wg_d = {n: D['w_%s%d' % (n, l)] for n, nc_ in WIN_GROUPS}
    kTa_o = D['kTa_loc']
    kTb_o = D['kTb_loc']
    qTa_o = D['qTa_s']
    qTb_o = D['qTb_s']
    va_o = D['va_loc']
    vb_o = D['vb_loc']
    gates_o = D['gates_s']
    mod_o = D['mod_s']

    cosT = k.fa(T)
    sinT = k.fa(T)
    small = k.fa(256)
    work_f = k.fa(4096)
    hT = k.ba(8 * T)
    wring = [k.ba(8192) for _ in range(3)]
    work_b = k.ba(2048)
    stage = [k.ba(2048) for _ in range(2)]
    vst = [k.ba(1024) for _ in range(2)]

    ccol = small.sub(0, 8)
    cact = k.ba(8)
    bada = small.sub(8, 48)
    mod = small.sub(56, 48)
    gains = small.sub(104, 4)
    invf = small.sub(108, 1)

    k.dma('sp', ccol.v(), ccol_d.v())
    k.dma('sp', bada.v(), bada_d.v())
    k.dma('sp', gains.v(), gains_d.v())
    k.dma('sp', invf.v(), invf_d.v())

    ang = work_f.sub(0, T)
    kk = work_f.sub(T, T)
    posi_v = V(kk.v().ap.bitcast(I32), kk.v().keys)
    k.dma('sp', posi_v, pos_d.v())
    k.cp('dve', ang.v(), posi_v)
    k.ts('dve', ang.v(), ang.v(), invf.v(), None, ALU.mult)
    k.ts('dve', kk.v(), ang.v(), 1.0 / TWO_PI, 12582912.0, ALU.mult, ALU.add)
    k.ts('dve', kk.v(), kk.v(), 12582912.0, None, ALU.subtract)
    k.stt('dve', ang.v(), kk.v(), -C1, ang.v(), ALU.mult, ALU.add)
    k.stt('dve', ang.v(), kk.v(), -C2, ang.v(), ALU.mult, ALU.add)
    k.stt('dve', ang.v(), kk.v(), -C3, ang.v(), ALU.mult, ALU.add)
    k.ts('dve', ang.v(), ang.v(), math.pi, -math.pi, ALU.min, ALU.max)
    k.act(sinT.v(), ang.v(), AF.Sin)
    k.act(kk.v(), ang.v(), AF.Sin, scale=0.5)
    k.tt('dve', kk.v(), kk.v(), kk.v(), ALU.mult)
    k.ts('dve', cosT.v(), kk.v(), -2.0, 1.0, ALU.mult, ALU.add)

    k.act(cact.v(), ccol.v(), AF.Silu)
    psm = k.ps[3].sub(0, 48)
    for s in range(6):
        wb = wring[s % 3]
        k.dma('pool', wb.v(), V(wada_d.ap[s], wada_d.v(key=s).keys))
        for cc in range(8):
            for kc in range(8):
                k.mm(psm.v(s * 8 + cc, s * 8 + cc + 1),
                     wb.v(kc * 1024 + cc * 128, kc * 1024 + cc * 128 + 128),
                     cact.v(kc, kc + 1), start=(kc == 0), stop=(kc == 7))
    k.tt('dve', mod.v(), psm.v(), bada.v(), ALU.add)
    k.dma('sp', mod_o.v(), mod.v())
    sc1 = small.sub(152, 8)
    k.ts('dve', sc1.v(), mod.v(8, 16), 1.0, None, ALU.add)

    rms_mod(k, xT, hT, c, sc1, mod.sub(0, 8), work_f, work_b)

    raw = [work_f.sub(i * 512, 512) for i in range(2)]
    rst = [work_f.sub(1024 + i * 512, 512) for i in range(2)]
    t1 = [work_f.sub(2048 + i * 512, 512) for i in range(2)]
    t2 = [work_f.sub(3072 + i * 512, 512) for i in range(2)]
    sqb = [work_b.sub(i * 512, 512) for i in range(2)]
    qnb = [work_b.sub(1024 + i * 512, 512) for i in range(2)]
    cnt = [0]

    def qk_block(psb, gcol, outv, tb):
        i = cnt[0] % 2
        cnt[0] += 1
        k.act(sqb[i].v(), psb.v(), AF.Square)
        k.ts('dve', raw[i].v(), psb.v(), gcol, None, ALU.mult)
        ps2 = k.ps[2].sub(i * 512, 512)
        k.mm(ps2.v(), c['onesbd'].v(), sqb[i].v())
        k.act(rst[i].v(), ps2.v(), AF.Ln, scale=1.0 / 64, bias=EPS)
        k.act(rst[i].v(), rst[i].v(), AF.Exp, scale=-0.5)
        k.tt('pool', qnb[i].v(), raw[i].v(), rst[i].v(), ALU.mult)
        ps3 = k.ps[3].sub(i * 512, 512)
        k.mm(ps3.v(), c['rotT'].v(), qnb[i].v())
        k.tt('dve', t1[i].v(), qnb[i].v(), cosT.v(tb * 512, tb * 512 + 512), ALU.mult)
        k.tt('dve', t2[i].v(), ps3.v(), sinT.v(tb * 512, tb * 512 + 512), ALU.mult)
        k.tt('pool', outv, t1[i].v(), t2[i].v(), ALU.add)

    pcnt = [0]
    def wload_g(gj):
        nm_, nc__ = WIN_GROUPS[gj]
        k.dma('pool', wring[gj % 3].v(0, 8 * nc__), wg_d[nm_].v())

    wload_g(0)
    wload_g(1)
    for gi, (name, ncol) in enumerate(WIN_GROUPS):
        wb = wring[gi % 3]
        if gi + 2 < len(WIN_GROUPS):
            wload_g(gi + 2)
        if name in ('ka', 'kb', 'qa', 'qb'):
            npair = ncol // 128
            gidx = {'qa': 0, 'ka': 1, 'qb': 2, 'kb': 3}[name]
            od = {'ka': kTa_o, 'kb': kTb_o, 'qa': qTa_o, 'qb': qTb_o}[name]
            for pr in range(npair):
                st = stage[pr % 2]
                for tb in range(4):
                    psb = k.ps[pcnt[0] % 2].sub(512 * ((pcnt[0] // 2) % 2), 512)
                    pcnt[0] += 1
                    for kc in range(8):
                        k.mm(psb.v(), wb.v(kc * ncol + pr * 128, kc * ncol + pr * 128 + 128),
                             hT.v(kc * T + tb * 512, kc * T + tb * 512 + 512), start=(kc == 0), stop=(kc == 7))
                    qk_block(psb, gains.v(gidx, gidx + 1), st.v(tb * 512, tb * 512 + 512), tb)
                if name in ('ka', 'kb'):
                    odp = od[pr // 2]
                    k.dma('sp', V(odp.ap[(pr % 2) * 128:(pr % 2 + 1) * 128, :], odp.v().keys), st.v())
                else:
                    k.dma('sp', V(od.ap[:, pr * T:(pr + 1) * T], od.v().keys), st.v())
        elif name in ('va', 'vb'):
            nh_, dh_, d_ = (4, 129, 128) if name == 'va' else (12, 64, 64)
            od = va_o if name == 'va' else vb_o
            w_ = nh_ * dh_
            for i in range(2):
                k.memset('pool', vst[i].v(0, w_), 1.0)
            for tt_ in range(16):
                st = vst[tt_ % 2]
                for n0 in range(0, ncol, 512):
                    nn = min(512, ncol - n0)
                    psb = k.ps[pcnt[0] % 2].sub(512 * ((pcnt[0] // 2) % 2), 512)
                    pcnt[0] += 1
                    for kc in range(8):
                        k.mm(psb.v(0, nn), hT.v(kc * T + tt_ * 128, kc * T + tt_ * 128 + 128),
                             wb.v(kc * ncol + n0, kc * ncol + n0 + nn), start=(kc == 0), stop=(kc == 7))
                    h0 = n0 // d_
                    nhh = nn // d_
                    outv = V(st.v(h0 * dh_, (h0 + nhh) * dh_, pat="p (h c) -> p h c", c=dh_).ap[:, :, 0:d_],
                             st.keys(h0 * dh_, (h0 + nhh) * dh_))
                    inv = V(psb.v(0, nn, pat="p (h c) -> p h c", c=d_).ap, psb.keys(0, nn))
                    k.cp('act', outv, inv)
                if name == 'va':
                    for h4 in range(4):
                        k.dma('sp', V(od[h4].ap[tt_ * 128:(tt_ + 1) * 128, :], od[h4].v().keys), st.v(h4 * 129, h4 * 129 + 129))
                else:
                    for g3 in range(3):
                        k.dma('sp', V(od[g3].ap[tt_ * 128:(tt_ + 1) * 128, :], od[g3].v().keys), st.v(g3 * 256, g3 * 256 + 256))
        else:
            gi_ = 0 if name == 'ga' else 1
            for cc in range(8):
                st = stage[cc % 2]
                for tb in range(4):
                    psb = k.ps[pcnt[0] % 2].sub(512 * ((pcnt[0] // 2) % 2), 512)
                    pcnt[0] += 1
                    for kc in range(8):
                        k.mm(psb.v(), wb.v(kc * ncol + cc * 128, kc * ncol + cc * 128 + 128),
                             hT.v(kc * T + tb * 512, kc * T + tb * 512 + 512), start=(kc == 0), stop=(kc == 7))
                    k.act(st.v(tb * 512, tb * 512 + 512), psb.v(), AF.Sigmoid)
                o0 = (gi_ * 8 + cc) * T
                k.dma('sp', V(gates_o.ap[:, o0:o0 + T], gates_o.v().keys), st.v())
    return


def body_B(k, c, xT, D, l):
    mod_d = D['mod_s']
    qTa_d = D['qTa_s']
    qTb_d = D['qTb_s']
    kTa_d = D['kTa_all']
    va_d = D['va_all']
    kTb_d = D['kTb_all']
    vb_d = D['vb_all']
    gates_d = D['gates_s']
    lam_d = D['lamp%d' % l]
    subln_d = D['subln%d' % l]
    wpa_d = D['w_pa%d' % l]
    wpb_d = D['w_pb%d' % l]
    wo_d = D['w_o%d' % l]
    wr_d = D['w_r%d' % l]
    br_d = D['b_r%d' % l]
    sel_d = D['sel']
    ehd_d = D['ehd']
    we_d = D['w_e%d' % l]

    small = k.fa(1024)
    fwork = k.fa(25600 - k.fo)
    mod = small.sub(0, 48)
    lamp = small.sub(48, 258)
    subln = small.sub(320, 128)
    br = small.sub(448, 36)
    sc2 = small.sub(484, 8)
    lamcol = small.sub(492, 4)
    tmp64 = small.sub(512, 128)
    wr = small.sub(640, 288)

    k.dma('sp', mod.v(), mod_d.v())
    k.dma('sp', lamp.v(), lam_d.v())
    k.dma('sp', subln.v(), subln_d.v())
    k.dma('sp', br.v(), br_d.v())
    k.dma('sp', wr.v(), wr_d.v())

    k.tt('dve', tmp64.v(0, 64), lamp.v(0, 64), lamp.v(64, 128), ALU.mult)
    k.tt('dve', tmp64.v(64, 128), lamp.v(128, 192), lamp.v(192, 256), ALU.mult)
    k.red('dve', lamcol.v(0, 2), tmp64.v(0, 128, pat="p (a w) -> p a w", w=64), ALU.add)
    k.act(lamcol.v(0, 2), lamcol.v(0, 2), AF.Exp)
    k.tt('dve', lamcol.v(3, 4), lamcol.v(0, 1), lamcol.v(1, 2), ALU.subtract)
    k.tt('dve', lamcol.v(0, 1), lamcol.v(3, 4), lamp.v(256, 257), ALU.add)
    k.ts('dve', lamcol.v(1, 2), lamcol.v(0, 1), -1.0, None, ALU.mult)
    k.ts('dve', sc2.v(), mod.v(32, 40), 1.0, None, ALU.add)

    AB = k.AB_
    b0 = k.bo
    qTa = AB.sub(b0 + 0, 8192)
    oaT = AB.sub(b0 + 8192, 8192)
    obT = AB.sub(b0 + 16384, 4096)
    qTb = AB.sub(b0 + 20480, 12288)
    kring = [AB.sub(b0 + 32768 + i * 2048, 2048) for i in range(3)]
    vring = [AB.sub(b0 + 38912 + i * 2048, 2048) for i in range(3)]
    PT = [AB.sub(b0 + 45104 + i * 1024, 1024) for i in range(2)]

    k.dma('sp', qTa.v(), qTa_d.v())
    PT4 = [AB.sub(b0 + 45056 + i * 1024, 1024) for i in range(4)]
    tsum = AB.sub(b0 + 49152, 1024)
    acc = fwork.sub(1024, 1024)
    dsb = fwork.sub(2048, 512)
    rsb = fwork.sub(2560, 512)
    r2 = fwork.sub(3072, 512)
    slcol = fwork.sub(0, 1)
    k.tt('dve', slcol.v(), subln.v(0, 1), lamp.v(257, 258), ALU.mult)
    ld = [0]
    for h in range(4):
        for qb in range(4):
            OT = k.ps[2]

            SB = [0, 1, 3]

            def qk(kt, kbuf):
                pss = k.ps[SB[kt % 3]]
                for m in range(2):
                    k.mm(pss.v(m * 512, m * 512 + 512),
                         kbuf.v((kt % 16) * 128, (kt % 16) * 128 + 128, p0=m * 64, p1=m * 64 + 64),
                         qTa.v(h * T + qb * 512, h * T + qb * 512 + 512, p0=m * 64, p1=m * 64 + 64))

            bufs = {}

            def load(ch):
                i = ld[0] % 3
                ld[0] += 1
                kb_, vb_ = kring[i], vring[i]
                k.dma('sp', kb_.v(), V(kTa_d[h // 2].ap[ch * 256 + (h % 2) * 128:ch * 256 + (h % 2) * 128 + 128, :], kTa_d[h // 2].v().keys))
                src = va_d[h].ap[ch * 2048:(ch + 1) * 2048, 0:128].rearrange("(t p) c -> p t c", p=128)
                k.dma('sp', V(vb_.v(0, 2048, pat="p (t c) -> p t c", c=128).ap, vb_.keys(0, 2048)), V(src, va_d[h].v().keys))
                bufs[ch] = (kb_, vb_)

            load(0)
            load(1)
            qk(0, bufs[0][0])
            qk(1, bufs[0][0])
            for kt in range(64):
                ch = kt // 16
                if kt % 16 == 0 and ch + 2 < 4:
                    load(ch + 2)
                if kt + 2 < 64:
                    qk(kt + 2, bufs[(kt + 2) // 16][0])
                pt = PT4[kt % 4]
                k.act(pt.v(), k.ps[SB[kt % 3]].v(), AF.Exp, scale=0.125)
                vb_ = bufs[ch][1]
                for m in range(2):
                    k.mm(OT.v(m * 512, m * 512 + 512), vb_.v((kt % 16) * 128, (kt % 16) * 128 + 128),
                         pt.v(m * 512, m * 512 + 512), start=(kt == 0), stop=(kt == 63))
                if kt % 4 == 1:
                    k.tt('dve', tsum.v(), PT4[(kt - 1) % 4].v(), pt.v(), ALU.add)
                elif kt % 4 == 3:
                    k.tt('dve', tsum.v(), tsum.v(), PT4[(kt - 1) % 4].v(), ALU.add)
                    k.tt('dve', tsum.v(), tsum.v(), pt.v(), ALU.add)
                    if kt == 3:
                        k.cp('dve', acc.v(), tsum.v())
                    else:
                        k.tt('dve', acc.v(), acc.v(), tsum.v(), ALU.add)
            k.cp('dve', tsum.v(), acc.v())
            for m in range(2):
                psd = k.ps[0].sub(m * 512, 512)
                k.mm(psd.v(), c['ones_b'].v(), tsum.v(m * 512, m * 512 + 512))
            k.recip(rsb.v(), k.ps[0].v(0, 512))
            k.recip(r2.v(), k.ps[0].v(512, 1024))
            k.tt('dve', dsb.v(), OT.v(0, 512), rsb.v(), ALU.mult)
            k.tt('dve', r2.v(), OT.v(512, 1024), r2.v(), ALU.mult)
            k.stt('dve', dsb.v(), r2.v(), lamcol.v(1, 2), dsb.v(), ALU.mult, ALU.add)
            sqA = PT4[0].sub(0, 512)
            k.act(sqA.v(), dsb.v(), AF.Square)
            pss_ = k.ps[1].sub(0, 512)
            k.mm(pss_.v(), c['ones_b'].v(), sqA.v())
            k.act(rsb.v(), pss_.v(), AF.Sqrt, scale=1.0 / 128, bias=EPS)
            k.recip(rsb.v(), rsb.v())
            k.stt('dve', oaT.v(h * T + qb * 512, h * T + qb * 512 + 512), dsb.v(), slcol.v(), rsb.v(), ALU.mult, ALU.mult)

    k.dma('sp', qTb.v(), qTb_d.v())
    kbb = [AB.sub(b0 + i * 4096, 4096) for i in range(2)]
    vtr = [AB.sub(b0 + 32768 + i * 260, 260) for i in range(8)]
    numT = fwork.sub(512, 2 * T)
    denT = fwork.sub(512 + 2 * T, T)
    Osb = fwork.sub(512 + 3 * T, 260)
    vld = [0]
    vtile_no = [0]
    U32 = mybir.dt.uint32
    idx_t = k.idx_t
    kidx = Buf("idx_t", idx_t, 4, 0, 24)
    vidx = Buf("idx_t", idx_t, 4, 24, 69)
    vmask = fwork.sub(7200, 69)
    k.dma('sp', vmask.v(), D['vmask'].v())
    kTb_view = [d_.ap.rearrange("r (h c) -> (r h) c", h=2) for d_ in kTb_d]
    vstg = [AB.sub(b0 + 32768 + 8 * 260 + i * 256, 256) for i in range(4)]

    def igather(outv, src_ap, src_d, idxv):
        k.S.add('pool', lambda e: e.indirect_dma_start(out=outv.ap, out_offset=None, in_=src_ap,
                                                       in_offset=bass.IndirectOffsetOnAxis(ap=idxv.ap.bitcast(U32), axis=0)),
                reads=src_d.v().keys + idxv.keys, writes=outv.keys, dma=True)
    for g, dil in enumerate([1, 4, 16]):
        for j in range(2):
            for seg in range(4):
                col = (g * 2 + j) * 4 + seg
                igather(kbb[j].v(seg * 1024, seg * 1024 + 1024), kTb_view[g], kTb_d[g], kidx.v(col, col + 1))
        ntile = 16 // dil
        for r in range(dil):
            vt = {}

            def vload(n):
                i = vld[0] % 8
                vld[0] += 1
                tno = vtile_no[0]
                vtile_no[0] += 1
                vs_ = vstg[tno % 4]
                igather(vs_.v(), vb_d[g].ap, vb_d[g], vidx.v(tno, tno + 1))
                k.ts('dve', V(vtr[i].v(0, 260, pat="p (h c) -> p h c", c=65).ap[:, :, 0:64], vtr[i].keys(0, 260)),
                     V(vs_.v(0, 256, pat="p (h c) -> p h c", c=64).ap, vs_.keys(0, 256)), vmask.v(tno, tno + 1), None, ALU.mult)
                k.ts('dve', V(vtr[i].v(0, 260, pat="p (h c) -> p h c", c=65).ap[:, :, 64], vtr[i].keys(0, 260)),
                     c['ones_b'].v(0, 4), vmask.v(tno, tno + 1), None, ALU.mult)
                vt[n] = vtr[i]

            vload(0)
            for m in range(ntile):
                vload(m + 1)
                pss = k.ps[m % 2]
                i0 = 128 * m * dil + r
                for hh in range(4):
                    j, half = hh // 2, hh % 2
                    for ab in range(2):
                        n = m + ab
                        w0 = PADW + (128 * n - 64) * dil + r
                        col = (hh * 2 + ab) * 128
                        k.mm(pss.v(col, col + 128), c['ident_b'].v(), c['maskA' if ab == 0 else 'maskB'].v(), start=True, stop=False)
                        k.mm(pss.v(col, col + 128),
                             kbb[j].v(w0, w0 + 128 * dil - (dil - 1), p0=half * 64, p1=half * 64 + 64, step=dil),
                             qTb.v((g * 2 + j) * T + i0, (g * 2 + j) * T + i0 + 128 * dil - (dil - 1), p0=half * 64, p1=half * 64 + 64, step=dil),
                             start=False, stop=True)
                pt = PT[m % 2]
                k.act(pt.v(), pss.v(), AF.Exp, scale=0.125)
                Ops = k.ps[2 + (m % 2)].sub(0, 260)
                for hh in range(4):
                    for ab in range(2):
                        col = (hh * 2 + ab) * 128
                        k.mm(Ops.v(hh * 65, hh * 65 + 65), pt.v(col, col + 128), vt[m + ab].v(hh * 65, hh * 65 + 65),
                             start=(ab == 0), stop=(ab == 1))
                k.cp('dve', V(Osb.v(0, 256, pat="p (h c) -> p h c", c=64).ap, Osb.keys(0, 256)),
                     V(Ops.v(0, 260, pat="p (h c) -> p h c", c=65).ap[:, :, 0:64], Ops.keys(0, 260)))
                k.cp('dve', Osb.v(256, 260), V(Ops.v(0, 260, pat="p (h c) -> p h c", c=65).ap[:, :, 64], Ops.keys(0, 260)))
                pT_ = k.ps[2 + (m % 2)]
                for j in range(2):
                    k.tr(pT_.v(512 + j * 128, 512 + j * 128 + 128), Osb.v(j * 128, j * 128 + 128), c['ident_f'].v())
                k.tr(pT_.v(768, 896, p0=0, p1=4), Osb.v(256, 260), c['ident_f'].v())
                for j in range(2):
                    dst = numT.v(j * T + i0, j * T + i0 + 128 * dil - (dil - 1), step=dil)
                    if g == 0:
                        k.cp('dve', dst, pT_.v(512 + j * 128, 512 + j * 128 + 128))
                    else:
                        k.tt('dve', dst, pT_.v(512 + j * 128, 512 + j * 128 + 128), dst, ALU.add)
                dst = denT.v(i0, i0 + 128 * dil - (dil - 1), p0=0, p1=4, step=dil)
                if g == 0:
                    k.cp('dve', dst, pT_.v(768, 896, p0=0, p1=4))
                else:
                    k.tt('dve', dst, pT_.v(768, 896, p0=0, p1=4), dst, ALU.add)
    ehd_t = fwork.sub(512 + 3 * T + 260, 256)
    k.dma('sp', ehd_t.v(p0=0, p1=4), ehd_d.v())
    k.recip(denT.v(p0=0, p1=4), denT.v(p0=0, p1=4))
    for j in range(2):
        for tb in range(4):
            psb = k.ps[tb % 2].sub(0, 512)
            k.mm(psb.v(), ehd_t.v(j * 128, j * 128 + 128, p0=0, p1=4), denT.v(tb * 512, tb * 512 + 512, p0=0, p1=4))
            k.tt('dve', obT.v(j * T + tb * 512, j * T + tb * 512 + 512), numT.v(j * T + tb * 512, j * T + tb * 512 + 512), psb.v(), ALU.mult)

    wo = AB.sub(b0 + 20480, 8192)
    wpa = AB.sub(b0 + 28672, 4096)
    gat = AB.sub(b0 + 32768, 8192)
    mrg = AB.sub(b0 + 40960, 4096)
    wpb = AB.sub(b0 + 45056, 2048)
    k.dma('pool', wo.v(), wo_d.v())
    k.dma('pool', wpa.v(), wpa_d.v())
    k.dma('pool', wpb.v(), wpb_d.v())
    m1 = fwork.sub(0, 512)
    for tb in range(4):
        for gi_ in range(2):
            src = gates_d.ap[:, gi_ * 8 * T: (gi_ + 1) * 8 * T].rearrange("p (c t) -> p c t", t=T)[:, :, tb * 512:(tb + 1) * 512]
            k.dma('sp', V(gat.v(gi_ * 4096, gi_ * 4096 + 4096, pat="p (c t) -> p c t", t=512).ap, gat.keys(gi_ * 4096, gi_ * 4096 + 4096)),
                  V(src, gates_d.v().keys))
        for cc in range(8):
            psa = k.ps[cc % 2].sub(0, 512)
            psb = k.ps[cc % 2].sub(512, 512)
            for kc in range(4):
                k.mm(psa.v(), wpa.v(kc * 1024 + cc * 128, kc * 1024 + cc * 128 + 128),
                     oaT.v(kc * T + tb * 512, kc * T + tb * 512 + 512), start=(kc == 0), stop=(kc == 3))
            for kc in range(2):
                k.mm(psb.v(), wpb.v(kc * 1024 + cc * 128, kc * 1024 + cc * 128 + 128),
                     obT.v(kc * T + tb * 512, kc * T + tb * 512 + 512), start=(kc == 0), stop=(kc == 1))
            k.tt('dve', m1.v(), psa.v(), gat.v(cc * 512, cc * 512 + 512), ALU.mult)
            k.tt('dve', mrg.v(cc * 512, cc * 512 + 512), psb.v(), gat.v(4096 + cc * 512, 4096 + cc * 512 + 512), ALU.mult)
            k.tt('pool', mrg.v(cc * 512, cc * 512 + 512), mrg.v(cc * 512, cc * 512 + 512), m1.v(), ALU.add)
        for cc in range(8):
            psy = k.ps[2 + cc % 2].sub(0, 512)
            for kc in range(8):
                k.mm(psy.v(), wo.v(kc * 1024 + cc * 128, kc * 1024 + cc * 128 + 128),
                     mrg.v(kc * 512, kc * 512 + 512), start=(kc == 0), stop=(kc == 7))
            xs = xT.v(cc * T + tb * 512, cc * T + tb * 512 + 512)
            k.stt('dve', xs, psy.v(), mod.v(16 + cc, 17 + cc), xs, ALU.mult, ALU.add)

    hT = AB.sub(b0, 8 * T)
    wring = [AB.sub(b0 + 16384 + i * 6144, 6144) for i in range(4)]
    hid = [AB.sub(b0 + 40960 + i * 4096, 4096) for i in range(2)]
    wb2 = AB.sub(b0 + 49152, 1024)
    h32b = fwork.sub(2048, 4096)
    lg = fwork.sub(6144, 36 * 16)
    comb = fwork.sub(6144 + 576, 32 * 16)
    rw = fwork.sub(6144 + 576 + 512, 96)

    def hook(tb, cc):
        if cc is not None:
            return h32b.sub(cc * 512, 512)
        for q in range(4):
            tt_ = tb * 4 + q
            psl = k.ps[2 + q % 2].sub(0, 36)
            for kc in range(8):
                k.mm(psl.v(), h32b.v(kc * 512 + q * 128, kc * 512 + q * 128 + 128), wr.v(kc * 36, kc * 36 + 36),
                     start=(kc == 0), stop=(kc == 7))
            k.tt('dve', lg.v(tt_ * 36, tt_ * 36 + 36), psl.v(), br.v(), ALU.add)
        return None

    rms_mod(k, xT, hT, c, sc2, mod.sub(24, 8), fwork.sub(0, 2048), wb2, h32_hook=hook)

    Rb = fwork.sub(0, 2048)
    m1 = Rb.sub(0, 16); e1 = Rb.sub(16, 64); s1 = Rb.sub(80, 16); gval = Rb.sub(96, 16); oh = Rb.sub(112, 64)
    ig = Rb.sub(176, 128); tmpg = Rb.sub(304, 128); v1 = Rb.sub(432, 16); mk1 = Rb.sub(448, 128); ig2 = Rb.sub(576, 128)
    v2 = Rb.sub(704, 16); mk2 = Rb.sub(720, 128); dd = Rb.sub(848, 16); ee = Rb.sub(864, 16); w1 = Rb.sub(880, 16)
    w2 = Rb.sub(896, 16); cig = Rb.sub(912, 128); tmp2 = Rb.sub(1040, 128)

    def v3(buf, n):
        return V(buf.v(0, 16 * n, pat="p (t e) -> p t e", e=n).ap, buf.keys(0, 16 * n))

    def bc(buf, n):
        return V(buf.v(0, 16).ap.unsqueeze(2).to_broadcast([128, 16, n]), buf.keys(0, 16))

    def lgv(a_, b_):
        return V(lg.v(0, 576, pat="p (t e) -> p t e", e=36).ap[:, :, a_:b_], lg.keys(0, 576))

    def oh_g(g):
        return V(oh.v(0, 64, pat="p (t e) -> p t e", e=4).ap[:, :, g:g + 1].to_broadcast([128, 16, 8]), oh.keys(0, 64))

    def comb_g(g):
        return V(comb.v(0, 512, pat="p (t g e) -> p t g e", g=4, e=8).ap[:, :, g, :], comb.keys(0, 512))

    k.red('dve', m1.v(), lgv(0, 4), ALU.max)
    k.tt('dve', v3(e1, 4), lgv(0, 4), bc(m1, 4), ALU.subtract)
    k.act(e1.v(), e1.v(), AF.Exp)
    k.red('dve', s1.v(), v3(e1, 4), ALU.add)
    k.recip(gval.v(), s1.v())
    k.tt('dve', v3(oh, 4), lgv(0, 4), bc(m1, 4), ALU.is_equal)
    for g in range(4):
        dst = ig if g == 0 else tmpg
        k.tt('dve', v3(dst, 8), lgv(4 + 8 * g, 12 + 8 * g), oh_g(g), ALU.mult)
        if g > 0:
            k.tt('dve', ig.v(), ig.v(), tmpg.v(), ALU.add)
    k.red('dve', v1.v(), v3(ig, 8), ALU.max)
    k.tt('dve', v3(mk1, 8), v3(ig, 8), bc(v1, 8), ALU.is_equal)
    k.stt('dve', ig2.v(), mk1.v(), -1e30, ig.v(), ALU.mult, ALU.add)
    k.red('dve', v2.v(), v3(ig2, 8), ALU.max)
    k.tt('dve', v3(mk2, 8), v3(ig2, 8), bc(v2, 8), ALU.is_equal)
    k.tt('dve', dd.v(), v1.v(), v2.v(), ALU.subtract)
    k.act(ee.v(), dd.v(), AF.Exp, scale=-1.0)
    k.ts('dve', w1.v(), ee.v(), 1.0, None, ALU.add)
    k.recip(w1.v(), w1.v())
    k.tt('dve', w2.v(), ee.v(), w1.v(), ALU.mult)
    k.tt('dve', w1.v(), w1.v(), gval.v(), ALU.mult)
    k.tt('dve', w2.v(), w2.v(), gval.v(), ALU.mult)
    k.tt('dve', v3(cig, 8), v3(mk1, 8), bc(w1, 8), ALU.mult)
    k.tt('dve', v3(tmp2, 8), v3(mk2, 8), bc(w2, 8), ALU.mult)
    k.tt('dve', cig.v(), cig.v(), tmp2.v(), ALU.add)
    for g in range(4):
        k.tt('dve', comb_g(g), v3(cig, 8), oh_g(g), ALU.mult)
    CTt = fwork.sub(0, 2048)
    for tt_ in range(16):
        pst = k.ps[2 + tt_ % 2].sub(512, 128)
        k.tr(pst.v(p0=0, p1=32), comb.v(tt_ * 32, tt_ * 32 + 32), c['ident_f'].v())
        k.cp('dve', CTt.v(tt_ * 128, tt_ * 128 + 128, p0=0, p1=32), pst.v(p0=0, p1=32))
    selt = fwork.sub(2048, 4096)
    k.dma('sp', selt.v(p0=0, p1=32), sel_d.v())

    G = 2
    sg = [fwork.sub(6144 + i * 512, 512) for i in range(2)]

    def wload(e):
        k.dma('pool', wring[e % 4].v(), V(we_d.ap[e], we_d.v(key=e).keys))

    for e in range(4):
        wload(e)
    hcnt = [0]
    for eg in range(NEXP // G):
        if eg >= 1 and (eg + 1) * G < NEXP:
            for ei in range(G):
                wload((eg + 1) * G + ei)
        for tb in range(4):
            hb = hid[hcnt[0] % 2]
            hcnt[0] += 1
            for ei in range(G):
                e = eg * G + ei
                w = wring[e % 4]
                psc = k.ps[3].sub(512, 512)
                k.mm(psc.v(), selt.v(e * 128, e * 128 + 128, p0=0, p1=32), CTt.v(tb * 512, tb * 512 + 512, p0=0, p1=32))
                for ch in range(2):
                    psg = k.ps[ch].sub(0, 512)
                    psu = k.ps[ch].sub(512, 512)
                    for kc in range(8):
                        k.mm(psg.v(), w.v(kc * 256 + ch * 128, kc * 256 + ch * 128 + 128),
                             hT.v(kc * T + tb * 512, kc * T + tb * 512 + 512), start=(kc == 0), stop=(kc == 7))
                    for kc in range(8):
                        k.mm(psu.v(), w.v(2048 + kc * 256 + ch * 128, 2048 + kc * 256 + ch * 128 + 128),
                             hT.v(kc * T + tb * 512, kc * T + tb * 512 + 512), start=(kc == 0), stop=(kc == 7))
                    s_ = sg[ch]
                    k.act(s_.v(), psg.v(), AF.Silu)
                    k.tt('dve', s_.v(), psu.v(), s_.v(), ALU.mult)
                    k.tt('dve', hb.v((ei * 2 + ch) * 512, (ei * 2 + ch) * 512 + 512), psc.v(), s_.v(), ALU.mult)
            for cc in range(8):
                psy = k.ps[2].sub((cc % 2) * 512, 512)
                for ei in range(G):
                    w = wring[(eg * G + ei) % 4]
                    for ch in range(2):
                        k.mm(psy.v(), w.v(4096 + ch * 1024 + cc * 128, 4096 + ch * 1024 + cc * 128 + 128),
                             hb.v((ei * 2 + ch) * 512, (ei * 2 + ch) * 512 + 512),
                             start=(ei == 0 and ch == 0), stop=(ei == G - 1 and ch == 1))
                xs = xT.v(cc * T + tb * 512, cc * T + tb * 512 + 512)
                k.stt('dve', xs, psy.v(), mod.v(40 + cc, 41 + cc), xs, ALU.mult, ALU.add)
    return


def build_F():
    k = K(nf32=25600, nbf16=51200)
    nc = k.nc
    D = {}

    def ext(name, shape, dt):
        D[name] = k.din(name, shape, dt)

    def itn(name, shape, dt):
        D[name] = DBuf(name, nc.dram_tensor(name, list(shape), dt).ap())

    ext('xT', [128, 8 * T], F32); ext('c_col', [128, 8], F32); ext('posb', [128, T], I32); ext('invf', [128, 1], F32)
    ext('sel', [32, 32 * 128], F32); ext('ehd', [4, 256], F32); ext('idx', [128, 93], I32); ext('vmask', [128, 69], F32)
    for l in range(2):
        ext('w_ada%d' % l, [6, 128, 8 * 1024], F32); ext('b_ada%d' % l, [128, 48], F32); ext('gains%d' % l, [128, 4], F32)
        for n, nc_ in WIN_GROUPS:
            ext('w_%s%d' % (n, l), [128, 8 * nc_], F32)
        ext('lamp%d' % l, [128, 258], F32); ext('subln%d' % l, [128, 128], F32)
        ext('w_pa%d' % l, [128, 4096], F32); ext('w_pb%d' % l, [128, 2048], F32); ext('w_o%d' % l, [128, 8192], F32)
        ext('w_r%d' % l, [128, 288], F32); ext('b_r%d' % l, [128, 36], F32); ext('w_e%d' % l, [NEXP, 128, 6144], F32)
    def pieces(nm, n, ls, as_):
        D[nm + '_loc'] = [DBuf('%s_loc%d' % (nm, i), nc.dram_tensor('%s_loc%d' % (nm, i), ls, BF16).ap()) for i in range(n)]
        D[nm + '_all'] = [DBuf('%s_all%d' % (nm, i), nc.dram_tensor('%s_all%d' % (nm, i), as_, BF16).ap()) for i in range(n)]
    pieces('kTa', 2, [256, 2048], [1024, 2048])
    pieces('va', 4, [T, 129], [SEQ, 129])
    pieces('kTb', 3, [256, 2048], [1024, 2048])
    pieces('vb', 3, [T, 256], [SEQ, 256])
    itn('qTa_s', [128, 4 * T], BF16); itn('qTb_s', [128, 6 * T], BF16)
    itn('gates_s', [128, 16 * T], BF16); itn('mod_s', [128, 48], F32)
    out_d = k.dout('outT', [128, 8 * T], F32)
    c = load_consts(k, True)
    xT = k.fa(8 * T)
    k.idx_t = k.es.enter_context(nc.sbuf_tensor("idx_t", [128, 93], I32))
    idxb = Buf("idx_t", k.idx_t, 4, 0, 93)
    k.dma('sp', idxb.v(), D['idx'].v())
    k.dma('sp', xT.v(), D['xT'].v())
    base = (k.fo, k.bo)
    rg = [[0, 1, 2, 3], [4, 5, 6, 7]]
    for l in range(2):
        k.fo, k.bo = base
        def after_kv():
            for nm, i_ in [('kTa', 0), ('va', 0), ('va', 1), ('kTa', 1), ('va', 2), ('va', 3),
                           ('kTb', 0), ('kTb', 1), ('kTb', 2), ('vb', 0), ('vb', 1), ('vb', 2)]:
                loc, al = D[nm + '_loc'][i_], D[nm + '_all'][i_]
                k.S.add('pool', lambda e, loc=loc, al=al: e.collective_compute(
                    "AllGather", ALU.bypass, replica_groups=rg, ins=[loc.ap.opt()], outs=[al.ap.opt()]),
                    reads=loc.v().keys, writes=al.v().keys, dma=True, cc=True)
        k.after_kv = after_kv
        body_A(k, c, xT, D, l)
        after_kv()
        k.fo, k.bo = base
        body_B(k, c, xT, D, l)
    k.dma('sp', out_d.v(), xT.v())
    return k.finalize()


_cache = {}


def _fm(w):
    K_, N = w.shape
    return np.ascontiguousarray(w.reshape(K_ // 128, 128, N).transpose(1, 0, 2).reshape(128, (K_ // 128) * N))


def _index_tables(jr):
    p = np.arange(128)
    idx = np.zeros((128, 93), np.int64)
    vmask = np.zeros((128, 69), np.float32)
    for jp in range(6):
        for seg in range(4):
            rank = [jr - 1, jr, jr, jr + 1][seg]
            half = [1, 0, 1, 0][seg]
            rank = min(max(rank, 0), 3)
            idx[:, jp * 4 + seg] = rank * 512 + ((jp % 2) * 128 + p) * 2 + half
    t = 0
    for g, dil in enumerate([1, 4, 16]):
        for r in range(dil):
            for n in range(16 // dil + 1):
                a_k = 128 * n - 64 + p
                gpos = jr * T + a_k * dil + r
                valid = (gpos >= 0) & (gpos < SEQ)
                gp = np.where(valid, gpos, 0)
                idx[:, 24 + t] = gp
                vmask[:, t] = valid.astype(np.float32)
                t += 1
    assert t == 69
    return idx.astype(np.int32), vmask


def kernel(x, c, positions, w_ada, b_ada, w_in, qn_a, kn_a, lam_q1, lam_k1, lam_q2, lam_k2,
           subln_a, qn_b, kn_b, w_pa, w_pb, w_o, w_r1, b_r1, w_r2, b_r2, w_e_gate, w_e_up, w_e_down):
    x = np.asarray(x, np.float32)
    consts = host_consts()
    invf = (np.float32(10000.0) ** (-np.arange(0, 64, 2, dtype=np.float32) / np.float32(64))).astype(np.float32)
    invf_col = invf[(np.arange(128) % 64) % 32].reshape(128, 1).astype(np.float32)
    cores = list(range(NCORES))
    sel = np.zeros((32, 32 * 128), np.float32)
    for e in range(32):
        sel[e, e * 128:(e + 1) * 128] = 1.0
    ehd = np.zeros((4, 256), np.float32)
    for h in range(4):
        j, half = h // 2, h % 2
        ehd[h, j * 128 + half * 64: j * 128 + half * 64 + 64] = 1.0
    cuts = np.cumsum([0, 512, 512, 512, 768, 768, 768, 1024, 1024])
    names = ['qa', 'ka', 'va', 'qb', 'kb', 'vb', 'ga', 'gb']
    shared = {"consts": consts, "invf": invf_col, "sel": sel, "ehd": ehd}
    pidx = np.arange(128) % 64
    for l in range(2):
        wl = np.asarray(w_in[l], np.float32)
        for i, n in enumerate(names):
            shared["w_%s%d" % (n, l)] = _fm(wl[:, cuts[i]:cuts[i + 1]])
        shared["w_ada%d" % l] = np.ascontiguousarray(
            np.asarray(w_ada[l], np.float32).reshape(8, 128, 6, 1024).transpose(2, 1, 0, 3).reshape(6, 128, 8 * 1024))
        shared["b_ada%d" % l] = np.ascontiguousarray(np.asarray(b_ada[l], np.float32).reshape(48, 128).T)
        shared["gains%d" % l] = np.stack([np.asarray(qn_a[l])[pidx], np.asarray(kn_a[l])[pidx],
                                          np.asarray(qn_b[l])[pidx], np.asarray(kn_b[l])[pidx]], axis=1).astype(np.float32)
        lam_init = 0.8 - 0.6 * math.exp(-0.3 * l)
        lamp = np.concatenate([np.asarray(lam_q1[l]), np.asarray(lam_k1[l]), np.asarray(lam_q2[l]), np.asarray(lam_k2[l]),
                               np.array([lam_init, 1.0 - lam_init])]).astype(np.float32)
        shared["lamp%d" % l] = np.ascontiguousarray(np.broadcast_to(lamp[None, :], (128, 258)))
        shared["subln%d" % l] = np.ascontiguousarray(np.broadcast_to(np.asarray(subln_a[l], np.float32)[:, None], (128, 128)))
        shared["w_r%d" % l] = _fm(np.concatenate([np.asarray(w_r1[l], np.float32), np.asarray(w_r2[l], np.float32)], axis=1))
        shared["b_r%d" % l] = np.ascontiguousarray(np.broadcast_to(
            np.concatenate([np.asarray(b_r1[l]), np.asarray(b_r2[l])]).astype(np.float32)[None, :], (128, 36)))
        we = np.empty((NEXP, 128, 6144), np.float32)
        for e in range(NEXP):
            we[e, :, 0:2048] = _fm(np.asarray(w_e_gate[l, e], np.float32))
            we[e, :, 2048:4096] = _fm(np.asarray(w_e_up[l, e], np.float32))
            we[e, :, 4096:6144] = _fm(np.asarray(w_e_down[l, e], np.float32))
        shared["w_e%d" % l] = we
        shared["w_pa%d" % l] = _fm(np.asarray(w_pa[l], np.float32))
        shared["w_pb%d" % l] = _fm(np.asarray(w_pb[l], np.float32))
        shared["w_o%d" % l] = _fm(np.asarray(w_o[l], np.float32))
    in_maps = []
    for ci in cores:
        b, j = ci // 4, ci % 4
        xs = x[b, j * T:(j + 1) * T, :]
        idx, vmask = _index_tables(j)
        m = dict(shared)
        m["xT"] = np.ascontiguousarray(xs.T.reshape(8, 128, T).transpose(1, 0, 2).reshape(128, 8 * T))
        m["c_col"] = np.ascontiguousarray(np.asarray(c[b], np.float32).reshape(8, 128).T)
        m["posb"] = np.ascontiguousarray(np.broadcast_to(np.asarray(positions[b, j * T:(j + 1) * T], np.int32)[None, :], (128, T)))
        m["idx"] = idx
        m["vmask"] = vmask
        in_maps.append(m)
    if 'F' not in _cache:
        _cache['F'] = build_F()
    res = run_bass_kernel_spmd(_cache['F'], in_maps, core_ids=cores).results
    out = np.empty((2, SEQ, D), np.float32)
    for ci in cores:
        b, j = ci // 4, ci % 4
        o = np.asarray(res[ci]["outT"], np.float32)
        out[b, j * T:(j + 1) * T, :] = o.reshape(128, 8, T).transpose(1, 0, 2).reshape(D, T).T
    return out
```
